# Optimizing a Trainium2 kernel written in Bass

```python
import math
import jax, jax.numpy as jnp
from jax import lax
import numpy as np

D_MODEL = 1024
BATCH = 32
SEQ = 2048
DEPTH = 1

MEM_LEN = 256
EPS = 1e-6
MLA_HEADS = 8
MLA_NOPE = 64
MLA_ROPE = 32
MLA_V = 64
MLA_Q_RANK = 384
MLA_KV_RANK = 256
ROPE_THETA = 10000.0
Q_BLOCK = 128
ML_HEADS = 8
ML_DK = 64
ML_DV = 64
ML_CHUNK = 64
CONV_WIDTH = 5
MEM_HEADS = 4
MEM_HEAD_DIM = 128
MLA_WIDTH = MLA_HEADS * MLA_V
ML_WIDTH = ML_HEADS * ML_DV
MEM_WIDTH = MEM_HEADS * MEM_HEAD_DIM
N_BRANCH = 3
BRANCH_WIDTH = 512
N_ML_GATES = 4 * ML_HEADS
N_IN = N_BRANCH * D_MODEL + MLA_Q_RANK + MLA_KV_RANK + MLA_ROPE + 3 * ML_WIDTH + N_ML_GATES + MEM_WIDTH
D_FF = -(-8 * D_MODEL // (3 * 256)) * 256

kernel_name = "hybrid_mla_mlstm_memory_gated_block"


def rms_norm(x, g):
    xf = x.astype(jnp.float32)
    y = xf * lax.rsqrt(jnp.mean(xf * xf, axis=-1, keepdims=True) + EPS)
    return (y * g.astype(jnp.float32)).astype(x.dtype)


def rope_angles(positions):
    half = MLA_ROPE // 2
    inv = ROPE_THETA ** (-jnp.arange(half, dtype=jnp.float32) / half)
    ang = positions.astype(jnp.float32)[..., None] * inv
    return jnp.cos(ang), jnp.sin(ang)


def apply_rope(x, cos, sin):
    half = x.shape[-1] // 2
    x1, x2 = x[..., :half], x[..., half:]
    out = jnp.concatenate([x1 * cos - x2 * sin, x1 * sin + x2 * cos], axis=-1)
    return out.astype(x.dtype)


def split_columns(z):
    sizes = (N_BRANCH * D_MODEL, MLA_Q_RANK, MLA_KV_RANK, MLA_ROPE,
             ML_WIDTH, ML_WIDTH, ML_WIDTH, N_ML_GATES, MEM_WIDTH)
    idx = np.cumsum(sizes)[:-1].tolist()
    return jnp.split(z, idx, axis=-1)


def mla_attention(c_q, c_kv, k_rope_in, positions, g_q, g_kv, w_uq, w_uk, w_uv):
    B, S, _ = c_q.shape
    cq = rms_norm(c_q, g_q)
    ckv = rms_norm(c_kv, g_kv)
    q = jnp.einsum('bsr,rhd->bshd', cq, w_uq)
    q_nope, q_rope = q[..., :MLA_NOPE], q[..., MLA_NOPE:]
    cos, sin = rope_angles(positions)
    q_rope = apply_rope(q_rope, cos[:, :, None, :], sin[:, :, None, :])
    k_rope = apply_rope(k_rope_in, cos, sin)
    k_nope = jnp.einsum('bsr,rhd->bshd', ckv, w_uk)
    v = jnp.einsum('bsr,rhd->bshd', ckv, w_uv)
    scale = (MLA_NOPE + MLA_ROPE) ** -0.5
    nb = S // Q_BLOCK
    qn_b = q_nope.reshape(B, nb, Q_BLOCK, MLA_HEADS, MLA_NOPE).swapaxes(0, 1)
    qr_b = q_rope.reshape(B, nb, Q_BLOCK, MLA_HEADS, MLA_ROPE).swapaxes(0, 1)

    def block(args):
        qn, qr = args
        s = (jnp.einsum('bqhd,bkhd->bhqk', qn, k_nope)
             + jnp.einsum('bqhd,bkd->bhqk', qr, k_rope))
        p = jax.nn.softmax(s.astype(jnp.float32) * scale, axis=-1).astype(v.dtype)
        return jnp.einsum('bhqk,bkhd->bqhd', p, v)

    o = lax.map(block, (qn_b, qr_b))
    return o.swapaxes(0, 1).reshape(B, S, MLA_WIDTH)


def mlstm_chunkwise(q, k, v, i_pre, f_pre):
    B, H, S, dk = q.shape
    dv = v.shape[-1]
    L = ML_CHUNK
    nc = S // L
    to_chunks = lambda t: jnp.moveaxis(t.reshape(B, H, nc, L, *t.shape[3:]), 2, 0)
    qc, kc, vc = to_chunks(q), to_chunks(k), to_chunks(v)
    lf = to_chunks(jax.nn.log_sigmoid(f_pre))
    ic = to_chunks(i_pre)
    mask = jnp.tril(jnp.ones((L, L), dtype=bool))

    def step(carry, inp):
        C, n, m = carry
        qb, kb, vb, lfb, ib = inp
        b = jnp.cumsum(lfb, axis=-1)
        dmat = jnp.where(mask, b[..., :, None] - b[..., None, :] + ib[..., None, :], -jnp.inf)
        inter = b + m[..., None]
        m_t = jnp.maximum(inter, dmat.max(axis=-1))
        w_inter = jnp.exp(inter - m_t)
        s = jnp.einsum('bhtd,bhsd->bhts', qb, kb) * jnp.exp(dmat - m_t[..., None])
        num = (jnp.einsum('bhts,bhse->bhte', s, vb)
               + w_inter[..., None] * jnp.einsum('bhed,bhtd->bhte', C, qb))
        den = s.sum(axis=-1) + w_inter * jnp.einsum('bhd,bhtd->bht', n, qb)
        h = num / jnp.maximum(jnp.abs(den), jnp.exp(-m_t))[..., None]
        bL = b[..., -1]
        g = bL[..., None] - b + ib
        m_new = jnp.maximum(bL + m, g.max(axis=-1))
        decay = jnp.exp(bL + m - m_new)
        ws = jnp.exp(g - m_new[..., None])
        C_new = decay[..., None, None] * C + jnp.einsum('bhs,bhse,bhsd->bhed', ws, vb, kb)
        n_new = decay[..., None] * n + jnp.einsum('bhs,bhsd->bhd', ws, kb)
        return (C_new, n_new, m_new), h

    init = (jnp.zeros((B, H, dv, dk), jnp.float32),
            jnp.zeros((B, H, dk), jnp.float32),
            jnp.full((B, H), -jnp.inf, jnp.float32))
    _, hs = lax.scan(step, init, (qc, kc, vc, lf, ic))
    return jnp.moveaxis(hs, 0, 2).reshape(B, H, S, dv)


def mlstm_branch(u, v_in, o_pre, gate_pre, conv_w, w_q, w_k, gate_bias, g_head):
    B, S, _ = u.shape
    dtype = u.dtype
    c = lax.conv_general_dilated(u, conv_w[:, None, :], window_strides=(1,), padding='SAME',
                                 dimension_numbers=('NWC', 'WIO', 'NWC'),
                                 feature_group_count=ML_WIDTH)
    c = jax.nn.silu(c).reshape(B, S, ML_HEADS, ML_DK)
    q = jnp.einsum('bshc,hcd->bhsd', c, w_q).astype(jnp.float32)
    k = (jnp.einsum('bshc,hcd->bhsd', c, w_k) * (ML_DK ** -0.5)).astype(jnp.float32)
    v = v_in.reshape(B, S, ML_HEADS, ML_DV).transpose(0, 2, 1, 3).astype(jnp.float32)
    gates = (gate_pre.reshape(B, S, 4, ML_HEADS) + gate_bias).astype(jnp.float32)
    gates = gates.transpose(2, 0, 3, 1)
    flip = lambda t: jnp.flip(t, axis=2)
    q2 = jnp.concatenate([q, flip(q)], axis=1)
    k2 = jnp.concatenate([k, flip(k)], axis=1)
    v2 = jnp.concatenate([v, flip(v)], axis=1)
    i2 = jnp.concatenate([gates[0], flip(gates[2])], axis=1)
    f2 = jnp.concatenate([gates[1], flip(gates[3])], axis=1)
    h2 = mlstm_chunkwise(q2, k2, v2, i2, f2)
    h = h2[:, :ML_HEADS] + flip(h2[:, ML_HEADS:])
    h = h.transpose(0, 2, 1, 3)
    h = h * lax.rsqrt(jnp.mean(h * h, axis=-1, keepdims=True) + EPS)
    h = h.reshape(B, S, ML_WIDTH) * g_head.astype(jnp.float32)
    return (jax.nn.sigmoid(o_pre.astype(jnp.float32)) * h).astype(dtype)


def memory_attention(q_in, mem_n, w_kv):
    B, S, _ = q_in.shape
    M = mem_n.shape[1]
    q = q_in.reshape(B, S, MEM_HEADS, MEM_HEAD_DIM)
    kv = mem_n @ w_kv
    k = kv[..., :MEM_WIDTH].reshape(B, M, MEM_HEADS, MEM_HEAD_DIM)
    v = kv[..., MEM_WIDTH:].reshape(B, M, MEM_HEADS, MEM_HEAD_DIM)
    s = jnp.einsum('bshd,bmhd->bhsm', q, k).astype(jnp.float32) * (MEM_HEAD_DIM ** -0.5)
    p = jax.nn.softmax(s, axis=-1).astype(v.dtype)
    return jnp.einsum('bhsm,bmhd->bshd', p, v).reshape(B, S, MEM_WIDTH)


def hybrid_layer(x, mem, positions, g_mix, w_in, mla_g_q, mla_g_kv, mla_w_uq, mla_w_uk, mla_w_uv,
                 ml_conv_w, ml_w_q, ml_w_k, ml_gate_bias, ml_g_head, mem_g, mem_w_kv,
                 w_branch, w_out, g_ffn, w_ffn_gate, w_ffn_up, w_ffn_down):
    B, S, D = x.shape
    h = rms_norm(x, g_mix)
    z = h @ w_in
    (z_gate, z_cq, z_ckv, z_kr, z_mu, z_mv, z_mo, z_mg, z_memq) = split_columns(z)
    gates = jax.nn.sigmoid(z_gate.astype(jnp.float32)).reshape(B, S, N_BRANCH, D).astype(x.dtype)
    o_mla = mla_attention(z_cq, z_ckv, z_kr, positions, mla_g_q, mla_g_kv, mla_w_uq, mla_w_uk, mla_w_uv)
    o_ml = mlstm_branch(z_mu, z_mv, z_mo, z_mg, ml_conv_w, ml_w_q, ml_w_k, ml_gate_bias, ml_g_head)
    o_mem = memory_attention(z_memq, rms_norm(mem, mem_g), mem_w_kv)
    merged = (gates[:, :, 0] * (o_mla @ w_branch[0])
              + gates[:, :, 1] * (o_ml @ w_branch[1])
              + gates[:, :, 2] * (o_mem @ w_branch[2]))
    x = x + merged @ w_out
    h2 = rms_norm(x, g_ffn)
    ffn = (jax.nn.silu(h2 @ w_ffn_gate) * (h2 @ w_ffn_up)) @ w_ffn_down
    return x + ffn


def setup_inputs(seed: int = 0) -> dict:
    key = jax.random.key(seed)
    ks = jax.random.split(key, 32)
    f32 = jnp.float32
    nrm = lambda k, shape, fan_in: jax.random.normal(k, shape, f32) * (fan_in ** -0.5)
    gain = lambda k, shape: 1.0 + 0.02 * jax.random.normal(k, shape, f32)
    x = jax.random.normal(ks[0], (BATCH, SEQ, D_MODEL), f32)
    mem = jax.random.normal(ks[1], (BATCH, MEM_LEN, D_MODEL), f32)
    offsets = jax.random.randint(ks[2], (BATCH, 1), 0, 4096, dtype=jnp.int32)
    positions = offsets + jnp.arange(SEQ, dtype=jnp.int32)[None, :]
    f_base = jnp.linspace(3.0, 6.0, ML_HEADS, dtype=f32)
    i_base = jnp.zeros((ML_HEADS,), f32)
    gate_base = jnp.stack([i_base, f_base, i_base, f_base])
    ml_gate_bias = gate_base[None] + 0.01 * jax.random.normal(ks[3], (DEPTH, 4, ML_HEADS), f32)
    return {
        "x": x,
        "mem": mem,
        "positions": positions,
        "g_mix": gain(ks[4], (DEPTH, D_MODEL)),
        "w_in": nrm(ks[5], (DEPTH, D_MODEL, N_IN), D_MODEL),
        "mla_g_q": gain(ks[6], (DEPTH, MLA_Q_RANK)),
        "mla_g_kv": gain(ks[7], (DEPTH, MLA_KV_RANK)),
        "mla_w_uq": nrm(ks[8], (DEPTH, MLA_Q_RANK, MLA_HEADS, MLA_NOPE + MLA_ROPE), MLA_Q_RANK),
        "mla_w_uk": nrm(ks[9], (DEPTH, MLA_KV_RANK, MLA_HEADS, MLA_NOPE), MLA_KV_RANK),
        "mla_w_uv": nrm(ks[10], (DEPTH, MLA_KV_RANK, MLA_HEADS, MLA_V), MLA_KV_RANK),
        "ml_conv_w": nrm(ks[11], (DEPTH, CONV_WIDTH, ML_WIDTH), CONV_WIDTH),
        "ml_w_q": nrm(ks[12], (DEPTH, ML_HEADS, ML_DK, ML_DK), ML_DK),
        "ml_w_k": nrm(ks[13], (DEPTH, ML_HEADS, ML_DK, ML_DK), ML_DK),
        "ml_gate_bias": ml_gate_bias,
        "ml_g_head": gain(ks[14], (DEPTH, ML_WIDTH)),
        "mem_g": gain(ks[15], (DEPTH, D_MODEL)),
        "mem_w_kv": nrm(ks[16], (DEPTH, D_MODEL, 2 * MEM_WIDTH), D_MODEL),
        "w_branch": nrm(ks[17], (DEPTH, N_BRANCH, BRANCH_WIDTH, D_MODEL), BRANCH_WIDTH),
        "w_out": nrm(ks[18], (DEPTH, D_MODEL, D_MODEL), D_MODEL),
        "g_ffn": gain(ks[19], (DEPTH, D_MODEL)),
        "w_ffn_gate": nrm(ks[20], (DEPTH, D_MODEL, D_FF), D_MODEL),
        "w_ffn_up": nrm(ks[21], (DEPTH, D_MODEL, D_FF), D_MODEL),
        "w_ffn_down": nrm(ks[22], (DEPTH, D_FF, D_MODEL), D_FF),
        "g_final": gain(ks[23], (D_MODEL,)),
    }


def reference(x, mem, positions, g_mix, w_in, mla_g_q, mla_g_kv, mla_w_uq, mla_w_uk, mla_w_uv,
              ml_conv_w, ml_w_q, ml_w_k, ml_gate_bias, ml_g_head, mem_g, mem_w_kv,
              w_branch, w_out, g_ffn, w_ffn_gate, w_ffn_up, w_ffn_down, g_final):
    for l in range(DEPTH):
        x = hybrid_layer(x, mem, positions, g_mix[l], w_in[l], mla_g_q[l], mla_g_kv[l],
                         mla_w_uq[l], mla_w_uk[l], mla_w_uv[l], ml_conv_w[l], ml_w_q[l],
                         ml_w_k[l], ml_gate_bias[l], ml_g_head[l], mem_g[l], mem_w_kv[l],
                         w_branch[l], w_out[l], g_ffn[l], w_ffn_gate[l], w_ffn_up[l],
                         w_ffn_down[l])
    return rms_norm(x, g_final)
```

```python
import contextlib
import math
import numpy as np
import concourse.bass as bass
import concourse.mybir as mybir
from concourse.bass_utils import run_bass_kernel_spmd

F32 = mybir.dt.float32
BF16 = mybir.dt.bfloat16
I32 = mybir.dt.int32
AF = mybir.ActivationFunctionType
ALU = mybir.AluOpType

NCORES = 8
SEQ_PER_CORE = 4
S = 2048
D = 1024
NT = S // 128
EPS = 1e-6
N_IN = 5824
C_CQ, C_MU, C_MV, C_MO, C_MG, C_MQ = 3072, 3744, 4256, 4768, 5280, 5312
DFF = 2816
NFF = DFF // 128
K_ID, K_MF, K_MB, K_TF, K_TB, K_I0, K_I1, K_RP = 0, 128, 192, 256, 384, 512, 640, 768
K_N = 772


class Trk:
    __slots__ = ("w", "r")

    def __init__(self):
        self.w = None
        self.r = {}


class Eng:
    def __init__(self, sch, name, h):
        self.sch = sch
        self.name = name
        self.h = h
        self.seen = {}
        self.pending = False
        self.semidx = sch.newsem()
        self.cnt = 0


class Sched:
    def __init__(self, nc, es):
        self.nc = nc
        self.es = es
        self.sems = []
        self.trk = {}
        self.dsem = {}
        self.final = []
        self.alias = {}
        self.ninst = 0
        self.pe = Eng(self, "pe", nc.tensor)
        self.dve = Eng(self, "dve", nc.vector)
        self.act = Eng(self, "act", nc.scalar)
        self.pool = Eng(self, "pool", nc.gpsimd)
        self.sp = Eng(self, "sp", nc.sync)

    def newsem(self):
        h = self.es.enter_context(self.nc.semaphore("s%d" % len(self.sems)))
        self.sems.append(h)
        return len(self.sems) - 1

    def _exp(self, keys):
        out = []
        for k in keys:
            a = self.alias.get(k)
            if a is None:
                out.append(k)
            else:
                out.extend(a)
        return out

    def set_alias(self, key, lo, hi):
        self.alias[key] = [("TW", g) for g in range(lo // 512, (hi + 511) // 512)]

    def _deps(self, reads, writes):
        d = {}
        for k in reads:
            t = self.trk.get(k)
            if t is not None and t.w is not None and d.get(t.w[0], 0) < t.w[1]:
                d[t.w[0]] = t.w[1]
        for k in writes:
            t = self.trk.get(k)
            if t is None:
                continue
            if t.w is not None and d.get(t.w[0], 0) < t.w[1]:
                d[t.w[0]] = t.w[1]
            for si, v in t.r.items():
                if d.get(si, 0) < v:
                    d[si] = v
        return d

    def _wait(self, eng, d, skip_own=False):
        for si, v in d.items():
            if skip_own and si == eng.semidx:
                continue
            if eng.seen.get(si, 0) >= v:
                continue
            eng.h.wait_ge(self.sems[si], v)
            self.ninst += 1
            eng.seen[si] = v

    def _mark(self, tok, reads, writes):
        for k in reads:
            t = self.trk.get(k)
            if t is None:
                t = self.trk[k] = Trk()
            if t.r.get(tok[0], 0) < tok[1]:
                t.r[tok[0]] = tok[1]
        for k in writes:
            t = self.trk.get(k)
            if t is None:
                t = self.trk[k] = Trk()
            t.w = tok
            t.r = {}

    def op(self, eng, fn, reads=(), writes=(), sig=True):
        reads = self._exp(reads)
        writes = self._exp(writes)
        d = self._deps(reads, writes)
        self._wait(eng, d, skip_own=(eng is self.pe))
        ins = fn()
        self.ninst += 1
        if sig or eng is not self.pe:
            if eng.cnt >= 30000 and not eng.pending:
                eng.semidx = self.newsem()
                eng.cnt = 0
            eng.cnt += 1
            ins.then_inc(self.sems[eng.semidx], 1)
            tok = (eng.semidx, eng.cnt)
            eng.pending = False
        else:
            tok = (eng.semidx, eng.cnt + 1)
            eng.pending = True
        self._mark(tok, reads, writes)
        return tok

    def dma(self, q, out, in_, reads=(), writes=(), grp=None, final=False):
        reads = self._exp(reads)
        writes = self._exp(writes)
        d = self._deps(reads, writes)
        g = self.dsem.get(grp)
        if g is None:
            g = self.dsem[grp] = [self.newsem(), 0]
        d.pop(g[0], None)
        self._wait(q, d)
        ins = q.h.dma_start(out=out, in_=in_)
        self.ninst += 1
        g[1] += 16
        ins.then_inc(self.sems[g[0]], 16)
        tok = (g[0], g[1])
        self._mark(tok, reads, writes)
        if final:
            self.final.append(tok)
        return tok

    def retoken(self, keys, tok):
        for k in keys:
            t = self.trk.get(k)
            if t is None:
                t = self.trk[k] = Trk()
            t.w = tok

    def barrier(self):
        engs = [self.pe, self.dve, self.act, self.pool]
        assert not self.pe.pending
        for e in engs + [self.sp]:
            d = {}
            for o in engs:
                if o is not e and o.cnt > 0:
                    d[o.semidx] = o.cnt
            self._wait(e, d)

    def finish(self):
        d = {}
        for si, v in self.final:
            if d.get(si, 0) < v:
                d[si] = v
        self._wait(self.sp, d)


class StopBuild(Exception):
    pass


def build_program(nseq=SEQ_PER_CORE, dbg=False, stage=99):
    nc = bass.Bass("TRN2", target_bir_lowering=False)
    es = contextlib.ExitStack()
    with es:
        sch = Sched(nc, es)
        try:
            _emit(nc, es, nseq, dbg, sch, stage)
        except StopBuild:
            pass
        sch.finish()
        _emit.ninst = sch.ninst
        _emit.nsem = len(sch.sems)
    return nc


def _emit(nc, es, nseq, dbg, sch, stage):
    pe, dve, act, pool, sp = sch.pe, sch.dve, sch.act, sch.pool, sch.sp
    T, V, A, G = nc.tensor, nc.vector, nc.scalar, nc.gpsimd

    def din(name, shape, dt=F32):
        return nc.dram_tensor(name, list(shape), dt, kind="ExternalInput").ap()

    def dscr(name, shape, dt=BF16):
        return nc.dram_tensor(name, list(shape), dt, kind="Internal").ap()

    x_d = din("x", [SEQ_PER_CORE, S, D])
    mem_d = din("mem", [SEQ_PER_CORE, 256, D])
    pos_d = din("positions", [SEQ_PER_CORE, S], I32)
    gmix_d = din("g_mix", [1, D])
    win_d = din("w_in", [D, N_IN])
    gq_d = din("mla_g_q", [1, 384])
    gkv_d = din("mla_g_kv", [1, 256])
    wuq_d = din("mla_w_uq", [384, 768])
    wuk_d = din("mla_w_uk", [256, 512])
    wuv_d = din("mla_w_uv", [256, 512])
    convw_d = din("ml_conv_w", [5, 512])
    wq_d = din("ml_w_q", [512, 64])
    wk_d = din("ml_w_k", [512, 64])
    gbias_d = din("ml_gate_bias", [1, 32])
    ghead_d = din("ml_g_head", [1, 512])
    memg_d = din("mem_g", [1, D])
    wkv_d = din("mem_w_kv", [D, 1024])
    wbr_d = din("w_branch", [1536, D])
    wout_d = din("w_out", [D, D])
    gffn_d = din("g_ffn", [1, D])
    wg_d = din("w_ffn_gate", [D, DFF])
    wu_d = din("w_ffn_up", [D, DFF])
    wd_d = din("w_ffn_down", [DFF, D])
    gfin_d = din("g_final", [1, D])
    cst_d = din("cst", [128, K_N])
    out_d = nc.dram_tensor("out", [SEQ_PER_CORE, S, D], F32, kind="ExternalOutput").ap()
    if dbg:
        dbg_d = nc.dram_tensor("dbg_oT", [3, 128, 4, S], BF16, kind="ExternalOutput").ap()

    win_b = dscr("win_b", [D, N_IN])
    wuq_b = dscr("wuq_b", [384, 768])
    wuk_b = dscr("wuk_b", [256, 512])
    wuv_b = dscr("wuv_b", [256, 512])
    wq_b = dscr("wq_b", [512, 64])
    wk_b = dscr("wk_b", [512, 64])
    wkv_b = dscr("wkv_b", [D, 1024])
    wbr_b = dscr("wbr_b", [1536, D])
    wout_b = dscr("wout_b", [D, D])
    wg_b = dscr("wg_b", [D, DFF])
    wu_b = dscr("wu_b", [D, DFF])
    wd_b = dscr("wd_b", [DFF, D])
    oT_d = dscr("oT_scr", [3, 128, 4, S])

    dumps = {}

    def dump(name, ap, reads):
        shp = [int(v) for v in ap.shape]
        t = nc.dram_tensor("dmp_" + name, shp, ap.dtype, kind="ExternalOutput").ap()
        sch.dma(sp, t, ap, reads=reads, grp=("dmp", name), final=True)

    def checkpoint(st):
        if stage <= st:
            sch.barrier()
            raise StopBuild()

    def sbt(name, cols, dt):
        return es.enter_context(nc.sbuf_tensor(name, [128, cols], dt))

    T_hT = sbt("hT", 8 * S, BF16)
    T_big = sbt("big", 4 * 2080, F32)
    T_A = sbt("ra", 11264, BF16)
    T_V = sbt("rv", 16 * 520, BF16)
    T_C = sbt("rc", 16 * 512, BF16)
    T_W = sbt("rw", 13824, BF16)
    xs = sbt("xs", 2 * 1024, F32)
    xn = sbt("xn", 2 * 1024, BF16)
    cst = sbt("cstt", K_N, F32)
    identb = sbt("identb", 128, BF16)
    gfin_bc = sbt("gfin_bc", 1024, F32)
    ghead_bc = sbt("ghead_bc", 512, F32)
    gbias_bc = sbt("gbias_bc", 32, F32)
    gvecT = sbt("gvecT", 32, F32)
    convw = sbt("convw", 20, F32)
    stat = sbt("stat", 64, F32)
    epsb = sbt("epsb", 1, F32)
    oneb = sbt("oneb", 1, F32)
    gA = sbt("gA", 16 * 16, F32)
    gE = sbt("gE", 16 * 16, F32)
    gWt = sbt("gWt", 16 * 16, F32)
    gDec = sbt("gDec", 16 * 32, F32)
    gtmp = sbt("gtmp", 128, F32)
    kst = sbt("kst", 2 * 96, BF16)
    PTt = sbt("PTt", 3 * 512, BF16)
    tmpf = sbt("tmpf", 2 * 512, F32)
    stg = sbt("stg", 2 * 512, BF16)
    posi = sbt("posi", S, I32)
    posf = sbt("posf", S, F32)

    hT = T_hT[:].rearrange("p (k t) -> p k t", k=8)
    bigb = T_big[:].bitcast(BF16)

    pbk = [es.enter_context(nc.psum_tensor("pb%d" % i, [128, 512], F32)) for i in range(6)]
    ptk = [es.enter_context(nc.psum_tensor("pt%d" % i, [128, 1024], BF16)) for i in range(2)]
    rot = {}

    def bank(grp, ids):
        i = rot.get(grp, 0)
        rot[grp] = i + 1
        b = ids[i % len(ids)]
        return pbk[b], ("pb", b)

    def tbank():
        i = rot.get("pt", 0)
        rot["pt"] = i + 1
        return ptk[i % 2], ("pt", i % 2)

    sch.dma(sp, cst[:], cst_d, writes=["cst"], grp="cst")
    sch.dma(pool, identb[:], cst_d[:, K_ID:K_ID + 128], writes=["ident"], grp="ident")
    sch.dma(sp, gfin_bc[:], gfin_d.partition_broadcast(128), writes=["gfin"], grp="c2")
    sch.dma(sp, ghead_bc[:], ghead_d.partition_broadcast(128), writes=["ghead"], grp="c3")
    sch.dma(sp, gbias_bc[:], gbias_d.partition_broadcast(128), writes=["gbias"], grp="c4")
    with nc.allow_non_contiguous_dma("tiny gain-vector transposes"):
        sch.dma(sp, gvecT[:, 0:8], gmix_d.rearrange("o (k p) -> p (o k)", p=128), writes=["gv0"], grp="c5")
        sch.dma(sp, gvecT[:, 8:16], gffn_d.rearrange("o (k p) -> p (o k)", p=128), writes=["gv1"], grp="c6")
        sch.dma(sp, gvecT[:, 16:24], memg_d.rearrange("o (k p) -> p (o k)", p=128), writes=["gv2"], grp="c7")
        sch.dma(sp, gvecT[:, 24:27], gq_d.rearrange("o (k p) -> p (o k)", p=128), writes=["gv3"], grp="c8")
        sch.dma(sp, gvecT[:, 27:29], gkv_d.rearrange("o (k p) -> p (o k)", p=128), writes=["gv4"], grp="c9")
        for ct_ in range(4):
            sch.dma(sp, convw[:, ct_ * 5:(ct_ + 1) * 5], convw_d[:, ct_ * 128:(ct_ + 1) * 128].rearrange("j p -> p j"),
                    writes=["convw"], grp="c10")
    GV = ["gv0", "gv1", "gv2", "gv3", "gv4"]
    sch.op(dve, lambda: V.memset(epsb[:], EPS), writes=["epsb"])
    sch.op(dve, lambda: V.memset(kst[:], 0.0), writes=[("kst", 0), ("kst", 1)])
    sch.op(dve, lambda: V.memset(oneb[:], 1.0), writes=["oneb"])

    def conv_w(dst, src, rows, key):
        tok = None
        r0 = 0
        while r0 < rows:
            r1 = min(rows, r0 + 128)
            tok = sch.dma(pool, dst[r0:r1, :], src[r0:r1, :], grp=("cv", key))
            r0 = r1
        sch.retoken([("w", key)], tok)

    conv_w(win_b, win_d, D, "win")
    conv_w(wuq_b, wuq_d, 384, "wuq")
    conv_w(wuk_b, wuk_d, 256, "wuk")
    conv_w(wuv_b, wuv_d, 256, "wuv")
    conv_w(wq_b, wq_d, 512, "wq")
    conv_w(wk_b, wk_d, 512, "wk")
    conv_w(wkv_b, wkv_d, D, "wkv")
    conv_w(wbr_b, wbr_d, 1536, "wbr")
    conv_w(wout_b, wout_d, D, "wout")
    conv_w(wg_b, wg_d, D, "wg")
    conv_w(wu_b, wu_d, D, "wu")
    conv_w(wd_b, wd_d, DFF, "wd")

    checkpoint(0)

    def wload(dst, src2d, key, wkey, kc):
        if kc == 1:
            srcv = src2d
        else:
            srcv = src2d.rearrange("(k p) n -> p k n", p=128)
        sch.dma(sp, dst, srcv, reads=[("w", wkey)], writes=[key], grp=key)

    def rms_to_T(src_ap, src_keys, gcol, dst_fn, dst_keys, slot):
        st = stat[:, slot * 4:slot * 4 + 1]
        st2 = stat[:, slot * 4 + 1:slot * 4 + 2]
        xnv = xn[:, slot * 1024:(slot + 1) * 1024]
        sch.op(act, lambda: A.activation(out=xnv, in_=src_ap, func=AF.Square, scale=1.0 / 32.0, accum_out=st),
               reads=src_keys, writes=[("xn", slot), ("st", slot)])
        sch.op(act, lambda: A.activation(out=st2, in_=st, func=AF.Sqrt, bias=epsb[:], scale=1.0),
               reads=[("st", slot), "epsb"], writes=[("st2", slot)])
        sch.op(dve, lambda: V.reciprocal(out=st2, in_=st2), reads=[("st2", slot)], writes=[("st2", slot)])
        sch.op(act, lambda: A.activation(out=xnv, in_=src_ap, func=AF.Copy, scale=st2),
               reads=list(src_keys) + [("st2", slot)], writes=[("xn", slot)])
        pt, ptkey = tbank()
        ptv = pt[:].rearrange("p (k t) -> p k t", k=8)
        for kc in range(8):
            sch.op(pe, lambda kc=kc: T.transpose(ptv[:, kc, :], xnv[:, kc * 128:(kc + 1) * 128], identb[:]),
                   reads=[("xn", slot), "ident"], writes=[ptkey], sig=(kc == 7))
        gb = gvecT[:, gcol:gcol + 8].unsqueeze(2).broadcast_to([128, 8, 128])
        sch.op(dve, lambda: V.tensor_tensor(out=dst_fn(), in0=ptv, in1=gb, op=ALU.mult),
               reads=[ptkey] + GV, writes=dst_keys)

    def mm_acc(out_ap, pairs, okey, rkeys):
        n = len(pairs)
        for i, (l, r) in enumerate(pairs):
            sch.op(pe, lambda l=l, r=r, i=i: T.matmul(out_ap, lhsT=l, rhs=r, start=(i == 0), stop=(i == n - 1)),
                   reads=rkeys, writes=[okey], sig=(i == n - 1))

    def htkeys(blk):
        return [("hT", blk * 4 + i) for i in range(4)]

    for b in range(nseq):
        for tt in range(NT):
            slot = tt % 2
            xsv = xs[:, slot * 1024:(slot + 1) * 1024]
            sch.dma(sp, xsv, x_d[b, tt * 128:(tt + 1) * 128, :], writes=[("xs", slot)], grp=("xs", slot))
            rms_to_T(xsv, [("xs", slot)], 0, lambda tt=tt: hT[:, :, tt * 128:(tt + 1) * 128], [("hT", tt)], slot)

        if stage == 1:
            dump("hT", T_hT[:], [("hT", t) for t in range(NT)])
        checkpoint(1)
        WinB = T_W[:, 0:5376].rearrange("p (k n) -> p k n", k=8)
        Wuq = T_W[:, 5376:7680].rearrange("p (k n) -> p k n", k=3)
        WuqS = T_W[:, 7680:9984].rearrange("p (k n) -> p k n", k=3)
        Wuk = T_W[:, 9984:11008].rearrange("p (k n) -> p k n", k=2)
        Wuv = T_W[:, 11008:12032].rearrange("p (k n) -> p k n", k=2)
        for nm, lo, hi in (("W0", 0, 5376), ("W1", 5376, 7680), ("W2", 7680, 9984), ("W3", 9984, 11008), ("W4", 11008, 12032)):
            sch.set_alias(nm, lo, hi)
        wload(WinB, win_b[:, C_CQ:C_CQ + 672], "W0", "win", 8)
        wload(Wuq, wuq_b, "W1", "wuq", 3)
        wuq_b3 = wuq_b.rearrange("(k p) (h c) -> p k h c", p=128, c=96)
        WuqS4 = WuqS.rearrange("p k (h c) -> p k h c", c=96)
        for kc in range(3):
            sch.dma(sp, WuqS4[:, kc, :, 0:64], wuq_b3[:, kc, :, 0:64], reads=[("w", "wuq")], writes=["W2"], grp="W2")
            sch.dma(sp, WuqS4[:, kc, :, 64:80], wuq_b3[:, kc, :, 80:96], reads=[("w", "wuq")], writes=["W2"], grp="W2")
            tk = sch.dma(sp, WuqS4[:, kc, :, 80:96], wuq_b3[:, kc, :, 64:80], reads=[("w", "wuq")], writes=["W2"], grp="W2")
        wload(Wuk, wuk_b, "W3", "wuk", 2)
        wload(Wuv, wuv_b, "W4", "wuv", 2)
        W2K = ["W2"]

        cqnT = T_A[:, 0:6144].rearrange("p (k t) -> p k t", k=3)
        ckvnT = T_A[:, 6144:10240].rearrange("p (k t) -> p k t", k=2)
        KhT = [bigb[:, 0:2048], bigb[:, 2048:4096]]
        QhT = [bigb[:, 4096:6144], bigb[:, 6144:8192]]
        cosT = bigb[:, 8192:10240]
        sinT = bigb[:, 10240:12288]
        zst = [bigb[:, 12288:12960], bigb[:, 12960:13632]]
        Vaug = T_V[:].rearrange("p (t h c) -> p t h c", t=16, h=8)
        otok = T_C[:].rearrange("p (t c) -> p t c", t=16)

        sch.dma(sp, posi[:], pos_d[b:b + 1, :].partition_broadcast(128), writes=["posi"], grp="posi")
        sch.op(dve, lambda: V.tensor_copy(out=posf[64:96, :], in_=posi[64:96, :]), reads=["posi"], writes=["posf"])
        TWO_PI = 2.0 * math.pi
        for (tab, offc, key) in ((cosT, K_RP + 2, "cosT"), (sinT, K_RP + 1, "sinT")):
            for q4 in range(2):
                seg = slice(q4 * 1024, (q4 + 1) * 1024)
                yv = tmpf[64:96, :]
                ki = posi[64:96, 0:1024]
                kf = xs[64:96, 0:1024]
                mv = xs[64:96, 1024:2048]
                sch.op(dve, lambda: V.tensor_scalar(out=yv, in0=posf[64:96, seg], scalar1=cst[64:96, K_RP:K_RP + 1],
                                                    scalar2=cst[64:96, offc:offc + 1], op0=ALU.mult, op1=ALU.add),
                       reads=["posf", "cst"], writes=["tmpf"])
                sch.op(dve, lambda: V.tensor_scalar(out=ki, in0=yv, scalar1=1.0 / TWO_PI, scalar2=0.5, op0=ALU.mult, op1=ALU.add),
                       reads=["tmpf", "posf"], writes=["posi"])
                sch.op(dve, lambda: V.tensor_copy(out=kf, in_=ki), reads=["posi"], writes=[("xs", 0)])
                sch.op(dve, lambda: V.scalar_tensor_tensor(out=yv, in0=kf, scalar=-TWO_PI, in1=yv, op0=ALU.mult, op1=ALU.add),
                       reads=[("xs", 0), "tmpf"], writes=["tmpf"])
                sch.op(dve, lambda: V.tensor_scalar(out=mv, in0=yv, scalar1=-math.pi, scalar2=TWO_PI, op0=ALU.is_lt, op1=ALU.mult),
                       reads=["tmpf"], writes=[("xs", 1)])
                sch.op(dve, lambda: V.tensor_tensor(out=yv, in0=yv, in1=mv, op=ALU.add), reads=["tmpf", ("xs", 1)], writes=["tmpf"])
                sch.op(act, lambda: A.activation(out=tab[64:96, seg], in_=yv, func=AF.Sin), reads=["tmpf"], writes=[key])

        sch.op(dve, lambda: V.memset(Vaug[:, :, :, 64:65], 1.0), writes=["Vaug"])

        for tt in range(NT):
            slot = tt % 2
            pz0, k0 = bank("z0", [0, 1])
            pz1, k1 = bank("z1", [2, 3])
            mm_acc(pz0[:, 0:384], [(hT[:, kc, tt * 128:(tt + 1) * 128], WinB[:, kc, 0:384]) for kc in range(8)],
                   k0, [("hT", tt), "W0"])
            mm_acc(pz1[:, 0:288], [(hT[:, kc, tt * 128:(tt + 1) * 128], WinB[:, kc, 384:672]) for kc in range(8)],
                   k1, [("hT", tt), "W0"])
            st = stat[:, 16 + slot * 4:16 + slot * 4 + 2]
            junk = xn[:, slot * 1024:slot * 1024 + 384]
            sch.op(act, lambda: A.activation(out=junk, in_=pz0[:, 0:384], func=AF.Square, scale=384.0 ** -0.5,
                                             accum_out=st[:, 0:1]), reads=[k0], writes=[("xn", slot), ("bst", slot)])
            sch.op(act, lambda: A.activation(out=junk[:, 0:256], in_=pz1[:, 0:256], func=AF.Square, scale=1.0 / 16.0,
                                             accum_out=st[:, 1:2]), reads=[k1], writes=[("xn", slot), ("bst", slot)])
            sch.op(act, lambda: A.activation(out=st, in_=st, func=AF.Sqrt, bias=epsb[:], scale=1.0),
                   reads=[("bst", slot), "epsb"], writes=[("bst", slot)])
            sch.op(dve, lambda: V.reciprocal(out=st, in_=st), reads=[("bst", slot)], writes=[("bst", slot)])
            z = zst[slot]
            sch.op(act, lambda: A.activation(out=z[:, 0:384], in_=pz0[:, 0:384], func=AF.Copy, scale=st[:, 0:1]),
                   reads=[k0, ("bst", slot)], writes=[("zst", slot)])
            sch.op(dve, lambda: V.tensor_scalar(out=z[:, 384:640], in0=pz1[:, 0:256], scalar1=st[:, 1:2], scalar2=None,
                                                op0=ALU.mult), reads=[k1, ("bst", slot)], writes=[("zst", slot)])
            ks = kst[:, slot * 96:(slot + 1) * 96]
            sch.op(act, lambda: A.copy(out=z[:, 640:672], in_=pz1[:, 256:288]), reads=[k1], writes=[("zst", slot)])
            pt, ptkey = tbank()
            ptv = pt[:].rearrange("p (k t) -> p k t", k=8)
            for j in range(5):
                sch.op(pe, lambda j=j: T.transpose(ptv[:, j, :], z[:, j * 128:(j + 1) * 128], identb[:]),
                       reads=[("zst", slot), "ident"], writes=[ptkey], sig=False)
            sch.op(dve, lambda: V.tensor_copy(out=ks[:, 64:96], in_=z[:, 640:672]), reads=[("zst", slot)], writes=[("kst", slot)])
            sch.op(pe, lambda: T.transpose(ptv[0:96, 5, :], ks, identb[:]), reads=[("kst", slot), "ident"], writes=[ptkey])
            sch.op(dve, lambda: V.tensor_copy(out=ks[:, 64:80], in_=z[:, 656:672]), reads=[("zst", slot), ptkey], writes=[("kst", slot)])
            sch.op(dve, lambda: V.tensor_copy(out=ks[:, 80:96], in_=z[:, 640:656]), reads=[("zst", slot)], writes=[("kst", slot)])
            sch.op(pe, lambda: T.transpose(ptv[0:96, 6, :], ks, identb[:]), reads=[("kst", slot), "ident"], writes=[ptkey])
            tsl = slice(tt * 128, (tt + 1) * 128)
            gq = gvecT[:, 24:27].unsqueeze(2).broadcast_to([128, 3, 128])
            gk = gvecT[:, 27:29].unsqueeze(2).broadcast_to([128, 2, 128])
            sch.op(dve, lambda: V.tensor_tensor(out=cqnT[:, :, tsl], in0=ptv[:, 0:3, :], in1=gq, op=ALU.mult),
                   reads=[ptkey] + GV, writes=[("cqnT", tt)])
            sch.op(dve, lambda: V.tensor_tensor(out=ckvnT[:, :, tsl], in0=ptv[:, 3:5, :], in1=gk, op=ALU.mult),
                   reads=[ptkey] + GV, writes=[("ckvnT", tt)])
            t1 = tmpf[64:96, 0:128]
            t2 = tmpf[64:96, 512:640]
            sch.op(dve, lambda: V.tensor_tensor(out=t1, in0=ptv[64:96, 5, :], in1=cosT[64:96, tsl], op=ALU.mult),
                   reads=[ptkey, "cosT"], writes=["tmpf"])
            sch.op(dve, lambda: V.tensor_tensor(out=t2, in0=ptv[64:96, 6, :], in1=sinT[64:96, tsl], op=ALU.mult),
                   reads=[ptkey, "sinT"], writes=["tmpf"])
            sch.op(dve, lambda: V.tensor_tensor(out=KhT[0][64:96, tsl], in0=t1, in1=t2, op=ALU.add),
                   reads=["tmpf"], writes=[("KhT", 0)])
            sch.op(act, lambda: A.copy(out=KhT[1][64:96, tsl], in_=KhT[0][64:96, tsl]),
                   reads=[("KhT", 0)], writes=[("KhT", 1)])
            pv, kv = bank("z0", [0, 1])
            mm_acc(pv[:, :], [(ckvnT[:, kc, tsl], Wuv[:, kc, :]) for kc in range(2)], kv, [("ckvnT", tt), "W4"])
            sch.op(act, lambda: A.copy(out=Vaug[:, tt, :, 0:64], in_=pv[:].rearrange("p (h c) -> p h c", h=8)),
                   reads=[kv], writes=["Vaug"])

        if stage == 2:
            dump("cq", T_A[:, 0:10240], [("cqnT", t) for t in range(NT)] + [("ckvnT", t) for t in range(NT)])
            dump("kh", bigb[:, 0:2048], [("KhT", 0)])
            dump("cos", bigb[:, 8192:12288], ["cosT", "sinT"])
            dump("va", T_V[:], ["Vaug"])
        checkpoint(2)
        ALLCQ = [("cqnT", t) for t in range(NT)]
        ALLCKV = [("ckvnT", t) for t in range(NT)]
        att_scale = 96.0 ** -0.5
        for h in range(8):
            s_ = h % 2
            for blk in range(4):
                bs = slice(blk * 512, (blk + 1) * 512)
                pk, kk = bank("kq", [0, 1])
                mm_acc(pk[0:64, :], [(Wuk[:, kc, h * 64:(h + 1) * 64], ckvnT[:, kc, bs]) for kc in range(2)], kk,
                       ALLCKV[blk * 4:blk * 4 + 4] + ["W3"])
                sch.op(act, lambda: A.copy(out=KhT[s_][0:64, bs], in_=pk[0:64, :]), reads=[kk], writes=[("KhT", s_)])
                pq, kq = bank("kq", [0, 1])
                pqs, kqs = bank("kq2", [2, 3])
                mm_acc(pq[0:96, :], [(Wuq[:, kc, h * 96:(h + 1) * 96], cqnT[:, kc, bs]) for kc in range(3)], kq,
                       ALLCQ[blk * 4:blk * 4 + 4] + ["W1"])
                mm_acc(pqs[0:96, :], [(WuqS[:, kc, h * 96:(h + 1) * 96], cqnT[:, kc, bs]) for kc in range(3)], kqs,
                       ALLCQ[blk * 4:blk * 4 + 4] + W2K)
                sch.op(act, lambda: A.copy(out=QhT[s_][0:64, bs], in_=pq[0:64, :]), reads=[kq], writes=[("QhT", s_)])
                t1 = tmpf[64:96, 0:512]
                t2 = tmpf[64:96, 512:1024]
                sch.op(dve, lambda: V.tensor_tensor(out=t1, in0=pq[64:96, :], in1=cosT[64:96, bs], op=ALU.mult),
                       reads=[kq, "cosT"], writes=["tmpf"])
                sch.op(dve, lambda: V.tensor_tensor(out=t2, in0=pqs[64:96, :], in1=sinT[64:96, bs], op=ALU.mult),
                       reads=[kqs, "sinT"], writes=["tmpf"])
                sch.op(dve, lambda: V.tensor_tensor(out=QhT[s_][64:96, bs], in0=t1, in1=t2, op=ALU.add),
                       reads=["tmpf"], writes=[("QhT", s_)])
            for qb in range(4):
                qs = slice(qb * 512, (qb + 1) * 512)
                pO, kO = bank("pO", [4, 5])
                pOv = pO[:, 0:260].rearrange("p (j c) -> p j c", j=4)
                for kt in range(16):
                    pS, kS = bank("pS", [0, 1, 2, 3])
                    sch.op(pe, lambda: T.matmul(pS[:, :], lhsT=KhT[s_][0:96, kt * 128:(kt + 1) * 128], rhs=QhT[s_][0:96, qs],
                                                start=True, stop=True), reads=[("KhT", s_), ("QhT", s_)], writes=[kS])
                    ps_ = kt % 3
                    P_ = PTt[:, ps_ * 512:(ps_ + 1) * 512]
                    sch.op(act, lambda: A.activation(out=P_, in_=pS[:, :], func=AF.Exp, scale=att_scale),
                           reads=[kS], writes=[("PT", ps_)])
                    for j in range(4):
                        sch.op(pe, lambda j=j: T.matmul(pOv[:, j, :], lhsT=P_[:, j * 128:(j + 1) * 128], rhs=Vaug[:, kt, h, :],
                                                        start=(kt == 0 and j == 0), stop=(kt == 15), skip_group_check=True),
                               reads=[("PT", ps_), "Vaug"], writes=[kO], sig=(j == 3))
                rec = stat[:, 32:36]
                sch.op(dve, lambda: V.reciprocal(out=rec, in_=pOv[:, :, 64]), reads=[kO], writes=["rec"])
                sch.op(dve, lambda: V.tensor_tensor(out=otok[:, qb * 4:(qb + 1) * 4, h * 64:(h + 1) * 64], in0=pOv[:, :, 0:64],
                                                    in1=rec.unsqueeze(2).broadcast_to([128, 4, 64]), op=ALU.mult),
                       reads=[kO, "rec"], writes=[("otok", qb)])

        def flush_oT(br):
            for blk in range(4):
                sl = blk % 2
                sv = stg[:, sl * 512:(sl + 1) * 512]
                for j in range(4):
                    pt, ptkey = tbank()
                    ptv = pt[:].rearrange("p (k t) -> p k t", k=8)
                    for i in range(4):
                        sch.op(pe, lambda i=i: T.transpose(ptv[:, i, :], otok[:, blk * 4 + i, j * 128:(j + 1) * 128], identb[:]),
                               reads=[("otok", blk), "ident"], writes=[ptkey], sig=(i == 3))
                    st_ = stg[:, sl * 512:(sl + 1) * 512]
                    if j % 2 == 0:
                        sch.op(act, lambda: A.copy(out=st_.rearrange("p (i t) -> p i t", i=4), in_=ptv[:, 0:4, :]),
                               reads=[ptkey], writes=[("stg", sl)])
                    else:
                        sch.op(dve, lambda: V.tensor_copy(out=st_.rearrange("p (i t) -> p i t", i=4), in_=ptv[:, 0:4, :]),
                               reads=[ptkey], writes=[("stg", sl)])
                    sch.dma(sp, oT_d[br, :, j, blk * 512:(blk + 1) * 512], st_, reads=[("stg", sl)],
                            writes=[("oTd", br, blk, j)], grp=("stg", sl))
                    sl = (sl + 1) % 2

        if stage == 3:
            dump("otok", T_C[:], [("otok", q) for q in range(4)])
            dump("kh", bigb[:, 0:8192], [("KhT", 0), ("KhT", 1), ("QhT", 0), ("QhT", 1)])
        checkpoint(3)
        flush_oT(0)
        sch.barrier()

        checkpoint(4)
        WinU = T_W[:, 0:4096].rearrange("p (k n) -> p k n", k=8)
        WinVG = T_W[:, 4096:8448].rearrange("p (k n) -> p k n", k=8)
        WqBD = T_W[:, 8448:8960].rearrange("p (j n) -> p j n", j=4)
        WkBD = T_W[:, 8960:9472].rearrange("p (j n) -> p j n", j=4)
        for nm, lo, hi in (("W0", 0, 4096), ("W1", 4096, 8448), ("W1b", 4096, 8448), ("W3", 8448, 8960), ("W4", 8960, 9472)):
            sch.set_alias(nm, lo, hi)
        wload(WinU, win_b[:, C_MU:C_MU + 512], "W0", "win", 8)
        sch.dma(sp, WinVG[:, :, 0:512], win_b[:, C_MV:C_MV + 512].rearrange("(k p) n -> p k n", p=128),
                reads=[("w", "win")], writes=["W1"], grp="W1")
        sch.dma(sp, WinVG[:, :, 512:544], win_b[:, C_MG:C_MG + 32].rearrange("(k p) n -> p k n", p=128),
                reads=[("w", "win")], writes=["W1"], grp="W1")
        sch.op(dve, lambda: V.memset(T_W[:, 8448:9472], 0.0), writes=["W3", "W4"])
        for (Wt, src, key) in ((WqBD, wq_b, "W3"), (WkBD, wk_b, "W4")):
            s4 = src.rearrange("(j r c) d -> r c j d", j=4, r=2)
            for r in range(2):
                sch.dma(sp, Wt[r * 64:(r + 1) * 64, :, r * 64:(r + 1) * 64], s4[r], reads=[("w", "wq"), ("w", "wk")],
                        writes=[key], grp=key)
        uT = T_big[:].rearrange("p (c t) -> p c t", c=4)
        Hh = T_big[:, 0:8192].rearrange("p (t c) -> p t c", t=16)
        cT = T_C[:].rearrange("p (c t) -> p c t", c=4)
        vaug = Vaug
        accf = T_A[:].bitcast(F32)
        sch.op(dve, lambda: V.memset(uT[:, :, 14:16], 0.0), writes=["uTpad"])
        sch.op(dve, lambda: V.memset(uT[:, :, 2064:2066], 0.0), writes=["uTpad"])
        sch.op(dve, lambda: V.memset(vaug[:, :, :, 64:65], 1.0), writes=["Vaug"])
        checkpoint(4.1)
        for ct in range(4):
            for blk in range(4):
                pu, ku = bank("cu", [0, 1])
                mm_acc(pu[:, :], [(WinU[:, kc, ct * 128:(ct + 1) * 128], hT[:, kc, blk * 512:(blk + 1) * 512]) for kc in range(8)],
                       ku, htkeys(blk) + ["W0"])
                sch.op(act, lambda: A.copy(out=uT[:, ct, 16 + blk * 512:16 + (blk + 1) * 512], in_=pu[:, :]),
                       reads=[ku], writes=[("uT", ct)])
        checkpoint(4.2)
        for tt in range(NT):
            pv, kv = bank("cv", [2, 3])
            pg, kg = bank("cg", [4, 5])
            mm_acc(pv[:, :], [(hT[:, kc, tt * 128:(tt + 1) * 128], WinVG[:, kc, 0:512]) for kc in range(8)], kv, [("hT", tt), "W1"])
            mm_acc(pg[:, 0:32], [(hT[:, kc, tt * 128:(tt + 1) * 128], WinVG[:, kc, 512:544]) for kc in range(8)], kg,
                   [("hT", tt), "W1"])
            sch.op(act, lambda: A.copy(out=vaug[:, tt, :, 0:64], in_=pv[:].rearrange("p (h c) -> p h c", h=8)),
                   reads=[kv], writes=["Vaug"])
            Gt = gtmp[:, 0:32]
            sch.op(dve, lambda: V.tensor_tensor(out=Gt, in0=pg[:, 0:32], in1=gbias_bc[:], op=ALU.add),
                   reads=[kg, "gbias"], writes=["Gt"])
            G4 = Gt.rearrange("p (a h) -> p a h", a=4)
            Fv = gtmp[:, 0:32].rearrange("p (d a h) -> p d a h", d=2, a=2)[:, :, 1, :]
            Iv = gtmp[:, 0:32].rearrange("p (d a h) -> p d a h", d=2, a=2)[:, :, 0, :]
            sp_ = gtmp[:, 32:48].rearrange("p (d h) -> p d h", d=2)
            sch.op(act, lambda: A.activation(out=sp_, in_=Fv, func=AF.Exp, scale=-1.0), reads=["Gt"], writes=["spg"])
            sch.op(act, lambda: A.activation(out=sp_, in_=sp_, func=AF.Ln, bias=oneb[:], scale=1.0), reads=["spg", "oneb"], writes=["spg"])
            pc, kc_ = bank("cc", [0, 1])
            sch.op(pe, lambda: T.matmul(pc[:, 0:8], lhsT=cst[:, K_TF:K_TF + 128], rhs=gtmp[:, 32:40], start=True, stop=True),
                   reads=["spg", "cst"], writes=[kc_], sig=False)
            sch.op(pe, lambda: T.matmul(pc[:, 8:16], lhsT=cst[:, K_TB:K_TB + 128], rhs=gtmp[:, 40:48], start=True, stop=True),
                   reads=["spg", "cst"], writes=[kc_], sig=False)
            sch.op(pe, lambda: T.matmul(pc[:, 16:32], lhsT=cst[:, K_I0:K_I0 + 128], rhs=gtmp[:, 32:48], start=True, stop=True),
                   reads=["spg", "cst"], writes=[kc_], sig=False)
            sch.op(pe, lambda: T.matmul(pc[:, 32:48], lhsT=cst[:, K_I1:K_I1 + 128], rhs=gtmp[:, 32:48], start=True, stop=True),
                   reads=["spg", "cst"], writes=[kc_])
            tsl16 = slice(tt * 16, (tt + 1) * 16)
            ic = gtmp[:, 48:64].rearrange("p (d h) -> p d h", d=2)
            sch.op(dve, lambda: V.tensor_tensor(out=ic, in0=Iv, in1=pc[:, 0:16].rearrange("p (d h) -> p d h", d=2), op=ALU.add),
                   reads=["Gt", kc_], writes=["ic"])
            sch.op(act, lambda: A.activation(out=gA[:, tsl16], in_=gtmp[:, 48:64], func=AF.Exp), reads=["ic"], writes=[("gA", tt)])
            sch.op(act, lambda: A.activation(out=gE[:, tsl16], in_=pc[:, 0:16], func=AF.Exp, scale=-1.0), reads=[kc_], writes=[("gE", tt)])
            sch.op(act, lambda: A.activation(out=gDec[:, tt * 32:(tt + 1) * 32], in_=pc[:, 16:48], func=AF.Exp, scale=-1.0),
                   reads=[kc_], writes=[("gDec", tt)])
            sch.op(dve, lambda: V.tensor_tensor(out=gWt[0:64, tsl16], in0=gA[0:64, tsl16], in1=gDec[0:64, tt * 32:tt * 32 + 16], op=ALU.mult),
                   reads=[("gA", tt), ("gDec", tt)], writes=[("gW", tt)])
            sch.op(dve, lambda: V.tensor_tensor(out=gWt[64:128, tsl16], in0=gA[64:128, tsl16], in1=gDec[64:128, tt * 32 + 16:tt * 32 + 32],
                                                op=ALU.mult), reads=[("gA", tt), ("gDec", tt)], writes=[("gW", tt)])
        checkpoint(4.5)
        for ct in range(4):
            eng, E_ = dve, V
            acc = accf[:, (ct % 2) * 2048:(ct % 2 + 1) * 2048]
            ak = ("acc", ct % 2)
            sch.op(eng, lambda: E_.tensor_scalar(out=acc, in0=uT[:, ct, 14:14 + 2048], scalar1=convw[:, ct * 5:ct * 5 + 1], scalar2=None,
                                                 op0=ALU.mult), reads=[("uT", ct), "uTpad", "convw"], writes=[ak])
            for j in range(1, 5):
                sch.op(eng, lambda j=j: E_.scalar_tensor_tensor(out=acc, in0=uT[:, ct, 14 + j:14 + j + 2048],
                                                                scalar=convw[:, ct * 5 + j:ct * 5 + j + 1], in1=acc,
                                                                op0=ALU.mult, op1=ALU.add),
                       reads=[("uT", ct), "uTpad", "convw", ak], writes=[ak])
            sch.op(act, lambda: A.activation(out=cT[:, ct, :], in_=acc, func=AF.Silu), reads=[ak], writes=[("cT", ct)])
        sch.barrier()

        if stage == 5:
            dump("cT", T_C[:], [("cT", c_) for c_ in range(4)])
            dump("ga", gA[:], [("gA", t) for t in range(NT)])
            dump("ge", gE[:], [("gE", t) for t in range(NT)])
            dump("gw", gWt[:], [("gW", t) for t in range(NT)])
            dump("gd", gDec[:], [("gDec", t) for t in range(NT)])
        checkpoint(5)
        def tA(off, n):
            return T_A[:, off:off + n]
        qTt = [tA(d * 512, 512).rearrange("p (j t) -> p j t", j=4) for d in range(2)]
        qTz = [[tA(1024 + (d * 2 + r) * 512, 512).rearrange("p (j t) -> p j t", j=4) for r in range(2)] for d in range(2)]
        kTz = [[tA(3072 + (d * 2 + r) * 512, 512).rearrange("p (j t) -> p j t", j=4) for r in range(2)] for d in range(2)]
        ktk = [tA(5120 + d * 512, 512) for d in range(2)]
        PTm = [tA(6144 + i * 1024, 1024).bitcast(F32) for i in range(2)]
        vaT = [tA(8192 + i * 1040, 1040).bitcast(F32).rearrange("p (h c) -> p h c", h=8) for i in range(2)]
        vwz = [T_W[:, 11288 + i * 520:11288 + (i + 1) * 520].rearrange("p (h c) -> p h c", h=8) for i in range(2)]
        Cbf = [T_W[:, 10768 + d * 260:10768 + (d + 1) * 260].rearrange("p (j c) -> p j c", j=4) for d in range(2)]
        Cst = [T_W[:, 9728 + d * 520:9728 + (d + 1) * 520].bitcast(F32).rearrange("p (j c) -> p j c", j=4) for d in range(2)]
        pm = [cst[:, K_I0:K_I0 + 1], cst[:, K_I1:K_I1 + 1]]
        pm8 = stat[:, 28:30]
        sch.op(dve, lambda: V.tensor_scalar(out=pm8[:, 0:1], in0=pm[0], scalar1=0.125, scalar2=None, op0=ALU.mult), reads=["cst"], writes=["pm8"])
        sch.op(dve, lambda: V.tensor_scalar(out=pm8[:, 1:2], in0=pm[1], scalar1=0.125, scalar2=None, op0=ALU.mult), reads=["cst", "pm8"], writes=["pm8"])
        for d in range(2):
            sch.op(dve, lambda d=d: V.memset(Cst[d], 0.0), writes=[("Cst", d)])
            sch.op(dve, lambda d=d: V.memset(Cbf[d], 0.0), writes=[("Cbf", d)])
            sch.op(dve, lambda d=d: V.memset(PTm[d], 0.0), writes=[("PTm", d)])
            sch.op(dve, lambda d=d: V.memset(vwz[d], 0.0), writes=[("vw", d)])
        sch.op(dve, lambda: V.memset(Hh, 0.0), writes=[("H", t) for t in range(NT)])
        cnt2 = [0]
        for it in range(32):
            for d in range(2):
                c = it if d == 0 else 31 - it
                m = c // 2
                r0 = c % 2
                rs = slice(r0 * 64, (r0 + 1) * 64)
                new_tile = (it % 2 == 0)
                tsl = slice(m * 128, (m + 1) * 128)
                if new_tile:
                    pq, kq = bank("mq", [0, 1])
                    pk, kk = bank("mk", [2, 3])
                    pk2, kk2 = bank("mk2", [4, 5])
                    for j in range(4):
                        sch.op(pe, lambda j=j: T.matmul(pq[:, j * 128:(j + 1) * 128], lhsT=WqBD[:, j, :], rhs=cT[:, j, tsl], start=True, stop=True),
                               reads=[("cT", j), "W3"], writes=[kq], sig=(j == 3))
                    for j in range(4):
                        sch.op(pe, lambda j=j: T.matmul(pk[:, j * 128:(j + 1) * 128], lhsT=WkBD[:, j, :], rhs=cT[:, j, tsl], start=True, stop=True),
                               reads=[("cT", j), "W4"], writes=[kk], sig=(j == 3))
                    for j in range(4):
                        sch.op(pe, lambda j=j: T.matmul(pk2[:, j * 128:(j + 1) * 128], lhsT=cT[:, j, tsl], rhs=WkBD[:, j, :], start=True, stop=True),
                               reads=[("cT", j), "W4"], writes=[kk2], sig=(j == 3))
                    pq3 = pq[:].rearrange("p (j t) -> p j t", j=4)
                    pk3 = pk[:].rearrange("p (j t) -> p j t", j=4)
                    sch.op(act, lambda: A.copy(out=qTt[d], in_=pq3), reads=[kq], writes=[("qTt", d)])
                    for r in range(2):
                        sch.op(act, lambda r=r: A.activation(out=qTz[d][r], in_=pq3, func=AF.Copy, scale=pm[r]),
                               reads=[kq, "cst"], writes=[("qTz", d, r)])
                        sch.op(act, lambda r=r: A.activation(out=kTz[d][r], in_=pk3, func=AF.Copy, scale=pm8[:, r:r + 1]),
                               reads=[kk, "pm8"], writes=[("kTz", d, r)])
                    sch.op(dve, lambda: V.tensor_scalar(out=ktk[d], in0=pk2[:, :], scalar1=0.125, scalar2=None, op0=ALU.mult),
                           reads=[kk2], writes=[("ktk", d)])
                if it == 0 and d == 0:
                    checkpoint(5.1)
                i2 = cnt2[0] % 2
                cnt2[0] += 1
                col = slice(m * 16 + d * 8, m * 16 + d * 8 + 8)
                sch.op(pool, lambda: G.tensor_tensor(out=vaT[i2], in0=vaug[:, m, :, :],
                                                     in1=gA[:, col].unsqueeze(2).broadcast_to([128, 8, 65]), op=ALU.mult),
                       reads=["Vaug", ("gA", m)], writes=[("va", i2)])
                sch.op(pool, lambda: G.tensor_tensor(out=vwz[r0][rs], in0=vaug[rs, m, :, :],
                                                     in1=gWt[rs, col].unsqueeze(2).broadcast_to([64, 8, 65]), op=ALU.mult),
                       reads=["Vaug", ("gW", m)], writes=[("vw", r0)])
                if it == 0 and d == 0:
                    checkpoint(5.2)
                pS, kS = bank("mS", [0, 1, 2, 3])
                pSv = pS[:].rearrange("p (h t) -> p h t", h=8)
                for h in range(8):
                    j, r = h // 2, h % 2
                    sch.op(pe, lambda h=h, j=j, r=r: T.matmul(pSv[rs, h, :], lhsT=kTz[d][r][:, j, rs], rhs=qTt[d][:, j, rs],
                                                             start=True, stop=True),
                           reads=[("kTz", d, r), ("qTt", d)], writes=[kS], sig=(h == 7))
                if it == 0 and d == 0:
                    checkpoint(5.25)
                mk = cst[rs, (K_MF if d == 0 else K_MB):(K_MF if d == 0 else K_MB) + 64]
                sch.op(dve, lambda: V.tensor_tensor(out=PTm[r0][rs].rearrange("p (h t) -> p h t", h=8), in0=pSv[rs],
                                                    in1=mk.unsqueeze(1).broadcast_to([64, 8, 64]), op=ALU.mult),
                       reads=[kS, "cst"], writes=[("PTm", r0)])
                if it == 0 and d == 0:
                    checkpoint(5.3)
                pN0, kN0 = bank("mN0", [4])
                pN1, kN1 = bank("mN1", [5])
                for h in range(8):
                    j, r = h // 2, h % 2
                    pn, kn = (pN0, kN0) if h < 4 else (pN1, kN1)
                    o_ = pn[rs, (h % 4) * 65:(h % 4 + 1) * 65]
                    sch.op(pe, lambda h=h, o_=o_: T.matmul(o_, lhsT=PTm[r0][:, h * 64:(h + 1) * 64], rhs=vaT[i2][:, h, :], start=True, stop=False),
                           reads=[("PTm", r0), ("va", i2)], writes=[kn], sig=False)
                    sch.op(pe, lambda h=h, j=j, r=r, o_=o_: T.matmul(o_, lhsT=qTz[d][r][:, j, rs], rhs=Cbf[d][:, j, :], start=False, stop=True),
                           reads=[("qTz", d, r), ("Cbf", d)], writes=[kn], sig=(h % 4 == 3))
                if it == 0 and d == 0:
                    checkpoint(5.4)
                pC, kC = bank("mC", [0, 1, 2, 3])
                pCv = pC[:, 0:260].rearrange("p (j c) -> p j c", j=4)
                for h in range(8):
                    j, r = h // 2, h % 2
                    pr = slice(r * 64, (r + 1) * 64)
                    sch.op(pe, lambda h=h, j=j, pr=pr: T.matmul(pCv[pr, j, :], lhsT=ktk[d][:, h * 64:(h + 1) * 64], rhs=vwz[r0][:, h, :],
                                                               start=True, stop=True),
                           reads=[("ktk", d), ("vw", r0)], writes=[kC], sig=(h == 7))
                if it == 0 and d == 0:
                    checkpoint(5.5)
                ecol = gE[rs, col]
                dn = stat[rs, 40:48]
                for hb, (pn, kn) in enumerate(((pN0, kN0), (pN1, kN1))):
                    pnv = pn[rs, 0:260].rearrange("p (h c) -> p h c", h=4)
                    sch.op(dve, lambda pnv=pnv, hb=hb: V.tensor_tensor(out=dn[:, hb * 4:(hb + 1) * 4], in0=pnv[:, :, 64],
                                                                      in1=ecol[:, hb * 4:(hb + 1) * 4], op=ALU.mult),
                           reads=[kn, ("gE", m)], writes=["dn"])
                dn2 = stat[rs, 20:28]
                sch.op(dve, lambda: V.tensor_scalar(out=dn2, in0=dn, scalar1=-1.0, scalar2=1.0, op0=ALU.mult, op1=ALU.max),
                       reads=["dn"], writes=["dn2"])
                sch.op(dve, lambda: V.tensor_tensor(out=dn, in0=dn, in1=dn2, op=ALU.max), reads=["dn", "dn2"], writes=["dn"])
                sch.op(dve, lambda: V.reciprocal(out=dn, in_=dn), reads=["dn"], writes=["dn"])
                sch.op(dve, lambda: V.tensor_tensor(out=dn, in0=dn, in1=ecol, op=ALU.mult), reads=["dn", ("gE", m)], writes=["dn"])
                hn = tmpf[rs, 0:512].rearrange("p (h c) -> p h c", h=8)
                for hb, (pn, kn) in enumerate(((pN0, kN0), (pN1, kN1))):
                    pnv = pn[rs, 0:260].rearrange("p (h c) -> p h c", h=4)
                    sch.op(dve, lambda pnv=pnv, hb=hb: V.tensor_tensor(out=hn[:, hb * 4:(hb + 1) * 4, :], in0=pnv[:, :, 0:64],
                                                                      in1=dn[:, hb * 4:(hb + 1) * 4].unsqueeze(2).broadcast_to([64, 4, 64]),
                                                                      op=ALU.mult), reads=[kn, "dn"], writes=["hn"])
                sch.op(pool, lambda: G.tensor_tensor(out=Hh[rs, m, :], in0=Hh[rs, m, :], in1=tmpf[rs, 0:512], op=ALU.add),
                       reads=["hn", ("H", m)], writes=[("H", m)])
                if it == 0 and d == 0:
                    checkpoint(5.6)
                par = r0
                dcy = gDec[:, m * 32 + par * 16 + d * 8:m * 32 + par * 16 + d * 8 + 8]
                dv4 = dcy.rearrange("p (j r) -> p j r", r=2)
                for r in range(2):
                    pr = slice(r * 64, (r + 1) * 64)
                    sch.op(dve, lambda r=r, pr=pr: V.tensor_tensor(out=Cst[d][pr], in0=Cst[d][pr],
                                                                  in1=dv4[pr, :, r].unsqueeze(2).broadcast_to([64, 4, 65]), op=ALU.mult),
                           reads=[("gDec", m), ("Cst", d)], writes=[("Cst", d)])
                sch.op(dve, lambda: V.tensor_tensor(out=Cst[d], in0=Cst[d], in1=pCv, op=ALU.add), reads=[kC, ("Cst", d)], writes=[("Cst", d)])
                sch.op(act, lambda: A.copy(out=Cbf[d], in_=Cst[d]), reads=[("Cst", d)], writes=[("Cbf", d)])

        if stage == 6:
            dump("H", T_big[:, 0:8192], [("H", t) for t in range(NT)])
        checkpoint(6)
        WinO = T_W[:, 0:4096].rearrange("p (k n) -> p k n", k=8)
        wload(WinO, win_b[:, C_MO:C_MO + 512], "W0", "win", 8)
        sch.barrier()
        otok2 = T_A[:, 0:8192].rearrange("p (t c) -> p t c", t=16)
        for tt in range(NT):
            po, ko = bank("co", [0, 1])
            mm_acc(po[:, :], [(hT[:, kc, tt * 128:(tt + 1) * 128], WinO[:, kc, :]) for kc in range(8)], ko, [("hT", tt), "W0"])
            sg = tmpf[:, 512:1024]
            sch.op(act, lambda: A.activation(out=sg, in_=po[:, :], func=AF.Sigmoid), reads=[ko], writes=["sg"])
            sq = tmpf[:, 0:512]
            Ht = Hh[:, tt, :]
            sch.op(act, lambda: A.activation(out=sq, in_=Ht, func=AF.Square, scale=0.125), reads=[("H", tt)], writes=["sq"])
            ms = stat[:, 48:56]
            sch.op(dve, lambda: V.tensor_reduce(out=ms, in_=sq.rearrange("p (h c) -> p h c", h=8), axis=mybir.AxisListType.X, op=ALU.add),
                   reads=["sq"], writes=["ms"])
            sch.op(act, lambda: A.activation(out=ms, in_=ms, func=AF.Sqrt, bias=epsb[:], scale=1.0), reads=["ms", "epsb"], writes=["ms"])
            sch.op(dve, lambda: V.reciprocal(out=ms, in_=ms), reads=["ms"], writes=["ms"])
            sch.op(dve, lambda: V.tensor_tensor(out=sq.rearrange("p (h c) -> p h c", h=8), in0=Ht.rearrange("p (h c) -> p h c", h=8),
                                                in1=ms.unsqueeze(2).broadcast_to([128, 8, 64]), op=ALU.mult),
                   reads=[("H", tt), "ms"], writes=["sq"])
            sch.op(pool, lambda: G.tensor_tensor(out=sq, in0=sq, in1=ghead_bc[:], op=ALU.mult), reads=["sq", "ghead"], writes=["sq"])
            sch.op(dve, lambda: V.tensor_tensor(out=otok2[:, tt, :], in0=sq, in1=sg, op=ALU.mult), reads=["sq", "sg"],
                   writes=[("otok", tt // 4)])
        otok_save = otok
        otok = otok2
        flush_oT(1)
        otok = otok_save
        sch.barrier()

        checkpoint(7)
        WkvA = T_W[:, 0:4096].rearrange("p (k n) -> p k n", k=8)
        WkvB = T_W[:, 4096:8192].rearrange("p (k n) -> p k n", k=8)
        WinQ = T_W[:, 8192:12288].rearrange("p (k n) -> p k n", k=8)
        for nm, lo, hi in (("W0", 0, 4096), ("W1", 4096, 8192), ("W2", 8192, 12288)):
            sch.set_alias(nm, lo, hi)
        wload(WkvA, wkv_b[:, 0:512], "W0", "wkv", 8)
        wload(WkvB, wkv_b[:, 512:1024], "W1", "wkv", 8)
        wload(WinQ, win_b[:, C_MQ:C_MQ + 512], "W2", "win", 8)
        memT = T_A[:, 0:2048].rearrange("p (k t) -> p k t", k=8)
        kmT = T_A[:, 2048:3072].rearrange("p (h t) -> p h t", h=4)
        vm = T_A[:, 3072:4104].rearrange("p (t h c) -> p t h c", t=2, h=4)
        qm = [T_A[:, 4104 + i * 512:4104 + (i + 1) * 512] for i in range(2)]
        sch.op(dve, lambda: V.memset(vm[:, :, :, 128:129], 1.0), writes=["vm"])
        for t2 in range(2):
            slot = t2
            xsv = xs[:, slot * 1024:(slot + 1) * 1024]
            sch.dma(sp, xsv, mem_d[b, t2 * 128:(t2 + 1) * 128, :], writes=[("xs", slot)], grp=("xs", slot))
            rms_to_T(xsv, [("xs", slot)], 16, lambda t2=t2: memT[:, :, t2 * 128:(t2 + 1) * 128], [("memT", t2)], slot)
        checkpoint(7.1)
        MT = [("memT", 0), ("memT", 1)]
        for h in range(4):
            pk, kk = bank("dk", [0, 1])
            mm_acc(pk[:, 0:256], [(WkvA[:, kc, h * 128:(h + 1) * 128], memT[:, kc, :]) for kc in range(8)], kk, MT + ["W0"])
            sch.op(act, lambda: A.copy(out=kmT[:, h, :], in_=pk[:, 0:256]), reads=[kk], writes=["kmT"])
        for t2 in range(2):
            pv, kv = bank("dv", [2, 3])
            mm_acc(pv[:, :], [(memT[:, kc, t2 * 128:(t2 + 1) * 128], WkvB[:, kc, :]) for kc in range(8)], kv, MT + ["W1"])
            sch.op(act, lambda: A.copy(out=vm[:, t2, :, 0:128], in_=pv[:].rearrange("p (h c) -> p h c", h=4)), reads=[kv], writes=["vm"])
        checkpoint(7.2)
        mscale = 128.0 ** -0.5
        qc = 0
        for h in range(4):
            for qb in range(4):
                qs = slice(qb * 512, (qb + 1) * 512)
                pq, kq = bank("dq", [0, 1])
                mm_acc(pq[:, :], [(WinQ[:, kc, h * 128:(h + 1) * 128], hT[:, kc, qs]) for kc in range(8)], kq, htkeys(qb) + ["W2"])
                qi = qc % 2
                qc += 1
                sch.op(act, lambda: A.copy(out=qm[qi], in_=pq[:, :]), reads=[kq], writes=[("qm", qi)])
                pOa, kOa = bank("dOa", [4])
                pOb, kOb = bank("dOb", [5])
                for kt in range(2):
                    pS, kS = bank("dS", [2, 3])
                    sch.op(pe, lambda: T.matmul(pS[:, :], lhsT=kmT[:, h, kt * 128:(kt + 1) * 128], rhs=qm[qi], start=True, stop=True),
                           reads=["kmT", ("qm", qi)], writes=[kS])
                    ps_ = (qc * 2 + kt) % 3
                    P_ = PTt[:, ps_ * 512:(ps_ + 1) * 512]
                    sch.op(act, lambda: A.activation(out=P_, in_=pS[:, :], func=AF.Exp, scale=mscale), reads=[kS], writes=[("PT", ps_)])
                    for j in range(4):
                        pO_, kO_ = (pOa, kOa) if j < 2 else (pOb, kOb)
                        sch.op(pe, lambda j=j, pO_=pO_: T.matmul(pO_[:, (j % 2) * 129:(j % 2 + 1) * 129], lhsT=P_[:, j * 128:(j + 1) * 128],
                                                                 rhs=vm[:, kt, h, :], start=(kt == 0 and j % 2 == 0), stop=(kt == 1),
                                                                 skip_group_check=True),
                               reads=[("PT", ps_), "vm"], writes=[kO_], sig=(j % 2 == 1))
                if h == 0 and qb == 0:
                    checkpoint(7.3)
                rec = stat[:, 32:36]
                for half, (pO_, kO_) in enumerate(((pOa, kOa), (pOb, kOb))):
                    pv2 = pO_[:, 0:258].rearrange("p (j c) -> p j c", j=2)
                    rc = rec[:, half * 2:half * 2 + 2]
                    sch.op(dve, lambda: V.reciprocal(out=rc, in_=pv2[:, :, 128]), reads=[kO_], writes=[("rec", half)])
                    sch.op(dve, lambda: V.tensor_tensor(out=otok[:, qb * 4 + half * 2:qb * 4 + half * 2 + 2, h * 128:(h + 1) * 128],
                                                        in0=pv2[:, :, 0:128], in1=rc.unsqueeze(2).broadcast_to([128, 2, 128]), op=ALU.mult),
                           reads=[kO_, ("rec", half)], writes=[("otok", qb)])
        checkpoint(7.4)
        flush_oT(2)
        sch.barrier()

        if dbg:
            for br in range(3):
                for j in range(4):
                    sch.dma(pool, dbg_d[br, :, j, :], oT_d[br, :, j, :], reads=[("oTd", br, k, j) for k in range(4)], writes=[("dbg", br, j)],
                            grp=("dbg", br, j), final=True)

        checkpoint(8)
        x1 = T_big[:, 0:4096].rearrange("p (t c) -> p t c", t=4)
        mrg = bigb[:, 8192:12288].rearrange("p (k t) -> p k t", k=8)
        h2T = bigb[:, 12288:16384].rearrange("p (k t) -> p k t", k=8)
        aT = T_A[:].rearrange("p (c t) -> p c t", c=NFF)
        oTs = [T_C[:, br * 2048:(br + 1) * 2048].rearrange("p (j t) -> p j t", j=4) for br in range(3)]
        sgb = [T_V[:, i * 512:(i + 1) * 512] for i in range(3)]
        prd = [T_V[:, 1536 + i * 1024:1536 + (i + 1) * 1024].bitcast(F32) for i in range(3)]
        WE = [T_W[:, i * 4608:(i + 1) * 4608] for i in range(2)]
        for i in range(2):
            sch.set_alias(("WE", i), i * 4608, (i + 1) * 4608)
        for i in range(4):
            sch.set_alias(("WF", i), i * 2048, (i + 1) * 2048)
        sch.set_alias("WO", 9216, 13312)
        sch.set_alias("WD", 0, 11264)
        for blk in range(4):
            bs = slice(blk * 512, (blk + 1) * 512)
            for br in range(3):
                sch.dma(sp, oTs[br], oT_d[br, :, :, bs], reads=[("oTd", br, blk, j) for j in range(4)], writes=[("oTs", br)], grp=("oTs", br))
            for f in range(8):
                ws = f % 2
                Wg_ = WE[ws][:, 0:3072].rearrange("p (k b n) -> p k b n", k=8, b=3)
                Wb_ = WE[ws][:, 3072:4608].rearrange("p (k b n) -> p k b n", k=4, b=3)
                for br in range(3):
                    sch.dma(sp, Wg_[:, :, br, :], win_b[:, br * 1024 + f * 128:br * 1024 + (f + 1) * 128].rearrange("(k p) n -> p k n", p=128),
                            reads=[("w", "win")], writes=[("WE", ws)], grp=("WE", ws))
                    sch.dma(sp, Wb_[:, :, br, :], wbr_b[br * 512:(br + 1) * 512, f * 128:(f + 1) * 128].rearrange("(k p) n -> p k n", p=128),
                            reads=[("w", "wbr")], writes=[("WE", ws)], grp=("WE", ws))
                for br in range(3):
                    pg, kg = bank("eg", [0, 1])
                    pb_, kb_ = bank("eb", [2, 3])
                    mm_acc(pg[:, :], [(Wg_[:, kc, br, :], hT[:, kc, bs]) for kc in range(8)], kg, htkeys(blk) + [("WE", ws)])
                    mm_acc(pb_[:, :], [(Wb_[:, kc, br, :], oTs[br][:, kc, :]) for kc in range(4)], kb_, [("oTs", br), ("WE", ws)])
                    sch.op(act, lambda: A.activation(out=sgb[br], in_=pg[:, :], func=AF.Sigmoid), reads=[kg], writes=[("sgb", br)])
                    sch.op(dve, lambda: V.tensor_tensor(out=prd[br], in0=pb_[:, :], in1=sgb[br], op=ALU.mult),
                           reads=[kb_, ("sgb", br)], writes=[("prd", br)])
                sch.op(pool, lambda: G.tensor_tensor(out=prd[0], in0=prd[0], in1=prd[1], op=ALU.add),
                       reads=[("prd", 0), ("prd", 1)], writes=[("prd", 0)])
                sch.op(pool, lambda: G.tensor_tensor(out=mrg[:, f, :], in0=prd[0], in1=prd[2], op=ALU.add),
                       reads=[("prd", 0), ("prd", 2)], writes=[("mrg", f)])
            MRG = [("mrg", f) for f in range(8)]
            Wo = T_W[:, 9216:13312].rearrange("p (k n) -> p k n", k=8)
            for nh in range(2):
                wload(Wo, wout_b[:, nh * 512:(nh + 1) * 512], "WO", "wout", 8)
                for tt in range(4):
                    slot = tt % 2
                    gt = blk * 4 + tt
                    xh = xs[:, slot * 1024:slot * 1024 + 512]
                    sch.dma(sp, xh, x_d[b, gt * 128:(gt + 1) * 128, nh * 512:(nh + 1) * 512], writes=[("xs", slot)], grp=("xs", slot))
                    py, ky = bank("ey", [4, 5])
                    mm_acc(py[:, :], [(mrg[:, kc, tt * 128:(tt + 1) * 128], Wo[:, kc, :]) for kc in range(8)], ky, MRG + ["WO"])
                    sch.op(dve, lambda: V.tensor_tensor(out=x1[:, tt, nh * 512:(nh + 1) * 512], in0=py[:, :], in1=xh,
                                                        op=ALU.add), reads=[ky, ("xs", slot)], writes=[("x1", tt)])
            for tt in range(4):
                rms_to_T(x1[:, tt, :], [("x1", tt)], 8, lambda tt=tt: h2T[:, :, tt * 128:(tt + 1) * 128], [("h2T", tt)], tt % 2)
            H2 = [("h2T", t) for t in range(4)]
            WF = [T_W[:, i * 2048:(i + 1) * 2048].rearrange("p (g k n) -> p g k n", g=2, k=8) for i in range(4)]
            for c in range(NFF):
                ws = c % 4
                sch.dma(sp, WF[ws][:, 0], wg_b[:, c * 128:(c + 1) * 128].rearrange("(k p) n -> p k n", p=128),
                        reads=[("w", "wg")], writes=[("WF", ws)], grp=("WF", ws))
                sch.dma(sp, WF[ws][:, 1], wu_b[:, c * 128:(c + 1) * 128].rearrange("(k p) n -> p k n", p=128),
                        reads=[("w", "wu")], writes=[("WF", ws)], grp=("WF", ws))
                pg, kg = bank("fg", [0, 1])
                pu, ku = bank("fu", [2, 3])
                mm_acc(pg[:, :], [(WF[ws][:, 0, kc, :], h2T[:, kc, :]) for kc in range(8)], kg, H2 + [("WF", ws)])
                mm_acc(pu[:, :], [(WF[ws][:, 1, kc, :], h2T[:, kc, :]) for kc in range(8)], ku, H2 + [("WF", ws)])
                si = c % 3
                sch.op(act, lambda: A.activation(out=prd[si], in_=pg[:, :], func=AF.Silu), reads=[kg], writes=[("prd", si)])
                sch.op(dve, lambda: V.tensor_tensor(out=aT[:, c, :], in0=pu[:, :], in1=prd[si], op=ALU.mult),
                       reads=[ku, ("prd", si)], writes=[("aT", c)])
            for hh in range(2):
                Wd_ = T_W[:, 0:11264].rearrange("p (c n) -> p c n", c=11)
                sch.dma(sp, Wd_, wd_b[hh * 1408:(hh + 1) * 1408, :].rearrange("(c p) n -> p c n", p=128),
                        reads=[("w", "wd")], writes=["WD"], grp="WD")
                for tt in range(4):
                    for nh in range(2):
                        py, ky = bank("ey", [4, 5])
                        mm_acc(py[:, :], [(aT[:, hh * 11 + c, tt * 128:(tt + 1) * 128], Wd_[:, c, nh * 512:(nh + 1) * 512]) for c in range(11)],
                               ky, [("aT", hh * 11 + c) for c in range(11)] + ["WD"])
                        sch.op(dve, lambda: V.tensor_tensor(out=x1[:, tt, nh * 512:(nh + 1) * 512], in0=py[:, :],
                                                            in1=x1[:, tt, nh * 512:(nh + 1) * 512], op=ALU.add),
                               reads=[ky, ("x1", tt)], writes=[("x1", tt)])
            for tt in range(4):
                slot = tt % 2
                gt = blk * 4 + tt
                st = stat[:, 56 + slot * 2:56 + slot * 2 + 1]
                xsv = xs[:, slot * 1024:(slot + 1) * 1024]
                xnv = xn[:, slot * 1024:(slot + 1) * 1024]
                sch.op(act, lambda: A.activation(out=xnv, in_=x1[:, tt, :], func=AF.Square, scale=1.0 / 32.0, accum_out=st),
                       reads=[("x1", tt)], writes=[("xn", slot), ("fst", slot)])
                sch.op(act, lambda: A.activation(out=st, in_=st, func=AF.Sqrt, bias=epsb[:], scale=1.0), reads=[("fst", slot), "epsb"],
                       writes=[("fst", slot)])
                sch.op(dve, lambda: V.reciprocal(out=st, in_=st), reads=[("fst", slot)], writes=[("fst", slot)])
                sch.op(act, lambda: A.activation(out=xsv, in_=x1[:, tt, :], func=AF.Copy, scale=st), reads=[("x1", tt), ("fst", slot)],
                       writes=[("xs", slot)])
                sch.op(pool, lambda: G.tensor_tensor(out=xsv, in0=xsv, in1=gfin_bc[:], op=ALU.mult), reads=[("xs", slot), "gfin"],
                       writes=[("xs", slot)])
                sch.dma(sp, out_d[b, gt * 128:(gt + 1) * 128, :], xsv, reads=[("xs", slot)], grp=("xs", slot), final=True)
        sch.barrier()


def make_consts():
    c = np.zeros((128, K_N), np.float32)
    c[:, K_ID:K_ID + 128] = np.eye(128, dtype=np.float32)
    p = np.arange(128)
    s = (p % 64)[:, None]
    t = np.arange(64)[None, :]
    c[:, K_MF:K_MF + 64] = (s <= t)
    c[:, K_MB:K_MB + 64] = (s >= t)
    ss = p[:, None]
    tt = p[None, :]
    same = (ss // 64) == (tt // 64)
    c[:, K_TF:K_TF + 128] = same & (ss <= tt)
    c[:, K_TB:K_TB + 128] = same & (ss >= tt)
    c[:, K_I0:K_I0 + 128] = (ss // 64 == 0) & (tt >= 0)
    c[:, K_I1:K_I1 + 128] = (ss // 64 == 1) & (tt >= 0)
    inv = (10000.0 ** (-np.arange(16, dtype=np.float32) / 16.0)).astype(np.float32)
    for i in range(32):
        c[64 + i, K_RP] = inv[i % 16]
        c[64 + i, K_RP + 1] = math.pi if i < 16 else 0.0
        c[64 + i, K_RP + 2] = 0.5 * math.pi
    c[:, K_RP + 3] = -math.pi
    return c


_CACHE = {}


def kernel(**inputs):
    f = lambda a: np.ascontiguousarray(np.asarray(a, dtype=np.float32))
    if "nc" not in _CACHE:
        _CACHE["nc"] = build_program()
    nc = _CACHE["nc"]
    shared = {
        "g_mix": f(inputs["g_mix"]).reshape(1, D),
        "w_in": f(inputs["w_in"]).reshape(D, N_IN),
        "mla_g_q": f(inputs["mla_g_q"]).reshape(1, 384),
        "mla_g_kv": f(inputs["mla_g_kv"]).reshape(1, 256),
        "mla_w_uq": f(inputs["mla_w_uq"]).reshape(384, 768),
        "mla_w_uk": f(inputs["mla_w_uk"]).reshape(256, 512),
        "mla_w_uv": f(inputs["mla_w_uv"]).reshape(256, 512),
        "ml_conv_w": f(inputs["ml_conv_w"]).reshape(5, 512),
        "ml_w_q": f(inputs["ml_w_q"]).reshape(512, 64),
        "ml_w_k": f(inputs["ml_w_k"]).reshape(512, 64),
        "ml_gate_bias": f(inputs["ml_gate_bias"]).reshape(1, 32),
        "ml_g_head": f(inputs["ml_g_head"]).reshape(1, 512),
        "mem_g": f(inputs["mem_g"]).reshape(1, D),
        "mem_w_kv": f(inputs["mem_w_kv"]).reshape(D, 1024),
        "w_branch": f(inputs["w_branch"]).reshape(1536, D),
        "w_out": f(inputs["w_out"]).reshape(D, D),
        "g_ffn": f(inputs["g_ffn"]).reshape(1, D),
        "w_ffn_gate": f(inputs["w_ffn_gate"]).reshape(D, DFF),
        "w_ffn_up": f(inputs["w_ffn_up"]).reshape(D, DFF),
        "w_ffn_down": f(inputs["w_ffn_down"]).reshape(DFF, D),
        "g_final": f(inputs["g_final"]).reshape(1, D),
        "cst": make_consts(),
    }
    x = f(inputs["x"])
    mem = f(inputs["mem"])
    pos = np.ascontiguousarray(np.asarray(inputs["positions"], dtype=np.int32))
    in_maps = []
    for c in range(NCORES):
        sl = slice(c * SEQ_PER_CORE, (c + 1) * SEQ_PER_CORE)
        m = dict(shared)
        m["x"] = x[sl]
        m["mem"] = mem[sl]
        m["positions"] = pos[sl]
        in_maps.append(m)
    res = run_bass_kernel_spmd(nc, in_maps, core_ids=list(range(NCORES)))
    out = np.concatenate([np.asarray(r["out"]) for r in res.results], axis=0)
    return out.astype(np.float32, copy=False)
```

```python
import contextlib
import math
import numpy as np
import concourse.bass as bass
import concourse.mybir as mybir
from concourse.bass_utils import run_bass_kernel_spmd

F32 = mybir.dt.float32
BF16 = mybir.dt.bfloat16
I32 = mybir.dt.int32
AF = mybir.ActivationFunctionType
ALU = mybir.AluOpType

NCORES = 8
SEQ_PER_CORE = 4
S = 2048
D = 1024
NT = S // 128
EPS = 1e-6
N_IN = 5824
C_CQ, C_MU, C_MV, C_MO, C_MG, C_MQ = 3072, 3744, 4256, 4768, 5280, 5312
DFF = 2816
NFF = DFF // 128
K_ID, K_MF, K_MB, K_TF, K_TB, K_I0, K_I1, K_RP = 0, 128, 192, 256, 384, 512, 640, 768
K_N = 772


class Trk:
    __slots__ = ("w", "r")

    def __init__(self):
        self.w = None
        self.r = {}


class Eng:
    def __init__(self, sch, name, h):
        self.sch = sch
        self.name = name
        self.h = h
        self.seen = {}
        self.pending = False
        self.semidx = sch.newsem()
        self.cnt = 0


class Sched:
    def __init__(self, nc, es):
        self.nc = nc
        self.es = es
        self.sems = []
        self.trk = {}
        self.dsem = {}
        self.final = []
        self.alias = {}
        self.ninst = 0
        self.pe = Eng(self, "pe", nc.tensor)
        self.dve = Eng(self, "dve", nc.vector)
        self.act = Eng(self, "act", nc.scalar)
        self.pool = Eng(self, "pool", nc.gpsimd)
        self.sp = Eng(self, "sp", nc.sync)

    def newsem(self):
        h = self.es.enter_context(self.nc.semaphore("s%d" % len(self.sems)))
        self.sems.append(h)
        return len(self.sems) - 1

    def _exp(self, keys):
        out = []
        for k in keys:
            a = self.alias.get(k)
            if a is None:
                out.append(k)
            else:
                out.extend(a)
        return out

    def set_alias(self, key, lo, hi):
        self.alias[key] = [("TW", g) for g in range(lo // 512, (hi + 511) // 512)]

    def _deps(self, reads, writes, skip_waw_sem=None):
        d = {}
        for k in reads:
            t = self.trk.get(k)
            if t is not None and t.w is not None and d.get(t.w[0], 0) < t.w[1]:
                d[t.w[0]] = t.w[1]
        for k in writes:
            t = self.trk.get(k)
            if t is None:
                continue
            if t.w is not None and t.w[0] != skip_waw_sem and d.get(t.w[0], 0) < t.w[1]:
                d[t.w[0]] = t.w[1]
            for si, v in t.r.items():
                if d.get(si, 0) < v:
                    d[si] = v
        return d

    def _wait(self, eng, d, skip_own=False):
        for si, v in d.items():
            if skip_own and si == eng.semidx:
                continue
            if eng.seen.get(si, 0) >= v:
                continue
            eng.h.wait_ge(self.sems[si], v)
            self.ninst += 1
            eng.seen[si] = v

    def _mark(self, tok, reads, writes):
        for k in reads:
            t = self.trk.get(k)
            if t is None:
                t = self.trk[k] = Trk()
            if t.r.get(tok[0], 0) < tok[1]:
                t.r[tok[0]] = tok[1]
        for k in writes:
            t = self.trk.get(k)
            if t is None:
                t = self.trk[k] = Trk()
            t.w = tok
            t.r = {}

    def op(self, eng, fn, reads=(), writes=(), sig=True):
        reads = self._exp(reads)
        writes = self._exp(writes)
        d = self._deps(reads, writes)
        self._wait(eng, d, skip_own=(eng is self.pe))
        ins = fn()
        self.ninst += 1
        if sig or eng is not self.pe:
            if eng.cnt >= 30000 and not eng.pending:
                eng.semidx = self.newsem()
                eng.cnt = 0
            eng.cnt += 1
            ins.then_inc(self.sems[eng.semidx], 1)
            tok = (eng.semidx, eng.cnt)
            eng.pending = False
        else:
            tok = (eng.semidx, eng.cnt + 1)
            eng.pending = True
        self._mark(tok, reads, writes)
        return tok

    def dma(self, q, out, in_, reads=(), writes=(), grp=None, final=False):
        reads = self._exp(reads)
        writes = self._exp(writes)
        g = self.dsem.get(grp)
        if g is None:
            g = self.dsem[grp] = [self.newsem(), 0]
        d = self._deps(reads, writes, skip_waw_sem=g[0])
        self._wait(q, d)
        ins = q.h.dma_start(out=out, in_=in_)
        self.ninst += 1
        g[1] += 16
        ins.then_inc(self.sems[g[0]], 16)
        tok = (g[0], g[1])
        self._mark(tok, reads, writes)
        if final:
            self.final.append(tok)
        return tok

    def retoken(self, keys, tok):
        for k in keys:
            t = self.trk.get(k)
            if t is None:
                t = self.trk[k] = Trk()
            t.w = tok

    def barrier(self):
        engs = [self.pe, self.dve, self.act, self.pool]
        assert not self.pe.pending
        for e in engs + [self.sp]:
            d = {}
            for o in engs:
                if o is not e and o.cnt > 0:
                    d[o.semidx] = o.cnt
            self._wait(e, d)

    def finish(self):
        d = {}
        for si, v in self.final:
            if d.get(si, 0) < v:
                d[si] = v
        self._wait(self.sp, d)


class StopBuild(Exception):
    pass


def build_program(nseq=SEQ_PER_CORE, dbg=False, stage=99):
    nc = bass.Bass("TRN2", target_bir_lowering=False)
    es = contextlib.ExitStack()
    with es:
        sch = Sched(nc, es)
        try:
            _emit(nc, es, nseq, dbg, sch, stage)
        except StopBuild:
            pass
        sch.finish()
        _emit.ninst = sch.ninst
        _emit.nsem = len(sch.sems)
    return nc


def _emit(nc, es, nseq, dbg, sch, stage):
    pe, dve, act, pool, sp = sch.pe, sch.dve, sch.act, sch.pool, sch.sp
    T, V, A, G = nc.tensor, nc.vector, nc.scalar, nc.gpsimd

    def din(name, shape, dt=F32):
        return nc.dram_tensor(name, list(shape), dt, kind="ExternalInput").ap()

    def dscr(name, shape, dt=BF16):
        return nc.dram_tensor(name, list(shape), dt, kind="Internal").ap()

    x_d = din("x", [SEQ_PER_CORE, S, D])
    mem_d = din("mem", [SEQ_PER_CORE, 256, D])
    pos_d = din("positions", [SEQ_PER_CORE, S], I32)
    gmix_d = din("g_mix", [1, D])
    win_d = din("w_in", [D, N_IN])
    gq_d = din("mla_g_q", [1, 384])
    gkv_d = din("mla_g_kv", [1, 256])
    wuq_d = din("mla_w_uq", [384, 768])
    wuk_d = din("mla_w_uk", [256, 512])
    wuv_d = din("mla_w_uv", [256, 512])
    convw_d = din("ml_conv_w", [5, 512])
    wq_d = din("ml_w_q", [512, 64])
    wk_d = din("ml_w_k", [512, 64])
    gbias_d = din("ml_gate_bias", [1, 32])
    ghead_d = din("ml_g_head", [1, 512])
    memg_d = din("mem_g", [1, D])
    wkv_d = din("mem_w_kv", [D, 1024])
    wbr_d = din("w_branch", [1536, D])
    wout_d = din("w_out", [D, D])
    gffn_d = din("g_ffn", [1, D])
    wg_d = din("w_ffn_gate", [D, DFF])
    wu_d = din("w_ffn_up", [D, DFF])
    wd_d = din("w_ffn_down", [DFF, D])
    gfin_d = din("g_final", [1, D])
    cst_d = din("cst", [128, K_N])
    out_d = nc.dram_tensor("out", [SEQ_PER_CORE, S, D], F32, kind="ExternalOutput").ap()
    if dbg:
        dbg_d = nc.dram_tensor("dbg_oT", [3, 128, 4, S], BF16, kind="ExternalOutput").ap()

    win_b = dscr("win_b", [D, N_IN])
    wuq_b = dscr("wuq_b", [384, 768])
    wuk_b = dscr("wuk_b", [256, 512])
    wuv_b = dscr("wuv_b", [256, 512])
    wq_b = dscr("wq_b", [512, 64])
    wk_b = dscr("wk_b", [512, 64])
    wkv_b = dscr("wkv_b", [D, 1024])
    wbr_b = dscr("wbr_b", [1536, D])
    wout_b = dscr("wout_b", [D, D])
    wg_b = dscr("wg_b", [D, DFF])
    wu_b = dscr("wu_b", [D, DFF])
    wd_b = dscr("wd_b", [DFF, D])
    oT_d = dscr("oT_scr", [3, 128, 4, S])

    dumps = {}

    def dump(name, ap, reads):
        shp = [int(v) for v in ap.shape]
        t = nc.dram_tensor("dmp_" + name, shp, ap.dtype, kind="ExternalOutput").ap()
        sch.dma(sp, t, ap, reads=reads, grp=("dmp", name), final=True)

    def checkpoint(st):
        if stage <= st:
            sch.barrier()
            raise StopBuild()

    def sbt(name, cols, dt):
        return es.enter_context(nc.sbuf_tensor(name, [128, cols], dt))

    T_hT = sbt("hT", 8 * S, BF16)
    T_big = sbt("big", 4 * 2080, F32)
    T_A = sbt("ra", 11264, BF16)
    T_V = sbt("rv", 16 * 520, BF16)
    T_C = sbt("rc", 16 * 512, BF16)
    T_W = sbt("rw", 13824, BF16)
    xs = sbt("xs", 2 * 1024, F32)
    xn = sbt("xn", 2 * 1024, BF16)
    cst = sbt("cstt", K_N, F32)
    identb = sbt("identb", 128, BF16)
    gfin_bc = sbt("gfin_bc", 1024, F32)
    ghead_bc = sbt("ghead_bc", 512, F32)
    gbias_bc = sbt("gbias_bc", 32, F32)
    gvecT = sbt("gvecT", 32, F32)
    convw = sbt("convw", 20, F32)
    stat = sbt("stat", 64, F32)
    epsb = sbt("epsb", 1, F32)
    oneb = sbt("oneb", 1, F32)
    gA = sbt("gA", 16 * 16, F32)
    gE = sbt("gE", 16 * 16, F32)
    gWt = sbt("gWt", 16 * 16, F32)
    gDec = sbt("gDec", 16 * 32, F32)
    gtmp = sbt("gtmp", 128, F32)
    kst = sbt("kst", 2 * 96, BF16)
    PTt = sbt("PTt", 3 * 512, BF16)
    tmpf = sbt("tmpf", 2 * 512, F32)
    stg = sbt("stg", 2 * 512, BF16)
    posi = sbt("posi", S, I32)
    posf = sbt("posf", S, F32)

    hT = T_hT[:].rearrange("p (k t) -> p k t", k=8)
    bigb = T_big[:].bitcast(BF16)

    pbk = [es.enter_context(nc.psum_tensor("pb%d" % i, [128, 512], F32)) for i in range(6)]
    ptk = [es.enter_context(nc.psum_tensor("pt%d" % i, [128, 1024], BF16)) for i in range(2)]
    pbk.append(ptk[0][:].bitcast(F32))
    pbk.append(ptk[1][:].bitcast(F32))
    rot = {}

    def bank(grp, ids):
        i = rot.get(grp, 0)
        rot[grp] = i + 1
        b = ids[i % len(ids)]
        return pbk[b], (("pb", b) if b < 6 else ("pt", b - 6))

    def tbank():
        i = rot.get("pt", 0)
        rot["pt"] = i + 1
        return ptk[i % 2], ("pt", i % 2)

    sch.dma(sp, cst[:], cst_d, writes=["cst"], grp="cst")
    sch.dma(pool, identb[:], cst_d[:, K_ID:K_ID + 128], writes=["ident"], grp="ident")
    sch.dma(sp, gfin_bc[:], gfin_d.partition_broadcast(128), writes=["gfin"], grp="c2")
    sch.dma(sp, ghead_bc[:], ghead_d.partition_broadcast(128), writes=["ghead"], grp="c3")
    sch.dma(sp, gbias_bc[:], gbias_d.partition_broadcast(128), writes=["gbias"], grp="c4")
    with nc.allow_non_contiguous_dma("tiny gain-vector transposes"):
        sch.dma(sp, gvecT[:, 0:8], gmix_d.rearrange("o (k p) -> p (o k)", p=128), writes=["gv0"], grp="c5")
        sch.dma(sp, gvecT[:, 8:16], gffn_d.rearrange("o (k p) -> p (o k)", p=128), writes=["gv1"], grp="c6")
        sch.dma(sp, gvecT[:, 16:24], memg_d.rearrange("o (k p) -> p (o k)", p=128), writes=["gv2"], grp="c7")
        sch.dma(sp, gvecT[:, 24:27], gq_d.rearrange("o (k p) -> p (o k)", p=128), writes=["gv3"], grp="c8")
        sch.dma(sp, gvecT[:, 27:29], gkv_d.rearrange("o (k p) -> p (o k)", p=128), writes=["gv4"], grp="c9")
        for ct_ in range(4):
            sch.dma(sp, convw[:, ct_ * 5:(ct_ + 1) * 5], convw_d[:, ct_ * 128:(ct_ + 1) * 128].rearrange("j p -> p j"),
                    writes=["convw"], grp="c10")
    GV = ["gv0", "gv1", "gv2", "gv3", "gv4"]
    sch.op(dve, lambda: V.memset(epsb[:], EPS), writes=["epsb"])
    sch.op(dve, lambda: V.memset(kst[:], 0.0), writes=[("kst", 0), ("kst", 1)])
    sch.op(dve, lambda: V.memset(oneb[:], 1.0), writes=["oneb"])

    def conv_w(dst, src, rows, key):
        tok = None
        r0 = 0
        while r0 < rows:
            r1 = min(rows, r0 + 128)
            tok = sch.dma(pool, dst[r0:r1, :], src[r0:r1, :], grp=("cv", key))
            r0 = r1
        sch.retoken([("w", key)], tok)

    conv_w(win_b, win_d, D, "win")
    conv_w(wuq_b, wuq_d, 384, "wuq")
    conv_w(wuk_b, wuk_d, 256, "wuk")
    conv_w(wuv_b, wuv_d, 256, "wuv")
    conv_w(wq_b, wq_d, 512, "wq")
    conv_w(wk_b, wk_d, 512, "wk")
    conv_w(wkv_b, wkv_d, D, "wkv")
    conv_w(wbr_b, wbr_d, 1536, "wbr")
    conv_w(wout_b, wout_d, D, "wout")
    conv_w(wg_b, wg_d, D, "wg")
    conv_w(wu_b, wu_d, D, "wu")
    conv_w(wd_b, wd_d, DFF, "wd")

    checkpoint(0)

    def wload(dst, src2d, key, wkey, kc):
        if kc == 1:
            srcv = src2d
        else:
            srcv = src2d.rearrange("(k p) n -> p k n", p=128)
        sch.dma(sp, dst, srcv, reads=[("w", wkey)], writes=[key], grp=key)

    def rms_to_T(src_ap, src_keys, gcol, dst_fn, dst_keys, slot):
        st = stat[:, slot * 4:slot * 4 + 1]
        st2 = stat[:, slot * 4 + 1:slot * 4 + 2]
        xnv = xn[:, slot * 1024:(slot + 1) * 1024]
        sch.op(act, lambda: A.activation(out=xnv, in_=src_ap, func=AF.Square, scale=1.0 / 32.0, accum_out=st),
               reads=src_keys, writes=[("xn", slot), ("st", slot)])
        sch.op(act, lambda: A.activation(out=st2, in_=st, func=AF.Sqrt, bias=epsb[:], scale=1.0),
               reads=[("st", slot), "epsb"], writes=[("st2", slot)])
        sch.op(dve, lambda: V.reciprocal(out=st2, in_=st2), reads=[("st2", slot)], writes=[("st2", slot)])
        sch.op(act, lambda: A.activation(out=xnv, in_=src_ap, func=AF.Copy, scale=st2),
               reads=list(src_keys) + [("st2", slot)], writes=[("xn", slot)])
        pt, ptkey = tbank()
        ptv = pt[:].rearrange("p (k t) -> p k t", k=8)
        for kc in range(8):
            sch.op(pe, lambda kc=kc: T.transpose(ptv[:, kc, :], xnv[:, kc * 128:(kc + 1) * 128], identb[:]),
                   reads=[("xn", slot), "ident"], writes=[ptkey], sig=(kc == 7))
        gb = gvecT[:, gcol:gcol + 8].unsqueeze(2).broadcast_to([128, 8, 128])
        sch.op(dve, lambda: V.tensor_tensor(out=dst_fn(), in0=ptv, in1=gb, op=ALU.mult),
               reads=[ptkey] + GV, writes=dst_keys)

    def mm_acc(out_ap, pairs, okey, rkeys):
        n = len(pairs)
        for i, (l, r) in enumerate(pairs):
            sch.op(pe, lambda l=l, r=r, i=i: T.matmul(out_ap, lhsT=l, rhs=r, start=(i == 0), stop=(i == n - 1)),
                   reads=rkeys, writes=[okey], sig=(i == n - 1))

    def htkeys(blk):
        return [("hT", blk * 4 + i) for i in range(4)]

    for b in range(nseq):
        for tt in range(NT):
            slot = tt % 2
            xsv = xs[:, slot * 1024:(slot + 1) * 1024]
            sch.dma(sp, xsv, x_d[b, tt * 128:(tt + 1) * 128, :], writes=[("xs", slot)], grp=("xs", slot))
            rms_to_T(xsv, [("xs", slot)], 0, lambda tt=tt: hT[:, :, tt * 128:(tt + 1) * 128], [("hT", tt)], slot)

        if stage == 1:
            dump("hT", T_hT[:], [("hT", t) for t in range(NT)])
        checkpoint(1)
        WinB = T_W[:, 0:5376].rearrange("p (k n) -> p k n", k=8)
        Wuq = T_W[:, 5376:7680].rearrange("p (k n) -> p k n", k=3)
        WuqS = T_W[:, 7680:9984].rearrange("p (k n) -> p k n", k=3)
        Wuk = T_W[:, 9984:11008].rearrange("p (k n) -> p k n", k=2)
        Wuv = T_W[:, 11008:12032].rearrange("p (k n) -> p k n", k=2)
        for nm, lo, hi in (("W0", 0, 5376), ("W1", 5376, 7680), ("W2", 7680, 9984), ("W3", 9984, 11008), ("W4", 11008, 12032)):
            sch.set_alias(nm, lo, hi)
        wload(WinB, win_b[:, C_CQ:C_CQ + 672], "W0", "win", 8)
        wload(Wuq, wuq_b, "W1", "wuq", 3)
        wuq_b3 = wuq_b.rearrange("(k p) (h c) -> p k h c", p=128, c=96)
        WuqS4 = WuqS.rearrange("p k (h c) -> p k h c", c=96)
        for kc in range(3):
            sch.dma(sp, WuqS4[:, kc, :, 0:64], wuq_b3[:, kc, :, 0:64], reads=[("w", "wuq")], writes=["W2"], grp="W2")
            sch.dma(sp, WuqS4[:, kc, :, 64:80], wuq_b3[:, kc, :, 80:96], reads=[("w", "wuq")], writes=["W2"], grp="W2")
            tk = sch.dma(sp, WuqS4[:, kc, :, 80:96], wuq_b3[:, kc, :, 64:80], reads=[("w", "wuq")], writes=["W2"], grp="W2")
        wload(Wuk, wuk_b, "W3", "wuk", 2)
        wload(Wuv, wuv_b, "W4", "wuv", 2)
        W2K = ["W2"]

        cqnT = T_A[:, 0:6144].rearrange("p (k t) -> p k t", k=3)
        ckvnT = T_A[:, 6144:10240].rearrange("p (k t) -> p k t", k=2)
        KhT = [bigb[:, 0:2048], bigb[:, 2048:4096]]
        QhT = [bigb[:, 4096:6144], bigb[:, 6144:8192]]
        cosT = bigb[:, 8192:10240]
        sinT = bigb[:, 10240:12288]
        zst = [bigb[:, 12288:12960], bigb[:, 12960:13632]]
        Vaug = T_V[:].rearrange("p (t h c) -> p t h c", t=16, h=8)
        otok = T_C[:].rearrange("p (t c) -> p t c", t=16)

        sch.dma(sp, posi[:], pos_d[b:b + 1, :].partition_broadcast(128), writes=["posi"], grp="posi")
        sch.op(dve, lambda: V.tensor_copy(out=posf[64:96, :], in_=posi[64:96, :]), reads=["posi"], writes=["posf"])
        TWO_PI = 2.0 * math.pi
        for (tab, offc, key) in ((cosT, K_RP + 2, "cosT"), (sinT, K_RP + 1, "sinT")):
            for q4 in range(2):
                seg = slice(q4 * 1024, (q4 + 1) * 1024)
                yv = tmpf[64:96, :]
                ki = posi[64:96, 0:1024]
                kf = xs[64:96, 0:1024]
                mv = xs[64:96, 1024:2048]
                sch.op(dve, lambda: V.tensor_scalar(out=yv, in0=posf[64:96, seg], scalar1=cst[64:96, K_RP:K_RP + 1],
                                                    scalar2=cst[64:96, offc:offc + 1], op0=ALU.mult, op1=ALU.add),
                       reads=["posf", "cst"], writes=["tmpf"])
                sch.op(dve, lambda: V.tensor_scalar(out=ki, in0=yv, scalar1=1.0 / TWO_PI, scalar2=0.5, op0=ALU.mult, op1=ALU.add),
                       reads=["tmpf", "posf"], writes=["posi"])
                sch.op(dve, lambda: V.tensor_copy(out=kf, in_=ki), reads=["posi"], writes=[("xs", 0)])
                sch.op(dve, lambda: V.scalar_tensor_tensor(out=yv, in0=kf, scalar=-TWO_PI, in1=yv, op0=ALU.mult, op1=ALU.add),
                       reads=[("xs", 0), "tmpf"], writes=["tmpf"])
                sch.op(dve, lambda: V.tensor_scalar(out=mv, in0=yv, scalar1=-math.pi, scalar2=TWO_PI, op0=ALU.is_lt, op1=ALU.mult),
                       reads=["tmpf"], writes=[("xs", 1)])
                sch.op(dve, lambda: V.tensor_tensor(out=yv, in0=yv, in1=mv, op=ALU.add), reads=["tmpf", ("xs", 1)], writes=["tmpf"])
                sch.op(act, lambda: A.activation(out=tab[64:96, seg], in_=yv, func=AF.Sin), reads=["tmpf"], writes=[key])

        sch.op(dve, lambda: V.memset(Vaug[:, :, :, 64:65], 1.0), writes=["Vaug"])

        for tt in range(NT):
            slot = tt % 2
            pz0, k0 = bank("z0", [0, 1])
            pz1, k1 = bank("z1", [2, 3])
            mm_acc(pz0[:, 0:384], [(hT[:, kc, tt * 128:(tt + 1) * 128], WinB[:, kc, 0:384]) for kc in range(8)],
                   k0, [("hT", tt), "W0"])
            mm_acc(pz1[:, 0:288], [(hT[:, kc, tt * 128:(tt + 1) * 128], WinB[:, kc, 384:672]) for kc in range(8)],
                   k1, [("hT", tt), "W0"])
            st = stat[:, 16 + slot * 4:16 + slot * 4 + 2]
            junk = xn[:, slot * 1024:slot * 1024 + 384]
            sch.op(act, lambda: A.activation(out=junk, in_=pz0[:, 0:384], func=AF.Square, scale=384.0 ** -0.5,
                                             accum_out=st[:, 0:1]), reads=[k0], writes=[("xn", slot), ("bst", slot)])
            sch.op(act, lambda: A.activation(out=junk[:, 0:256], in_=pz1[:, 0:256], func=AF.Square, scale=1.0 / 16.0,
                                             accum_out=st[:, 1:2]), reads=[k1], writes=[("xn", slot), ("bst", slot)])
            sch.op(act, lambda: A.activation(out=st, in_=st, func=AF.Sqrt, bias=epsb[:], scale=1.0),
                   reads=[("bst", slot), "epsb"], writes=[("bst", slot)])
            sch.op(dve, lambda: V.reciprocal(out=st, in_=st), reads=[("bst", slot)], writes=[("bst", slot)])
            z = zst[slot]
            sch.op(act, lambda: A.activation(out=z[:, 0:384], in_=pz0[:, 0:384], func=AF.Copy, scale=st[:, 0:1]),
                   reads=[k0, ("bst", slot)], writes=[("zst", slot)])
            sch.op(dve, lambda: V.tensor_scalar(out=z[:, 384:640], in0=pz1[:, 0:256], scalar1=st[:, 1:2], scalar2=None,
                                                op0=ALU.mult), reads=[k1, ("bst", slot)], writes=[("zst", slot)])
            ks = kst[:, slot * 96:(slot + 1) * 96]
            sch.op(act, lambda: A.copy(out=z[:, 640:672], in_=pz1[:, 256:288]), reads=[k1], writes=[("zst", slot)])
            pt, ptkey = tbank()
            ptv = pt[:].rearrange("p (k t) -> p k t", k=8)
            for j in range(5):
                sch.op(pe, lambda j=j: T.transpose(ptv[:, j, :], z[:, j * 128:(j + 1) * 128], identb[:]),
                       reads=[("zst", slot), "ident"], writes=[ptkey], sig=False)
            sch.op(dve, lambda: V.tensor_copy(out=ks[:, 64:96], in_=z[:, 640:672]), reads=[("zst", slot)], writes=[("kst", slot)])
            sch.op(pe, lambda: T.transpose(ptv[0:96, 5, :], ks, identb[:]), reads=[("kst", slot), "ident"], writes=[ptkey])
            sch.op(dve, lambda: V.tensor_copy(out=ks[:, 64:80], in_=z[:, 656:672]), reads=[("zst", slot), ptkey], writes=[("kst", slot)])
            sch.op(dve, lambda: V.tensor_copy(out=ks[:, 80:96], in_=z[:, 640:656]), reads=[("zst", slot)], writes=[("kst", slot)])
            sch.op(pe, lambda: T.transpose(ptv[0:96, 6, :], ks, identb[:]), reads=[("kst", slot), "ident"], writes=[ptkey])
            tsl = slice(tt * 128, (tt + 1) * 128)
            gq = gvecT[:, 24:27].unsqueeze(2).broadcast_to([128, 3, 128])
            gk = gvecT[:, 27:29].unsqueeze(2).broadcast_to([128, 2, 128])
            sch.op(dve, lambda: V.tensor_tensor(out=cqnT[:, :, tsl], in0=ptv[:, 0:3, :], in1=gq, op=ALU.mult),
                   reads=[ptkey] + GV, writes=[("cqnT", tt)])
            sch.op(dve, lambda: V.tensor_tensor(out=ckvnT[:, :, tsl], in0=ptv[:, 3:5, :], in1=gk, op=ALU.mult),
                   reads=[ptkey] + GV, writes=[("ckvnT", tt)])
            t1 = tmpf[64:96, 0:128]
            t2 = tmpf[64:96, 512:640]
            sch.op(dve, lambda: V.tensor_tensor(out=t1, in0=ptv[64:96, 5, :], in1=cosT[64:96, tsl], op=ALU.mult),
                   reads=[ptkey, "cosT"], writes=["tmpf"])
            sch.op(dve, lambda: V.tensor_tensor(out=t2, in0=ptv[64:96, 6, :], in1=sinT[64:96, tsl], op=ALU.mult),
                   reads=[ptkey, "sinT"], writes=["tmpf"])
            sch.op(dve, lambda: V.tensor_tensor(out=KhT[0][64:96, tsl], in0=t1, in1=t2, op=ALU.add),
                   reads=["tmpf"], writes=[("KhT", 0)])
            sch.op(act, lambda: A.copy(out=KhT[1][64:96, tsl], in_=KhT[0][64:96, tsl]),
                   reads=[("KhT", 0)], writes=[("KhT", 1)])
            pv, kv = bank("z0", [0, 1])
            mm_acc(pv[:, :], [(ckvnT[:, kc, tsl], Wuv[:, kc, :]) for kc in range(2)], kv, [("ckvnT", tt), "W4"])
            sch.op(act, lambda: A.copy(out=Vaug[:, tt, :, 0:64], in_=pv[:].rearrange("p (h c) -> p h c", h=8)),
                   reads=[kv], writes=["Vaug"])

        if stage == 2:
            dump("cq", T_A[:, 0:10240], [("cqnT", t) for t in range(NT)] + [("ckvnT", t) for t in range(NT)])
            dump("kh", bigb[:, 0:2048], [("KhT", 0)])
            dump("cos", bigb[:, 8192:12288], ["cosT", "sinT"])
            dump("va", T_V[:], ["Vaug"])
        checkpoint(2)
        ALLCQ = [("cqnT", t) for t in range(NT)]
        ALLCKV = [("ckvnT", t) for t in range(NT)]
        att_scale = 96.0 ** -0.5
        def produce_kq(h):
            s_ = h % 2
            for blk in range(4):
                bs = slice(blk * 512, (blk + 1) * 512)
                pk, kk = bank("kq", [6, 7])
                mm_acc(pk[0:64, :], [(Wuk[:, kc, h * 64:(h + 1) * 64], ckvnT[:, kc, bs]) for kc in range(2)], kk,
                       ALLCKV[blk * 4:blk * 4 + 4] + ["W3"])
                sch.op(dve, lambda: V.tensor_copy(out=KhT[s_][0:64, bs], in_=pk[0:64, :]), reads=[kk], writes=[("KhT", s_)])
                pq, kq = bank("kq", [6, 7])
                pqs, kqs = bank("kq", [6, 7])
                mm_acc(pq[0:96, :], [(Wuq[:, kc, h * 96:(h + 1) * 96], cqnT[:, kc, bs]) for kc in range(3)], kq,
                       ALLCQ[blk * 4:blk * 4 + 4] + ["W1"])
                mm_acc(pqs[0:96, :], [(WuqS[:, kc, h * 96:(h + 1) * 96], cqnT[:, kc, bs]) for kc in range(3)], kqs,
                       ALLCQ[blk * 4:blk * 4 + 4] + W2K)
                sch.op(dve, lambda: V.tensor_copy(out=QhT[s_][0:64, bs], in_=pq[0:64, :]), reads=[kq], writes=[("QhT", s_)])
                t1 = tmpf[64:96, 0:512]
                t2 = tmpf[64:96, 512:1024]
                sch.op(dve, lambda: V.tensor_tensor(out=t1, in0=pq[64:96, :], in1=cosT[64:96, bs], op=ALU.mult),
                       reads=[kq, "cosT"], writes=["tmpf"])
                sch.op(dve, lambda: V.tensor_tensor(out=t2, in0=pqs[64:96, :], in1=sinT[64:96, bs], op=ALU.mult),
                       reads=[kqs, "sinT"], writes=["tmpf"])
                sch.op(pool, lambda: G.tensor_tensor(out=QhT[s_][64:96, bs], in0=t1, in1=t2, op=ALU.add),
                       reads=["tmpf"], writes=[("QhT", s_)])

        def attend(h):
            s_ = h % 2
            for qb in range(4):
                qs = slice(qb * 512, (qb + 1) * 512)
                pO, kO = bank("pO", [4, 5])
                pOv = pO[:, 0:260].rearrange("p (j c) -> p j c", j=4)

                def pv(kt, P_, ps_):
                    for j in range(4):
                        sch.op(pe, lambda j=j: T.matmul(pOv[:, j, :], lhsT=P_[:, j * 128:(j + 1) * 128], rhs=Vaug[:, kt, h, :],
                                                        start=(kt == 0 and j == 0), stop=(kt == 15), skip_group_check=True),
                               reads=[("PT", ps_), "Vaug"], writes=[kO], sig=(j == 3))

                prev = None
                for kt in range(16):
                    pS, kS = bank("pS", [0, 1, 2, 3])
                    sch.op(pe, lambda: T.matmul(pS[:, :], lhsT=KhT[s_][0:96, kt * 128:(kt + 1) * 128], rhs=QhT[s_][0:96, qs],
                                                start=True, stop=True), reads=[("KhT", s_), ("QhT", s_)], writes=[kS])
                    ps_ = rot.get("ptslot", 0) % 3
                    rot["ptslot"] = rot.get("ptslot", 0) + 1
                    P_ = PTt[:, ps_ * 512:(ps_ + 1) * 512]
                    sch.op(act, lambda: A.activation(out=P_, in_=pS[:, :], func=AF.Exp, scale=att_scale),
                           reads=[kS], writes=[("PT", ps_)])
                    if prev is not None:
                        pv(*prev)
                    prev = (kt, P_, ps_)
                pv(*prev)
                rec = stat[:, 32 + (qb % 2) * 4:36 + (qb % 2) * 4]
                sch.op(dve, lambda: V.reciprocal(out=rec, in_=pOv[:, :, 64]), reads=[kO], writes=[("rec", qb % 2)])
                sch.op(dve, lambda: V.tensor_tensor(out=otok[:, qb * 4:(qb + 1) * 4, h * 64:(h + 1) * 64], in0=pOv[:, :, 0:64],
                                                    in1=rec.unsqueeze(2).broadcast_to([128, 4, 64]), op=ALU.mult),
                       reads=[kO, ("rec", qb % 2)], writes=[("otok", qb)])

        produce_kq(0)
        for h in range(8):
            if h + 1 < 8:
                produce_kq(h + 1)
            attend(h)

        def flush_oT(br):
            for blk in range(4):
                sl = blk % 2
                sv = stg[:, sl * 512:(sl + 1) * 512]
                for j in range(4):
                    pt, ptkey = tbank()
                    ptv = pt[:].rearrange("p (k t) -> p k t", k=8)
                    for i in range(4):
                        sch.op(pe, lambda i=i: T.transpose(ptv[:, i, :], otok[:, blk * 4 + i, j * 128:(j + 1) * 128], identb[:]),
                               reads=[("otok", blk), "ident"], writes=[ptkey], sig=(i == 3))
                    st_ = stg[:, sl * 512:(sl + 1) * 512]
                    if j % 2 == 0:
                        sch.op(act, lambda: A.copy(out=st_.rearrange("p (i t) -> p i t", i=4), in_=ptv[:, 0:4, :]),
                               reads=[ptkey], writes=[("stg", sl)])
                    else:
                        sch.op(dve, lambda: V.tensor_copy(out=st_.rearrange("p (i t) -> p i t", i=4), in_=ptv[:, 0:4, :]),
                               reads=[ptkey], writes=[("stg", sl)])
                    sch.dma(sp, oT_d[br, :, j, blk * 512:(blk + 1) * 512], st_, reads=[("stg", sl)],
                            writes=[("oTd", br, blk, j)], grp=("stg", sl))
                    sl = (sl + 1) % 2

        if stage == 3:
            dump("otok", T_C[:], [("otok", q) for q in range(4)])
            dump("kh", bigb[:, 0:8192], [("KhT", 0), ("KhT", 1), ("QhT", 0), ("QhT", 1)])
        checkpoint(3)
        flush_oT(0)
        sch.barrier()

        checkpoint(4)
        WinU = T_W[:, 0:4096].rearrange("p (k n) -> p k n", k=8)
        WinVG = T_W[:, 4096:8448].rearrange("p (k n) -> p k n", k=8)
        WqBD = T_W[:, 8448:8960].rearrange("p (j n) -> p j n", j=4)
        WkBD = T_W[:, 8960:9472].rearrange("p (j n) -> p j n", j=4)
        for nm, lo, hi in (("W0", 0, 4096), ("W1", 4096, 8448), ("W1b", 4096, 8448), ("W3", 8448, 8960), ("W4", 8960, 9472)):
            sch.set_alias(nm, lo, hi)
        wload(WinU, win_b[:, C_MU:C_MU + 512], "W0", "win", 8)
        sch.dma(sp, WinVG[:, :, 0:512], win_b[:, C_MV:C_MV + 512].rearrange("(k p) n -> p k n", p=128),
                reads=[("w", "win")], writes=["W1"], grp="W1")
        sch.dma(sp, WinVG[:, :, 512:544], win_b[:, C_MG:C_MG + 32].rearrange("(k p) n -> p k n", p=128),
                reads=[("w", "win")], writes=["W1"], grp="W1")
        sch.op(dve, lambda: V.memset(T_W[:, 8448:9472], 0.0), writes=["W3", "W4"])
        for (Wt, src, key) in ((WqBD, wq_b, "W3"), (WkBD, wk_b, "W4")):
            s4 = src.rearrange("(j r c) d -> r c j d", j=4, r=2)
            for r in range(2):
                sch.dma(sp, Wt[r * 64:(r + 1) * 64, :, r * 64:(r + 1) * 64], s4[r], reads=[("w", "wq"), ("w", "wk")],
                        writes=[key], grp=key)
        uT = T_big[:].rearrange("p (c t) -> p c t", c=4)
        Hh = T_big[:, 0:8192].rearrange("p (t c) -> p t c", t=16)
        cT = T_C[:].rearrange("p (c t) -> p c t", c=4)
        vaug = Vaug
        accf = T_A[:].bitcast(F32)
        sch.op(dve, lambda: V.memset(uT[:, :, 14:16], 0.0), writes=["uTpad"])
        sch.op(dve, lambda: V.memset(uT[:, :, 2064:2066], 0.0), writes=["uTpad"])
        sch.op(dve, lambda: V.memset(vaug[:, :, :, 64:65], 1.0), writes=["Vaug"])
        checkpoint(4.1)
        for ct in range(4):
            for blk in range(4):
                pu, ku = bank("cu", [0, 1])
                mm_acc(pu[:, :], [(WinU[:, kc, ct * 128:(ct + 1) * 128], hT[:, kc, blk * 512:(blk + 1) * 512]) for kc in range(8)],
                       ku, htkeys(blk) + ["W0"])
                sch.op(act, lambda: A.copy(out=uT[:, ct, 16 + blk * 512:16 + (blk + 1) * 512], in_=pu[:, :]),
                       reads=[ku], writes=[("uT", ct)])
        checkpoint(4.2)
        for tt in range(NT):
            pv, kv = bank("cv", [2, 3])
            pg, kg = bank("cg", [4, 5])
            mm_acc(pv[:, :], [(hT[:, kc, tt * 128:(tt + 1) * 128], WinVG[:, kc, 0:512]) for kc in range(8)], kv, [("hT", tt), "W1"])
            mm_acc(pg[:, 0:32], [(hT[:, kc, tt * 128:(tt + 1) * 128], WinVG[:, kc, 512:544]) for kc in range(8)], kg,
                   [("hT", tt), "W1"])
            sch.op(act, lambda: A.copy(out=vaug[:, tt, :, 0:64], in_=pv[:].rearrange("p (h c) -> p h c", h=8)),
                   reads=[kv], writes=["Vaug"])
            Gt = gtmp[:, 0:32]
            sch.op(dve, lambda: V.tensor_tensor(out=Gt, in0=pg[:, 0:32], in1=gbias_bc[:], op=ALU.add),
                   reads=[kg, "gbias"], writes=["Gt"])
            G4 = Gt.rearrange("p (a h) -> p a h", a=4)
            Fv = gtmp[:, 0:32].rearrange("p (d a h) -> p d a h", d=2, a=2)[:, :, 1, :]
            Iv = gtmp[:, 0:32].rearrange("p (d a h) -> p d a h", d=2, a=2)[:, :, 0, :]
            sp_ = gtmp[:, 32:48].rearrange("p (d h) -> p d h", d=2)
            sch.op(act, lambda: A.activation(out=sp_, in_=Fv, func=AF.Exp, scale=-1.0), reads=["Gt"], writes=["spg"])
            sch.op(act, lambda: A.activation(out=sp_, in_=sp_, func=AF.Ln, bias=oneb[:], scale=1.0), reads=["spg", "oneb"], writes=["spg"])
            pc, kc_ = bank("cc", [0, 1])
            sch.op(pe, lambda: T.matmul(pc[:, 0:8], lhsT=cst[:, K_TF:K_TF + 128], rhs=gtmp[:, 32:40], start=True, stop=True),
                   reads=["spg", "cst"], writes=[kc_], sig=False)
            sch.op(pe, lambda: T.matmul(pc[:, 8:16], lhsT=cst[:, K_TB:K_TB + 128], rhs=gtmp[:, 40:48], start=True, stop=True),
                   reads=["spg", "cst"], writes=[kc_], sig=False)
            sch.op(pe, lambda: T.matmul(pc[:, 16:32], lhsT=cst[:, K_I0:K_I0 + 128], rhs=gtmp[:, 32:48], start=True, stop=True),
                   reads=["spg", "cst"], writes=[kc_], sig=False)
            sch.op(pe, lambda: T.matmul(pc[:, 32:48], lhsT=cst[:, K_I1:K_I1 + 128], rhs=gtmp[:, 32:48], start=True, stop=True),
                   reads=["spg", "cst"], writes=[kc_])
            tsl16 = slice(tt * 16, (tt + 1) * 16)
            ic = gtmp[:, 48:64].rearrange("p (d h) -> p d h", d=2)
            sch.op(dve, lambda: V.tensor_tensor(out=ic, in0=Iv, in1=pc[:, 0:16].rearrange("p (d h) -> p d h", d=2), op=ALU.add),
                   reads=["Gt", kc_], writes=["ic"])
            sch.op(act, lambda: A.activation(out=gA[:, tsl16], in_=gtmp[:, 48:64], func=AF.Exp), reads=["ic"], writes=[("gA", tt)])
            sch.op(act, lambda: A.activation(out=gE[:, tsl16], in_=pc[:, 0:16], func=AF.Exp, scale=-1.0), reads=[kc_], writes=[("gE", tt)])
            sch.op(act, lambda: A.activation(out=gDec[:, tt * 32:(tt + 1) * 32], in_=pc[:, 16:48], func=AF.Exp, scale=-1.0),
                   reads=[kc_], writes=[("gDec", tt)])
            sch.op(dve, lambda: V.tensor_tensor(out=gWt[0:64, tsl16], in0=gA[0:64, tsl16], in1=gDec[0:64, tt * 32:tt * 32 + 16], op=ALU.mult),
                   reads=[("gA", tt), ("gDec", tt)], writes=[("gW", tt)])
            sch.op(dve, lambda: V.tensor_tensor(out=gWt[64:128, tsl16], in0=gA[64:128, tsl16], in1=gDec[64:128, tt * 32 + 16:tt * 32 + 32],
                                                op=ALU.mult), reads=[("gA", tt), ("gDec", tt)], writes=[("gW", tt)])
        checkpoint(4.5)
        for ct in range(4):
            eng, E_ = dve, V
            acc = accf[:, (ct % 2) * 2048:(ct % 2 + 1) * 2048]
            ak = ("acc", ct % 2)
            sch.op(eng, lambda: E_.tensor_scalar(out=acc, in0=uT[:, ct, 14:14 + 2048], scalar1=convw[:, ct * 5:ct * 5 + 1], scalar2=None,
                                                 op0=ALU.mult), reads=[("uT", ct), "uTpad", "convw"], writes=[ak])
            for j in range(1, 5):
                sch.op(eng, lambda j=j: E_.scalar_tensor_tensor(out=acc, in0=uT[:, ct, 14 + j:14 + j + 2048],
                                                                scalar=convw[:, ct * 5 + j:ct * 5 + j + 1], in1=acc,
                                                                op0=ALU.mult, op1=ALU.add),
                       reads=[("uT", ct), "uTpad", "convw", ak], writes=[ak])
            sch.op(act, lambda: A.activation(out=cT[:, ct, :], in_=acc, func=AF.Silu), reads=[ak], writes=[("cT", ct)])
        sch.barrier()

        if stage == 5:
            dump("cT", T_C[:], [("cT", c_) for c_ in range(4)])
            dump("ga", gA[:], [("gA", t) for t in range(NT)])
            dump("ge", gE[:], [("gE", t) for t in range(NT)])
            dump("gw", gWt[:], [("gW", t) for t in range(NT)])
            dump("gd", gDec[:], [("gDec", t) for t in range(NT)])
        checkpoint(5)
        def tA(off, n):
            return T_A[:, off:off + n]
        qTt = [tA(d * 512, 512).rearrange("p (j t) -> p j t", j=4) for d in range(2)]
        qTz = [[tA(1024 + (d * 2 + r) * 512, 512).rearrange("p (j t) -> p j t", j=4) for r in range(2)] for d in range(2)]
        kTz = [[tA(3072 + (d * 2 + r) * 512, 512).rearrange("p (j t) -> p j t", j=4) for r in range(2)] for d in range(2)]
        ktk = [tA(5120 + d * 512, 512) for d in range(2)]
        PTm = [tA(6144 + i * 1024, 1024).bitcast(F32) for i in range(2)]
        vaT = [tA(8192 + i * 1040, 1040).bitcast(F32).rearrange("p (h c) -> p h c", h=8) for i in range(2)]
        vwz = [T_W[:, 11288 + i * 520:11288 + (i + 1) * 520].rearrange("p (h c) -> p h c", h=8) for i in range(2)]
        Cbf = [T_W[:, 10768 + d * 260:10768 + (d + 1) * 260].rearrange("p (j c) -> p j c", j=4) for d in range(2)]
        Cst = [T_W[:, 9728 + d * 520:9728 + (d + 1) * 520].bitcast(F32).rearrange("p (j c) -> p j c", j=4) for d in range(2)]
        pm = [cst[:, K_I0:K_I0 + 1], cst[:, K_I1:K_I1 + 1]]
        pm8 = stat[:, 28:30]
        sch.op(dve, lambda: V.tensor_scalar(out=pm8[:, 0:1], in0=pm[0], scalar1=0.125, scalar2=None, op0=ALU.mult), reads=["cst"], writes=["pm8"])
        sch.op(dve, lambda: V.tensor_scalar(out=pm8[:, 1:2], in0=pm[1], scalar1=0.125, scalar2=None, op0=ALU.mult), reads=["cst", "pm8"], writes=["pm8"])
        for d in range(2):
            sch.op(dve, lambda d=d: V.memset(Cst[d], 0.0), writes=[("Cst", d)])
            sch.op(dve, lambda d=d: V.memset(Cbf[d], 0.0), writes=[("Cbf", d)])
            sch.op(dve, lambda d=d: V.memset(PTm[d], 0.0), writes=[("PTm", d)])
            sch.op(dve, lambda d=d: V.memset(vwz[d], 0.0), writes=[("vw", d)])
        sch.op(dve, lambda: V.memset(Hh, 0.0), writes=[("H", t) for t in range(NT)])
        cnt2 = [0]
        for it in range(32):
            for d in range(2):
                c = it if d == 0 else 31 - it
                m = c // 2
                r0 = c % 2
                rs = slice(r0 * 64, (r0 + 1) * 64)
                new_tile = (it % 2 == 0)
                tsl = slice(m * 128, (m + 1) * 128)
                if new_tile:
                    pq, kq = bank("mq", [0, 1])
                    pk, kk = bank("mk", [2, 3])
                    pk2, kk2 = bank("mk2", [4, 5])
                    for j in range(4):
                        sch.op(pe, lambda j=j: T.matmul(pq[:, j * 128:(j + 1) * 128], lhsT=WqBD[:, j, :], rhs=cT[:, j, tsl], start=True, stop=True),
                               reads=[("cT", j), "W3"], writes=[kq], sig=(j == 3))
                    for j in range(4):
                        sch.op(pe, lambda j=j: T.matmul(pk[:, j * 128:(j + 1) * 128], lhsT=WkBD[:, j, :], rhs=cT[:, j, tsl], start=True, stop=True),
                               reads=[("cT", j), "W4"], writes=[kk], sig=(j == 3))
                    for j in range(4):
                        sch.op(pe, lambda j=j: T.matmul(pk2[:, j * 128:(j + 1) * 128], lhsT=cT[:, j, tsl], rhs=WkBD[:, j, :], start=True, stop=True),
                               reads=[("cT", j), "W4"], writes=[kk2], sig=(j == 3))
                    pq3 = pq[:].rearrange("p (j t) -> p j t", j=4)
                    pk3 = pk[:].rearrange("p (j t) -> p j t", j=4)
                    sch.op(act, lambda: A.copy(out=qTt[d], in_=pq3), reads=[kq], writes=[("qTt", d)])
                    for r in range(2):
                        sch.op(act, lambda r=r: A.activation(out=qTz[d][r], in_=pq3, func=AF.Copy, scale=pm[r]),
                               reads=[kq, "cst"], writes=[("qTz", d, r)])
                        sch.op(act, lambda r=r: A.activation(out=kTz[d][r], in_=pk3, func=AF.Copy, scale=pm8[:, r:r + 1]),
                               reads=[kk, "pm8"], writes=[("kTz", d, r)])
                    sch.op(dve, lambda: V.tensor_scalar(out=ktk[d], in0=pk2[:, :], scalar1=0.125, scalar2=None, op0=ALU.mult),
                           reads=[kk2], writes=[("ktk", d)])
                if it == 0 and d == 0:
                    checkpoint(5.1)
                i2 = cnt2[0] % 2
                cnt2[0] += 1
                col = slice(m * 16 + d * 8, m * 16 + d * 8 + 8)
                sch.op(pool, lambda: G.tensor_tensor(out=vaT[i2], in0=vaug[:, m, :, :],
                                                     in1=gA[:, col].unsqueeze(2).broadcast_to([128, 8, 65]), op=ALU.mult),
                       reads=["Vaug", ("gA", m)], writes=[("va", i2)])
                sch.op(pool, lambda: G.tensor_tensor(out=vwz[r0][rs], in0=vaug[rs, m, :, :],
                                                     in1=gWt[rs, col].unsqueeze(2).broadcast_to([64, 8, 65]), op=ALU.mult),
                       reads=["Vaug", ("gW", m)], writes=[("vw", r0)])
                if it == 0 and d == 0:
                    checkpoint(5.2)
                pS, kS = bank("mS", [0, 1, 2, 3])
                pSv = pS[:].rearrange("p (h t) -> p h t", h=8)
                for h in range(8):
                    j, r = h // 2, h % 2
                    sch.op(pe, lambda h=h, j=j, r=r: T.matmul(pSv[rs, h, :], lhsT=kTz[d][r][:, j, rs], rhs=qTt[d][:, j, rs],
                                                             start=True, stop=True),
                           reads=[("kTz", d, r), ("qTt", d)], writes=[kS], sig=(h == 7))
                if it == 0 and d == 0:
                    checkpoint(5.25)
                mk = cst[rs, (K_MF if d == 0 else K_MB):(K_MF if d == 0 else K_MB) + 64]
                sch.op(dve, lambda: V.tensor_tensor(out=PTm[r0][rs].rearrange("p (h t) -> p h t", h=8), in0=pSv[rs],
                                                    in1=mk.unsqueeze(1).broadcast_to([64, 8, 64]), op=ALU.mult),
                       reads=[kS, "cst"], writes=[("PTm", r0)])
                if it == 0 and d == 0:
                    checkpoint(5.3)
                pN0, kN0 = bank("mN0", [4])
                pN1, kN1 = bank("mN1", [5])
                for h in range(8):
                    j, r = h // 2, h % 2
                    pn, kn = (pN0, kN0) if h < 4 else (pN1, kN1)
                    o_ = pn[rs, (h % 4) * 65:(h % 4 + 1) * 65]
                    sch.op(pe, lambda h=h, o_=o_: T.matmul(o_, lhsT=PTm[r0][:, h * 64:(h + 1) * 64], rhs=vaT[i2][:, h, :], start=True, stop=False),
                           reads=[("PTm", r0), ("va", i2)], writes=[kn], sig=False)
                    sch.op(pe, lambda h=h, j=j, r=r, o_=o_: T.matmul(o_, lhsT=qTz[d][r][:, j, rs], rhs=Cbf[d][:, j, :], start=False, stop=True),
                           reads=[("qTz", d, r), ("Cbf", d)], writes=[kn], sig=(h % 4 == 3))
                if it == 0 and d == 0:
                    checkpoint(5.4)
                pC, kC = bank("mC", [0, 1, 2, 3])
                pCv = pC[:, 0:260].rearrange("p (j c) -> p j c", j=4)
                for h in range(8):
                    j, r = h // 2, h % 2
                    pr = slice(r * 64, (r + 1) * 64)
                    sch.op(pe, lambda h=h, j=j, pr=pr: T.matmul(pCv[pr, j, :], lhsT=ktk[d][:, h * 64:(h + 1) * 64], rhs=vwz[r0][:, h, :],
                                                               start=True, stop=True),
                           reads=[("ktk", d), ("vw", r0)], writes=[kC], sig=(h == 7))
                if it == 0 and d == 0:
                    checkpoint(5.5)
                ecol = gE[rs, col]
                dn = stat[rs, 40:48]
                for hb, (pn, kn) in enumerate(((pN0, kN0), (pN1, kN1))):
                    pnv = pn[rs, 0:260].rearrange("p (h c) -> p h c", h=4)
                    sch.op(dve, lambda pnv=pnv, hb=hb: V.tensor_tensor(out=dn[:, hb * 4:(hb + 1) * 4], in0=pnv[:, :, 64],
                                                                      in1=ecol[:, hb * 4:(hb + 1) * 4], op=ALU.mult),
                           reads=[kn, ("gE", m)], writes=["dn"])
                dn2 = stat[rs, 20:28]
                sch.op(dve, lambda: V.tensor_scalar(out=dn2, in0=dn, scalar1=-1.0, scalar2=1.0, op0=ALU.mult, op1=ALU.max),
                       reads=["dn"], writes=["dn2"])
                sch.op(dve, lambda: V.tensor_tensor(out=dn, in0=dn, in1=dn2, op=ALU.max), reads=["dn", "dn2"], writes=["dn"])
                sch.op(dve, lambda: V.reciprocal(out=dn, in_=dn), reads=["dn"], writes=["dn"])
                sch.op(dve, lambda: V.tensor_tensor(out=dn, in0=dn, in1=ecol, op=ALU.mult), reads=["dn", ("gE", m)], writes=["dn"])
                hn = tmpf[rs, 0:512].rearrange("p (h c) -> p h c", h=8)
                for hb, (pn, kn) in enumerate(((pN0, kN0), (pN1, kN1))):
                    pnv = pn[rs, 0:260].rearrange("p (h c) -> p h c", h=4)
                    sch.op(dve, lambda pnv=pnv, hb=hb: V.tensor_tensor(out=hn[:, hb * 4:(hb + 1) * 4, :], in0=pnv[:, :, 0:64],
                                                                      in1=dn[:, hb * 4:(hb + 1) * 4].unsqueeze(2).broadcast_to([64, 4, 64]),
                                                                      op=ALU.mult), reads=[kn, "dn"], writes=["hn"])
                sch.op(pool, lambda: G.tensor_tensor(out=Hh[rs, m, :], in0=Hh[rs, m, :], in1=tmpf[rs, 0:512], op=ALU.add),
                       reads=["hn", ("H", m)], writes=[("H", m)])
                if it == 0 and d == 0:
                    checkpoint(5.6)
                par = r0
                dcy = gDec[:, m * 32 + par * 16 + d * 8:m * 32 + par * 16 + d * 8 + 8]
                dv4 = dcy.rearrange("p (j r) -> p j r", r=2)
                for r in range(2):
                    pr = slice(r * 64, (r + 1) * 64)
                    sch.op(dve, lambda r=r, pr=pr: V.tensor_tensor(out=Cst[d][pr], in0=Cst[d][pr],
                                                                  in1=dv4[pr, :, r].unsqueeze(2).broadcast_to([64, 4, 65]), op=ALU.mult),
                           reads=[("gDec", m), ("Cst", d)], writes=[("Cst", d)])
                sch.op(dve, lambda: V.tensor_tensor(out=Cst[d], in0=Cst[d], in1=pCv, op=ALU.add), reads=[kC, ("Cst", d)], writes=[("Cst", d)])
                sch.op(act, lambda: A.copy(out=Cbf[d], in_=Cst[d]), reads=[("Cst", d)], writes=[("Cbf", d)])

        if stage == 6:
            dump("H", T_big[:, 0:8192], [("H", t) for t in range(NT)])
        checkpoint(6)
        WinO = T_W[:, 0:4096].rearrange("p (k n) -> p k n", k=8)
        wload(WinO, win_b[:, C_MO:C_MO + 512], "W0", "win", 8)
        sch.barrier()
        otok2 = T_A[:, 0:8192].rearrange("p (t c) -> p t c", t=16)
        for tt in range(NT):
            po, ko = bank("co", [0, 1])
            mm_acc(po[:, :], [(hT[:, kc, tt * 128:(tt + 1) * 128], WinO[:, kc, :]) for kc in range(8)], ko, [("hT", tt), "W0"])
            sg = tmpf[:, 512:1024]
            sch.op(act, lambda: A.activation(out=sg, in_=po[:, :], func=AF.Sigmoid), reads=[ko], writes=["sg"])
            sq = tmpf[:, 0:512]
            Ht = Hh[:, tt, :]
            sch.op(act, lambda: A.activation(out=sq, in_=Ht, func=AF.Square, scale=0.125), reads=[("H", tt)], writes=["sq"])
            ms = stat[:, 48:56]
            sch.op(dve, lambda: V.tensor_reduce(out=ms, in_=sq.rearrange("p (h c) -> p h c", h=8), axis=mybir.AxisListType.X, op=ALU.add),
                   reads=["sq"], writes=["ms"])
            sch.op(act, lambda: A.activation(out=ms, in_=ms, func=AF.Sqrt, bias=epsb[:], scale=1.0), reads=["ms", "epsb"], writes=["ms"])
            sch.op(dve, lambda: V.reciprocal(out=ms, in_=ms), reads=["ms"], writes=["ms"])
            sch.op(dve, lambda: V.tensor_tensor(out=sq.rearrange("p (h c) -> p h c", h=8), in0=Ht.rearrange("p (h c) -> p h c", h=8),
                                                in1=ms.unsqueeze(2).broadcast_to([128, 8, 64]), op=ALU.mult),
                   reads=[("H", tt), "ms"], writes=["sq"])
            sch.op(pool, lambda: G.tensor_tensor(out=sq, in0=sq, in1=ghead_bc[:], op=ALU.mult), reads=["sq", "ghead"], writes=["sq"])
            sch.op(dve, lambda: V.tensor_tensor(out=otok2[:, tt, :], in0=sq, in1=sg, op=ALU.mult), reads=["sq", "sg"],
                   writes=[("otok", tt // 4)])
        otok_save = otok
        otok = otok2
        flush_oT(1)
        otok = otok_save
        sch.barrier()

        checkpoint(7)
        WkvA = T_W[:, 0:4096].rearrange("p (k n) -> p k n", k=8)
        WkvB = T_W[:, 4096:8192].rearrange("p (k n) -> p k n", k=8)
        WinQ = T_W[:, 8192:12288].rearrange("p (k n) -> p k n", k=8)
        for nm, lo, hi in (("W0", 0, 4096), ("W1", 4096, 8192), ("W2", 8192, 12288)):
            sch.set_alias(nm, lo, hi)
        wload(WkvA, wkv_b[:, 0:512], "W0", "wkv", 8)
        wload(WkvB, wkv_b[:, 512:1024], "W1", "wkv", 8)
        wload(WinQ, win_b[:, C_MQ:C_MQ + 512], "W2", "win", 8)
        memT = T_A[:, 0:2048].rearrange("p (k t) -> p k t", k=8)
        kmT = T_A[:, 2048:3072].rearrange("p (h t) -> p h t", h=4)
        vm = T_A[:, 3072:4104].rearrange("p (t h c) -> p t h c", t=2, h=4)
        qm = [T_A[:, 4104 + i * 512:4104 + (i + 1) * 512] for i in range(2)]
        sch.op(dve, lambda: V.memset(vm[:, :, :, 128:129], 1.0), writes=["vm"])
        for t2 in range(2):
            slot = t2
            xsv = xs[:, slot * 1024:(slot + 1) * 1024]
            sch.dma(sp, xsv, mem_d[b, t2 * 128:(t2 + 1) * 128, :], writes=[("xs", slot)], grp=("xs", slot))
            rms_to_T(xsv, [("xs", slot)], 16, lambda t2=t2: memT[:, :, t2 * 128:(t2 + 1) * 128], [("memT", t2)], slot)
        checkpoint(7.1)
        MT = [("memT", 0), ("memT", 1)]
        for h in range(4):
            pk, kk = bank("dk", [0, 1])
            mm_acc(pk[:, 0:256], [(WkvA[:, kc, h * 128:(h + 1) * 128], memT[:, kc, :]) for kc in range(8)], kk, MT + ["W0"])
            sch.op(act, lambda: A.copy(out=kmT[:, h, :], in_=pk[:, 0:256]), reads=[kk], writes=["kmT"])
        for t2 in range(2):
            pv, kv = bank("dv", [2, 3])
            mm_acc(pv[:, :], [(memT[:, kc, t2 * 128:(t2 + 1) * 128], WkvB[:, kc, :]) for kc in range(8)], kv, MT + ["W1"])
            sch.op(act, lambda: A.copy(out=vm[:, t2, :, 0:128], in_=pv[:].rearrange("p (h c) -> p h c", h=4)), reads=[kv], writes=["vm"])
        checkpoint(7.2)
        mscale = 128.0 ** -0.5
        qc = 0
        for h in range(4):
            for qb in range(4):
                qs = slice(qb * 512, (qb + 1) * 512)
                pq, kq = bank("dq", [0, 1])
                mm_acc(pq[:, :], [(WinQ[:, kc, h * 128:(h + 1) * 128], hT[:, kc, qs]) for kc in range(8)], kq, htkeys(qb) + ["W2"])
                qi = qc % 2
                qc += 1
                sch.op(act, lambda: A.copy(out=qm[qi], in_=pq[:, :]), reads=[kq], writes=[("qm", qi)])
                pOa, kOa = bank("dOa", [4])
                pOb, kOb = bank("dOb", [5])
                for kt in range(2):
                    pS, kS = bank("dS", [2, 3])
                    sch.op(pe, lambda: T.matmul(pS[:, :], lhsT=kmT[:, h, kt * 128:(kt + 1) * 128], rhs=qm[qi], start=True, stop=True),
                           reads=["kmT", ("qm", qi)], writes=[kS])
                    ps_ = (qc * 2 + kt) % 3
                    P_ = PTt[:, ps_ * 512:(ps_ + 1) * 512]
                    sch.op(act, lambda: A.activation(out=P_, in_=pS[:, :], func=AF.Exp, scale=mscale), reads=[kS], writes=[("PT", ps_)])
                    for j in range(4):
                        pO_, kO_ = (pOa, kOa) if j < 2 else (pOb, kOb)
                        sch.op(pe, lambda j=j, pO_=pO_: T.matmul(pO_[:, (j % 2) * 129:(j % 2 + 1) * 129], lhsT=P_[:, j * 128:(j + 1) * 128],
                                                                 rhs=vm[:, kt, h, :], start=(kt == 0 and j % 2 == 0), stop=(kt == 1),
                                                                 skip_group_check=True),
                               reads=[("PT", ps_), "vm"], writes=[kO_], sig=(j % 2 == 1))
                if h == 0 and qb == 0:
                    checkpoint(7.3)
                rec = stat[:, 32:36]
                for half, (pO_, kO_) in enumerate(((pOa, kOa), (pOb, kOb))):
                    pv2 = pO_[:, 0:258].rearrange("p (j c) -> p j c", j=2)
                    rc = rec[:, half * 2:half * 2 + 2]
                    sch.op(dve, lambda: V.reciprocal(out=rc, in_=pv2[:, :, 128]), reads=[kO_], writes=[("rec", half)])
                    sch.op(dve, lambda: V.tensor_tensor(out=otok[:, qb * 4 + half * 2:qb * 4 + half * 2 + 2, h * 128:(h + 1) * 128],
                                                        in0=pv2[:, :, 0:128], in1=rc.unsqueeze(2).broadcast_to([128, 2, 128]), op=ALU.mult),
                           reads=[kO_, ("rec", half)], writes=[("otok", qb)])
        checkpoint(7.4)
        flush_oT(2)
        sch.barrier()

        if dbg:
            for br in range(3):
                for j in range(4):
                    sch.dma(pool, dbg_d[br, :, j, :], oT_d[br, :, j, :], reads=[("oTd", br, k, j) for k in range(4)], writes=[("dbg", br, j)],
                            grp=("dbg", br, j), final=True)

        checkpoint(8)
        x1 = T_big[:, 0:4096].rearrange("p (t c) -> p t c", t=4)
        mrg = bigb[:, 8192:12288].rearrange("p (k t) -> p k t", k=8)
        h2T = bigb[:, 12288:16384].rearrange("p (k t) -> p k t", k=8)
        aT = T_A[:].rearrange("p (c t) -> p c t", c=NFF)
        oTs = [T_C[:, br * 2048:(br + 1) * 2048].rearrange("p (j t) -> p j t", j=4) for br in range(3)]
        sgb = [T_V[:, i * 512:(i + 1) * 512] for i in range(3)]
        prd = [T_V[:, 1536 + i * 1024:1536 + (i + 1) * 1024].bitcast(F32) for i in range(3)]
        WE = [T_W[:, i * 4608:(i + 1) * 4608] for i in range(2)]
        for i in range(2):
            sch.set_alias(("WE", i), i * 4608, (i + 1) * 4608)
        for i in range(4):
            sch.set_alias(("WF", i), i * 2048, (i + 1) * 2048)
        sch.set_alias("WO", 9216, 13312)
        sch.set_alias("WD", 0, 11264)
        for blk in range(4):
            bs = slice(blk * 512, (blk + 1) * 512)
            for br in range(3):
                sch.dma(sp, oTs[br], oT_d[br, :, :, bs], reads=[("oTd", br, blk, j) for j in range(4)], writes=[("oTs", br)], grp=("oTs", br))
            for f in range(8):
                ws = f % 2
                Wg_ = WE[ws][:, 0:3072].rearrange("p (k b n) -> p k b n", k=8, b=3)
                Wb_ = WE[ws][:, 3072:4608].rearrange("p (k b n) -> p k b n", k=4, b=3)
                for br in range(3):
                    sch.dma(sp, Wg_[:, :, br, :], win_b[:, br * 1024 + f * 128:br * 1024 + (f + 1) * 128].rearrange("(k p) n -> p k n", p=128),
                            reads=[("w", "win")], writes=[("WE", ws)], grp=("WE", ws))
                    sch.dma(sp, Wb_[:, :, br, :], wbr_b[br * 512:(br + 1) * 512, f * 128:(f + 1) * 128].rearrange("(k p) n -> p k n", p=128),
                            reads=[("w", "wbr")], writes=[("WE", ws)], grp=("WE", ws))
                for br in range(3):
                    pg, kg = bank("eg", [0, 1])
                    pb_, kb_ = bank("eb", [2, 3])
                    mm_acc(pg[:, :], [(Wg_[:, kc, br, :], hT[:, kc, bs]) for kc in range(8)], kg, htkeys(blk) + [("WE", ws)])
                    mm_acc(pb_[:, :], [(Wb_[:, kc, br, :], oTs[br][:, kc, :]) for kc in range(4)], kb_, [("oTs", br), ("WE", ws)])
                    sch.op(act, lambda: A.activation(out=sgb[br], in_=pg[:, :], func=AF.Sigmoid), reads=[kg], writes=[("sgb", br)])
                    sch.op(dve, lambda: V.tensor_tensor(out=prd[br], in0=pb_[:, :], in1=sgb[br], op=ALU.mult),
                           reads=[kb_, ("sgb", br)], writes=[("prd", br)])
                sch.op(pool, lambda: G.tensor_tensor(out=prd[0], in0=prd[0], in1=prd[1], op=ALU.add),
                       reads=[("prd", 0), ("prd", 1)], writes=[("prd", 0)])
                sch.op(pool, lambda: G.tensor_tensor(out=mrg[:, f, :], in0=prd[0], in1=prd[2], op=ALU.add),
                       reads=[("prd", 0), ("prd", 2)], writes=[("mrg", f)])
            MRG = [("mrg", f) for f in range(8)]
            Wo = T_W[:, 9216:13312].rearrange("p (k n) -> p k n", k=8)
            for nh in range(2):
                wload(Wo, wout_b[:, nh * 512:(nh + 1) * 512], "WO", "wout", 8)
                for tt in range(4):
                    slot = tt % 2
                    gt = blk * 4 + tt
                    xh = xs[:, slot * 1024:slot * 1024 + 512]
                    sch.dma(sp, xh, x_d[b, gt * 128:(gt + 1) * 128, nh * 512:(nh + 1) * 512], writes=[("xs", slot)], grp=("xs", slot))
                    py, ky = bank("ey", [4, 5])
                    mm_acc(py[:, :], [(mrg[:, kc, tt * 128:(tt + 1) * 128], Wo[:, kc, :]) for kc in range(8)], ky, MRG + ["WO"])
                    sch.op(dve, lambda: V.tensor_tensor(out=x1[:, tt, nh * 512:(nh + 1) * 512], in0=py[:, :], in1=xh,
                                                        op=ALU.add), reads=[ky, ("xs", slot)], writes=[("x1", tt)])
            for tt in range(4):
                rms_to_T(x1[:, tt, :], [("x1", tt)], 8, lambda tt=tt: h2T[:, :, tt * 128:(tt + 1) * 128], [("h2T", tt)], tt % 2)
            H2 = [("h2T", t) for t in range(4)]
            WF = [T_W[:, i * 2048:(i + 1) * 2048].rearrange("p (g k n) -> p g k n", g=2, k=8) for i in range(4)]
            for c in range(NFF):
                ws = c % 4
                sch.dma(sp, WF[ws][:, 0], wg_b[:, c * 128:(c + 1) * 128].rearrange("(k p) n -> p k n", p=128),
                        reads=[("w", "wg")], writes=[("WF", ws)], grp=("WF", ws))
                sch.dma(sp, WF[ws][:, 1], wu_b[:, c * 128:(c + 1) * 128].rearrange("(k p) n -> p k n", p=128),
                        reads=[("w", "wu")], writes=[("WF", ws)], grp=("WF", ws))
                pg, kg = bank("fg", [0, 1])
                pu, ku = bank("fu", [2, 3])
                mm_acc(pg[:, :], [(WF[ws][:, 0, kc, :], h2T[:, kc, :]) for kc in range(8)], kg, H2 + [("WF", ws)])
                mm_acc(pu[:, :], [(WF[ws][:, 1, kc, :], h2T[:, kc, :]) for kc in range(8)], ku, H2 + [("WF", ws)])
                si = c % 3
                sch.op(act, lambda: A.activation(out=prd[si], in_=pg[:, :], func=AF.Silu), reads=[kg], writes=[("prd", si)])
                sch.op(dve, lambda: V.tensor_tensor(out=aT[:, c, :], in0=pu[:, :], in1=prd[si], op=ALU.mult),
                       reads=[ku, ("prd", si)], writes=[("aT", c)])
            for hh in range(2):
                Wd_ = T_W[:, 0:11264].rearrange("p (c n) -> p c n", c=11)
                sch.dma(sp, Wd_, wd_b[hh * 1408:(hh + 1) * 1408, :].rearrange("(c p) n -> p c n", p=128),
                        reads=[("w", "wd")], writes=["WD"], grp="WD")
                for tt in range(4):
                    for nh in range(2):
                        py, ky = bank("ey", [4, 5])
                        mm_acc(py[:, :], [(aT[:, hh * 11 + c, tt * 128:(tt + 1) * 128], Wd_[:, c, nh * 512:(nh + 1) * 512]) for c in range(11)],
                               ky, [("aT", hh * 11 + c) for c in range(11)] + ["WD"])
                        sch.op(dve, lambda: V.tensor_tensor(out=x1[:, tt, nh * 512:(nh + 1) * 512], in0=py[:, :],
                                                            in1=x1[:, tt, nh * 512:(nh + 1) * 512], op=ALU.add),
                               reads=[ky, ("x1", tt)], writes=[("x1", tt)])
            for tt in range(4):
                slot = tt % 2
                gt = blk * 4 + tt
                st = stat[:, 56 + slot * 2:56 + slot * 2 + 1]
                xsv = xs[:, slot * 1024:(slot + 1) * 1024]
                xnv = xn[:, slot * 1024:(slot + 1) * 1024]
                sch.op(act, lambda: A.activation(out=xnv, in_=x1[:, tt, :], func=AF.Square, scale=1.0 / 32.0, accum_out=st),
                       reads=[("x1", tt)], writes=[("xn", slot), ("fst", slot)])
                sch.op(act, lambda: A.activation(out=st, in_=st, func=AF.Sqrt, bias=epsb[:], scale=1.0), reads=[("fst", slot), "epsb"],
                       writes=[("fst", slot)])
                sch.op(dve, lambda: V.reciprocal(out=st, in_=st), reads=[("fst", slot)], writes=[("fst", slot)])
                sch.op(act, lambda: A.activation(out=xsv, in_=x1[:, tt, :], func=AF.Copy, scale=st), reads=[("x1", tt), ("fst", slot)],
                       writes=[("xs", slot)])
                sch.op(pool, lambda: G.tensor_tensor(out=xsv, in0=xsv, in1=gfin_bc[:], op=ALU.mult), reads=[("xs", slot), "gfin"],
                       writes=[("xs", slot)])
                sch.dma(sp, out_d[b, gt * 128:(gt + 1) * 128, :], xsv, reads=[("xs", slot)], grp=("xs", slot), final=True)
        sch.barrier()


def make_consts():
    c = np.zeros((128, K_N), np.float32)
    c[:, K_ID:K_ID + 128] = np.eye(128, dtype=np.float32)
    p = np.arange(128)
    s = (p % 64)[:, None]
    t = np.arange(64)[None, :]
    c[:, K_MF:K_MF + 64] = (s <= t)
    c[:, K_MB:K_MB + 64] = (s >= t)
    ss = p[:, None]
    tt = p[None, :]
    same = (ss // 64) == (tt // 64)
    c[:, K_TF:K_TF + 128] = same & (ss <= tt)
    c[:, K_TB:K_TB + 128] = same & (ss >= tt)
    c[:, K_I0:K_I0 + 128] = (ss // 64 == 0) & (tt >= 0)
    c[:, K_I1:K_I1 + 128] = (ss // 64 == 1) & (tt >= 0)
    inv = (10000.0 ** (-np.arange(16, dtype=np.float32) / 16.0)).astype(np.float32)
    for i in range(32):
        c[64 + i, K_RP] = inv[i % 16]
        c[64 + i, K_RP + 1] = math.pi if i < 16 else 0.0
        c[64 + i, K_RP + 2] = 0.5 * math.pi
    c[:, K_RP + 3] = -math.pi
    return c


_CACHE = {}


def kernel(**inputs):
    f = lambda a: np.ascontiguousarray(np.asarray(a, dtype=np.float32))
    if "nc" not in _CACHE:
        _CACHE["nc"] = build_program()
    nc = _CACHE["nc"]
    shared = {
        "g_mix": f(inputs["g_mix"]).reshape(1, D),
        "w_in": f(inputs["w_in"]).reshape(D, N_IN),
        "mla_g_q": f(inputs["mla_g_q"]).reshape(1, 384),
        "mla_g_kv": f(inputs["mla_g_kv"]).reshape(1, 256),
        "mla_w_uq": f(inputs["mla_w_uq"]).reshape(384, 768),
        "mla_w_uk": f(inputs["mla_w_uk"]).reshape(256, 512),
        "mla_w_uv": f(inputs["mla_w_uv"]).reshape(256, 512),
        "ml_conv_w": f(inputs["ml_conv_w"]).reshape(5, 512),
        "ml_w_q": f(inputs["ml_w_q"]).reshape(512, 64),
        "ml_w_k": f(inputs["ml_w_k"]).reshape(512, 64),
        "ml_gate_bias": f(inputs["ml_gate_bias"]).reshape(1, 32),
        "ml_g_head": f(inputs["ml_g_head"]).reshape(1, 512),
        "mem_g": f(inputs["mem_g"]).reshape(1, D),
        "mem_w_kv": f(inputs["mem_w_kv"]).reshape(D, 1024),
        "w_branch": f(inputs["w_branch"]).reshape(1536, D),
        "w_out": f(inputs["w_out"]).reshape(D, D),
        "g_ffn": f(inputs["g_ffn"]).reshape(1, D),
        "w_ffn_gate": f(inputs["w_ffn_gate"]).reshape(D, DFF),
        "w_ffn_up": f(inputs["w_ffn_up"]).reshape(D, DFF),
        "w_ffn_down": f(inputs["w_ffn_down"]).reshape(DFF, D),
        "g_final": f(inputs["g_final"]).reshape(1, D),
        "cst": make_consts(),
    }
    x = f(inputs["x"])
    mem = f(inputs["mem"])
    pos = np.ascontiguousarray(np.asarray(inputs["positions"], dtype=np.int32))
    in_maps = []
    for c in range(NCORES):
        sl = slice(c * SEQ_PER_CORE, (c + 1) * SEQ_PER_CORE)
        m = dict(shared)
        m["x"] = x[sl]
        m["mem"] = mem[sl]
        m["positions"] = pos[sl]
        in_maps.append(m)
    res = run_bass_kernel_spmd(nc, in_maps, core_ids=list(range(NCORES)))
    out = np.concatenate([np.asarray(r["out"]) for r in res.results], axis=0)
    return out.astype(np.float32, copy=False)
```

```python
import contextlib
import math
import numpy as np
import concourse.bass as bass
import concourse.mybir as mybir
from concourse.bass_utils import run_bass_kernel_spmd

F32 = mybir.dt.float32
BF16 = mybir.dt.bfloat16
I32 = mybir.dt.int32
AF = mybir.ActivationFunctionType
ALU = mybir.AluOpType

NCORES = 8
SEQ_PER_CORE = 4
S = 2048
D = 1024
NT = S // 128
EPS = 1e-6
N_IN = 5824
C_CQ, C_MU, C_MV, C_MO, C_MG, C_MQ = 3072, 3744, 4256, 4768, 5280, 5312
DFF = 2816
NFF = DFF // 128
K_ID, K_MF, K_MB, K_TF, K_TB, K_I0, K_I1, K_RP = 0, 128, 192, 256, 384, 512, 640, 768
K_N = 772


class Trk:
    __slots__ = ("w", "r")

    def __init__(self):
        self.w = None
        self.r = {}


class Eng:
    def __init__(self, sch, name, h):
        self.sch = sch
        self.name = name
        self.h = h
        self.seen = {}
        self.pending = False
        self.semidx = sch.newsem()
        self.cnt = 0


class Sched:
    def __init__(self, nc, es):
        self.nc = nc
        self.es = es
        self.sems = []
        self.trk = {}
        self.dsem = {}
        self.final = []
        self.alias = {}
        self.ninst = 0
        self.pe = Eng(self, "pe", nc.tensor)
        self.dve = Eng(self, "dve", nc.vector)
        self.act = Eng(self, "act", nc.scalar)
        self.pool = Eng(self, "pool", nc.gpsimd)
        self.sp = Eng(self, "sp", nc.sync)

    def newsem(self):
        h = self.es.enter_context(self.nc.semaphore("s%d" % len(self.sems)))
        self.sems.append(h)
        return len(self.sems) - 1

    def _exp(self, keys):
        out = []
        for k in keys:
            a = self.alias.get(k)
            if a is None:
                out.append(k)
            else:
                out.extend(a)
        return out

    def set_alias(self, key, lo, hi):
        self.alias[key] = [("TW", g) for g in range(lo // 512, (hi + 511) // 512)]

    def _deps(self, reads, writes, skip_waw_sem=None):
        d = {}
        for k in reads:
            t = self.trk.get(k)
            if t is not None and t.w is not None and d.get(t.w[0], 0) < t.w[1]:
                d[t.w[0]] = t.w[1]
        for k in writes:
            t = self.trk.get(k)
            if t is None:
                continue
            if t.w is not None and t.w[0] != skip_waw_sem and d.get(t.w[0], 0) < t.w[1]:
                d[t.w[0]] = t.w[1]
            for si, v in t.r.items():
                if d.get(si, 0) < v:
                    d[si] = v
        return d

    def _wait(self, eng, d, skip_own=False):
        for si, v in d.items():
            if skip_own and si == eng.semidx:
                continue
            if eng.seen.get(si, 0) >= v:
                continue
            eng.h.wait_ge(self.sems[si], v)
            self.ninst += 1
            eng.seen[si] = v

    def _mark(self, tok, reads, writes):
        for k in reads:
            t = self.trk.get(k)
            if t is None:
                t = self.trk[k] = Trk()
            if t.r.get(tok[0], 0) < tok[1]:
                t.r[tok[0]] = tok[1]
        for k in writes:
            t = self.trk.get(k)
            if t is None:
                t = self.trk[k] = Trk()
            t.w = tok
            t.r = {}

    def op(self, eng, fn, reads=(), writes=(), sig=True):
        reads = self._exp(reads)
        writes = self._exp(writes)
        d = self._deps(reads, writes)
        self._wait(eng, d, skip_own=(eng is self.pe))
        ins = fn()
        self.ninst += 1
        if sig or eng is not self.pe:
            if eng.cnt >= 30000 and not eng.pending:
                eng.semidx = self.newsem()
                eng.cnt = 0
            eng.cnt += 1
            ins.then_inc(self.sems[eng.semidx], 1)
            tok = (eng.semidx, eng.cnt)
            eng.pending = False
        else:
            tok = (eng.semidx, eng.cnt + 1)
            eng.pending = True
        self._mark(tok, reads, writes)
        return tok

    def dma(self, q, out, in_, reads=(), writes=(), grp=None, final=False):
        reads = self._exp(reads)
        writes = self._exp(writes)
        g = self.dsem.get(grp)
        if g is None:
            g = self.dsem[grp] = [self.newsem(), 0]
        d = self._deps(reads, writes, skip_waw_sem=g[0])
        self._wait(q, d)
        ins = q.h.dma_start(out=out, in_=in_)
        self.ninst += 1
        g[1] += 16
        ins.then_inc(self.sems[g[0]], 16)
        tok = (g[0], g[1])
        self._mark(tok, reads, writes)
        if final:
            self.final.append(tok)
        return tok

    def retoken(self, keys, tok):
        for k in keys:
            t = self.trk.get(k)
            if t is None:
                t = self.trk[k] = Trk()
            t.w = tok

    def barrier(self):
        engs = [self.pe, self.dve, self.act, self.pool]
        assert not self.pe.pending
        for e in engs + [self.sp]:
            d = {}
            for o in engs:
                if o is not e and o.cnt > 0:
                    d[o.semidx] = o.cnt
            self._wait(e, d)

    def finish(self):
        d = {}
        for si, v in self.final:
            if d.get(si, 0) < v:
                d[si] = v
        self._wait(self.sp, d)


class StopBuild(Exception):
    pass


def build_program(nseq=SEQ_PER_CORE, dbg=False, stage=99):
    nc = bass.Bass("TRN2", target_bir_lowering=False)
    es = contextlib.ExitStack()
    with es:
        sch = Sched(nc, es)
        try:
            _emit(nc, es, nseq, dbg, sch, stage)
        except StopBuild:
            pass
        sch.finish()
        _emit.ninst = sch.ninst
        _emit.nsem = len(sch.sems)
    return nc


def _emit(nc, es, nseq, dbg, sch, stage):
    pe, dve, act, pool, sp = sch.pe, sch.dve, sch.act, sch.pool, sch.sp
    T, V, A, G = nc.tensor, nc.vector, nc.scalar, nc.gpsimd

    def din(name, shape, dt=F32):
        return nc.dram_tensor(name, list(shape), dt, kind="ExternalInput").ap()

    def dscr(name, shape, dt=BF16):
        return nc.dram_tensor(name, list(shape), dt, kind="Internal").ap()

    x_d = din("x", [SEQ_PER_CORE, S, D])
    mem_d = din("mem", [SEQ_PER_CORE, 256, D])
    pos_d = din("positions", [SEQ_PER_CORE, S], I32)
    gmix_d = din("g_mix", [1, D])
    win_d = din("w_in", [D, N_IN])
    gq_d = din("mla_g_q", [1, 384])
    gkv_d = din("mla_g_kv", [1, 256])
    wuq_d = din("mla_w_uq", [384, 768])
    wuk_d = din("mla_w_uk", [256, 512])
    wuv_d = din("mla_w_uv", [256, 512])
    convw_d = din("ml_conv_w", [5, 512])
    wq_d = din("ml_w_q", [512, 64])
    wk_d = din("ml_w_k", [512, 64])
    gbias_d = din("ml_gate_bias", [1, 32])
    ghead_d = din("ml_g_head", [1, 512])
    memg_d = din("mem_g", [1, D])
    wkv_d = din("mem_w_kv", [D, 1024])
    wbr_d = din("w_branch", [1536, D])
    wout_d = din("w_out", [D, D])
    gffn_d = din("g_ffn", [1, D])
    wg_d = din("w_ffn_gate", [D, DFF])
    wu_d = din("w_ffn_up", [D, DFF])
    wd_d = din("w_ffn_down", [DFF, D])
    gfin_d = din("g_final", [1, D])
    cst_d = din("cst", [128, K_N])
    out_d = nc.dram_tensor("out", [SEQ_PER_CORE, S, D], F32, kind="ExternalOutput").ap()
    if dbg:
        dbg_d = nc.dram_tensor("dbg_oT", [3, 128, 4, S], BF16, kind="ExternalOutput").ap()

    win_b = dscr("win_b", [D, N_IN])
    wuq_b = dscr("wuq_b", [384, 768])
    wuk_b = dscr("wuk_b", [256, 512])
    wuv_b = dscr("wuv_b", [256, 512])
    wq_b = dscr("wq_b", [512, 64])
    wk_b = dscr("wk_b", [512, 64])
    wkv_b = dscr("wkv_b", [D, 1024])
    wout_b = dscr("wout_b", [D, D])
    wd_b = dscr("wd_b", [DFF, D])
    oT_d = dscr("oT_scr", [3, 128, 4, S])
    wge_b = dscr("wge_b", [8, 128, 4608])
    wgu_b = dscr("wgu_b", [11, 128, 4096])

    dumps = {}

    def dump(name, ap, reads):
        shp = [int(v) for v in ap.shape]
        t = nc.dram_tensor("dmp_" + name, shp, ap.dtype, kind="ExternalOutput").ap()
        sch.dma(sp, t, ap, reads=reads, grp=("dmp", name), final=True)

    def checkpoint(st):
        if stage <= st:
            sch.barrier()
            raise StopBuild()

    def sbt(name, cols, dt):
        return es.enter_context(nc.sbuf_tensor(name, [128, cols], dt))

    T_hT = sbt("hT", 8 * S, BF16)
    T_big = sbt("big", 4 * 2080, F32)
    T_A = sbt("ra", 11264, BF16)
    T_V = sbt("rv", 16 * 520, BF16)
    T_C = sbt("rc", 16 * 512, BF16)
    T_W = sbt("rw", 13824, BF16)
    xs = sbt("xs", 2 * 1024, F32)
    xn = sbt("xn", 2 * 1024, BF16)
    cst = sbt("cstt", K_N, F32)
    identb = sbt("identb", 128, BF16)
    gfin_bc = sbt("gfin_bc", 1024, F32)
    ghead_bc = sbt("ghead_bc", 512, F32)
    gbias_bc = sbt("gbias_bc", 32, F32)
    gvecT = sbt("gvecT", 32, F32)
    convw = sbt("convw", 20, F32)
    stat = sbt("stat", 64, F32)
    epsb = sbt("epsb", 1, F32)
    oneb = sbt("oneb", 1, F32)
    gA = sbt("gA", 16 * 16, F32)
    gE = sbt("gE", 16 * 16, F32)
    gWt = sbt("gWt", 16 * 16, F32)
    gDec = sbt("gDec", 16 * 32, F32)
    gtmp = sbt("gtmp", 128, F32)
    kst = sbt("kst", 2 * 96, BF16)
    PTt = sbt("PTt", 3 * 1024, BF16)
    tmpf = sbt("tmpf", 2 * 512, F32)
    stg = sbt("stg", 2 * 512, BF16)
    posi = sbt("posi", S, I32)
    posf = sbt("posf", S, F32)

    hT = T_hT[:].rearrange("p (k t) -> p k t", k=8)
    bigb = T_big[:].bitcast(BF16)

    pdbl = [es.enter_context(nc.psum_tensor("pd%d" % i, [128, 1024], F32)) for i in range(3)]
    pbk = [pdbl[i // 2][:, (i % 2) * 512:(i % 2 + 1) * 512] for i in range(6)]
    ptk = [es.enter_context(nc.psum_tensor("pt%d" % i, [128, 1024], BF16)) for i in range(2)]
    pbk.append(ptk[0][:].bitcast(F32))
    pbk.append(ptk[1][:].bitcast(F32))
    rot = {}

    def bank(grp, ids):
        i = rot.get(grp, 0)
        rot[grp] = i + 1
        b = ids[i % len(ids)]
        return pbk[b], (("pb", b) if b < 6 else ("pt", b - 6))

    def tbank():
        i = rot.get("pt", 0)
        rot["pt"] = i + 1
        return ptk[i % 2], ("pt", i % 2)

    sch.dma(sp, cst[:], cst_d, writes=["cst"], grp="cst")
    sch.dma(pool, identb[:], cst_d[:, K_ID:K_ID + 128], writes=["ident"], grp="ident")
    sch.dma(sp, gfin_bc[:], gfin_d.partition_broadcast(128), writes=["gfin"], grp="c2")
    sch.dma(sp, ghead_bc[:], ghead_d.partition_broadcast(128), writes=["ghead"], grp="c3")
    sch.dma(sp, gbias_bc[:], gbias_d.partition_broadcast(128), writes=["gbias"], grp="c4")
    with nc.allow_non_contiguous_dma("tiny gain-vector transposes"):
        sch.dma(sp, gvecT[:, 0:8], gmix_d.rearrange("o (k p) -> p (o k)", p=128), writes=["gv0"], grp="c5")
        sch.dma(sp, gvecT[:, 8:16], gffn_d.rearrange("o (k p) -> p (o k)", p=128), writes=["gv1"], grp="c6")
        sch.dma(sp, gvecT[:, 16:24], memg_d.rearrange("o (k p) -> p (o k)", p=128), writes=["gv2"], grp="c7")
        sch.dma(sp, gvecT[:, 24:27], gq_d.rearrange("o (k p) -> p (o k)", p=128), writes=["gv3"], grp="c8")
        sch.dma(sp, gvecT[:, 27:29], gkv_d.rearrange("o (k p) -> p (o k)", p=128), writes=["gv4"], grp="c9")
        for ct_ in range(4):
            sch.dma(sp, convw[:, ct_ * 5:(ct_ + 1) * 5], convw_d[:, ct_ * 128:(ct_ + 1) * 128].rearrange("j p -> p j"),
                    writes=["convw"], grp="c10")
    GV = ["gv0", "gv1", "gv2", "gv3", "gv4"]
    sch.op(dve, lambda: V.memset(epsb[:], EPS), writes=["epsb"])
    sch.op(dve, lambda: V.memset(kst[:], 0.0), writes=[("kst", 0), ("kst", 1)])
    sch.op(dve, lambda: V.memset(oneb[:], 1.0), writes=["oneb"])

    def conv_w(dst, src, rows, key):
        tok = None
        r0 = 0
        while r0 < rows:
            r1 = min(rows, r0 + 128)
            tok = sch.dma(pool, dst[r0:r1, :], src[r0:r1, :], grp=("cv", key))
            r0 = r1
        sch.retoken([("w", key)], tok)

    tok = None
    for r0_ in range(0, D, 128):
        tok = sch.dma(pool, win_b[r0_:r0_ + 128, C_CQ:N_IN], win_d[r0_:r0_ + 128, C_CQ:N_IN], grp=("cv", "win"))
    sch.retoken([("w", "win")], tok)
    conv_w(wuq_b, wuq_d, 384, "wuq")
    conv_w(wuk_b, wuk_d, 256, "wuk")
    conv_w(wuv_b, wuv_d, 256, "wuv")
    conv_w(wq_b, wq_d, 512, "wq")
    conv_w(wk_b, wk_d, 512, "wk")
    conv_w(wkv_b, wkv_d, D, "wkv")
    tok = None
    for f_ in range(8):
        for br_ in range(3):
            dst = wge_b[f_, :, 0:3072].rearrange("p (k b n) -> p k b n", k=8, b=3)[:, :, br_, :]
            src = win_d[:, br_ * 1024 + f_ * 128:br_ * 1024 + (f_ + 1) * 128].rearrange("(k p) n -> p k n", p=128)
            tok = sch.dma(pool, dst, src, grp=("cv", "wge"))
            dst = wge_b[f_, :, 3072:4608].rearrange("p (k b n) -> p k b n", k=4, b=3)[:, :, br_, :]
            src = wbr_d[br_ * 512:(br_ + 1) * 512, f_ * 128:(f_ + 1) * 128].rearrange("(k p) n -> p k n", p=128)
            tok = sch.dma(pool, dst, src, grp=("cv", "wge"))
    sch.retoken([("w", "wge")], tok)
    conv_w(wout_b, wout_d, D, "wout")
    tok = None
    for u_ in range(11):
        for c2_ in range(2):
            c_ = 2 * u_ + c2_
            for gu_, src_d in enumerate((wg_d, wu_d)):
                o_ = (c2_ * 2 + gu_) * 1024
                dst = wgu_b[u_, :, o_:o_ + 1024].rearrange("p (k n) -> p k n", k=8)
                src = src_d[:, c_ * 128:(c_ + 1) * 128].rearrange("(k p) n -> p k n", p=128)
                tok = sch.dma(pool, dst, src, grp=("cv", "wgu"))
    sch.retoken([("w", "wgu")], tok)
    conv_w(wd_b, wd_d, DFF, "wd")

    checkpoint(0)

    def wload(dst, src2d, key, wkey, kc):
        if kc == 1:
            srcv = src2d
        else:
            srcv = src2d.rearrange("(k p) n -> p k n", p=128)
        sch.dma(sp, dst, srcv, reads=[("w", wkey)], writes=[key], grp=key)

    def rms_to_T(src_ap, src_keys, gcol, dst_fn, dst_keys, slot):
        st = stat[:, slot * 4:slot * 4 + 1]
        st2 = stat[:, slot * 4 + 1:slot * 4 + 2]
        xnv = xn[:, slot * 1024:(slot + 1) * 1024]
        sch.op(act, lambda: A.activation(out=xnv, in_=src_ap, func=AF.Square, scale=1.0 / 32.0, accum_out=st),
               reads=src_keys, writes=[("xn", slot), ("st", slot)])
        sch.op(act, lambda: A.activation(out=st2, in_=st, func=AF.Sqrt, bias=epsb[:], scale=1.0),
               reads=[("st", slot), "epsb"], writes=[("st2", slot)])
        sch.op(dve, lambda: V.reciprocal(out=st2, in_=st2), reads=[("st2", slot)], writes=[("st2", slot)])
        sch.op(act, lambda: A.activation(out=xnv, in_=src_ap, func=AF.Copy, scale=st2),
               reads=list(src_keys) + [("st2", slot)], writes=[("xn", slot)])
        pt, ptkey = tbank()
        ptv = pt[:].rearrange("p (k t) -> p k t", k=8)
        for kc in range(8):
            sch.op(pe, lambda kc=kc: T.transpose(ptv[:, kc, :], xnv[:, kc * 128:(kc + 1) * 128], identb[:]),
                   reads=[("xn", slot), "ident"], writes=[ptkey], sig=(kc == 7))
        gb = gvecT[:, gcol:gcol + 8].unsqueeze(2).broadcast_to([128, 8, 128])
        sch.op(dve, lambda: V.tensor_tensor(out=dst_fn(), in0=ptv, in1=gb, op=ALU.mult),
               reads=[ptkey] + GV, writes=dst_keys)

    def mm_acc(out_ap, pairs, okey, rkeys):
        n = len(pairs)
        for i, (l, r) in enumerate(pairs):
            sch.op(pe, lambda l=l, r=r, i=i: T.matmul(out_ap, lhsT=l, rhs=r, start=(i == 0), stop=(i == n - 1)),
                   reads=rkeys, writes=[okey], sig=(i == n - 1))

    def htkeys(blk):
        return [("hT", blk * 4 + i) for i in range(4)]

    for b in range(nseq):
        for tt in range(NT):
            slot = tt % 2
            xsv = xs[:, slot * 1024:(slot + 1) * 1024]
            sch.dma(sp, xsv, x_d[b, tt * 128:(tt + 1) * 128, :], writes=[("xs", slot)], grp=("xs", slot))
            rms_to_T(xsv, [("xs", slot)], 0, lambda tt=tt: hT[:, :, tt * 128:(tt + 1) * 128], [("hT", tt)], slot)

        if stage == 1:
            dump("hT", T_hT[:], [("hT", t) for t in range(NT)])
        checkpoint(1)
        WinB = T_W[:, 0:5376].rearrange("p (k n) -> p k n", k=8)
        Wuq = T_W[:, 5376:7680].rearrange("p (k n) -> p k n", k=3)
        WuqS = T_W[:, 7680:9984].rearrange("p (k n) -> p k n", k=3)
        Wuk = T_W[:, 9984:11008].rearrange("p (k n) -> p k n", k=2)
        Wuv = T_W[:, 11008:12032].rearrange("p (k n) -> p k n", k=2)
        for nm, lo, hi in (("W0", 0, 5376), ("W1", 5376, 7680), ("W2", 7680, 9984), ("W3", 9984, 11008), ("W4", 11008, 12032)):
            sch.set_alias(nm, lo, hi)
        wload(WinB, win_b[:, C_CQ:C_CQ + 672], "W0", "win", 8)
        wload(Wuq, wuq_b, "W1", "wuq", 3)
        wuq_b3 = wuq_b.rearrange("(k p) (h c) -> p k h c", p=128, c=96)
        WuqS4 = WuqS.rearrange("p k (h c) -> p k h c", c=96)
        for kc in range(3):
            sch.dma(sp, WuqS4[:, kc, :, 0:64], wuq_b3[:, kc, :, 0:64], reads=[("w", "wuq")], writes=["W2"], grp="W2")
            sch.dma(sp, WuqS4[:, kc, :, 64:80], wuq_b3[:, kc, :, 80:96], reads=[("w", "wuq")], writes=["W2"], grp="W2")
            tk = sch.dma(sp, WuqS4[:, kc, :, 80:96], wuq_b3[:, kc, :, 64:80], reads=[("w", "wuq")], writes=["W2"], grp="W2")
        wload(Wuk, wuk_b, "W3", "wuk", 2)
        wload(Wuv, wuv_b, "W4", "wuv", 2)
        W2K = ["W2"]

        cqnT = T_A[:, 0:6144].rearrange("p (k t) -> p k t", k=3)
        ckvnT = T_A[:, 6144:10240].rearrange("p (k t) -> p k t", k=2)
        KhT = [bigb[:, 0:2048], bigb[:, 2048:4096]]
        QhT = [bigb[:, 4096:6144], bigb[:, 6144:8192]]
        cosT = bigb[:, 8192:10240]
        sinT = bigb[:, 10240:12288]
        zst = [bigb[:, 12288:12960], bigb[:, 12960:13632]]
        Vaug = T_V[:].rearrange("p (t h c) -> p t h c", t=16, h=8)
        otok = T_C[:].rearrange("p (t c) -> p t c", t=16)

        sch.dma(sp, posi[:], pos_d[b:b + 1, :].partition_broadcast(128), writes=["posi"], grp="posi")
        sch.op(dve, lambda: V.tensor_copy(out=posf[64:96, :], in_=posi[64:96, :]), reads=["posi"], writes=["posf"])
        TWO_PI = 2.0 * math.pi
        for (tab, offc, key) in ((cosT, K_RP + 2, "cosT"), (sinT, K_RP + 1, "sinT")):
            for q4 in range(2):
                seg = slice(q4 * 1024, (q4 + 1) * 1024)
                yv = tmpf[64:96, :]
                ki = posi[64:96, 0:1024]
                kf = xs[64:96, 0:1024]
                mv = xs[64:96, 1024:2048]
                sch.op(dve, lambda: V.tensor_scalar(out=yv, in0=posf[64:96, seg], scalar1=cst[64:96, K_RP:K_RP + 1],
                                                    scalar2=cst[64:96, offc:offc + 1], op0=ALU.mult, op1=ALU.add),
                       reads=["posf", "cst"], writes=["tmpf"])
                sch.op(dve, lambda: V.tensor_scalar(out=ki, in0=yv, scalar1=1.0 / TWO_PI, scalar2=0.5, op0=ALU.mult, op1=ALU.add),
                       reads=["tmpf", "posf"], writes=["posi"])
                sch.op(dve, lambda: V.tensor_copy(out=kf, in_=ki), reads=["posi"], writes=[("xs", 0)])
                sch.op(dve, lambda: V.scalar_tensor_tensor(out=yv, in0=kf, scalar=-TWO_PI, in1=yv, op0=ALU.mult, op1=ALU.add),
                       reads=[("xs", 0), "tmpf"], writes=["tmpf"])
                sch.op(dve, lambda: V.tensor_scalar(out=mv, in0=yv, scalar1=-math.pi, scalar2=TWO_PI, op0=ALU.is_lt, op1=ALU.mult),
                       reads=["tmpf"], writes=[("xs", 1)])
                sch.op(dve, lambda: V.tensor_tensor(out=yv, in0=yv, in1=mv, op=ALU.add), reads=["tmpf", ("xs", 1)], writes=["tmpf"])
                sch.op(act, lambda: A.activation(out=tab[64:96, seg], in_=yv, func=AF.Sin), reads=["tmpf"], writes=[key])

        sch.op(dve, lambda: V.memset(Vaug[:, :, :, 64:65], 1.0), writes=["Vaug"])

        for tt in range(NT):
            slot = tt % 2
            pz0, k0 = bank("z0", [0, 1])
            pz1, k1 = bank("z1", [2, 3])
            mm_acc(pz0[:, 0:384], [(hT[:, kc, tt * 128:(tt + 1) * 128], WinB[:, kc, 0:384]) for kc in range(8)],
                   k0, [("hT", tt), "W0"])
            mm_acc(pz1[:, 0:288], [(hT[:, kc, tt * 128:(tt + 1) * 128], WinB[:, kc, 384:672]) for kc in range(8)],
                   k1, [("hT", tt), "W0"])
            st = stat[:, 16 + slot * 4:16 + slot * 4 + 2]
            junk = xn[:, slot * 1024:slot * 1024 + 384]
            sch.op(act, lambda: A.activation(out=junk, in_=pz0[:, 0:384], func=AF.Square, scale=384.0 ** -0.5,
                                             accum_out=st[:, 0:1]), reads=[k0], writes=[("xn", slot), ("bst", slot)])
            sch.op(act, lambda: A.activation(out=junk[:, 0:256], in_=pz1[:, 0:256], func=AF.Square, scale=1.0 / 16.0,
                                             accum_out=st[:, 1:2]), reads=[k1], writes=[("xn", slot), ("bst", slot)])
            sch.op(act, lambda: A.activation(out=st, in_=st, func=AF.Sqrt, bias=epsb[:], scale=1.0),
                   reads=[("bst", slot), "epsb"], writes=[("bst", slot)])
            sch.op(dve, lambda: V.reciprocal(out=st, in_=st), reads=[("bst", slot)], writes=[("bst", slot)])
            z = zst[slot]
            sch.op(act, lambda: A.activation(out=z[:, 0:384], in_=pz0[:, 0:384], func=AF.Copy, scale=st[:, 0:1]),
                   reads=[k0, ("bst", slot)], writes=[("zst", slot)])
            sch.op(dve, lambda: V.tensor_scalar(out=z[:, 384:640], in0=pz1[:, 0:256], scalar1=st[:, 1:2], scalar2=None,
                                                op0=ALU.mult), reads=[k1, ("bst", slot)], writes=[("zst", slot)])
            ks = kst[:, slot * 96:(slot + 1) * 96]
            sch.op(act, lambda: A.copy(out=z[:, 640:672], in_=pz1[:, 256:288]), reads=[k1], writes=[("zst", slot)])
            pt, ptkey = tbank()
            ptv = pt[:].rearrange("p (k t) -> p k t", k=8)
            for j in range(5):
                sch.op(pe, lambda j=j: T.transpose(ptv[:, j, :], z[:, j * 128:(j + 1) * 128], identb[:]),
                       reads=[("zst", slot), "ident"], writes=[ptkey], sig=False)
            sch.op(dve, lambda: V.tensor_copy(out=ks[:, 64:96], in_=z[:, 640:672]), reads=[("zst", slot)], writes=[("kst", slot)])
            sch.op(pe, lambda: T.transpose(ptv[0:96, 5, :], ks, identb[:]), reads=[("kst", slot), "ident"], writes=[ptkey])
            sch.op(dve, lambda: V.tensor_copy(out=ks[:, 64:80], in_=z[:, 656:672]), reads=[("zst", slot), ptkey], writes=[("kst", slot)])
            sch.op(dve, lambda: V.tensor_copy(out=ks[:, 80:96], in_=z[:, 640:656]), reads=[("zst", slot)], writes=[("kst", slot)])
            sch.op(pe, lambda: T.transpose(ptv[0:96, 6, :], ks, identb[:]), reads=[("kst", slot), "ident"], writes=[ptkey])
            tsl = slice(tt * 128, (tt + 1) * 128)
            gq = gvecT[:, 24:27].unsqueeze(2).broadcast_to([128, 3, 128])
            gk = gvecT[:, 27:29].unsqueeze(2).broadcast_to([128, 2, 128])
            sch.op(dve, lambda: V.tensor_tensor(out=cqnT[:, :, tsl], in0=ptv[:, 0:3, :], in1=gq, op=ALU.mult),
                   reads=[ptkey] + GV, writes=[("cqnT", tt)])
            sch.op(dve, lambda: V.tensor_tensor(out=ckvnT[:, :, tsl], in0=ptv[:, 3:5, :], in1=gk, op=ALU.mult),
                   reads=[ptkey] + GV, writes=[("ckvnT", tt)])
            t1 = tmpf[64:96, 0:128]
            t2 = tmpf[64:96, 512:640]
            sch.op(dve, lambda: V.tensor_tensor(out=t1, in0=ptv[64:96, 5, :], in1=cosT[64:96, tsl], op=ALU.mult),
                   reads=[ptkey, "cosT"], writes=["tmpf"])
            sch.op(dve, lambda: V.tensor_tensor(out=t2, in0=ptv[64:96, 6, :], in1=sinT[64:96, tsl], op=ALU.mult),
                   reads=[ptkey, "sinT"], writes=["tmpf"])
            sch.op(dve, lambda: V.tensor_tensor(out=KhT[0][64:96, tsl], in0=t1, in1=t2, op=ALU.add),
                   reads=["tmpf"], writes=[("KhT", 0)])
            sch.op(act, lambda: A.copy(out=KhT[1][64:96, tsl], in_=KhT[0][64:96, tsl]),
                   reads=[("KhT", 0)], writes=[("KhT", 1)])
            pv, kv = bank("z0", [0, 1])
            mm_acc(pv[:, :], [(ckvnT[:, kc, tsl], Wuv[:, kc, :]) for kc in range(2)], kv, [("ckvnT", tt), "W4"])
            sch.op(act, lambda: A.copy(out=Vaug[:, tt, :, 0:64], in_=pv[:].rearrange("p (h c) -> p h c", h=8)),
                   reads=[kv], writes=["Vaug"])

        if stage == 2:
            dump("cq", T_A[:, 0:10240], [("cqnT", t) for t in range(NT)] + [("ckvnT", t) for t in range(NT)])
            dump("kh", bigb[:, 0:2048], [("KhT", 0)])
            dump("cos", bigb[:, 8192:12288], ["cosT", "sinT"])
            dump("va", T_V[:], ["Vaug"])
        checkpoint(2)
        ALLCQ = [("cqnT", t) for t in range(NT)]
        ALLCKV = [("ckvnT", t) for t in range(NT)]
        att_scale = 96.0 ** -0.5
        def produce_kq(h):
            s_ = h % 2
            for blk in range(4):
                bs = slice(blk * 512, (blk + 1) * 512)
                pk, kk = bank("kq", [6, 7])
                mm_acc(pk[0:64, :], [(Wuk[:, kc, h * 64:(h + 1) * 64], ckvnT[:, kc, bs]) for kc in range(2)], kk,
                       ALLCKV[blk * 4:blk * 4 + 4] + ["W3"])
                sch.op(dve, lambda: V.tensor_copy(out=KhT[s_][0:64, bs], in_=pk[0:64, :]), reads=[kk], writes=[("KhT", s_)])
                pq, kq = bank("kq", [6, 7])
                pqs, kqs = bank("kq", [6, 7])
                mm_acc(pq[0:96, :], [(Wuq[:, kc, h * 96:(h + 1) * 96], cqnT[:, kc, bs]) for kc in range(3)], kq,
                       ALLCQ[blk * 4:blk * 4 + 4] + ["W1"])
                mm_acc(pqs[0:96, :], [(WuqS[:, kc, h * 96:(h + 1) * 96], cqnT[:, kc, bs]) for kc in range(3)], kqs,
                       ALLCQ[blk * 4:blk * 4 + 4] + W2K)
                sch.op(dve, lambda: V.tensor_copy(out=QhT[s_][0:64, bs], in_=pq[0:64, :]), reads=[kq], writes=[("QhT", s_)])
                t1 = tmpf[64:96, 0:512]
                t2 = tmpf[64:96, 512:1024]
                sch.op(dve, lambda: V.tensor_tensor(out=t1, in0=pq[64:96, :], in1=cosT[64:96, bs], op=ALU.mult),
                       reads=[kq, "cosT"], writes=["tmpf"])
                sch.op(dve, lambda: V.tensor_tensor(out=t2, in0=pqs[64:96, :], in1=sinT[64:96, bs], op=ALU.mult),
                       reads=[kqs, "sinT"], writes=["tmpf"])
                sch.op(pool, lambda: G.tensor_tensor(out=QhT[s_][64:96, bs], in0=t1, in1=t2, op=ALU.add),
                       reads=["tmpf"], writes=[("QhT", s_)])

        def attend(h):
            s_ = h % 2
            for qb in range(4):
                qs = slice(qb * 512, (qb + 1) * 512)
                pO, kO = bank("pO", [4, 5])
                pOv = pO[:, 0:260].rearrange("p (j c) -> p j c", j=4)

                def pv(kt, P_, ps_):
                    for j in range(4):
                        sch.op(pe, lambda j=j: T.matmul(pOv[:, j, :], lhsT=P_[:, j * 128:(j + 1) * 128], rhs=Vaug[:, kt, h, :],
                                                        start=(kt == 0 and j == 0), stop=(kt == 15), skip_group_check=True),
                               reads=[("PT", ps_), "Vaug"], writes=[kO], sig=(j == 3))

                prev = None
                for kp in range(8):
                    ip = rot.get("pspair", 0) % 2
                    rot["pspair"] = rot.get("pspair", 0) + 1
                    pSS = pdbl[ip]
                    kSS = [("pb", 2 * ip), ("pb", 2 * ip + 1)]
                    for u in range(2):
                        kt = 2 * kp + u
                        sch.op(pe, lambda: T.matmul(pSS[:, u * 512:(u + 1) * 512], lhsT=KhT[s_][0:96, kt * 128:(kt + 1) * 128],
                                                    rhs=QhT[s_][0:96, qs], start=True, stop=True),
                               reads=[("KhT", s_), ("QhT", s_)], writes=[kSS[u]])
                    ps_ = rot.get("ptslot", 0) % 3
                    rot["ptslot"] = rot.get("ptslot", 0) + 1
                    PP = PTt[:, ps_ * 1024:(ps_ + 1) * 1024]
                    sch.op(act, lambda: A.activation(out=PP, in_=pSS[:, :], func=AF.Exp, scale=att_scale),
                           reads=kSS, writes=[("PT", ps_)])
                    if prev is not None:
                        for a_ in prev:
                            pv(*a_)
                    prev = [(2 * kp + u, PP[:, u * 512:(u + 1) * 512], ps_) for u in range(2)]
                for a_ in prev:
                    pv(*a_)
                rec = stat[:, 32 + (qb % 2) * 4:36 + (qb % 2) * 4]
                sch.op(dve, lambda: V.reciprocal(out=rec, in_=pOv[:, :, 64]), reads=[kO], writes=[("rec", qb % 2)])
                sch.op(dve, lambda: V.tensor_tensor(out=otok[:, qb * 4:(qb + 1) * 4, h * 64:(h + 1) * 64], in0=pOv[:, :, 0:64],
                                                    in1=rec.unsqueeze(2).broadcast_to([128, 4, 64]), op=ALU.mult),
                       reads=[kO, ("rec", qb % 2)], writes=[("otok", qb)])

        produce_kq(0)
        for h in range(8):
            if h + 1 < 8:
                produce_kq(h + 1)
            attend(h)

        def flush_oT(br):
            for blk in range(4):
                sl = blk % 2
                sv = stg[:, sl * 512:(sl + 1) * 512]
                for j in range(4):
                    pt, ptkey = tbank()
                    ptv = pt[:].rearrange("p (k t) -> p k t", k=8)
                    for i in range(4):
                        sch.op(pe, lambda i=i: T.transpose(ptv[:, i, :], otok[:, blk * 4 + i, j * 128:(j + 1) * 128], identb[:]),
                               reads=[("otok", blk), "ident"], writes=[ptkey], sig=(i == 3))
                    st_ = stg[:, sl * 512:(sl + 1) * 512]
                    if j % 2 == 0:
                        sch.op(act, lambda: A.copy(out=st_.rearrange("p (i t) -> p i t", i=4), in_=ptv[:, 0:4, :]),
                               reads=[ptkey], writes=[("stg", sl)])
                    else:
                        sch.op(dve, lambda: V.tensor_copy(out=st_.rearrange("p (i t) -> p i t", i=4), in_=ptv[:, 0:4, :]),
                               reads=[ptkey], writes=[("stg", sl)])
                    sch.dma(sp, oT_d[br, :, j, blk * 512:(blk + 1) * 512], st_, reads=[("stg", sl)],
                            writes=[("oTd", br, blk, j)], grp=("stg", sl))
                    sl = (sl + 1) % 2

        if stage == 3:
            dump("otok", T_C[:], [("otok", q) for q in range(4)])
            dump("kh", bigb[:, 0:8192], [("KhT", 0), ("KhT", 1), ("QhT", 0), ("QhT", 1)])
        checkpoint(3)
        flush_oT(0)
        sch.barrier()

        checkpoint(4)
        WinU = T_W[:, 0:4096].rearrange("p (k n) -> p k n", k=8)
        WinVG = T_W[:, 4096:8448].rearrange("p (k n) -> p k n", k=8)
        WqBD = T_W[:, 8448:8960].rearrange("p (j n) -> p j n", j=4)
        WkBD = T_W[:, 8960:9472].rearrange("p (j n) -> p j n", j=4)
        for nm, lo, hi in (("W0", 0, 4096), ("W1", 4096, 8448), ("W1b", 4096, 8448), ("W3", 8448, 8960), ("W4", 8960, 9472)):
            sch.set_alias(nm, lo, hi)
        wload(WinU, win_b[:, C_MU:C_MU + 512], "W0", "win", 8)
        sch.dma(sp, WinVG[:, :, 0:512], win_b[:, C_MV:C_MV + 512].rearrange("(k p) n -> p k n", p=128),
                reads=[("w", "win")], writes=["W1"], grp="W1")
        sch.dma(sp, WinVG[:, :, 512:544], win_b[:, C_MG:C_MG + 32].rearrange("(k p) n -> p k n", p=128),
                reads=[("w", "win")], writes=["W1"], grp="W1")
        sch.op(dve, lambda: V.memset(T_W[:, 8448:9472], 0.0), writes=["W3", "W4"])
        for (Wt, src, key) in ((WqBD, wq_b, "W3"), (WkBD, wk_b, "W4")):
            s4 = src.rearrange("(j r c) d -> r c j d", j=4, r=2)
            for r in range(2):
                sch.dma(sp, Wt[r * 64:(r + 1) * 64, :, r * 64:(r + 1) * 64], s4[r], reads=[("w", "wq"), ("w", "wk")],
                        writes=[key], grp=key)
        uT = T_big[:].rearrange("p (c t) -> p c t", c=4)
        Hh = T_big[:, 0:8192].rearrange("p (t c) -> p t c", t=16)
        cT = T_C[:].rearrange("p (c t) -> p c t", c=4)
        vaug = Vaug
        accf = T_A[:].bitcast(F32)
        sch.op(dve, lambda: V.memset(uT[:, :, 14:16], 0.0), writes=["uTpad"])
        sch.op(dve, lambda: V.memset(uT[:, :, 2064:2066], 0.0), writes=["uTpad"])
        sch.op(dve, lambda: V.memset(vaug[:, :, :, 64:65], 1.0), writes=["Vaug"])
        checkpoint(4.1)
        for ct in range(4):
            for blk in range(4):
                pu, ku = bank("cu", [0, 1])
                mm_acc(pu[:, :], [(WinU[:, kc, ct * 128:(ct + 1) * 128], hT[:, kc, blk * 512:(blk + 1) * 512]) for kc in range(8)],
                       ku, htkeys(blk) + ["W0"])
                sch.op(act, lambda: A.copy(out=uT[:, ct, 16 + blk * 512:16 + (blk + 1) * 512], in_=pu[:, :]),
                       reads=[ku], writes=[("uT", ct)])
        checkpoint(4.2)
        for tt in range(NT):
            pv, kv = bank("cv", [2, 3])
            pg, kg = bank("cg", [4, 5])
            mm_acc(pv[:, :], [(hT[:, kc, tt * 128:(tt + 1) * 128], WinVG[:, kc, 0:512]) for kc in range(8)], kv, [("hT", tt), "W1"])
            mm_acc(pg[:, 0:32], [(hT[:, kc, tt * 128:(tt + 1) * 128], WinVG[:, kc, 512:544]) for kc in range(8)], kg,
                   [("hT", tt), "W1"])
            sch.op(act, lambda: A.copy(out=vaug[:, tt, :, 0:64], in_=pv[:].rearrange("p (h c) -> p h c", h=8)),
                   reads=[kv], writes=["Vaug"])
            Gt = gtmp[:, 0:32]
            sch.op(dve, lambda: V.tensor_tensor(out=Gt, in0=pg[:, 0:32], in1=gbias_bc[:], op=ALU.add),
                   reads=[kg, "gbias"], writes=["Gt"])
            G4 = Gt.rearrange("p (a h) -> p a h", a=4)
            Fv = gtmp[:, 0:32].rearrange("p (d a h) -> p d a h", d=2, a=2)[:, :, 1, :]
            Iv = gtmp[:, 0:32].rearrange("p (d a h) -> p d a h", d=2, a=2)[:, :, 0, :]
            sp_ = gtmp[:, 32:48].rearrange("p (d h) -> p d h", d=2)
            sch.op(act, lambda: A.activation(out=sp_, in_=Fv, func=AF.Exp, scale=-1.0), reads=["Gt"], writes=["spg"])
            sch.op(act, lambda: A.activation(out=sp_, in_=sp_, func=AF.Ln, bias=oneb[:], scale=1.0), reads=["spg", "oneb"], writes=["spg"])
            pc, kc_ = bank("cc", [0, 1])
            sch.op(pe, lambda: T.matmul(pc[:, 0:8], lhsT=cst[:, K_TF:K_TF + 128], rhs=gtmp[:, 32:40], start=True, stop=True),
                   reads=["spg", "cst"], writes=[kc_], sig=False)
            sch.op(pe, lambda: T.matmul(pc[:, 8:16], lhsT=cst[:, K_TB:K_TB + 128], rhs=gtmp[:, 40:48], start=True, stop=True),
                   reads=["spg", "cst"], writes=[kc_], sig=False)
            sch.op(pe, lambda: T.matmul(pc[:, 16:32], lhsT=cst[:, K_I0:K_I0 + 128], rhs=gtmp[:, 32:48], start=True, stop=True),
                   reads=["spg", "cst"], writes=[kc_], sig=False)
            sch.op(pe, lambda: T.matmul(pc[:, 32:48], lhsT=cst[:, K_I1:K_I1 + 128], rhs=gtmp[:, 32:48], start=True, stop=True),
                   reads=["spg", "cst"], writes=[kc_])
            tsl16 = slice(tt * 16, (tt + 1) * 16)
            ic = gtmp[:, 48:64].rearrange("p (d h) -> p d h", d=2)
            sch.op(dve, lambda: V.tensor_tensor(out=ic, in0=Iv, in1=pc[:, 0:16].rearrange("p (d h) -> p d h", d=2), op=ALU.add),
                   reads=["Gt", kc_], writes=["ic"])
            sch.op(act, lambda: A.activation(out=gA[:, tsl16], in_=gtmp[:, 48:64], func=AF.Exp), reads=["ic"], writes=[("gA", tt)])
            sch.op(act, lambda: A.activation(out=gE[:, tsl16], in_=pc[:, 0:16], func=AF.Exp, scale=-1.0), reads=[kc_], writes=[("gE", tt)])
            sch.op(act, lambda: A.activation(out=gDec[:, tt * 32:(tt + 1) * 32], in_=pc[:, 16:48], func=AF.Exp, scale=-1.0),
                   reads=[kc_], writes=[("gDec", tt)])
            sch.op(dve, lambda: V.tensor_tensor(out=gWt[0:64, tsl16], in0=gA[0:64, tsl16], in1=gDec[0:64, tt * 32:tt * 32 + 16], op=ALU.mult),
                   reads=[("gA", tt), ("gDec", tt)], writes=[("gW", tt)])
            sch.op(dve, lambda: V.tensor_tensor(out=gWt[64:128, tsl16], in0=gA[64:128, tsl16], in1=gDec[64:128, tt * 32 + 16:tt * 32 + 32],
                                                op=ALU.mult), reads=[("gA", tt), ("gDec", tt)], writes=[("gW", tt)])
        checkpoint(4.5)
        for ct in range(4):
            eng, E_ = dve, V
            acc = accf[:, (ct % 2) * 2048:(ct % 2 + 1) * 2048]
            ak = ("acc", ct % 2)
            sch.op(eng, lambda: E_.tensor_scalar(out=acc, in0=uT[:, ct, 14:14 + 2048], scalar1=convw[:, ct * 5:ct * 5 + 1], scalar2=None,
                                                 op0=ALU.mult), reads=[("uT", ct), "uTpad", "convw"], writes=[ak])
            for j in range(1, 5):
                sch.op(eng, lambda j=j: E_.scalar_tensor_tensor(out=acc, in0=uT[:, ct, 14 + j:14 + j + 2048],
                                                                scalar=convw[:, ct * 5 + j:ct * 5 + j + 1], in1=acc,
                                                                op0=ALU.mult, op1=ALU.add),
                       reads=[("uT", ct), "uTpad", "convw", ak], writes=[ak])
            sch.op(act, lambda: A.activation(out=cT[:, ct, :], in_=acc, func=AF.Silu), reads=[ak], writes=[("cT", ct)])
        sch.barrier()

        if stage == 5:
            dump("cT", T_C[:], [("cT", c_) for c_ in range(4)])
            dump("ga", gA[:], [("gA", t) for t in range(NT)])
            dump("ge", gE[:], [("gE", t) for t in range(NT)])
            dump("gw", gWt[:], [("gW", t) for t in range(NT)])
            dump("gd", gDec[:], [("gDec", t) for t in range(NT)])
        checkpoint(5)
        def tA(off, n):
            return T_A[:, off:off + n]
        qTt = [tA(d * 512, 512).rearrange("p (j t) -> p j t", j=4) for d in range(2)]
        qTz = [[tA(1024 + (d * 2 + r) * 512, 512).rearrange("p (j t) -> p j t", j=4) for r in range(2)] for d in range(2)]
        kTz = [[tA(3072 + (d * 2 + r) * 512, 512).rearrange("p (j t) -> p j t", j=4) for r in range(2)] for d in range(2)]
        ktk = [tA(5120 + d * 512, 512) for d in range(2)]
        PTm = [tA(6144 + i * 1024, 1024).bitcast(F32) for i in range(2)]
        vaT = [tA(8192 + i * 1040, 1040).bitcast(F32).rearrange("p (h c) -> p h c", h=8) for i in range(2)]
        vwz = [T_W[:, 11288 + i * 520:11288 + (i + 1) * 520].rearrange("p (h c) -> p h c", h=8) for i in range(2)]
        Cbf = [T_W[:, 10768 + d * 260:10768 + (d + 1) * 260].rearrange("p (j c) -> p j c", j=4) for d in range(2)]
        Cst = [T_W[:, 9728 + d * 520:9728 + (d + 1) * 520].bitcast(F32).rearrange("p (j c) -> p j c", j=4) for d in range(2)]
        pm = [cst[:, K_I0:K_I0 + 1], cst[:, K_I1:K_I1 + 1]]
        pm8 = stat[:, 28:30]
        sch.op(dve, lambda: V.tensor_scalar(out=pm8[:, 0:1], in0=pm[0], scalar1=0.125, scalar2=None, op0=ALU.mult), reads=["cst"], writes=["pm8"])
        sch.op(dve, lambda: V.tensor_scalar(out=pm8[:, 1:2], in0=pm[1], scalar1=0.125, scalar2=None, op0=ALU.mult), reads=["cst", "pm8"], writes=["pm8"])
        for d in range(2):
            sch.op(dve, lambda d=d: V.memset(Cst[d], 0.0), writes=[("Cst", d)])
            sch.op(dve, lambda d=d: V.memset(Cbf[d], 0.0), writes=[("Cbf", d)])
            sch.op(dve, lambda d=d: V.memset(PTm[d], 0.0), writes=[("PTm", d)])
            sch.op(dve, lambda d=d: V.memset(vwz[d], 0.0), writes=[("vw", d)])
        sch.op(dve, lambda: V.memset(Hh, 0.0), writes=[("H", t) for t in range(NT)])
        cnt2 = [0]
        for it in range(32):
            for d in range(2):
                c = it if d == 0 else 31 - it
                m = c // 2
                r0 = c % 2
                rs = slice(r0 * 64, (r0 + 1) * 64)
                new_tile = (it % 2 == 0)
                tsl = slice(m * 128, (m + 1) * 128)
                if new_tile:
                    pq, kq = bank("mq", [0, 1])
                    pk, kk = bank("mk", [2, 3])
                    pk2, kk2 = bank("mk2", [4, 5])
                    for j in range(4):
                        sch.op(pe, lambda j=j: T.matmul(pq[:, j * 128:(j + 1) * 128], lhsT=WqBD[:, j, :], rhs=cT[:, j, tsl], start=True, stop=True),
                               reads=[("cT", j), "W3"], writes=[kq], sig=(j == 3))
                    for j in range(4):
                        sch.op(pe, lambda j=j: T.matmul(pk[:, j * 128:(j + 1) * 128], lhsT=WkBD[:, j, :], rhs=cT[:, j, tsl], start=True, stop=True),
                               reads=[("cT", j), "W4"], writes=[kk], sig=(j == 3))
                    for j in range(4):
                        sch.op(pe, lambda j=j: T.matmul(pk2[:, j * 128:(j + 1) * 128], lhsT=cT[:, j, tsl], rhs=WkBD[:, j, :], start=True, stop=True),
                               reads=[("cT", j), "W4"], writes=[kk2], sig=(j == 3))
                    pq3 = pq[:].rearrange("p (j t) -> p j t", j=4)
                    pk3 = pk[:].rearrange("p (j t) -> p j t", j=4)
                    sch.op(act, lambda: A.copy(out=qTt[d], in_=pq3), reads=[kq], writes=[("qTt", d)])
                    for r in range(2):
                        sch.op(act, lambda r=r: A.activation(out=qTz[d][r], in_=pq3, func=AF.Copy, scale=pm[r]),
                               reads=[kq, "cst"], writes=[("qTz", d, r)])
                        sch.op(act, lambda r=r: A.activation(out=kTz[d][r], in_=pk3, func=AF.Copy, scale=pm8[:, r:r + 1]),
                               reads=[kk, "pm8"], writes=[("kTz", d, r)])
                    sch.op(dve, lambda: V.tensor_scalar(out=ktk[d], in0=pk2[:, :], scalar1=0.125, scalar2=None, op0=ALU.mult),
                           reads=[kk2], writes=[("ktk", d)])
                if it == 0 and d == 0:
                    checkpoint(5.1)
                i2 = cnt2[0] % 2
                cnt2[0] += 1
                col = slice(m * 16 + d * 8, m * 16 + d * 8 + 8)
                sch.op(pool, lambda: G.tensor_tensor(out=vaT[i2], in0=vaug[:, m, :, :],
                                                     in1=gA[:, col].unsqueeze(2).broadcast_to([128, 8, 65]), op=ALU.mult),
                       reads=["Vaug", ("gA", m)], writes=[("va", i2)])
                sch.op(pool, lambda: G.tensor_tensor(out=vwz[r0][rs], in0=vaug[rs, m, :, :],
                                                     in1=gWt[rs, col].unsqueeze(2).broadcast_to([64, 8, 65]), op=ALU.mult),
                       reads=["Vaug", ("gW", m)], writes=[("vw", r0)])
                if it == 0 and d == 0:
                    checkpoint(5.2)
                pS, kS = bank("mS", [0, 1, 2, 3])
                pSv = pS[:].rearrange("p (h t) -> p h t", h=8)
                for h in range(8):
                    j, r = h // 2, h % 2
                    sch.op(pe, lambda h=h, j=j, r=r: T.matmul(pSv[rs, h, :], lhsT=kTz[d][r][:, j, rs], rhs=qTt[d][:, j, rs],
                                                             start=True, stop=True),
                           reads=[("kTz", d, r), ("qTt", d)], writes=[kS], sig=(h == 7))
                if it == 0 and d == 0:
                    checkpoint(5.25)
                mk = cst[rs, (K_MF if d == 0 else K_MB):(K_MF if d == 0 else K_MB) + 64]
                sch.op(dve, lambda: V.tensor_tensor(out=PTm[r0][rs].rearrange("p (h t) -> p h t", h=8), in0=pSv[rs],
                                                    in1=mk.unsqueeze(1).broadcast_to([64, 8, 64]), op=ALU.mult),
                       reads=[kS, "cst"], writes=[("PTm", r0)])
                if it == 0 and d == 0:
                    checkpoint(5.3)
                pN0, kN0 = bank("mN0", [4])
                pN1, kN1 = bank("mN1", [5])
                for h in range(8):
                    j, r = h // 2, h % 2
                    pn, kn = (pN0, kN0) if h < 4 else (pN1, kN1)
                    o_ = pn[rs, (h % 4) * 65:(h % 4 + 1) * 65]
                    sch.op(pe, lambda h=h, o_=o_: T.matmul(o_, lhsT=PTm[r0][:, h * 64:(h + 1) * 64], rhs=vaT[i2][:, h, :], start=True, stop=False),
                           reads=[("PTm", r0), ("va", i2)], writes=[kn], sig=False)
                    sch.op(pe, lambda h=h, j=j, r=r, o_=o_: T.matmul(o_, lhsT=qTz[d][r][:, j, rs], rhs=Cbf[d][:, j, :], start=False, stop=True),
                           reads=[("qTz", d, r), ("Cbf", d)], writes=[kn], sig=(h % 4 == 3))
                if it == 0 and d == 0:
                    checkpoint(5.4)
                pC, kC = bank("mC", [0, 1, 2, 3])
                pCv = pC[:, 0:260].rearrange("p (j c) -> p j c", j=4)
                for h in range(8):
                    j, r = h // 2, h % 2
                    pr = slice(r * 64, (r + 1) * 64)
                    sch.op(pe, lambda h=h, j=j, pr=pr: T.matmul(pCv[pr, j, :], lhsT=ktk[d][:, h * 64:(h + 1) * 64], rhs=vwz[r0][:, h, :],
                                                               start=True, stop=True),
                           reads=[("ktk", d), ("vw", r0)], writes=[kC], sig=(h == 7))
                if it == 0 and d == 0:
                    checkpoint(5.5)
                ecol = gE[rs, col]
                dn = stat[rs, 40:48]
                for hb, (pn, kn) in enumerate(((pN0, kN0), (pN1, kN1))):
                    pnv = pn[rs, 0:260].rearrange("p (h c) -> p h c", h=4)
                    sch.op(dve, lambda pnv=pnv, hb=hb: V.tensor_tensor(out=dn[:, hb * 4:(hb + 1) * 4], in0=pnv[:, :, 64],
                                                                      in1=ecol[:, hb * 4:(hb + 1) * 4], op=ALU.mult),
                           reads=[kn, ("gE", m)], writes=["dn"])
                dn2 = stat[rs, 20:28]
                sch.op(dve, lambda: V.tensor_scalar(out=dn2, in0=dn, scalar1=-1.0, scalar2=1.0, op0=ALU.mult, op1=ALU.max),
                       reads=["dn"], writes=["dn2"])
                sch.op(dve, lambda: V.tensor_tensor(out=dn, in0=dn, in1=dn2, op=ALU.max), reads=["dn", "dn2"], writes=["dn"])
                sch.op(dve, lambda: V.reciprocal(out=dn, in_=dn), reads=["dn"], writes=["dn"])
                sch.op(dve, lambda: V.tensor_tensor(out=dn, in0=dn, in1=ecol, op=ALU.mult), reads=["dn", ("gE", m)], writes=["dn"])
                hn = tmpf[rs, 0:512].rearrange("p (h c) -> p h c", h=8)
                for hb, (pn, kn) in enumerate(((pN0, kN0), (pN1, kN1))):
                    pnv = pn[rs, 0:260].rearrange("p (h c) -> p h c", h=4)
                    sch.op(dve, lambda pnv=pnv, hb=hb: V.tensor_tensor(out=hn[:, hb * 4:(hb + 1) * 4, :], in0=pnv[:, :, 0:64],
                                                                      in1=dn[:, hb * 4:(hb + 1) * 4].unsqueeze(2).broadcast_to([64, 4, 64]),
                                                                      op=ALU.mult), reads=[kn, "dn"], writes=["hn"])
                sch.op(pool, lambda: G.tensor_tensor(out=Hh[rs, m, :], in0=Hh[rs, m, :], in1=tmpf[rs, 0:512], op=ALU.add),
                       reads=["hn", ("H", m)], writes=[("H", m)])
                if it == 0 and d == 0:
                    checkpoint(5.6)
                par = r0
                dcy = gDec[:, m * 32 + par * 16 + d * 8:m * 32 + par * 16 + d * 8 + 8]
                dv4 = dcy.rearrange("p (j r) -> p j r", r=2)
                for r in range(2):
                    pr = slice(r * 64, (r + 1) * 64)
                    sch.op(dve, lambda r=r, pr=pr: V.tensor_tensor(out=Cst[d][pr], in0=Cst[d][pr],
                                                                  in1=dv4[pr, :, r].unsqueeze(2).broadcast_to([64, 4, 65]), op=ALU.mult),
                           reads=[("gDec", m), ("Cst", d)], writes=[("Cst", d)])
                sch.op(dve, lambda: V.tensor_tensor(out=Cst[d], in0=Cst[d], in1=pCv, op=ALU.add), reads=[kC, ("Cst", d)], writes=[("Cst", d)])
                sch.op(act, lambda: A.copy(out=Cbf[d], in_=Cst[d]), reads=[("Cst", d)], writes=[("Cbf", d)])

        if stage == 6:
            dump("H", T_big[:, 0:8192], [("H", t) for t in range(NT)])
        checkpoint(6)
        WinO = T_W[:, 0:4096].rearrange("p (k n) -> p k n", k=8)
        wload(WinO, win_b[:, C_MO:C_MO + 512], "W0", "win", 8)
        sch.barrier()
        otok2 = T_A[:, 0:8192].rearrange("p (t c) -> p t c", t=16)
        for tt in range(NT):
            po, ko = bank("co", [0, 1])
            mm_acc(po[:, :], [(hT[:, kc, tt * 128:(tt + 1) * 128], WinO[:, kc, :]) for kc in range(8)], ko, [("hT", tt), "W0"])
            sg = tmpf[:, 512:1024]
            sch.op(act, lambda: A.activation(out=sg, in_=po[:, :], func=AF.Sigmoid), reads=[ko], writes=["sg"])
            sq = tmpf[:, 0:512]
            Ht = Hh[:, tt, :]
            sch.op(act, lambda: A.activation(out=sq, in_=Ht, func=AF.Square, scale=0.125), reads=[("H", tt)], writes=["sq"])
            ms = stat[:, 48:56]
            sch.op(dve, lambda: V.tensor_reduce(out=ms, in_=sq.rearrange("p (h c) -> p h c", h=8), axis=mybir.AxisListType.X, op=ALU.add),
                   reads=["sq"], writes=["ms"])
            sch.op(act, lambda: A.activation(out=ms, in_=ms, func=AF.Sqrt, bias=epsb[:], scale=1.0), reads=["ms", "epsb"], writes=["ms"])
            sch.op(dve, lambda: V.reciprocal(out=ms, in_=ms), reads=["ms"], writes=["ms"])
            sch.op(dve, lambda: V.tensor_tensor(out=sq.rearrange("p (h c) -> p h c", h=8), in0=Ht.rearrange("p (h c) -> p h c", h=8),
                                                in1=ms.unsqueeze(2).broadcast_to([128, 8, 64]), op=ALU.mult),
                   reads=[("H", tt), "ms"], writes=["sq"])
            sch.op(pool, lambda: G.tensor_tensor(out=sq, in0=sq, in1=ghead_bc[:], op=ALU.mult), reads=["sq", "ghead"], writes=["sq"])
            sch.op(dve, lambda: V.tensor_tensor(out=otok2[:, tt, :], in0=sq, in1=sg, op=ALU.mult), reads=["sq", "sg"],
                   writes=[("otok", tt // 4)])
        otok_save = otok
        otok = otok2
        flush_oT(1)
        otok = otok_save
        sch.barrier()

        checkpoint(7)
        WkvA = T_W[:, 0:4096].rearrange("p (k n) -> p k n", k=8)
        WkvB = T_W[:, 4096:8192].rearrange("p (k n) -> p k n", k=8)
        WinQ = T_W[:, 8192:12288].rearrange("p (k n) -> p k n", k=8)
        for nm, lo, hi in (("W0", 0, 4096), ("W1", 4096, 8192), ("W2", 8192, 12288)):
            sch.set_alias(nm, lo, hi)
        wload(WkvA, wkv_b[:, 0:512], "W0", "wkv", 8)
        wload(WkvB, wkv_b[:, 512:1024], "W1", "wkv", 8)
        wload(WinQ, win_b[:, C_MQ:C_MQ + 512], "W2", "win", 8)
        memT = T_A[:, 0:2048].rearrange("p (k t) -> p k t", k=8)
        kmT = T_A[:, 2048:3072].rearrange("p (h t) -> p h t", h=4)
        vm = T_A[:, 3072:4104].rearrange("p (t h c) -> p t h c", t=2, h=4)
        qm = [T_A[:, 4104 + i * 512:4104 + (i + 1) * 512] for i in range(2)]
        sch.op(dve, lambda: V.memset(vm[:, :, :, 128:129], 1.0), writes=["vm"])
        for t2 in range(2):
            slot = t2
            xsv = xs[:, slot * 1024:(slot + 1) * 1024]
            sch.dma(sp, xsv, mem_d[b, t2 * 128:(t2 + 1) * 128, :], writes=[("xs", slot)], grp=("xs", slot))
            rms_to_T(xsv, [("xs", slot)], 16, lambda t2=t2: memT[:, :, t2 * 128:(t2 + 1) * 128], [("memT", t2)], slot)
        checkpoint(7.1)
        MT = [("memT", 0), ("memT", 1)]
        for h in range(4):
            pk, kk = bank("dk", [0, 1])
            mm_acc(pk[:, 0:256], [(WkvA[:, kc, h * 128:(h + 1) * 128], memT[:, kc, :]) for kc in range(8)], kk, MT + ["W0"])
            sch.op(act, lambda: A.copy(out=kmT[:, h, :], in_=pk[:, 0:256]), reads=[kk], writes=["kmT"])
        for t2 in range(2):
            pv, kv = bank("dv", [2, 3])
            mm_acc(pv[:, :], [(memT[:, kc, t2 * 128:(t2 + 1) * 128], WkvB[:, kc, :]) for kc in range(8)], kv, MT + ["W1"])
            sch.op(act, lambda: A.copy(out=vm[:, t2, :, 0:128], in_=pv[:].rearrange("p (h c) -> p h c", h=4)), reads=[kv], writes=["vm"])
        checkpoint(7.2)
        mscale = 128.0 ** -0.5
        qc = 0
        for h in range(4):
            for qb in range(4):
                qs = slice(qb * 512, (qb + 1) * 512)
                pq, kq = bank("dq", [0, 1])
                mm_acc(pq[:, :], [(WinQ[:, kc, h * 128:(h + 1) * 128], hT[:, kc, qs]) for kc in range(8)], kq, htkeys(qb) + ["W2"])
                qi = qc % 2
                qc += 1
                sch.op(act, lambda: A.copy(out=qm[qi], in_=pq[:, :]), reads=[kq], writes=[("qm", qi)])
                pOa, kOa = bank("dOa", [4])
                pOb, kOb = bank("dOb", [5])
                for kt in range(2):
                    pS, kS = bank("dS", [2, 3])
                    sch.op(pe, lambda: T.matmul(pS[:, :], lhsT=kmT[:, h, kt * 128:(kt + 1) * 128], rhs=qm[qi], start=True, stop=True),
                           reads=["kmT", ("qm", qi)], writes=[kS])
                    ps_ = (qc * 2 + kt) % 3
                    P_ = PTt[:, ps_ * 512:(ps_ + 1) * 512]
                    sch.op(act, lambda: A.activation(out=P_, in_=pS[:, :], func=AF.Exp, scale=mscale), reads=[kS], writes=[("PT", ps_)])
                    for j in range(4):
                        pO_, kO_ = (pOa, kOa) if j < 2 else (pOb, kOb)
                        sch.op(pe, lambda j=j, pO_=pO_: T.matmul(pO_[:, (j % 2) * 129:(j % 2 + 1) * 129], lhsT=P_[:, j * 128:(j + 1) * 128],
                                                                 rhs=vm[:, kt, h, :], start=(kt == 0 and j % 2 == 0), stop=(kt == 1),
                                                                 skip_group_check=True),
                               reads=[("PT", ps_), "vm"], writes=[kO_], sig=(j % 2 == 1))
                if h == 0 and qb == 0:
                    checkpoint(7.3)
                rec = stat[:, 32:36]
                for half, (pO_, kO_) in enumerate(((pOa, kOa), (pOb, kOb))):
                    pv2 = pO_[:, 0:258].rearrange("p (j c) -> p j c", j=2)
                    rc = rec[:, half * 2:half * 2 + 2]
                    sch.op(dve, lambda: V.reciprocal(out=rc, in_=pv2[:, :, 128]), reads=[kO_], writes=[("rec", half)])
                    sch.op(dve, lambda: V.tensor_tensor(out=otok[:, qb * 4 + half * 2:qb * 4 + half * 2 + 2, h * 128:(h + 1) * 128],
                                                        in0=pv2[:, :, 0:128], in1=rc.unsqueeze(2).broadcast_to([128, 2, 128]), op=ALU.mult),
                           reads=[kO_, ("rec", half)], writes=[("otok", qb)])
        checkpoint(7.4)
        flush_oT(2)
        sch.barrier()

        if dbg:
            for br in range(3):
                for j in range(4):
                    sch.dma(pool, dbg_d[br, :, j, :], oT_d[br, :, j, :], reads=[("oTd", br, k, j) for k in range(4)], writes=[("dbg", br, j)],
                            grp=("dbg", br, j), final=True)

        checkpoint(8)
        x1 = T_big[:, 0:4096].rearrange("p (t c) -> p t c", t=4)
        mrg = bigb[:, 8192:12288].rearrange("p (k t) -> p k t", k=8)
        h2T = bigb[:, 12288:16384].rearrange("p (k t) -> p k t", k=8)
        aT = T_A[:].rearrange("p (c t) -> p c t", c=NFF)
        oTs = [T_C[:, br * 2048:(br + 1) * 2048].rearrange("p (j t) -> p j t", j=4) for br in range(3)]
        sgb = [T_V[:, i * 512:(i + 1) * 512] for i in range(3)]
        prd = [T_V[:, 1536 + i * 1024:1536 + (i + 1) * 1024].bitcast(F32) for i in range(3)]
        WS = [T_W[:, i * 4608:(i + 1) * 4608] for i in range(3)]
        for i in range(3):
            sch.set_alias(("WS", i), i * 4608, (i + 1) * 4608)
        units = []

        def e1_unit(blk, f):
            bs = slice(blk * 512, (blk + 1) * 512)

            def load(sl):
                sch.dma(sp, WS[sl][:, :], wge_b[f], reads=[("w", "wge")], writes=[("WS", sl)], grp=("WS", sl))

            def consume(sl):
                if f == 0:
                    for br in range(3):
                        sch.dma(sp, oTs[br], oT_d[br, :, :, bs], reads=[("oTd", br, blk, j) for j in range(4)], writes=[("oTs", br)],
                                grp=("oTs", br))
                Wg_ = WS[sl][:, 0:3072].rearrange("p (k b n) -> p k b n", k=8, b=3)
                Wb_ = WS[sl][:, 3072:4608].rearrange("p (k b n) -> p k b n", k=4, b=3)
                for br in range(3):
                    pg, kg = bank("eg", [0, 1])
                    pb_, kb_ = bank("eb", [2, 3])
                    mm_acc(pg[:, :], [(Wg_[:, kc, br, :], hT[:, kc, bs]) for kc in range(8)], kg, htkeys(blk) + [("WS", sl)])
                    mm_acc(pb_[:, :], [(Wb_[:, kc, br, :], oTs[br][:, kc, :]) for kc in range(4)], kb_, [("oTs", br), ("WS", sl)])
                    sch.op(act, lambda: A.activation(out=sgb[br], in_=pg[:, :], func=AF.Sigmoid), reads=[kg], writes=[("sgb", br)])
                    sch.op(dve, lambda: V.tensor_tensor(out=prd[br], in0=pb_[:, :], in1=sgb[br], op=ALU.mult),
                           reads=[kb_, ("sgb", br)], writes=[("prd", br)])
                sch.op(pool, lambda: G.tensor_tensor(out=prd[0], in0=prd[0], in1=prd[1], op=ALU.add),
                       reads=[("prd", 0), ("prd", 1)], writes=[("prd", 0)])
                sch.op(pool, lambda: G.tensor_tensor(out=mrg[:, f, :], in0=prd[0], in1=prd[2], op=ALU.add),
                       reads=[("prd", 0), ("prd", 2)], writes=[("mrg", f)])
            return load, consume

        MRG = [("mrg", f) for f in range(8)]
        H2 = [("h2T", t) for t in range(4)]

        def e2_unit(blk, nh):
            def load(sl):
                sch.dma(sp, WS[sl][:, 0:4096].rearrange("p (k n) -> p k n", k=8),
                        wout_b[:, nh * 512:(nh + 1) * 512].rearrange("(k p) n -> p k n", p=128),
                        reads=[("w", "wout")], writes=[("WS", sl)], grp=("WS", sl))

            def consume(sl):
                Wo = WS[sl][:, 0:4096].rearrange("p (k n) -> p k n", k=8)
                for tt in range(4):
                    slot = tt % 2
                    gt = blk * 4 + tt
                    xh = xs[:, slot * 1024:slot * 1024 + 512]
                    sch.dma(sp, xh, x_d[b, gt * 128:(gt + 1) * 128, nh * 512:(nh + 1) * 512], writes=[("xs", slot)], grp=("xs", slot))
                    py, ky = bank("ey", [4, 5])
                    mm_acc(py[:, :], [(mrg[:, kc, tt * 128:(tt + 1) * 128], Wo[:, kc, :]) for kc in range(8)], ky, MRG + [("WS", sl)])
                    sch.op(dve, lambda: V.tensor_tensor(out=x1[:, tt, nh * 512:(nh + 1) * 512], in0=py[:, :], in1=xh,
                                                        op=ALU.add), reads=[ky, ("xs", slot)], writes=[("x1", tt)])
                if nh == 1:
                    for tt in range(4):
                        rms_to_T(x1[:, tt, :], [("x1", tt)], 8, lambda tt=tt: h2T[:, :, tt * 128:(tt + 1) * 128], [("h2T", tt)], tt % 2)
            return load, consume

        def e3a_unit(blk, u):
            def load(sl):
                sch.dma(sp, WS[sl][:, 0:4096], wgu_b[u], reads=[("w", "wgu")], writes=[("WS", sl)], grp=("WS", sl))

            def consume(sl):
                WF = WS[sl][:, 0:4096].rearrange("p (c g k n) -> p c g k n", c=2, g=2, k=8)
                for c2 in range(2):
                    c = 2 * u + c2
                    pg, kg = bank("fg", [0, 1])
                    pu, ku = bank("fu", [2, 3])
                    mm_acc(pg[:, :], [(WF[:, c2, 0, kc, :], h2T[:, kc, :]) for kc in range(8)], kg, H2 + [("WS", sl)])
                    mm_acc(pu[:, :], [(WF[:, c2, 1, kc, :], h2T[:, kc, :]) for kc in range(8)], ku, H2 + [("WS", sl)])
                    si = c % 3
                    sch.op(act, lambda: A.activation(out=prd[si], in_=pg[:, :], func=AF.Silu), reads=[kg], writes=[("prd", si)])
                    sch.op(dve, lambda: V.tensor_tensor(out=aT[:, c, :], in0=pu[:, :], in1=prd[si], op=ALU.mult),
                           reads=[ku, ("prd", si)], writes=[("aT", c)])
            return load, consume

        def e3b_unit(blk, u):
            c0 = 4 * u
            ncu = min(4, NFF - c0)

            def load(sl):
                sch.dma(sp, WS[sl][:, 0:ncu * 1024].rearrange("p (c n) -> p c n", c=ncu),
                        wd_b[c0 * 128:(c0 + ncu) * 128, :].rearrange("(c p) n -> p c n", p=128),
                        reads=[("w", "wd")], writes=[("WS", sl)], grp=("WS", sl))

            def consume(sl):
                Wd_ = WS[sl][:, 0:ncu * 1024].rearrange("p (c n) -> p c n", c=ncu)
                for tt in range(4):
                    for nh in range(2):
                        py, ky = bank("ey", [4, 5])
                        mm_acc(py[:, :], [(aT[:, c0 + c, tt * 128:(tt + 1) * 128], Wd_[:, c, nh * 512:(nh + 1) * 512]) for c in range(ncu)],
                               ky, [("aT", c0 + c) for c in range(ncu)] + [("WS", sl)])
                        sch.op(dve, lambda: V.tensor_tensor(out=x1[:, tt, nh * 512:(nh + 1) * 512], in0=py[:, :],
                                                            in1=x1[:, tt, nh * 512:(nh + 1) * 512], op=ALU.add),
                               reads=[ky, ("x1", tt)], writes=[("x1", tt)])
                if c0 + ncu == NFF:
                    for tt in range(4):
                        slot = tt % 2
                        gt = blk * 4 + tt
                        st = stat[:, 56 + slot * 2:56 + slot * 2 + 1]
                        xsv = xs[:, slot * 1024:(slot + 1) * 1024]
                        xnv = xn[:, slot * 1024:(slot + 1) * 1024]
                        sch.op(act, lambda: A.activation(out=xnv, in_=x1[:, tt, :], func=AF.Square, scale=1.0 / 32.0, accum_out=st),
                               reads=[("x1", tt)], writes=[("xn", slot), ("fst", slot)])
                        sch.op(act, lambda: A.activation(out=st, in_=st, func=AF.Sqrt, bias=epsb[:], scale=1.0),
                               reads=[("fst", slot), "epsb"], writes=[("fst", slot)])
                        sch.op(dve, lambda: V.reciprocal(out=st, in_=st), reads=[("fst", slot)], writes=[("fst", slot)])
                        sch.op(act, lambda: A.activation(out=xsv, in_=x1[:, tt, :], func=AF.Copy, scale=st),
                               reads=[("x1", tt), ("fst", slot)], writes=[("xs", slot)])
                        sch.op(pool, lambda: G.tensor_tensor(out=xsv, in0=xsv, in1=gfin_bc[:], op=ALU.mult),
                               reads=[("xs", slot), "gfin"], writes=[("xs", slot)])
                        sch.dma(sp, out_d[b, gt * 128:(gt + 1) * 128, :], xsv, reads=[("xs", slot)], grp=("xs", slot), final=True)
            return load, consume

        for blk in range(4):
            units += [e1_unit(blk, f) for f in range(8)]
            units += [e2_unit(blk, nh) for nh in range(2)]
            units += [e3a_unit(blk, u) for u in range(11)]
            units += [e3b_unit(blk, u) for u in range(6)]
        units[0][0](0)
        units[1][0](1)
        for ui, (ld_, cons_) in enumerate(units):
            cons_(ui % 3)
            if ui + 2 < len(units):
                units[ui + 2][0]((ui + 2) % 3)
        sch.barrier()


def make_consts():
    c = np.zeros((128, K_N), np.float32)
    c[:, K_ID:K_ID + 128] = np.eye(128, dtype=np.float32)
    p = np.arange(128)
    s = (p % 64)[:, None]
    t = np.arange(64)[None, :]
    c[:, K_MF:K_MF + 64] = (s <= t)
    c[:, K_MB:K_MB + 64] = (s >= t)
    ss = p[:, None]
    tt = p[None, :]
    same = (ss // 64) == (tt // 64)
    c[:, K_TF:K_TF + 128] = same & (ss <= tt)
    c[:, K_TB:K_TB + 128] = same & (ss >= tt)
    c[:, K_I0:K_I0 + 128] = (ss // 64 == 0) & (tt >= 0)
    c[:, K_I1:K_I1 + 128] = (ss // 64 == 1) & (tt >= 0)
    inv = (10000.0 ** (-np.arange(16, dtype=np.float32) / 16.0)).astype(np.float32)
    for i in range(32):
        c[64 + i, K_RP] = inv[i % 16]
        c[64 + i, K_RP + 1] = math.pi if i < 16 else 0.0
        c[64 + i, K_RP + 2] = 0.5 * math.pi
    c[:, K_RP + 3] = -math.pi
    return c


_CACHE = {}


def kernel(**inputs):
    f = lambda a: np.ascontiguousarray(np.asarray(a, dtype=np.float32))
    if "nc" not in _CACHE:
        _CACHE["nc"] = build_program()
    nc = _CACHE["nc"]
    shared = {
        "g_mix": f(inputs["g_mix"]).reshape(1, D),
        "w_in": f(inputs["w_in"]).reshape(D, N_IN),
        "mla_g_q": f(inputs["mla_g_q"]).reshape(1, 384),
        "mla_g_kv": f(inputs["mla_g_kv"]).reshape(1, 256),
        "mla_w_uq": f(inputs["mla_w_uq"]).reshape(384, 768),
        "mla_w_uk": f(inputs["mla_w_uk"]).reshape(256, 512),
        "mla_w_uv": f(inputs["mla_w_uv"]).reshape(256, 512),
        "ml_conv_w": f(inputs["ml_conv_w"]).reshape(5, 512),
        "ml_w_q": f(inputs["ml_w_q"]).reshape(512, 64),
        "ml_w_k": f(inputs["ml_w_k"]).reshape(512, 64),
        "ml_gate_bias": f(inputs["ml_gate_bias"]).reshape(1, 32),
        "ml_g_head": f(inputs["ml_g_head"]).reshape(1, 512),
        "mem_g": f(inputs["mem_g"]).reshape(1, D),
        "mem_w_kv": f(inputs["mem_w_kv"]).reshape(D, 1024),
        "w_branch": f(inputs["w_branch"]).reshape(1536, D),
        "w_out": f(inputs["w_out"]).reshape(D, D),
        "g_ffn": f(inputs["g_ffn"]).reshape(1, D),
        "w_ffn_gate": f(inputs["w_ffn_gate"]).reshape(D, DFF),
        "w_ffn_up": f(inputs["w_ffn_up"]).reshape(D, DFF),
        "w_ffn_down": f(inputs["w_ffn_down"]).reshape(DFF, D),
        "g_final": f(inputs["g_final"]).reshape(1, D),
        "cst": make_consts(),
    }
    x = f(inputs["x"])
    mem = f(inputs["mem"])
    pos = np.ascontiguousarray(np.asarray(inputs["positions"], dtype=np.int32))
    in_maps = []
    for c in range(NCORES):
        sl = slice(c * SEQ_PER_CORE, (c + 1) * SEQ_PER_CORE)
        m = dict(shared)
        m["x"] = x[sl]
        m["mem"] = mem[sl]
        m["positions"] = pos[sl]
        in_maps.append(m)
    res = run_bass_kernel_spmd(nc, in_maps, core_ids=list(range(NCORES)))
    out = np.concatenate([np.asarray(r["out"]) for r in res.results], axis=0)
    return out.astype(np.float32, copy=False)
```

```python
import contextlib
import math
import numpy as np
import concourse.bass as bass
import concourse.mybir as mybir
from concourse.bass_utils import run_bass_kernel_spmd

F32 = mybir.dt.float32
BF16 = mybir.dt.bfloat16
I32 = mybir.dt.int32
AF = mybir.ActivationFunctionType
ALU = mybir.AluOpType

NCORES = 8
SEQ_PER_CORE = 4
S = 2048
D = 1024
NT = S // 128
EPS = 1e-6
N_IN = 5824
C_CQ, C_MU, C_MV, C_MO, C_MG, C_MQ = 3072, 3744, 4256, 4768, 5280, 5312
DFF = 2816
NFF = DFF // 128
K_ID, K_MF, K_MB, K_TF, K_TB, K_I0, K_I1, K_RP = 0, 128, 192, 256, 384, 512, 640, 768
K_N = 772


class Trk:
    __slots__ = ("w", "r")

    def __init__(self):
        self.w = None
        self.r = {}


class Eng:
    def __init__(self, sch, name, h):
        self.sch = sch
        self.name = name
        self.h = h
        self.seen = {}
        self.pending = False
        self.semidx = sch.newsem()
        self.cnt = 0


class Sched:
    def __init__(self, nc, es):
        self.nc = nc
        self.es = es
        self.sems = []
        self.trk = {}
        self.dsem = {}
        self.final = []
        self.alias = {}
        self.ninst = 0
        self.pe = Eng(self, "pe", nc.tensor)
        self.dve = Eng(self, "dve", nc.vector)
        self.act = Eng(self, "act", nc.scalar)
        self.pool = Eng(self, "pool", nc.gpsimd)
        self.sp = Eng(self, "sp", nc.sync)

    def newsem(self):
        h = self.es.enter_context(self.nc.semaphore("s%d" % len(self.sems)))
        self.sems.append(h)
        return len(self.sems) - 1

    def _exp(self, keys):
        out = []
        for k in keys:
            a = self.alias.get(k)
            if a is None:
                out.append(k)
            else:
                out.extend(a)
        return out

    def set_alias(self, key, lo, hi):
        self.alias[key] = [("TW", g) for g in range(lo // 512, (hi + 511) // 512)]

    def _deps(self, reads, writes, skip_waw_sem=None):
        d = {}
        for k in reads:
            t = self.trk.get(k)
            if t is not None and t.w is not None and d.get(t.w[0], 0) < t.w[1]:
                d[t.w[0]] = t.w[1]
        for k in writes:
            t = self.trk.get(k)
            if t is None:
                continue
            if t.w is not None and t.w[0] != skip_waw_sem and d.get(t.w[0], 0) < t.w[1]:
                d[t.w[0]] = t.w[1]
            for si, v in t.r.items():
                if d.get(si, 0) < v:
                    d[si] = v
        return d

    def _wait(self, eng, d, skip_own=False):
        for si, v in d.items():
            if skip_own and si == eng.semidx:
                continue
            if eng.seen.get(si, 0) >= v:
                continue
            eng.h.wait_ge(self.sems[si], v)
            self.ninst += 1
            eng.seen[si] = v

    def _mark(self, tok, reads, writes):
        for k in reads:
            t = self.trk.get(k)
            if t is None:
                t = self.trk[k] = Trk()
            if t.r.get(tok[0], 0) < tok[1]:
                t.r[tok[0]] = tok[1]
        for k in writes:
            t = self.trk.get(k)
            if t is None:
                t = self.trk[k] = Trk()
            t.w = tok
            t.r = {}

    def op(self, eng, fn, reads=(), writes=(), sig=True):
        reads = self._exp(reads)
        writes = self._exp(writes)
        d = self._deps(reads, writes)
        self._wait(eng, d, skip_own=(eng is self.pe))
        ins = fn()
        self.ninst += 1
        if sig or eng is not self.pe:
            if eng.cnt >= 30000 and not eng.pending:
                eng.semidx = self.newsem()
                eng.cnt = 0
            eng.cnt += 1
            ins.then_inc(self.sems[eng.semidx], 1)
            tok = (eng.semidx, eng.cnt)
            eng.pending = False
        else:
            tok = (eng.semidx, eng.cnt + 1)
            eng.pending = True
        self._mark(tok, reads, writes)
        return tok

    def dma(self, q, out, in_, reads=(), writes=(), grp=None, final=False):
        reads = self._exp(reads)
        writes = self._exp(writes)
        g = self.dsem.get(grp)
        if g is None:
            g = self.dsem[grp] = [self.newsem(), 0]
        d = self._deps(reads, writes, skip_waw_sem=g[0])
        self._wait(q, d)
        ins = q.h.dma_start(out=out, in_=in_)
        self.ninst += 1
        g[1] += 16
        ins.then_inc(self.sems[g[0]], 16)
        tok = (g[0], g[1])
        self._mark(tok, reads, writes)
        if final:
            self.final.append(tok)
        return tok

    def retoken(self, keys, tok):
        for k in keys:
            t = self.trk.get(k)
            if t is None:
                t = self.trk[k] = Trk()
            t.w = tok

    def barrier(self):
        engs = [self.pe, self.dve, self.act, self.pool]
        assert not self.pe.pending
        for e in engs + [self.sp]:
            d = {}
            for o in engs:
                if o is not e and o.cnt > 0:
                    d[o.semidx] = o.cnt
            self._wait(e, d)

    def finish(self):
        d = {}
        for si, v in self.final:
            if d.get(si, 0) < v:
                d[si] = v
        self._wait(self.sp, d)


class StopBuild(Exception):
    pass


def build_program(nseq=SEQ_PER_CORE, dbg=False, stage=99):
    nc = bass.Bass("TRN2", target_bir_lowering=False)
    es = contextlib.ExitStack()
    with es:
        sch = Sched(nc, es)
        try:
            _emit(nc, es, nseq, dbg, sch, stage)
        except StopBuild:
            pass
        sch.finish()
        _emit.ninst = sch.ninst
        _emit.nsem = len(sch.sems)
    return nc


def _emit(nc, es, nseq, dbg, sch, stage):
    pe, dve, act, pool, sp = sch.pe, sch.dve, sch.act, sch.pool, sch.sp
    T, V, A, G = nc.tensor, nc.vector, nc.scalar, nc.gpsimd

    def din(name, shape, dt=F32):
        return nc.dram_tensor(name, list(shape), dt, kind="ExternalInput").ap()

    def dscr(name, shape, dt=BF16):
        return nc.dram_tensor(name, list(shape), dt, kind="Internal").ap()

    x_d = din("x", [SEQ_PER_CORE, S, D])
    mem_d = din("mem", [SEQ_PER_CORE, 256, D])
    pos_d = din("positions", [SEQ_PER_CORE, S], I32)
    gmix_d = din("g_mix", [1, D])
    win_d = din("w_in", [D, N_IN])
    gq_d = din("mla_g_q", [1, 384])
    gkv_d = din("mla_g_kv", [1, 256])
    wuq_d = din("mla_w_uq", [384, 768])
    wuk_d = din("mla_w_uk", [256, 512])
    wuv_d = din("mla_w_uv", [256, 512])
    convw_d = din("ml_conv_w", [5, 512])
    wq_d = din("ml_w_q", [512, 64])
    wk_d = din("ml_w_k", [512, 64])
    gbias_d = din("ml_gate_bias", [1, 32])
    ghead_d = din("ml_g_head", [1, 512])
    memg_d = din("mem_g", [1, D])
    wkv_d = din("mem_w_kv", [D, 1024])
    wbr_d = din("w_branch", [1536, D])
    wout_d = din("w_out", [D, D])
    gffn_d = din("g_ffn", [1, D])
    wg_d = din("w_ffn_gate", [D, DFF])
    wu_d = din("w_ffn_up", [D, DFF])
    wd_d = din("w_ffn_down", [DFF, D])
    gfin_d = din("g_final", [1, D])
    cst_d = din("cst", [128, K_N])
    out_d = nc.dram_tensor("out", [SEQ_PER_CORE, S, D], F32, kind="ExternalOutput").ap()
    if dbg:
        dbg_d = nc.dram_tensor("dbg_oT", [3, 128, 4, S], BF16, kind="ExternalOutput").ap()

    win_b = dscr("win_b", [D, N_IN])
    wuq_b = dscr("wuq_b", [384, 768])
    wuk_b = dscr("wuk_b", [256, 512])
    wuv_b = dscr("wuv_b", [256, 512])
    wq_b = dscr("wq_b", [512, 64])
    wk_b = dscr("wk_b", [512, 64])
    wkv_b = dscr("wkv_b", [D, 1024])
    wout_b = dscr("wout_b", [D, D])
    wd_b = dscr("wd_b", [DFF, D])
    oT_d = dscr("oT_scr", [3, 128, 4, S])
    wge_b = dscr("wge_b", [8, 128, 4608])
    wgu_b = dscr("wgu_b", [11, 128, 4096])

    dumps = {}

    def dump(name, ap, reads):
        shp = [int(v) for v in ap.shape]
        t = nc.dram_tensor("dmp_" + name, shp, ap.dtype, kind="ExternalOutput").ap()
        sch.dma(sp, t, ap, reads=reads, grp=("dmp", name), final=True)

    def checkpoint(st):
        if stage <= st:
            sch.barrier()
            raise StopBuild()

    def sbt(name, cols, dt):
        return es.enter_context(nc.sbuf_tensor(name, [128, cols], dt))

    T_hT = sbt("hT", 8 * S, BF16)
    T_big = sbt("big", 4 * 2080, F32)
    T_A = sbt("ra", 11264, BF16)
    T_V = sbt("rv", 16 * 520, BF16)
    T_C = sbt("rc", 16 * 512, BF16)
    T_W = sbt("rw", 13824, BF16)
    xs = sbt("xs", 2 * 1024, F32)
    xn = sbt("xn", 2 * 1024, BF16)
    cst = sbt("cstt", K_N, F32)
    identb = sbt("identb", 128, BF16)
    gfin_bc = sbt("gfin_bc", 1024, F32)
    ghead_bc = sbt("ghead_bc", 512, F32)
    gbias_bc = sbt("gbias_bc", 32, F32)
    gvecT = sbt("gvecT", 32, F32)
    convw = sbt("convw", 20, F32)
    stat = sbt("stat", 128, F32)
    epsb = sbt("epsb", 1, F32)
    oneb = sbt("oneb", 1, F32)
    gA = sbt("gA", 16 * 16, F32)
    gE = sbt("gE", 16 * 16, F32)
    gWt = sbt("gWt", 16 * 16, F32)
    gDec = sbt("gDec", 16 * 32, F32)
    gtmp = sbt("gtmp", 128, F32)
    kst = sbt("kst", 2 * 96, BF16)
    PTt = sbt("PTt", 3 * 1024, BF16)
    tmpf = sbt("tmpf", 2 * 512, F32)
    stg = sbt("stg", 2 * 512, BF16)
    posi = sbt("posi", S, I32)
    posf = sbt("posf", S, F32)

    hT = T_hT[:].rearrange("p (k t) -> p k t", k=8)
    bigb = T_big[:].bitcast(BF16)

    pdbl = [es.enter_context(nc.psum_tensor("pd%d" % i, [128, 1024], F32)) for i in range(3)]
    pbk = [pdbl[i // 2][:, (i % 2) * 512:(i % 2 + 1) * 512] for i in range(6)]
    ptk = [es.enter_context(nc.psum_tensor("pt%d" % i, [128, 1024], BF16)) for i in range(2)]
    pbk.append(ptk[0][:].bitcast(F32))
    pbk.append(ptk[1][:].bitcast(F32))
    rot = {}

    def bank(grp, ids):
        i = rot.get(grp, 0)
        rot[grp] = i + 1
        b = ids[i % len(ids)]
        return pbk[b], (("pb", b) if b < 6 else ("pt", b - 6))

    def tbank():
        i = rot.get("pt", 0)
        rot["pt"] = i + 1
        return ptk[i % 2], ("pt", i % 2)

    sch.dma(sp, cst[:], cst_d, writes=["cst"], grp="cst")
    sch.dma(pool, identb[:], cst_d[:, K_ID:K_ID + 128], writes=["ident"], grp="ident")
    sch.dma(sp, gfin_bc[:], gfin_d.partition_broadcast(128), writes=["gfin"], grp="c2")
    sch.dma(sp, ghead_bc[:], ghead_d.partition_broadcast(128), writes=["ghead"], grp="c3")
    sch.dma(sp, gbias_bc[:], gbias_d.partition_broadcast(128), writes=["gbias"], grp="c4")
    with nc.allow_non_contiguous_dma("tiny gain-vector transposes"):
        sch.dma(sp, gvecT[:, 0:8], gmix_d.rearrange("o (k p) -> p (o k)", p=128), writes=["gv0"], grp="c5")
        sch.dma(sp, gvecT[:, 8:16], gffn_d.rearrange("o (k p) -> p (o k)", p=128), writes=["gv1"], grp="c6")
        sch.dma(sp, gvecT[:, 16:24], memg_d.rearrange("o (k p) -> p (o k)", p=128), writes=["gv2"], grp="c7")
        sch.dma(sp, gvecT[:, 24:27], gq_d.rearrange("o (k p) -> p (o k)", p=128), writes=["gv3"], grp="c8")
        sch.dma(sp, gvecT[:, 27:29], gkv_d.rearrange("o (k p) -> p (o k)", p=128), writes=["gv4"], grp="c9")
        for ct_ in range(4):
            sch.dma(sp, convw[:, ct_ * 5:(ct_ + 1) * 5], convw_d[:, ct_ * 128:(ct_ + 1) * 128].rearrange("j p -> p j"),
                    writes=["convw"], grp="c10")
    GV = ["gv0", "gv1", "gv2", "gv3", "gv4"]
    sch.op(dve, lambda: V.memset(epsb[:], EPS), writes=["epsb"])
    sch.op(dve, lambda: V.memset(kst[:], 0.0), writes=[("kst", 0), ("kst", 1)])
    sch.op(dve, lambda: V.memset(oneb[:], 1.0), writes=["oneb"])

    def conv_w(dst, src, rows, key):
        tok = None
        r0 = 0
        while r0 < rows:
            r1 = min(rows, r0 + 128)
            tok = sch.dma(pool, dst[r0:r1, :], src[r0:r1, :], grp=("cv", key))
            r0 = r1
        sch.retoken([("w", key)], tok)

    tok = None
    for r0_ in range(0, D, 128):
        tok = sch.dma(pool, win_b[r0_:r0_ + 128, C_CQ:N_IN], win_d[r0_:r0_ + 128, C_CQ:N_IN], grp=("cv", "win"))
    sch.retoken([("w", "win")], tok)
    conv_w(wuq_b, wuq_d, 384, "wuq")
    conv_w(wuk_b, wuk_d, 256, "wuk")
    conv_w(wuv_b, wuv_d, 256, "wuv")
    conv_w(wq_b, wq_d, 512, "wq")
    conv_w(wk_b, wk_d, 512, "wk")
    conv_w(wkv_b, wkv_d, D, "wkv")
    tok = None
    for f_ in range(8):
        for br_ in range(3):
            dst = wge_b[f_, :, 0:3072].rearrange("p (k b n) -> p k b n", k=8, b=3)[:, :, br_, :]
            src = win_d[:, br_ * 1024 + f_ * 128:br_ * 1024 + (f_ + 1) * 128].rearrange("(k p) n -> p k n", p=128)
            tok = sch.dma(pool, dst, src, grp=("cv", "wge"))
            dst = wge_b[f_, :, 3072:4608].rearrange("p (k b n) -> p k b n", k=4, b=3)[:, :, br_, :]
            src = wbr_d[br_ * 512:(br_ + 1) * 512, f_ * 128:(f_ + 1) * 128].rearrange("(k p) n -> p k n", p=128)
            tok = sch.dma(pool, dst, src, grp=("cv", "wge"))
    sch.retoken([("w", "wge")], tok)
    conv_w(wout_b, wout_d, D, "wout")
    tok = None
    for u_ in range(11):
        for c2_ in range(2):
            c_ = 2 * u_ + c2_
            for gu_, src_d in enumerate((wg_d, wu_d)):
                o_ = (c2_ * 2 + gu_) * 1024
                dst = wgu_b[u_, :, o_:o_ + 1024].rearrange("p (k n) -> p k n", k=8)
                src = src_d[:, c_ * 128:(c_ + 1) * 128].rearrange("(k p) n -> p k n", p=128)
                tok = sch.dma(pool, dst, src, grp=("cv", "wgu"))
    sch.retoken([("w", "wgu")], tok)
    conv_w(wd_b, wd_d, DFF, "wd")

    checkpoint(0)

    def wload(dst, src2d, key, wkey, kc):
        if kc == 1:
            srcv = src2d
        else:
            srcv = src2d.rearrange("(k p) n -> p k n", p=128)
        sch.dma(sp, dst, srcv, reads=[("w", wkey)], writes=[key], grp=key)

    def rms_to_T(src_ap, src_keys, gcol, dst_fn, dst_keys, slot):
        st = stat[:, slot * 4:slot * 4 + 1]
        st2 = stat[:, slot * 4 + 1:slot * 4 + 2]
        xnv = xn[:, slot * 1024:(slot + 1) * 1024]
        sch.op(act, lambda: A.activation(out=xnv, in_=src_ap, func=AF.Square, scale=1.0 / 32.0, accum_out=st),
               reads=src_keys, writes=[("xn", slot), ("st", slot)])
        sch.op(act, lambda: A.activation(out=st2, in_=st, func=AF.Sqrt, bias=epsb[:], scale=1.0),
               reads=[("st", slot), "epsb"], writes=[("st2", slot)])
        sch.op(dve, lambda: V.reciprocal(out=st2, in_=st2), reads=[("st2", slot)], writes=[("st2", slot)])
        sch.op(act, lambda: A.activation(out=xnv, in_=src_ap, func=AF.Copy, scale=st2),
               reads=list(src_keys) + [("st2", slot)], writes=[("xn", slot)])
        pt, ptkey = tbank()
        ptv = pt[:].rearrange("p (k t) -> p k t", k=8)
        for kc in range(8):
            sch.op(pe, lambda kc=kc: T.transpose(ptv[:, kc, :], xnv[:, kc * 128:(kc + 1) * 128], identb[:]),
                   reads=[("xn", slot), "ident"], writes=[ptkey], sig=(kc == 7))
        gb = gvecT[:, gcol:gcol + 8].unsqueeze(2).broadcast_to([128, 8, 128])
        sch.op(dve, lambda: V.tensor_tensor(out=dst_fn(), in0=ptv, in1=gb, op=ALU.mult),
               reads=[ptkey] + GV, writes=dst_keys)

    def mm_acc(out_ap, pairs, okey, rkeys):
        n = len(pairs)
        for i, (l, r) in enumerate(pairs):
            sch.op(pe, lambda l=l, r=r, i=i: T.matmul(out_ap, lhsT=l, rhs=r, start=(i == 0), stop=(i == n - 1)),
                   reads=rkeys, writes=[okey], sig=(i == n - 1))

    def htkeys(blk):
        return [("hT", blk * 4 + i) for i in range(4)]

    for b in range(nseq):
        for tt in range(NT):
            slot = tt % 2
            xsv = xs[:, slot * 1024:(slot + 1) * 1024]
            sch.dma(sp, xsv, x_d[b, tt * 128:(tt + 1) * 128, :], writes=[("xs", slot)], grp=("xs", slot))
            rms_to_T(xsv, [("xs", slot)], 0, lambda tt=tt: hT[:, :, tt * 128:(tt + 1) * 128], [("hT", tt)], slot)

        if stage == 1:
            dump("hT", T_hT[:], [("hT", t) for t in range(NT)])
        checkpoint(1)
        WinB = T_W[:, 0:5376].rearrange("p (k n) -> p k n", k=8)
        Wuq = T_W[:, 5376:7680].rearrange("p (k n) -> p k n", k=3)
        WuqS = T_W[:, 7680:9984].rearrange("p (k n) -> p k n", k=3)
        Wuk = T_W[:, 9984:11008].rearrange("p (k n) -> p k n", k=2)
        Wuv = T_W[:, 11008:12032].rearrange("p (k n) -> p k n", k=2)
        for nm, lo, hi in (("W0", 0, 5376), ("W1", 5376, 7680), ("W2", 7680, 9984), ("W3", 9984, 11008), ("W4", 11008, 12032)):
            sch.set_alias(nm, lo, hi)
        wload(WinB, win_b[:, C_CQ:C_CQ + 672], "W0", "win", 8)
        wload(Wuq, wuq_b, "W1", "wuq", 3)
        wuq_b3 = wuq_b.rearrange("(k p) (h c) -> p k h c", p=128, c=96)
        WuqS4 = WuqS.rearrange("p k (h c) -> p k h c", c=96)
        for kc in range(3):
            sch.dma(sp, WuqS4[:, kc, :, 0:64], wuq_b3[:, kc, :, 0:64], reads=[("w", "wuq")], writes=["W2"], grp="W2")
            sch.dma(sp, WuqS4[:, kc, :, 64:80], wuq_b3[:, kc, :, 80:96], reads=[("w", "wuq")], writes=["W2"], grp="W2")
            tk = sch.dma(sp, WuqS4[:, kc, :, 80:96], wuq_b3[:, kc, :, 64:80], reads=[("w", "wuq")], writes=["W2"], grp="W2")
        wload(Wuk, wuk_b, "W3", "wuk", 2)
        wload(Wuv, wuv_b, "W4", "wuv", 2)
        W2K = ["W2"]

        cqnT = T_A[:, 0:6144].rearrange("p (k t) -> p k t", k=3)
        ckvnT = T_A[:, 6144:10240].rearrange("p (k t) -> p k t", k=2)
        KhT = [bigb[:, 0:2048], bigb[:, 2048:4096]]
        QhT = [bigb[:, 4096:6144], bigb[:, 6144:8192]]
        cosT = bigb[:, 8192:10240]
        sinT = bigb[:, 10240:12288]
        zst = [bigb[:, 12288:12960], bigb[:, 12960:13632]]
        Vaug = T_V[:].rearrange("p (t h c) -> p t h c", t=16, h=8)
        otok = T_C[:].rearrange("p (t c) -> p t c", t=16)

        sch.dma(sp, posi[:], pos_d[b:b + 1, :].partition_broadcast(128), writes=["posi"], grp="posi")
        sch.op(dve, lambda: V.tensor_copy(out=posf[64:96, :], in_=posi[64:96, :]), reads=["posi"], writes=["posf"])
        TWO_PI = 2.0 * math.pi
        for (tab, offc, key) in ((cosT, K_RP + 2, "cosT"), (sinT, K_RP + 1, "sinT")):
            for q4 in range(2):
                seg = slice(q4 * 1024, (q4 + 1) * 1024)
                yv = tmpf[64:96, :]
                ki = posi[64:96, 0:1024]
                kf = xs[64:96, 0:1024]
                mv = xs[64:96, 1024:2048]
                sch.op(dve, lambda: V.tensor_scalar(out=yv, in0=posf[64:96, seg], scalar1=cst[64:96, K_RP:K_RP + 1],
                                                    scalar2=cst[64:96, offc:offc + 1], op0=ALU.mult, op1=ALU.add),
                       reads=["posf", "cst"], writes=["tmpf"])
                sch.op(dve, lambda: V.tensor_scalar(out=ki, in0=yv, scalar1=1.0 / TWO_PI, scalar2=0.5, op0=ALU.mult, op1=ALU.add),
                       reads=["tmpf", "posf"], writes=["posi"])
                sch.op(dve, lambda: V.tensor_copy(out=kf, in_=ki), reads=["posi"], writes=[("xs", 0)])
                sch.op(dve, lambda: V.scalar_tensor_tensor(out=yv, in0=kf, scalar=-TWO_PI, in1=yv, op0=ALU.mult, op1=ALU.add),
                       reads=[("xs", 0), "tmpf"], writes=["tmpf"])
                sch.op(dve, lambda: V.tensor_scalar(out=mv, in0=yv, scalar1=-math.pi, scalar2=TWO_PI, op0=ALU.is_lt, op1=ALU.mult),
                       reads=["tmpf"], writes=[("xs", 1)])
                sch.op(dve, lambda: V.tensor_tensor(out=yv, in0=yv, in1=mv, op=ALU.add), reads=["tmpf", ("xs", 1)], writes=["tmpf"])
                sch.op(act, lambda: A.activation(out=tab[64:96, seg], in_=yv, func=AF.Sin), reads=["tmpf"], writes=[key])

        sch.op(dve, lambda: V.memset(Vaug[:, :, :, 64:65], 1.0), writes=["Vaug"])

        for tt in range(NT):
            slot = tt % 2
            pz0, k0 = bank("z0", [0, 1])
            pz1, k1 = bank("z1", [2, 3])
            mm_acc(pz0[:, 0:384], [(hT[:, kc, tt * 128:(tt + 1) * 128], WinB[:, kc, 0:384]) for kc in range(8)],
                   k0, [("hT", tt), "W0"])
            mm_acc(pz1[:, 0:288], [(hT[:, kc, tt * 128:(tt + 1) * 128], WinB[:, kc, 384:672]) for kc in range(8)],
                   k1, [("hT", tt), "W0"])
            st = stat[:, 16 + slot * 4:16 + slot * 4 + 2]
            junk = xn[:, slot * 1024:slot * 1024 + 384]
            sch.op(act, lambda: A.activation(out=junk, in_=pz0[:, 0:384], func=AF.Square, scale=384.0 ** -0.5,
                                             accum_out=st[:, 0:1]), reads=[k0], writes=[("xn", slot), ("bst", slot)])
            sch.op(act, lambda: A.activation(out=junk[:, 0:256], in_=pz1[:, 0:256], func=AF.Square, scale=1.0 / 16.0,
                                             accum_out=st[:, 1:2]), reads=[k1], writes=[("xn", slot), ("bst", slot)])
            sch.op(act, lambda: A.activation(out=st, in_=st, func=AF.Sqrt, bias=epsb[:], scale=1.0),
                   reads=[("bst", slot), "epsb"], writes=[("bst", slot)])
            sch.op(dve, lambda: V.reciprocal(out=st, in_=st), reads=[("bst", slot)], writes=[("bst", slot)])
            z = zst[slot]
            sch.op(act, lambda: A.activation(out=z[:, 0:384], in_=pz0[:, 0:384], func=AF.Copy, scale=st[:, 0:1]),
                   reads=[k0, ("bst", slot)], writes=[("zst", slot)])
            sch.op(dve, lambda: V.tensor_scalar(out=z[:, 384:640], in0=pz1[:, 0:256], scalar1=st[:, 1:2], scalar2=None,
                                                op0=ALU.mult), reads=[k1, ("bst", slot)], writes=[("zst", slot)])
            ks = kst[:, slot * 96:(slot + 1) * 96]
            sch.op(act, lambda: A.copy(out=z[:, 640:672], in_=pz1[:, 256:288]), reads=[k1], writes=[("zst", slot)])
            pt, ptkey = tbank()
            ptv = pt[:].rearrange("p (k t) -> p k t", k=8)
            for j in range(5):
                sch.op(pe, lambda j=j: T.transpose(ptv[:, j, :], z[:, j * 128:(j + 1) * 128], identb[:]),
                       reads=[("zst", slot), "ident"], writes=[ptkey], sig=False)
            sch.op(dve, lambda: V.tensor_copy(out=ks[:, 64:96], in_=z[:, 640:672]), reads=[("zst", slot)], writes=[("kst", slot)])
            sch.op(pe, lambda: T.transpose(ptv[0:96, 5, :], ks, identb[:]), reads=[("kst", slot), "ident"], writes=[ptkey])
            sch.op(dve, lambda: V.tensor_copy(out=ks[:, 64:80], in_=z[:, 656:672]), reads=[("zst", slot), ptkey], writes=[("kst", slot)])
            sch.op(dve, lambda: V.tensor_copy(out=ks[:, 80:96], in_=z[:, 640:656]), reads=[("zst", slot)], writes=[("kst", slot)])
            sch.op(pe, lambda: T.transpose(ptv[0:96, 6, :], ks, identb[:]), reads=[("kst", slot), "ident"], writes=[ptkey])
            tsl = slice(tt * 128, (tt + 1) * 128)
            gq = gvecT[:, 24:27].unsqueeze(2).broadcast_to([128, 3, 128])
            gk = gvecT[:, 27:29].unsqueeze(2).broadcast_to([128, 2, 128])
            sch.op(dve, lambda: V.tensor_tensor(out=cqnT[:, :, tsl], in0=ptv[:, 0:3, :], in1=gq, op=ALU.mult),
                   reads=[ptkey] + GV, writes=[("cqnT", tt)])
            sch.op(dve, lambda: V.tensor_tensor(out=ckvnT[:, :, tsl], in0=ptv[:, 3:5, :], in1=gk, op=ALU.mult),
                   reads=[ptkey] + GV, writes=[("ckvnT", tt)])
            t1 = tmpf[64:96, 0:128]
            t2 = tmpf[64:96, 512:640]
            sch.op(dve, lambda: V.tensor_tensor(out=t1, in0=ptv[64:96, 5, :], in1=cosT[64:96, tsl], op=ALU.mult),
                   reads=[ptkey, "cosT"], writes=["tmpf"])
            sch.op(dve, lambda: V.tensor_tensor(out=t2, in0=ptv[64:96, 6, :], in1=sinT[64:96, tsl], op=ALU.mult),
                   reads=[ptkey, "sinT"], writes=["tmpf"])
            sch.op(dve, lambda: V.tensor_tensor(out=KhT[0][64:96, tsl], in0=t1, in1=t2, op=ALU.add),
                   reads=["tmpf"], writes=[("KhT", 0)])
            sch.op(act, lambda: A.copy(out=KhT[1][64:96, tsl], in_=KhT[0][64:96, tsl]),
                   reads=[("KhT", 0)], writes=[("KhT", 1)])
            pv, kv = bank("z0", [0, 1])
            mm_acc(pv[:, :], [(ckvnT[:, kc, tsl], Wuv[:, kc, :]) for kc in range(2)], kv, [("ckvnT", tt), "W4"])
            sch.op(act, lambda: A.copy(out=Vaug[:, tt, :, 0:64], in_=pv[:].rearrange("p (h c) -> p h c", h=8)),
                   reads=[kv], writes=["Vaug"])

        if stage == 2:
            dump("cq", T_A[:, 0:10240], [("cqnT", t) for t in range(NT)] + [("ckvnT", t) for t in range(NT)])
            dump("kh", bigb[:, 0:2048], [("KhT", 0)])
            dump("cos", bigb[:, 8192:12288], ["cosT", "sinT"])
            dump("va", T_V[:], ["Vaug"])
        checkpoint(2)
        ALLCQ = [("cqnT", t) for t in range(NT)]
        ALLCKV = [("ckvnT", t) for t in range(NT)]
        att_scale = 96.0 ** -0.5
        def produce_kq(h):
            s_ = h % 2
            for blk in range(4):
                bs = slice(blk * 512, (blk + 1) * 512)
                pk, kk = bank("kq", [6, 7])
                mm_acc(pk[0:64, :], [(Wuk[:, kc, h * 64:(h + 1) * 64], ckvnT[:, kc, bs]) for kc in range(2)], kk,
                       ALLCKV[blk * 4:blk * 4 + 4] + ["W3"])
                sch.op(dve, lambda: V.tensor_copy(out=KhT[s_][0:64, bs], in_=pk[0:64, :]), reads=[kk], writes=[("KhT", s_)])
                pq, kq = bank("kq", [6, 7])
                pqs, kqs = bank("kq", [6, 7])
                mm_acc(pq[0:96, :], [(Wuq[:, kc, h * 96:(h + 1) * 96], cqnT[:, kc, bs]) for kc in range(3)], kq,
                       ALLCQ[blk * 4:blk * 4 + 4] + ["W1"])
                mm_acc(pqs[0:96, :], [(WuqS[:, kc, h * 96:(h + 1) * 96], cqnT[:, kc, bs]) for kc in range(3)], kqs,
                       ALLCQ[blk * 4:blk * 4 + 4] + W2K)
                sch.op(dve, lambda: V.tensor_copy(out=QhT[s_][0:64, bs], in_=pq[0:64, :]), reads=[kq], writes=[("QhT", s_)])
                t1 = tmpf[64:96, 0:512]
                t2 = tmpf[64:96, 512:1024]
                sch.op(dve, lambda: V.tensor_tensor(out=t1, in0=pq[64:96, :], in1=cosT[64:96, bs], op=ALU.mult),
                       reads=[kq, "cosT"], writes=["tmpf"])
                sch.op(dve, lambda: V.tensor_tensor(out=t2, in0=pqs[64:96, :], in1=sinT[64:96, bs], op=ALU.mult),
                       reads=[kqs, "sinT"], writes=["tmpf"])
                sch.op(pool, lambda: G.tensor_tensor(out=QhT[s_][64:96, bs], in0=t1, in1=t2, op=ALU.add),
                       reads=["tmpf"], writes=[("QhT", s_)])

        def attend(h):
            s_ = h % 2
            for qb in range(4):
                qs = slice(qb * 512, (qb + 1) * 512)
                pO, kO = bank("pO", [4, 5])
                pOv = pO[:, 0:260].rearrange("p (j c) -> p j c", j=4)

                def pv(kt, P_, ps_):
                    for j in range(4):
                        sch.op(pe, lambda j=j: T.matmul(pOv[:, j, :], lhsT=P_[:, j * 128:(j + 1) * 128], rhs=Vaug[:, kt, h, :],
                                                        start=(kt == 0 and j == 0), stop=(kt == 15), skip_group_check=True),
                               reads=[("PT", ps_), "Vaug"], writes=[kO], sig=(j == 3))

                prev = None
                for kp in range(8):
                    ip = rot.get("pspair", 0) % 2
                    rot["pspair"] = rot.get("pspair", 0) + 1
                    pSS = pdbl[ip]
                    kSS = [("pb", 2 * ip), ("pb", 2 * ip + 1)]
                    for u in range(2):
                        kt = 2 * kp + u
                        sch.op(pe, lambda: T.matmul(pSS[:, u * 512:(u + 1) * 512], lhsT=KhT[s_][0:96, kt * 128:(kt + 1) * 128],
                                                    rhs=QhT[s_][0:96, qs], start=True, stop=True),
                               reads=[("KhT", s_), ("QhT", s_)], writes=[kSS[u]])
                    ps_ = rot.get("ptslot", 0) % 3
                    rot["ptslot"] = rot.get("ptslot", 0) + 1
                    PP = PTt[:, ps_ * 1024:(ps_ + 1) * 1024]
                    sch.op(act, lambda: A.activation(out=PP, in_=pSS[:, :], func=AF.Exp, scale=att_scale),
                           reads=kSS, writes=[("PT", ps_)])
                    if prev is not None:
                        for a_ in prev:
                            pv(*a_)
                    prev = [(2 * kp + u, PP[:, u * 512:(u + 1) * 512], ps_) for u in range(2)]
                for a_ in prev:
                    pv(*a_)
                rec = stat[:, 32 + (qb % 2) * 4:36 + (qb % 2) * 4]
                sch.op(dve, lambda: V.reciprocal(out=rec, in_=pOv[:, :, 64]), reads=[kO], writes=[("rec", qb % 2)])
                sch.op(dve, lambda: V.tensor_tensor(out=otok[:, qb * 4:(qb + 1) * 4, h * 64:(h + 1) * 64], in0=pOv[:, :, 0:64],
                                                    in1=rec.unsqueeze(2).broadcast_to([128, 4, 64]), op=ALU.mult),
                       reads=[kO, ("rec", qb % 2)], writes=[("otok", qb)])

        produce_kq(0)
        for h in range(8):
            if h + 1 < 8:
                produce_kq(h + 1)
            attend(h)

        def flush_oT(br):
            for blk in range(4):
                sl = blk % 2
                sv = stg[:, sl * 512:(sl + 1) * 512]
                for j in range(4):
                    pt, ptkey = tbank()
                    ptv = pt[:].rearrange("p (k t) -> p k t", k=8)
                    for i in range(4):
                        sch.op(pe, lambda i=i: T.transpose(ptv[:, i, :], otok[:, blk * 4 + i, j * 128:(j + 1) * 128], identb[:]),
                               reads=[("otok", blk), "ident"], writes=[ptkey], sig=(i == 3))
                    st_ = stg[:, sl * 512:(sl + 1) * 512]
                    if j % 2 == 0:
                        sch.op(act, lambda: A.copy(out=st_.rearrange("p (i t) -> p i t", i=4), in_=ptv[:, 0:4, :]),
                               reads=[ptkey], writes=[("stg", sl)])
                    else:
                        sch.op(dve, lambda: V.tensor_copy(out=st_.rearrange("p (i t) -> p i t", i=4), in_=ptv[:, 0:4, :]),
                               reads=[ptkey], writes=[("stg", sl)])
                    sch.dma(sp, oT_d[br, :, j, blk * 512:(blk + 1) * 512], st_, reads=[("stg", sl)],
                            writes=[("oTd", br, blk, j)], grp=("stg", sl))
                    sl = (sl + 1) % 2

        if stage == 3:
            dump("otok", T_C[:], [("otok", q) for q in range(4)])
            dump("kh", bigb[:, 0:8192], [("KhT", 0), ("KhT", 1), ("QhT", 0), ("QhT", 1)])
        checkpoint(3)
        flush_oT(0)
        sch.barrier()

        checkpoint(4)
        WinU = T_W[:, 0:4096].rearrange("p (k n) -> p k n", k=8)
        WinVG = T_W[:, 4096:8448].rearrange("p (k n) -> p k n", k=8)
        WqBD = T_W[:, 8448:8960].rearrange("p (j n) -> p j n", j=4)
        WkBD = T_W[:, 8960:9472].rearrange("p (j n) -> p j n", j=4)
        for nm, lo, hi in (("W0", 0, 4096), ("W1", 4096, 8448), ("W1b", 4096, 8448), ("W3", 8448, 8960), ("W4", 8960, 9472)):
            sch.set_alias(nm, lo, hi)
        wload(WinU, win_b[:, C_MU:C_MU + 512], "W0", "win", 8)
        sch.dma(sp, WinVG[:, :, 0:512], win_b[:, C_MV:C_MV + 512].rearrange("(k p) n -> p k n", p=128),
                reads=[("w", "win")], writes=["W1"], grp="W1")
        sch.dma(sp, WinVG[:, :, 512:544], win_b[:, C_MG:C_MG + 32].rearrange("(k p) n -> p k n", p=128),
                reads=[("w", "win")], writes=["W1"], grp="W1")
        sch.op(dve, lambda: V.memset(T_W[:, 8448:9472], 0.0), writes=["W3", "W4"])
        for (Wt, src, key) in ((WqBD, wq_b, "W3"), (WkBD, wk_b, "W4")):
            s4 = src.rearrange("(j r c) d -> r c j d", j=4, r=2)
            for r in range(2):
                sch.dma(sp, Wt[r * 64:(r + 1) * 64, :, r * 64:(r + 1) * 64], s4[r], reads=[("w", "wq"), ("w", "wk")],
                        writes=[key], grp=key)
        uT = T_big[:].rearrange("p (c t) -> p c t", c=4)
        Hh = T_big[:, 0:8192].rearrange("p (t c) -> p t c", t=16)
        cT = T_C[:].rearrange("p (c t) -> p c t", c=4)
        vaug = Vaug
        accf = T_A[:].bitcast(F32)
        sch.op(dve, lambda: V.memset(uT[:, :, 14:16], 0.0), writes=["uTpad"])
        sch.op(dve, lambda: V.memset(uT[:, :, 2064:2066], 0.0), writes=["uTpad"])
        sch.op(dve, lambda: V.memset(vaug[:, :, :, 64:65], 1.0), writes=["Vaug"])
        checkpoint(4.1)
        for ct in range(4):
            for blk in range(4):
                pu, ku = bank("cu", [0, 1])
                mm_acc(pu[:, :], [(WinU[:, kc, ct * 128:(ct + 1) * 128], hT[:, kc, blk * 512:(blk + 1) * 512]) for kc in range(8)],
                       ku, htkeys(blk) + ["W0"])
                sch.op(act, lambda: A.copy(out=uT[:, ct, 16 + blk * 512:16 + (blk + 1) * 512], in_=pu[:, :]),
                       reads=[ku], writes=[("uT", ct)])
        checkpoint(4.2)
        for tt in range(NT):
            pv, kv = bank("cv", [2, 3])
            pg, kg = bank("cg", [4, 5])
            mm_acc(pv[:, :], [(hT[:, kc, tt * 128:(tt + 1) * 128], WinVG[:, kc, 0:512]) for kc in range(8)], kv, [("hT", tt), "W1"])
            mm_acc(pg[:, 0:32], [(hT[:, kc, tt * 128:(tt + 1) * 128], WinVG[:, kc, 512:544]) for kc in range(8)], kg,
                   [("hT", tt), "W1"])
            sch.op(act, lambda: A.copy(out=vaug[:, tt, :, 0:64], in_=pv[:].rearrange("p (h c) -> p h c", h=8)),
                   reads=[kv], writes=["Vaug"])
            Gt = gtmp[:, 0:32]
            sch.op(dve, lambda: V.tensor_tensor(out=Gt, in0=pg[:, 0:32], in1=gbias_bc[:], op=ALU.add),
                   reads=[kg, "gbias"], writes=["Gt"])
            G4 = Gt.rearrange("p (a h) -> p a h", a=4)
            Fv = gtmp[:, 0:32].rearrange("p (d a h) -> p d a h", d=2, a=2)[:, :, 1, :]
            Iv = gtmp[:, 0:32].rearrange("p (d a h) -> p d a h", d=2, a=2)[:, :, 0, :]
            sp_ = gtmp[:, 32:48].rearrange("p (d h) -> p d h", d=2)
            sch.op(act, lambda: A.activation(out=sp_, in_=Fv, func=AF.Exp, scale=-1.0), reads=["Gt"], writes=["spg"])
            sch.op(act, lambda: A.activation(out=sp_, in_=sp_, func=AF.Ln, bias=oneb[:], scale=1.0), reads=["spg", "oneb"], writes=["spg"])
            pc, kc_ = bank("cc", [0, 1])
            sch.op(pe, lambda: T.matmul(pc[:, 0:8], lhsT=cst[:, K_TF:K_TF + 128], rhs=gtmp[:, 32:40], start=True, stop=True),
                   reads=["spg", "cst"], writes=[kc_], sig=False)
            sch.op(pe, lambda: T.matmul(pc[:, 8:16], lhsT=cst[:, K_TB:K_TB + 128], rhs=gtmp[:, 40:48], start=True, stop=True),
                   reads=["spg", "cst"], writes=[kc_], sig=False)
            sch.op(pe, lambda: T.matmul(pc[:, 16:32], lhsT=cst[:, K_I0:K_I0 + 128], rhs=gtmp[:, 32:48], start=True, stop=True),
                   reads=["spg", "cst"], writes=[kc_], sig=False)
            sch.op(pe, lambda: T.matmul(pc[:, 32:48], lhsT=cst[:, K_I1:K_I1 + 128], rhs=gtmp[:, 32:48], start=True, stop=True),
                   reads=["spg", "cst"], writes=[kc_])
            tsl16 = slice(tt * 16, (tt + 1) * 16)
            ic = gtmp[:, 48:64].rearrange("p (d h) -> p d h", d=2)
            sch.op(dve, lambda: V.tensor_tensor(out=ic, in0=Iv, in1=pc[:, 0:16].rearrange("p (d h) -> p d h", d=2), op=ALU.add),
                   reads=["Gt", kc_], writes=["ic"])
            sch.op(act, lambda: A.activation(out=gA[:, tsl16], in_=gtmp[:, 48:64], func=AF.Exp), reads=["ic"], writes=[("gA", tt)])
            sch.op(act, lambda: A.activation(out=gE[:, tsl16], in_=pc[:, 0:16], func=AF.Exp, scale=-1.0), reads=[kc_], writes=[("gE", tt)])
            sch.op(act, lambda: A.activation(out=gDec[:, tt * 32:(tt + 1) * 32], in_=pc[:, 16:48], func=AF.Exp, scale=-1.0),
                   reads=[kc_], writes=[("gDec", tt)])
            sch.op(dve, lambda: V.tensor_tensor(out=gWt[0:64, tsl16], in0=gA[0:64, tsl16], in1=gDec[0:64, tt * 32:tt * 32 + 16], op=ALU.mult),
                   reads=[("gA", tt), ("gDec", tt)], writes=[("gW", tt)])
            sch.op(dve, lambda: V.tensor_tensor(out=gWt[64:128, tsl16], in0=gA[64:128, tsl16], in1=gDec[64:128, tt * 32 + 16:tt * 32 + 32],
                                                op=ALU.mult), reads=[("gA", tt), ("gDec", tt)], writes=[("gW", tt)])
        checkpoint(4.5)
        for ct in range(4):
            eng, E_ = dve, V
            acc = accf[:, (ct % 2) * 2048:(ct % 2 + 1) * 2048]
            ak = ("acc", ct % 2)
            sch.op(eng, lambda: E_.tensor_scalar(out=acc, in0=uT[:, ct, 14:14 + 2048], scalar1=convw[:, ct * 5:ct * 5 + 1], scalar2=None,
                                                 op0=ALU.mult), reads=[("uT", ct), "uTpad", "convw"], writes=[ak])
            for j in range(1, 5):
                sch.op(eng, lambda j=j: E_.scalar_tensor_tensor(out=acc, in0=uT[:, ct, 14 + j:14 + j + 2048],
                                                                scalar=convw[:, ct * 5 + j:ct * 5 + j + 1], in1=acc,
                                                                op0=ALU.mult, op1=ALU.add),
                       reads=[("uT", ct), "uTpad", "convw", ak], writes=[ak])
            sch.op(act, lambda: A.activation(out=cT[:, ct, :], in_=acc, func=AF.Silu), reads=[ak], writes=[("cT", ct)])
        sch.barrier()

        if stage == 5:
            dump("cT", T_C[:], [("cT", c_) for c_ in range(4)])
            dump("ga", gA[:], [("gA", t) for t in range(NT)])
            dump("ge", gE[:], [("gE", t) for t in range(NT)])
            dump("gw", gWt[:], [("gW", t) for t in range(NT)])
            dump("gd", gDec[:], [("gDec", t) for t in range(NT)])
        checkpoint(5)
        def tA(off, n):
            return T_A[:, off:off + n]
        qTz = [[[tA(((d * 2 + sl) * 2 + r) * 512, 512).rearrange("p (j t) -> p j t", j=4) for r in range(2)] for sl in range(2)]
               for d in range(2)]
        kTf = [tA(4096 + d * 512, 512).rearrange("p (j t) -> p j t", j=4) for d in range(2)]
        ktkz = [[tA(5120 + (d * 2 + r0) * 512, 512) for r0 in range(2)] for d in range(2)]
        PTm = [[tA(7168 + (d * 2 + r0) * 512, 512) for r0 in range(2)] for d in range(2)]
        vwf = [tA(9216 + d * 520, 520).rearrange("p (h c) -> p h c", h=8) for d in range(2)]
        vaf = [[T_W[:, 11288 + (d * 2 + sl) * 520:11288 + (d * 2 + sl + 1) * 520].rearrange("p (h c) -> p h c", h=8)
                for sl in range(2)] for d in range(2)]
        Cbf = [T_W[:, 10768 + d * 260:10768 + (d + 1) * 260].rearrange("p (j c) -> p j c", j=4) for d in range(2)]
        Cst = [T_W[:, 9728 + d * 520:9728 + (d + 1) * 520].bitcast(F32).rearrange("p (j c) -> p j c", j=4) for d in range(2)]
        pm = [cst[:, K_I0:K_I0 + 1], cst[:, K_I1:K_I1 + 1]]
        pm8 = stat[:, 28:30]
        sch.op(dve, lambda: V.tensor_scalar(out=pm8[:, 0:1], in0=pm[0], scalar1=0.125, scalar2=None, op0=ALU.mult), reads=["cst"], writes=["pm8"])
        sch.op(dve, lambda: V.tensor_scalar(out=pm8[:, 1:2], in0=pm[1], scalar1=0.125, scalar2=None, op0=ALU.mult), reads=["cst", "pm8"], writes=["pm8"])
        for d in range(2):
            sch.op(dve, lambda d=d: V.memset(Cst[d], 0.0), writes=[("Cst", d)])
            sch.op(dve, lambda d=d: V.memset(Cbf[d], 0.0), writes=[("Cbf", d)])
            for r0 in range(2):
                sch.op(dve, lambda d=d, r0=r0: V.memset(PTm[d][r0], 0.0), writes=[("PTm", d, r0)])

        pcb = [[(pbk[6], ("pt", 0)), (pbk[7], ("pt", 1))], [(pbk[1], ("pb", 1)), (pbk[3], ("pb", 3))]]

        def geom(d, i):
            c = i if d == 0 else 31 - i
            return c, c // 2, c % 2, (i // 2) % 2

        def prep(d, i):
            c, m, r0, sl = geom(d, i)
            rs = slice(r0 * 64, (r0 + 1) * 64)
            tsl = slice(m * 128, (m + 1) * 128)
            col = slice(m * 16 + d * 8, m * 16 + d * 8 + 8)
            if i % 2 == 0:
                pq, kq = bank("mj", [2])
                for j in range(4):
                    sch.op(pe, lambda j=j: T.matmul(pq[:, j * 128:(j + 1) * 128], lhsT=WqBD[:, j, :], rhs=cT[:, j, tsl], start=True, stop=True),
                           reads=[("cT", j), "W3"], writes=[kq], sig=(j == 3))
                pq3 = pq[:].rearrange("p (j t) -> p j t", j=4)
                for r in range(2):
                    sch.op(act, lambda r=r: A.activation(out=qTz[d][sl][r], in_=pq3, func=AF.Copy, scale=pm[r]),
                           reads=[kq, "cst"], writes=[("qTz", d, sl, r)])
                pk, kk = bank("mj", [2])
                for j in range(4):
                    sch.op(pe, lambda j=j: T.matmul(pk[:, j * 128:(j + 1) * 128], lhsT=WkBD[:, j, :], rhs=cT[:, j, tsl], start=True, stop=True),
                           reads=[("cT", j), "W4"], writes=[kk], sig=(j == 3))
                sch.op(act, lambda: A.mul(out=kTf[d], in_=pk[:].rearrange("p (j t) -> p j t", j=4), mul=0.125),
                       reads=[kk], writes=[("kTf", d)])
                pk2, kk2 = bank("mj", [2])
                for j in range(4):
                    sch.op(pe, lambda j=j: T.matmul(pk2[:, j * 128:(j + 1) * 128], lhsT=cT[:, j, tsl], rhs=WkBD[:, j, :], start=True, stop=True),
                           reads=[("cT", j), "W4"], writes=[kk2], sig=(j == 3))
                for rr in range(2):
                    sch.op(act, lambda rr=rr: A.activation(out=ktkz[d][rr], in_=pk2[:, :], func=AF.Copy, scale=pm8[:, rr:rr + 1]),
                           reads=[kk2, "pm8"], writes=[("ktkz", d, rr)])
                sch.op(pool, lambda: G.tensor_tensor(out=vaf[d][sl], in0=vaug[:, m, :, :],
                                                     in1=gA[:, col].unsqueeze(2).broadcast_to([128, 8, 65]), op=ALU.mult),
                       reads=["Vaug", ("gA", m)], writes=[("vaf", d, sl)])
                sch.op(pool, lambda: G.tensor_tensor(out=vwf[d], in0=vaug[:, m, :, :],
                                                     in1=gWt[:, col].unsqueeze(2).broadcast_to([128, 8, 65]), op=ALU.mult),
                       reads=["Vaug", ("gW", m)], writes=[("vwf", d)])
            pS, kS = pbk[0], ("pb", 0)
            pSv = pS[:].rearrange("p (h t) -> p h t", h=8)
            for h in range(8):
                j, r = h // 2, h % 2
                sch.op(pe, lambda h=h, j=j, r=r: T.matmul(pSv[rs, h, :], lhsT=kTf[d][:, j, rs], rhs=qTz[d][sl][r][:, j, rs],
                                                         start=True, stop=True),
                       reads=[("kTf", d), ("qTz", d, sl, r)], writes=[kS], sig=(h == 7))
            mk = cst[rs, (K_MF if d == 0 else K_MB):(K_MF if d == 0 else K_MB) + 64]
            sch.op(dve, lambda: V.tensor_tensor(out=PTm[d][r0][rs].rearrange("p (h t) -> p h t", h=8), in0=pSv[rs],
                                                in1=mk.unsqueeze(1).broadcast_to([64, 8, 64]), op=ALU.mult),
                   reads=[kS, "cst"], writes=[("PTm", d, r0)])
            pC, kC = pcb[d][i % 2]
            pCv = pC[:, 0:260].rearrange("p (j c) -> p j c", j=4)
            for h in range(8):
                j, r = h // 2, h % 2
                pr = slice(r * 64, (r + 1) * 64)
                sch.op(pe, lambda h=h, j=j, pr=pr: T.matmul(pCv[pr, j, :], lhsT=ktkz[d][r0][:, h * 64:(h + 1) * 64], rhs=vwf[d][:, h, :],
                                                           start=True, stop=True),
                       reads=[("ktkz", d, r0), ("vwf", d)], writes=[kC], sig=(h == 7))

        def recur(d, i):
            c, m, r0, sl = geom(d, i)
            rs = slice(r0 * 64, (r0 + 1) * 64)
            col = slice(m * 16 + d * 8, m * 16 + d * 8 + 8)
            pN0, kN0 = pbk[4], ("pb", 4)
            pN1, kN1 = pbk[5], ("pb", 5)
            for h in range(8):
                j, r = h // 2, h % 2
                pn, kn = (pN0, kN0) if h < 4 else (pN1, kN1)
                o_ = pn[rs, (h % 4) * 65:(h % 4 + 1) * 65]
                sch.op(pe, lambda h=h, o_=o_: T.matmul(o_, lhsT=PTm[d][r0][:, h * 64:(h + 1) * 64], rhs=vaf[d][sl][:, h, :],
                                                       start=True, stop=False),
                       reads=[("PTm", d, r0), ("vaf", d, sl)], writes=[kn], sig=False)
                sch.op(pe, lambda h=h, j=j, r=r, o_=o_: T.matmul(o_, lhsT=qTz[d][sl][r][:, j, rs], rhs=Cbf[d][:, j, :], start=False, stop=True),
                       reads=[("qTz", d, sl, r), ("Cbf", d)], writes=[kn], sig=(h % 4 == 3))
            pC, kC = pcb[d][i % 2]
            pCv = pC[:, 0:260].rearrange("p (j c) -> p j c", j=4)
            dcy = gDec[:, m * 32 + r0 * 16 + d * 8:m * 32 + r0 * 16 + d * 8 + 8]
            dv4 = dcy.rearrange("p (j r) -> p j r", r=2)
            for r in range(2):
                pr = slice(r * 64, (r + 1) * 64)
                sch.op(dve, lambda r=r, pr=pr: V.tensor_tensor(out=Cst[d][pr], in0=Cst[d][pr],
                                                              in1=dv4[pr, :, r].unsqueeze(2).broadcast_to([64, 4, 65]), op=ALU.mult),
                       reads=[("gDec", m), ("Cst", d)], writes=[("Cst", d)])
            sch.op(dve, lambda: V.tensor_tensor(out=Cst[d], in0=Cst[d], in1=pCv, op=ALU.add), reads=[kC, ("Cst", d)], writes=[("Cst", d)])
            sch.op(act, lambda: A.copy(out=Cbf[d], in_=Cst[d]), reads=[("Cst", d)], writes=[("Cbf", d)])
            ecol = gE[rs, col]
            dn = stat[rs, 64 + d * 16:72 + d * 16]
            dn2 = stat[rs, 72 + d * 16:80 + d * 16]
            for hb, (pn, kn) in enumerate(((pN0, kN0), (pN1, kN1))):
                pnv = pn[rs, 0:260].rearrange("p (h c) -> p h c", h=4)
                sch.op(act, lambda pnv=pnv, hb=hb: A.activation(out=dn[:, hb * 4:(hb + 1) * 4], in_=pnv[:, :, 64], func=AF.Abs),
                       reads=[kn], writes=[("dn", d)])
            sch.op(dve, lambda: V.reciprocal(out=dn, in_=dn), reads=[("dn", d)], writes=[("dn", d)])
            sch.op(dve, lambda: V.tensor_tensor(out=dn, in0=dn, in1=ecol, op=ALU.min), reads=[("dn", d), ("gE", m)], writes=[("dn", d)])
            first = (i < 16)
            if first:
                hn = Hh[rs, m, :].rearrange("p (h c) -> p h c", h=8)
                hk = ("H", m)
            else:
                hn = tmpf[rs, d * 512:(d + 1) * 512].rearrange("p (h c) -> p h c", h=8)
                hk = ("hn", d)
            for hb, (pn, kn) in enumerate(((pN0, kN0), (pN1, kN1))):
                pnv = pn[rs, 0:260].rearrange("p (h c) -> p h c", h=4)
                sch.op(dve, lambda pnv=pnv, hb=hb: V.tensor_tensor(out=hn[:, hb * 4:(hb + 1) * 4, :], in0=pnv[:, :, 0:64],
                                                                  in1=dn[:, hb * 4:(hb + 1) * 4].unsqueeze(2).broadcast_to([64, 4, 64]),
                                                                  op=ALU.mult), reads=[kn, ("dn", d)], writes=[hk])
            if not first:
                sch.op(pool, lambda: G.tensor_tensor(out=Hh[rs, m, :], in0=Hh[rs, m, :], in1=tmpf[rs, d * 512:(d + 1) * 512], op=ALU.add),
                       reads=[("hn", d), ("H", m)], writes=[("H", m)])

        for d in range(2):
            prep(d, 0)
        for i in range(32):
            for d in range(2):
                if i + 1 < 32:
                    prep(d, i + 1)
                recur(d, i)

        if stage == 6:
            dump("H", T_big[:, 0:8192], [("H", t) for t in range(NT)])
        checkpoint(6)
        WinO = T_W[:, 0:4096].rearrange("p (k n) -> p k n", k=8)
        wload(WinO, win_b[:, C_MO:C_MO + 512], "W0", "win", 8)
        sch.barrier()
        otok2 = T_A[:, 0:8192].rearrange("p (t c) -> p t c", t=16)
        for tt in range(NT):
            po, ko = bank("co", [0, 1])
            mm_acc(po[:, :], [(hT[:, kc, tt * 128:(tt + 1) * 128], WinO[:, kc, :]) for kc in range(8)], ko, [("hT", tt), "W0"])
            sg = tmpf[:, 512:1024]
            sch.op(act, lambda: A.activation(out=sg, in_=po[:, :], func=AF.Sigmoid), reads=[ko], writes=["sg"])
            sq = tmpf[:, 0:512]
            Ht = Hh[:, tt, :]
            sch.op(act, lambda: A.activation(out=sq, in_=Ht, func=AF.Square, scale=0.125), reads=[("H", tt)], writes=["sq"])
            ms = stat[:, 48:56]
            sch.op(dve, lambda: V.tensor_reduce(out=ms, in_=sq.rearrange("p (h c) -> p h c", h=8), axis=mybir.AxisListType.X, op=ALU.add),
                   reads=["sq"], writes=["ms"])
            sch.op(act, lambda: A.activation(out=ms, in_=ms, func=AF.Sqrt, bias=epsb[:], scale=1.0), reads=["ms", "epsb"], writes=["ms"])
            sch.op(dve, lambda: V.reciprocal(out=ms, in_=ms), reads=["ms"], writes=["ms"])
            sch.op(dve, lambda: V.tensor_tensor(out=sq.rearrange("p (h c) -> p h c", h=8), in0=Ht.rearrange("p (h c) -> p h c", h=8),
                                                in1=ms.unsqueeze(2).broadcast_to([128, 8, 64]), op=ALU.mult),
                   reads=[("H", tt), "ms"], writes=["sq"])
            sch.op(pool, lambda: G.tensor_tensor(out=sq, in0=sq, in1=ghead_bc[:], op=ALU.mult), reads=["sq", "ghead"], writes=["sq"])
            sch.op(dve, lambda: V.tensor_tensor(out=otok2[:, tt, :], in0=sq, in1=sg, op=ALU.mult), reads=["sq", "sg"],
                   writes=[("otok", tt // 4)])
        otok_save = otok
        otok = otok2
        flush_oT(1)
        otok = otok_save
        sch.barrier()

        checkpoint(7)
        WkvA = T_W[:, 0:4096].rearrange("p (k n) -> p k n", k=8)
        WkvB = T_W[:, 4096:8192].rearrange("p (k n) -> p k n", k=8)
        WinQ = T_W[:, 8192:12288].rearrange("p (k n) -> p k n", k=8)
        for nm, lo, hi in (("W0", 0, 4096), ("W1", 4096, 8192), ("W2", 8192, 12288)):
            sch.set_alias(nm, lo, hi)
        wload(WkvA, wkv_b[:, 0:512], "W0", "wkv", 8)
        wload(WkvB, wkv_b[:, 512:1024], "W1", "wkv", 8)
        wload(WinQ, win_b[:, C_MQ:C_MQ + 512], "W2", "win", 8)
        memT = T_A[:, 0:2048].rearrange("p (k t) -> p k t", k=8)
        kmT = T_A[:, 2048:3072].rearrange("p (h t) -> p h t", h=4)
        vm = T_A[:, 3072:4104].rearrange("p (t h c) -> p t h c", t=2, h=4)
        qm = [T_A[:, 4104 + i * 512:4104 + (i + 1) * 512] for i in range(2)]
        sch.op(dve, lambda: V.memset(vm[:, :, :, 128:129], 1.0), writes=["vm"])
        for t2 in range(2):
            slot = t2
            xsv = xs[:, slot * 1024:(slot + 1) * 1024]
            sch.dma(sp, xsv, mem_d[b, t2 * 128:(t2 + 1) * 128, :], writes=[("xs", slot)], grp=("xs", slot))
            rms_to_T(xsv, [("xs", slot)], 16, lambda t2=t2: memT[:, :, t2 * 128:(t2 + 1) * 128], [("memT", t2)], slot)
        checkpoint(7.1)
        MT = [("memT", 0), ("memT", 1)]
        for h in range(4):
            pk, kk = bank("dk", [0, 1])
            mm_acc(pk[:, 0:256], [(WkvA[:, kc, h * 128:(h + 1) * 128], memT[:, kc, :]) for kc in range(8)], kk, MT + ["W0"])
            sch.op(act, lambda: A.copy(out=kmT[:, h, :], in_=pk[:, 0:256]), reads=[kk], writes=["kmT"])
        for t2 in range(2):
            pv, kv = bank("dv", [2, 3])
            mm_acc(pv[:, :], [(memT[:, kc, t2 * 128:(t2 + 1) * 128], WkvB[:, kc, :]) for kc in range(8)], kv, MT + ["W1"])
            sch.op(act, lambda: A.copy(out=vm[:, t2, :, 0:128], in_=pv[:].rearrange("p (h c) -> p h c", h=4)), reads=[kv], writes=["vm"])
        checkpoint(7.2)
        mscale = 128.0 ** -0.5
        qc = 0
        for h in range(4):
            for qb in range(4):
                qs = slice(qb * 512, (qb + 1) * 512)
                pq, kq = bank("dq", [0, 1])
                mm_acc(pq[:, :], [(WinQ[:, kc, h * 128:(h + 1) * 128], hT[:, kc, qs]) for kc in range(8)], kq, htkeys(qb) + ["W2"])
                qi = qc % 2
                qc += 1
                sch.op(act, lambda: A.copy(out=qm[qi], in_=pq[:, :]), reads=[kq], writes=[("qm", qi)])
                pOa, kOa = bank("dOa", [4])
                pOb, kOb = bank("dOb", [5])
                for kt in range(2):
                    pS, kS = bank("dS", [2, 3])
                    sch.op(pe, lambda: T.matmul(pS[:, :], lhsT=kmT[:, h, kt * 128:(kt + 1) * 128], rhs=qm[qi], start=True, stop=True),
                           reads=["kmT", ("qm", qi)], writes=[kS])
                    ps_ = (qc * 2 + kt) % 3
                    P_ = PTt[:, ps_ * 512:(ps_ + 1) * 512]
                    sch.op(act, lambda: A.activation(out=P_, in_=pS[:, :], func=AF.Exp, scale=mscale), reads=[kS], writes=[("PT", ps_)])
                    for j in range(4):
                        pO_, kO_ = (pOa, kOa) if j < 2 else (pOb, kOb)
                        sch.op(pe, lambda j=j, pO_=pO_: T.matmul(pO_[:, (j % 2) * 129:(j % 2 + 1) * 129], lhsT=P_[:, j * 128:(j + 1) * 128],
                                                                 rhs=vm[:, kt, h, :], start=(kt == 0 and j % 2 == 0), stop=(kt == 1),
                                                                 skip_group_check=True),
                               reads=[("PT", ps_), "vm"], writes=[kO_], sig=(j % 2 == 1))
                if h == 0 and qb == 0:
                    checkpoint(7.3)
                rec = stat[:, 32:36]
                for half, (pO_, kO_) in enumerate(((pOa, kOa), (pOb, kOb))):
                    pv2 = pO_[:, 0:258].rearrange("p (j c) -> p j c", j=2)
                    rc = rec[:, half * 2:half * 2 + 2]
                    sch.op(dve, lambda: V.reciprocal(out=rc, in_=pv2[:, :, 128]), reads=[kO_], writes=[("rec", half)])
                    sch.op(dve, lambda: V.tensor_tensor(out=otok[:, qb * 4 + half * 2:qb * 4 + half * 2 + 2, h * 128:(h + 1) * 128],
                                                        in0=pv2[:, :, 0:128], in1=rc.unsqueeze(2).broadcast_to([128, 2, 128]), op=ALU.mult),
                           reads=[kO_, ("rec", half)], writes=[("otok", qb)])
        checkpoint(7.4)
        flush_oT(2)
        sch.barrier()

        if dbg:
            for br in range(3):
                for j in range(4):
                    sch.dma(pool, dbg_d[br, :, j, :], oT_d[br, :, j, :], reads=[("oTd", br, k, j) for k in range(4)], writes=[("dbg", br, j)],
                            grp=("dbg", br, j), final=True)

        checkpoint(8)
        x1 = T_big[:, 0:4096].rearrange("p (t c) -> p t c", t=4)
        mrg = bigb[:, 8192:12288].rearrange("p (k t) -> p k t", k=8)
        h2T = bigb[:, 12288:16384].rearrange("p (k t) -> p k t", k=8)
        aT = T_A[:].rearrange("p (c t) -> p c t", c=NFF)
        oTs = [T_C[:, br * 2048:(br + 1) * 2048].rearrange("p (j t) -> p j t", j=4) for br in range(3)]
        sgb = [T_V[:, i * 512:(i + 1) * 512] for i in range(3)]
        prd = [T_V[:, 1536 + i * 1024:1536 + (i + 1) * 1024].bitcast(F32) for i in range(3)]
        WS = [T_W[:, i * 4608:(i + 1) * 4608] for i in range(3)]
        for i in range(3):
            sch.set_alias(("WS", i), i * 4608, (i + 1) * 4608)
        units = []

        def e1_unit(blk, f):
            bs = slice(blk * 512, (blk + 1) * 512)

            def load(sl):
                sch.dma(sp, WS[sl][:, :], wge_b[f], reads=[("w", "wge")], writes=[("WS", sl)], grp=("WS", sl))

            def consume(sl):
                if f == 0:
                    for br in range(3):
                        sch.dma(sp, oTs[br], oT_d[br, :, :, bs], reads=[("oTd", br, blk, j) for j in range(4)], writes=[("oTs", br)],
                                grp=("oTs", br))
                Wg_ = WS[sl][:, 0:3072].rearrange("p (k b n) -> p k b n", k=8, b=3)
                Wb_ = WS[sl][:, 3072:4608].rearrange("p (k b n) -> p k b n", k=4, b=3)
                for br in range(3):
                    pg, kg = bank("eg", [0, 1])
                    pb_, kb_ = bank("eb", [2, 3])
                    mm_acc(pg[:, :], [(Wg_[:, kc, br, :], hT[:, kc, bs]) for kc in range(8)], kg, htkeys(blk) + [("WS", sl)])
                    mm_acc(pb_[:, :], [(Wb_[:, kc, br, :], oTs[br][:, kc, :]) for kc in range(4)], kb_, [("oTs", br), ("WS", sl)])
                    sch.op(act, lambda: A.activation(out=sgb[br], in_=pg[:, :], func=AF.Sigmoid), reads=[kg], writes=[("sgb", br)])
                    sch.op(dve, lambda: V.tensor_tensor(out=prd[br], in0=pb_[:, :], in1=sgb[br], op=ALU.mult),
                           reads=[kb_, ("sgb", br)], writes=[("prd", br)])
                sch.op(pool, lambda: G.tensor_tensor(out=prd[0], in0=prd[0], in1=prd[1], op=ALU.add),
                       reads=[("prd", 0), ("prd", 1)], writes=[("prd", 0)])
                sch.op(pool, lambda: G.tensor_tensor(out=mrg[:, f, :], in0=prd[0], in1=prd[2], op=ALU.add),
                       reads=[("prd", 0), ("prd", 2)], writes=[("mrg", f)])
            return load, consume

        MRG = [("mrg", f) for f in range(8)]
        H2 = [("h2T", t) for t in range(4)]

        def e2_unit(blk, nh):
            def load(sl):
                sch.dma(sp, WS[sl][:, 0:4096].rearrange("p (k n) -> p k n", k=8),
                        wout_b[:, nh * 512:(nh + 1) * 512].rearrange("(k p) n -> p k n", p=128),
                        reads=[("w", "wout")], writes=[("WS", sl)], grp=("WS", sl))

            def consume(sl):
                Wo = WS[sl][:, 0:4096].rearrange("p (k n) -> p k n", k=8)
                for tt in range(4):
                    slot = tt % 2
                    gt = blk * 4 + tt
                    xh = xs[:, slot * 1024:slot * 1024 + 512]
                    sch.dma(sp, xh, x_d[b, gt * 128:(gt + 1) * 128, nh * 512:(nh + 1) * 512], writes=[("xs", slot)], grp=("xs", slot))
                    py, ky = bank("ey", [4, 5])
                    mm_acc(py[:, :], [(mrg[:, kc, tt * 128:(tt + 1) * 128], Wo[:, kc, :]) for kc in range(8)], ky, MRG + [("WS", sl)])
                    sch.op(dve, lambda: V.tensor_tensor(out=x1[:, tt, nh * 512:(nh + 1) * 512], in0=py[:, :], in1=xh,
                                                        op=ALU.add), reads=[ky, ("xs", slot)], writes=[("x1", tt)])
                if nh == 1:
                    for tt in range(4):
                        rms_to_T(x1[:, tt, :], [("x1", tt)], 8, lambda tt=tt: h2T[:, :, tt * 128:(tt + 1) * 128], [("h2T", tt)], tt % 2)
            return load, consume

        def e3a_unit(blk, u):
            def load(sl):
                sch.dma(sp, WS[sl][:, 0:4096], wgu_b[u], reads=[("w", "wgu")], writes=[("WS", sl)], grp=("WS", sl))

            def consume(sl):
                WF = WS[sl][:, 0:4096].rearrange("p (c g k n) -> p c g k n", c=2, g=2, k=8)
                for c2 in range(2):
                    c = 2 * u + c2
                    pg, kg = bank("fg", [0, 1])
                    pu, ku = bank("fu", [2, 3])
                    mm_acc(pg[:, :], [(WF[:, c2, 0, kc, :], h2T[:, kc, :]) for kc in range(8)], kg, H2 + [("WS", sl)])
                    mm_acc(pu[:, :], [(WF[:, c2, 1, kc, :], h2T[:, kc, :]) for kc in range(8)], ku, H2 + [("WS", sl)])
                    si = c % 3
                    sch.op(act, lambda: A.activation(out=prd[si], in_=pg[:, :], func=AF.Silu), reads=[kg], writes=[("prd", si)])
                    sch.op(dve, lambda: V.tensor_tensor(out=aT[:, c, :], in0=pu[:, :], in1=prd[si], op=ALU.mult),
                           reads=[ku, ("prd", si)], writes=[("aT", c)])
            return load, consume

        def e3b_unit(blk, u):
            c0 = 4 * u
            ncu = min(4, NFF - c0)

            def load(sl):
                sch.dma(sp, WS[sl][:, 0:ncu * 1024].rearrange("p (c n) -> p c n", c=ncu),
                        wd_b[c0 * 128:(c0 + ncu) * 128, :].rearrange("(c p) n -> p c n", p=128),
                        reads=[("w", "wd")], writes=[("WS", sl)], grp=("WS", sl))

            def consume(sl):
                Wd_ = WS[sl][:, 0:ncu * 1024].rearrange("p (c n) -> p c n", c=ncu)
                for tt in range(4):
                    for nh in range(2):
                        py, ky = bank("ey", [4, 5])
                        mm_acc(py[:, :], [(aT[:, c0 + c, tt * 128:(tt + 1) * 128], Wd_[:, c, nh * 512:(nh + 1) * 512]) for c in range(ncu)],
                               ky, [("aT", c0 + c) for c in range(ncu)] + [("WS", sl)])
                        sch.op(dve, lambda: V.tensor_tensor(out=x1[:, tt, nh * 512:(nh + 1) * 512], in0=py[:, :],
                                                            in1=x1[:, tt, nh * 512:(nh + 1) * 512], op=ALU.add),
                               reads=[ky, ("x1", tt)], writes=[("x1", tt)])
                if c0 + ncu == NFF:
                    for tt in range(4):
                        slot = tt % 2
                        gt = blk * 4 + tt
                        st = stat[:, 56 + slot * 2:56 + slot * 2 + 1]
                        xsv = xs[:, slot * 1024:(slot + 1) * 1024]
                        xnv = xn[:, slot * 1024:(slot + 1) * 1024]
                        sch.op(act, lambda: A.activation(out=xnv, in_=x1[:, tt, :], func=AF.Square, scale=1.0 / 32.0, accum_out=st),
                               reads=[("x1", tt)], writes=[("xn", slot), ("fst", slot)])
                        sch.op(act, lambda: A.activation(out=st, in_=st, func=AF.Sqrt, bias=epsb[:], scale=1.0),
                               reads=[("fst", slot), "epsb"], writes=[("fst", slot)])
                        sch.op(dve, lambda: V.reciprocal(out=st, in_=st), reads=[("fst", slot)], writes=[("fst", slot)])
                        sch.op(act, lambda: A.activation(out=xsv, in_=x1[:, tt, :], func=AF.Copy, scale=st),
                               reads=[("x1", tt), ("fst", slot)], writes=[("xs", slot)])
                        sch.op(pool, lambda: G.tensor_tensor(out=xsv, in0=xsv, in1=gfin_bc[:], op=ALU.mult),
                               reads=[("xs", slot), "gfin"], writes=[("xs", slot)])
                        sch.dma(sp, out_d[b, gt * 128:(gt + 1) * 128, :], xsv, reads=[("xs", slot)], grp=("xs", slot), final=True)
            return load, consume

        for blk in range(4):
            units += [e1_unit(blk, f) for f in range(8)]
            units += [e2_unit(blk, nh) for nh in range(2)]
            units += [e3a_unit(blk, u) for u in range(11)]
            units += [e3b_unit(blk, u) for u in range(6)]
        units[0][0](0)
        units[1][0](1)
        for ui, (ld_, cons_) in enumerate(units):
            cons_(ui % 3)
            if ui + 2 < len(units):
                units[ui + 2][0]((ui + 2) % 3)
        sch.barrier()


def make_consts():
    c = np.zeros((128, K_N), np.float32)
    c[:, K_ID:K_ID + 128] = np.eye(128, dtype=np.float32)
    p = np.arange(128)
    s = (p % 64)[:, None]
    t = np.arange(64)[None, :]
    c[:, K_MF:K_MF + 64] = (s <= t)
    c[:, K_MB:K_MB + 64] = (s >= t)
    ss = p[:, None]
    tt = p[None, :]
    same = (ss // 64) == (tt // 64)
    c[:, K_TF:K_TF + 128] = same & (ss <= tt)
    c[:, K_TB:K_TB + 128] = same & (ss >= tt)
    c[:, K_I0:K_I0 + 128] = (ss // 64 == 0) & (tt >= 0)
    c[:, K_I1:K_I1 + 128] = (ss // 64 == 1) & (tt >= 0)
    inv = (10000.0 ** (-np.arange(16, dtype=np.float32) / 16.0)).astype(np.float32)
    for i in range(32):
        c[64 + i, K_RP] = inv[i % 16]
        c[64 + i, K_RP + 1] = math.pi if i < 16 else 0.0
        c[64 + i, K_RP + 2] = 0.5 * math.pi
    c[:, K_RP + 3] = -math.pi
    return c


_CACHE = {}


def kernel(**inputs):
    f = lambda a: np.ascontiguousarray(np.asarray(a, dtype=np.float32))
    if "nc" not in _CACHE:
        _CACHE["nc"] = build_program()
    nc = _CACHE["nc"]
    shared = {
        "g_mix": f(inputs["g_mix"]).reshape(1, D),
        "w_in": f(inputs["w_in"]).reshape(D, N_IN),
        "mla_g_q": f(inputs["mla_g_q"]).reshape(1, 384),
        "mla_g_kv": f(inputs["mla_g_kv"]).reshape(1, 256),
        "mla_w_uq": f(inputs["mla_w_uq"]).reshape(384, 768),
        "mla_w_uk": f(inputs["mla_w_uk"]).reshape(256, 512),
        "mla_w_uv": f(inputs["mla_w_uv"]).reshape(256, 512),
        "ml_conv_w": f(inputs["ml_conv_w"]).reshape(5, 512),
        "ml_w_q": f(inputs["ml_w_q"]).reshape(512, 64),
        "ml_w_k": f(inputs["ml_w_k"]).reshape(512, 64),
        "ml_gate_bias": f(inputs["ml_gate_bias"]).reshape(1, 32),
        "ml_g_head": f(inputs["ml_g_head"]).reshape(1, 512),
        "mem_g": f(inputs["mem_g"]).reshape(1, D),
        "mem_w_kv": f(inputs["mem_w_kv"]).reshape(D, 1024),
        "w_branch": f(inputs["w_branch"]).reshape(1536, D),
        "w_out": f(inputs["w_out"]).reshape(D, D),
        "g_ffn": f(inputs["g_ffn"]).reshape(1, D),
        "w_ffn_gate": f(inputs["w_ffn_gate"]).reshape(D, DFF),
        "w_ffn_up": f(inputs["w_ffn_up"]).reshape(D, DFF),
        "w_ffn_down": f(inputs["w_ffn_down"]).reshape(DFF, D),
        "g_final": f(inputs["g_final"]).reshape(1, D),
        "cst": make_consts(),
    }
    x = f(inputs["x"])
    mem = f(inputs["mem"])
    pos = np.ascontiguousarray(np.asarray(inputs["positions"], dtype=np.int32))
    in_maps = []
    for c in range(NCORES):
        sl = slice(c * SEQ_PER_CORE, (c + 1) * SEQ_PER_CORE)
        m = dict(shared)
        m["x"] = x[sl]
        m["mem"] = mem[sl]
        m["positions"] = pos[sl]
        in_maps.append(m)
    res = run_bass_kernel_spmd(nc, in_maps, core_ids=list(range(NCORES)))
    out = np.concatenate([np.asarray(r["out"]) for r in res.results], axis=0)
    return out.astype(np.float32, copy=False)
```

```python
import contextlib
import math
import numpy as np
import concourse.bass as bass
import concourse.mybir as mybir
from concourse.bass_utils import run_bass_kernel_spmd

F32 = mybir.dt.float32
BF16 = mybir.dt.bfloat16
I32 = mybir.dt.int32
AF = mybir.ActivationFunctionType
ALU = mybir.AluOpType

NCORES = 8
SEQ_PER_CORE = 4
S = 2048
D = 1024
NT = S // 128
EPS = 1e-6
N_IN = 5824
C_CQ, C_MU, C_MV, C_MO, C_MG, C_MQ = 3072, 3744, 4256, 4768, 5280, 5312
DFF = 2816
NFF = DFF // 128
K_ID, K_MF, K_MB, K_TF, K_TB, K_I0, K_I1, K_RP = 0, 128, 192, 256, 384, 512, 640, 768
K_N = 772


class Trk:
    __slots__ = ("w", "r")

    def __init__(self):
        self.w = None
        self.r = {}


class Eng:
    def __init__(self, sch, name, h):
        self.sch = sch
        self.name = name
        self.h = h
        self.seen = {}
        self.pending = False
        self.semidx = sch.newsem()
        self.cnt = 0


class Sched:
    def __init__(self, nc, es):
        self.nc = nc
        self.es = es
        self.sems = []
        self.trk = {}
        self.dsem = {}
        self.final = []
        self.alias = {}
        self.ninst = 0
        self.pe = Eng(self, "pe", nc.tensor)
        self.dve = Eng(self, "dve", nc.vector)
        self.act = Eng(self, "act", nc.scalar)
        self.pool = Eng(self, "pool", nc.gpsimd)
        self.sp = Eng(self, "sp", nc.sync)

    def newsem(self):
        h = self.es.enter_context(self.nc.semaphore("s%d" % len(self.sems)))
        self.sems.append(h)
        return len(self.sems) - 1

    def _exp(self, keys):
        out = []
        for k in keys:
            a = self.alias.get(k)
            if a is None:
                out.append(k)
            else:
                out.extend(a)
        return out

    def set_alias(self, key, lo, hi):
        self.alias[key] = [("TW", g) for g in range(lo // 512, (hi + 511) // 512)]

    def _deps(self, reads, writes, skip_waw_sem=None):
        d = {}
        for k in reads:
            t = self.trk.get(k)
            if t is not None and t.w is not None and d.get(t.w[0], 0) < t.w[1]:
                d[t.w[0]] = t.w[1]
        for k in writes:
            t = self.trk.get(k)
            if t is None:
                continue
            if t.w is not None and t.w[0] != skip_waw_sem and d.get(t.w[0], 0) < t.w[1]:
                d[t.w[0]] = t.w[1]
            for si, v in t.r.items():
                if d.get(si, 0) < v:
                    d[si] = v
        return d

    def _wait(self, eng, d, skip_own=False):
        for si, v in d.items():
            if skip_own and si == eng.semidx:
                continue
            if eng.seen.get(si, 0) >= v:
                continue
            eng.h.wait_ge(self.sems[si], v)
            self.ninst += 1
            eng.seen[si] = v

    def _mark(self, tok, reads, writes):
        for k in reads:
            t = self.trk.get(k)
            if t is None:
                t = self.trk[k] = Trk()
            if t.r.get(tok[0], 0) < tok[1]:
                t.r[tok[0]] = tok[1]
        for k in writes:
            t = self.trk.get(k)
            if t is None:
                t = self.trk[k] = Trk()
            t.w = tok
            t.r = {}

    def op(self, eng, fn, reads=(), writes=(), sig=True):
        reads = self._exp(reads)
        writes = self._exp(writes)
        d = self._deps(reads, writes)
        self._wait(eng, d, skip_own=(eng is self.pe))
        ins = fn()
        self.ninst += 1
        if sig or eng is not self.pe:
            if eng.cnt >= 30000 and not eng.pending:
                eng.semidx = self.newsem()
                eng.cnt = 0
            eng.cnt += 1
            ins.then_inc(self.sems[eng.semidx], 1)
            tok = (eng.semidx, eng.cnt)
            eng.pending = False
        else:
            tok = (eng.semidx, eng.cnt + 1)
            eng.pending = True
        self._mark(tok, reads, writes)
        return tok

    def dma(self, q, out, in_, reads=(), writes=(), grp=None, final=False):
        reads = self._exp(reads)
        writes = self._exp(writes)
        g = self.dsem.get(grp)
        if g is None:
            g = self.dsem[grp] = [self.newsem(), 0]
        d = self._deps(reads, writes, skip_waw_sem=g[0])
        self._wait(q, d)
        ins = q.h.dma_start(out=out, in_=in_)
        self.ninst += 1
        g[1] += 16
        ins.then_inc(self.sems[g[0]], 16)
        tok = (g[0], g[1])
        self._mark(tok, reads, writes)
        if final:
            self.final.append(tok)
        return tok

    def retoken(self, keys, tok):
        for k in keys:
            t = self.trk.get(k)
            if t is None:
                t = self.trk[k] = Trk()
            t.w = tok

    def barrier(self):
        engs = [self.pe, self.dve, self.act, self.pool]
        assert not self.pe.pending
        for e in engs + [self.sp]:
            d = {}
            for o in engs:
                if o is not e and o.cnt > 0:
                    d[o.semidx] = o.cnt
            self._wait(e, d)

    def finish(self):
        d = {}
        for si, v in self.final:
            if d.get(si, 0) < v:
                d[si] = v
        self._wait(self.sp, d)


class StopBuild(Exception):
    pass


def build_program(nseq=SEQ_PER_CORE, dbg=False, stage=99):
    nc = bass.Bass("TRN2", target_bir_lowering=False)
    es = contextlib.ExitStack()
    with es:
        sch = Sched(nc, es)
        try:
            _emit(nc, es, nseq, dbg, sch, stage)
        except StopBuild:
            pass
        sch.finish()
        _emit.ninst = sch.ninst
        _emit.nsem = len(sch.sems)
    return nc


def _emit(nc, es, nseq, dbg, sch, stage):
    pe, dve, act, pool, sp = sch.pe, sch.dve, sch.act, sch.pool, sch.sp
    T, V, A, G = nc.tensor, nc.vector, nc.scalar, nc.gpsimd

    def din(name, shape, dt=F32):
        return nc.dram_tensor(name, list(shape), dt, kind="ExternalInput").ap()

    def dscr(name, shape, dt=BF16):
        return nc.dram_tensor(name, list(shape), dt, kind="Internal").ap()

    x_d = din("x", [SEQ_PER_CORE, S, D])
    mem_d = din("mem", [SEQ_PER_CORE, 256, D])
    pos_d = din("positions", [SEQ_PER_CORE, S], I32)
    gmix_d = din("g_mix", [1, D])
    win_d = din("w_in", [D, N_IN])
    gq_d = din("mla_g_q", [1, 384])
    gkv_d = din("mla_g_kv", [1, 256])
    wuq_d = din("mla_w_uq", [384, 768])
    wuk_d = din("mla_w_uk", [256, 512])
    wuv_d = din("mla_w_uv", [256, 512])
    convw_d = din("ml_conv_w", [5, 512])
    wq_d = din("ml_w_q", [512, 64])
    wk_d = din("ml_w_k", [512, 64])
    gbias_d = din("ml_gate_bias", [1, 32])
    ghead_d = din("ml_g_head", [1, 512])
    memg_d = din("mem_g", [1, D])
    wkv_d = din("mem_w_kv", [D, 1024])
    wbr_d = din("w_branch", [1536, D])
    wout_d = din("w_out", [D, D])
    gffn_d = din("g_ffn", [1, D])
    wg_d = din("w_ffn_gate", [D, DFF])
    wu_d = din("w_ffn_up", [D, DFF])
    wd_d = din("w_ffn_down", [DFF, D])
    gfin_d = din("g_final", [1, D])
    cst_d = din("cst", [128, K_N])
    out_d = nc.dram_tensor("out", [SEQ_PER_CORE, S, D], F32, kind="ExternalOutput").ap()
    if dbg:
        dbg_d = nc.dram_tensor("dbg_oT", [3, 128, 4, S], BF16, kind="ExternalOutput").ap()

    win_b = dscr("win_b", [D, N_IN])
    wuq_b = dscr("wuq_b", [384, 768])
    wuk_b = dscr("wuk_b", [256, 512])
    wuv_b = dscr("wuv_b", [256, 512])
    wq_b = dscr("wq_b", [512, 64])
    wk_b = dscr("wk_b", [512, 64])
    wkv_b = dscr("wkv_b", [D, 1024])
    wout_b = dscr("wout_b", [D, D])
    wd_b = dscr("wd_b", [DFF, D])
    oT_d = dscr("oT_scr", [3, 128, 4, S])
    wge_b = dscr("wge_b", [8, 128, 4608])
    wgu_b = dscr("wgu_b", [11, 128, 4096])

    dumps = {}

    def dump(name, ap, reads):
        shp = [int(v) for v in ap.shape]
        t = nc.dram_tensor("dmp_" + name, shp, ap.dtype, kind="ExternalOutput").ap()
        sch.dma(sp, t, ap, reads=reads, grp=("dmp", name), final=True)

    def checkpoint(st):
        if stage <= st:
            sch.barrier()
            raise StopBuild()

    def sbt(name, cols, dt):
        return es.enter_context(nc.sbuf_tensor(name, [128, cols], dt))

    T_hT = sbt("hT", 8 * S, BF16)
    T_big = sbt("big", 4 * 2080, F32)
    T_A = sbt("ra", 11264, BF16)
    T_V = sbt("rv", 16 * 520, BF16)
    T_C = sbt("rc", 16 * 512, BF16)
    T_W = sbt("rw", 13824, BF16)
    xs = sbt("xs", 2 * 1024, F32)
    xn = sbt("xn", 2 * 1024, BF16)
    cst = sbt("cstt", K_N, F32)
    identb = sbt("identb", 128, BF16)
    gfin_bc = sbt("gfin_bc", 1024, F32)
    ghead_bc = sbt("ghead_bc", 512, F32)
    gbias_bc = sbt("gbias_bc", 32, F32)
    gvecT = sbt("gvecT", 32, F32)
    convw = sbt("convw", 20, F32)
    stat = sbt("stat", 128, F32)
    epsb = sbt("epsb", 1, F32)
    oneb = sbt("oneb", 1, F32)
    gA = sbt("gA", 16 * 16, F32)
    gE = sbt("gE", 16 * 16, F32)
    gWt = sbt("gWt", 16 * 16, F32)
    gDec = sbt("gDec", 16 * 32, F32)
    gtmp = sbt("gtmp", 128, F32)
    kst = sbt("kst", 4 * 96, BF16)
    PTt = sbt("PTt", 3 * 1024, BF16)
    tmpf = sbt("tmpf", 2 * 512, F32)
    stg = sbt("stg", 2 * 512, BF16)
    posi = sbt("posi", S, I32)
    posf = sbt("posf", S, F32)

    hT = T_hT[:].rearrange("p (k t) -> p k t", k=8)
    bigb = T_big[:].bitcast(BF16)

    pdbl = [es.enter_context(nc.psum_tensor("pd%d" % i, [128, 1024], F32)) for i in range(3)]
    pbk = [pdbl[i // 2][:, (i % 2) * 512:(i % 2 + 1) * 512] for i in range(6)]
    ptk = [es.enter_context(nc.psum_tensor("pt%d" % i, [128, 1024], BF16)) for i in range(2)]
    pbk.append(ptk[0][:].bitcast(F32))
    pbk.append(ptk[1][:].bitcast(F32))
    rot = {}

    def bank(grp, ids):
        i = rot.get(grp, 0)
        rot[grp] = i + 1
        b = ids[i % len(ids)]
        return pbk[b], (("pb", b) if b < 6 else ("pt", b - 6))

    def tbank():
        i = rot.get("pt", 0)
        rot["pt"] = i + 1
        return ptk[i % 2], ("pt", i % 2)

    sch.dma(sp, cst[:], cst_d, writes=["cst"], grp="cst")
    sch.dma(pool, identb[:], cst_d[:, K_ID:K_ID + 128], writes=["ident"], grp="ident")
    sch.dma(sp, gfin_bc[:], gfin_d.partition_broadcast(128), writes=["gfin"], grp="c2")
    sch.dma(sp, ghead_bc[:], ghead_d.partition_broadcast(128), writes=["ghead"], grp="c3")
    sch.dma(sp, gbias_bc[:], gbias_d.partition_broadcast(128), writes=["gbias"], grp="c4")
    with nc.allow_non_contiguous_dma("tiny gain-vector transposes"):
        sch.dma(sp, gvecT[:, 0:8], gmix_d.rearrange("o (k p) -> p (o k)", p=128), writes=["gv0"], grp="c5")
        sch.dma(sp, gvecT[:, 8:16], gffn_d.rearrange("o (k p) -> p (o k)", p=128), writes=["gv1"], grp="c6")
        sch.dma(sp, gvecT[:, 16:24], memg_d.rearrange("o (k p) -> p (o k)", p=128), writes=["gv2"], grp="c7")
        sch.dma(sp, gvecT[:, 24:27], gq_d.rearrange("o (k p) -> p (o k)", p=128), writes=["gv3"], grp="c8")
        sch.dma(sp, gvecT[:, 27:29], gkv_d.rearrange("o (k p) -> p (o k)", p=128), writes=["gv4"], grp="c9")
        for ct_ in range(4):
            sch.dma(sp, convw[:, ct_ * 5:(ct_ + 1) * 5], convw_d[:, ct_ * 128:(ct_ + 1) * 128].rearrange("j p -> p j"),
                    writes=["convw"], grp="c10")
    GV = ["gv0", "gv1", "gv2", "gv3", "gv4"]
    sch.op(dve, lambda: V.memset(epsb[:], EPS), writes=["epsb"])
    sch.op(dve, lambda: V.memset(kst[:], 0.0), writes=[("kst", 0), ("kst", 1), ("kst2", 0), ("kst2", 1)])
    sch.op(dve, lambda: V.memset(oneb[:], 1.0), writes=["oneb"])

    def conv_w(dst, src, rows, key):
        tok = None
        r0 = 0
        while r0 < rows:
            r1 = min(rows, r0 + 128)
            tok = sch.dma(pool, dst[r0:r1, :], src[r0:r1, :], grp=("cv", key))
            r0 = r1
        sch.retoken([("w", key)], tok)

    tok = None
    for r0_ in range(0, D, 256):
        tok = sch.dma(pool, win_b[r0_:r0_ + 256, C_CQ:C_MU], win_d[r0_:r0_ + 256, C_CQ:C_MU], grp=("cv", "winB"))
    sch.retoken([("w", "winB")], tok)
    conv_w(wuq_b, wuq_d, 384, "wuq")
    conv_w(wuk_b, wuk_d, 256, "wuk")
    conv_w(wuv_b, wuv_d, 256, "wuv")
    tok = None
    for r0_ in range(0, D, 128):
        tok = sch.dma(pool, win_b[r0_:r0_ + 128, C_MU:N_IN], win_d[r0_:r0_ + 128, C_MU:N_IN], grp=("cv", "win"))
    sch.retoken([("w", "win")], tok)
    conv_w(wq_b, wq_d, 512, "wq")
    conv_w(wk_b, wk_d, 512, "wk")
    conv_w(wkv_b, wkv_d, D, "wkv")
    tok = None
    for f_ in range(8):
        for br_ in range(3):
            dst = wge_b[f_, :, 0:3072].rearrange("p (k b n) -> p k b n", k=8, b=3)[:, :, br_, :]
            src = win_d[:, br_ * 1024 + f_ * 128:br_ * 1024 + (f_ + 1) * 128].rearrange("(k p) n -> p k n", p=128)
            tok = sch.dma(pool, dst, src, grp=("cv", "wge"))
            dst = wge_b[f_, :, 3072:4608].rearrange("p (k b n) -> p k b n", k=4, b=3)[:, :, br_, :]
            src = wbr_d[br_ * 512:(br_ + 1) * 512, f_ * 128:(f_ + 1) * 128].rearrange("(k p) n -> p k n", p=128)
            tok = sch.dma(pool, dst, src, grp=("cv", "wge"))
    sch.retoken([("w", "wge")], tok)
    conv_w(wout_b, wout_d, D, "wout")
    tok = None
    for u_ in range(11):
        for c2_ in range(2):
            c_ = 2 * u_ + c2_
            for gu_, src_d in enumerate((wg_d, wu_d)):
                o_ = (c2_ * 2 + gu_) * 1024
                dst = wgu_b[u_, :, o_:o_ + 1024].rearrange("p (k n) -> p k n", k=8)
                src = src_d[:, c_ * 128:(c_ + 1) * 128].rearrange("(k p) n -> p k n", p=128)
                tok = sch.dma(pool, dst, src, grp=("cv", "wgu"))
    sch.retoken([("w", "wgu")], tok)
    conv_w(wd_b, wd_d, DFF, "wd")

    checkpoint(0)

    def wload(dst, src2d, key, wkey, kc):
        if kc == 1:
            srcv = src2d
        else:
            srcv = src2d.rearrange("(k p) n -> p k n", p=128)
        sch.dma(sp, dst, srcv, reads=[("w", wkey)], writes=[key], grp=key)

    def rms_a(src_ap, src_keys, slot):
        st = stat[:, slot * 4:slot * 4 + 1]
        st2 = stat[:, slot * 4 + 1:slot * 4 + 2]
        xnv = xn[:, slot * 1024:(slot + 1) * 1024]
        sch.op(act, lambda: A.activation(out=xnv, in_=src_ap, func=AF.Square, scale=1.0 / 32.0, accum_out=st),
               reads=src_keys, writes=[("xn", slot), ("st", slot)])
        sch.op(act, lambda: A.activation(out=st2, in_=st, func=AF.Sqrt, bias=epsb[:], scale=1.0),
               reads=[("st", slot), "epsb"], writes=[("st2", slot)])
        sch.op(dve, lambda: V.reciprocal(out=st2, in_=st2), reads=[("st2", slot)], writes=[("st2", slot)])
        sch.op(act, lambda: A.activation(out=xnv, in_=src_ap, func=AF.Copy, scale=st2),
               reads=list(src_keys) + [("st2", slot)], writes=[("xn", slot)])

    def rms_b(gcol, dst_fn, dst_keys, slot):
        xnv = xn[:, slot * 1024:(slot + 1) * 1024]
        pt, ptkey = tbank()
        ptv = pt[:].rearrange("p (k t) -> p k t", k=8)
        for kc in range(8):
            sch.op(pe, lambda kc=kc: T.transpose(ptv[:, kc, :], xnv[:, kc * 128:(kc + 1) * 128], identb[:]),
                   reads=[("xn", slot), "ident"], writes=[ptkey], sig=(kc == 7))
        gb = gvecT[:, gcol:gcol + 8].unsqueeze(2).broadcast_to([128, 8, 128])
        sch.op(dve, lambda: V.tensor_tensor(out=dst_fn(), in0=ptv, in1=gb, op=ALU.mult),
               reads=[ptkey] + GV, writes=dst_keys)

    def rms_to_T(src_ap, src_keys, gcol, dst_fn, dst_keys, slot):
        rms_a(src_ap, src_keys, slot)
        rms_b(gcol, dst_fn, dst_keys, slot)

    def mm_acc(out_ap, pairs, okey, rkeys):
        n = len(pairs)
        for i, (l, r) in enumerate(pairs):
            sch.op(pe, lambda l=l, r=r, i=i: T.matmul(out_ap, lhsT=l, rhs=r, start=(i == 0), stop=(i == n - 1)),
                   reads=rkeys, writes=[okey], sig=(i == n - 1))

    def htkeys(blk):
        return [("hT", blk * 4 + i) for i in range(4)]

    for b in range(nseq):
        for k in range(NT + 1):
            if k < NT:
                slot = k % 2
                xsv = xs[:, slot * 1024:(slot + 1) * 1024]
                sch.dma(sp, xsv, x_d[b, k * 128:(k + 1) * 128, :], writes=[("xs", slot)], grp=("xs", slot))
                rms_a(xsv, [("xs", slot)], slot)
            if k >= 1:
                rms_b(0, lambda tt=k - 1: hT[:, :, tt * 128:(tt + 1) * 128], [("hT", k - 1)], (k - 1) % 2)

        if stage == 1:
            dump("hT", T_hT[:], [("hT", t) for t in range(NT)])
        checkpoint(1)
        WinB = T_W[:, 0:5376].rearrange("p (k n) -> p k n", k=8)
        Wuq = T_W[:, 5376:7680].rearrange("p (k n) -> p k n", k=3)
        WuqS = T_W[:, 7680:9984].rearrange("p (k n) -> p k n", k=3)
        Wuk = T_W[:, 9984:11008].rearrange("p (k n) -> p k n", k=2)
        Wuv = T_W[:, 11008:12032].rearrange("p (k n) -> p k n", k=2)
        for nm, lo, hi in (("W0", 0, 5376), ("W1", 5376, 7680), ("W2", 7680, 9984), ("W3", 9984, 11008), ("W4", 11008, 12032)):
            sch.set_alias(nm, lo, hi)
        wload(WinB, win_b[:, C_CQ:C_CQ + 672], "W0", "winB", 8)
        wload(Wuq, wuq_b, "W1", "wuq", 3)
        wuq_b3 = wuq_b.rearrange("(k p) (h c) -> p k h c", p=128, c=96)
        WuqS4 = WuqS.rearrange("p k (h c) -> p k h c", c=96)
        for kc in range(3):
            sch.dma(sp, WuqS4[:, kc, :, 0:64], wuq_b3[:, kc, :, 0:64], reads=[("w", "wuq")], writes=["W2"], grp="W2")
            sch.dma(sp, WuqS4[:, kc, :, 64:80], wuq_b3[:, kc, :, 80:96], reads=[("w", "wuq")], writes=["W2"], grp="W2")
            tk = sch.dma(sp, WuqS4[:, kc, :, 80:96], wuq_b3[:, kc, :, 64:80], reads=[("w", "wuq")], writes=["W2"], grp="W2")
        wload(Wuk, wuk_b, "W3", "wuk", 2)
        wload(Wuv, wuv_b, "W4", "wuv", 2)
        W2K = ["W2"]

        cqnT = T_A[:, 0:6144].rearrange("p (k t) -> p k t", k=3)
        ckvnT = T_A[:, 6144:10240].rearrange("p (k t) -> p k t", k=2)
        KhT = [bigb[:, 0:2048], bigb[:, 2048:4096]]
        QhT = [bigb[:, 4096:6144], bigb[:, 6144:8192]]
        cosT = bigb[:, 8192:10240]
        sinT = bigb[:, 10240:12288]
        zst = [bigb[:, 12288:12960], bigb[:, 12960:13632]]
        Vaug = T_V[:].rearrange("p (t h c) -> p t h c", t=16, h=8)
        otok = T_C[:].rearrange("p (t c) -> p t c", t=16)

        sch.dma(sp, posi[:], pos_d[b:b + 1, :].partition_broadcast(128), writes=["posi"], grp="posi")
        sch.op(dve, lambda: V.tensor_copy(out=posf[64:96, :], in_=posi[64:96, :]), reads=["posi"], writes=["posf"])
        TWO_PI = 2.0 * math.pi
        for (tab, offc, key) in ((cosT, K_RP + 2, "cosT"), (sinT, K_RP + 1, "sinT")):
            for q4 in range(2):
                seg = slice(q4 * 1024, (q4 + 1) * 1024)
                yv = tmpf[64:96, :]
                ki = posi[64:96, 0:1024]
                kf = xs[64:96, 0:1024]
                mv = xs[64:96, 1024:2048]
                sch.op(dve, lambda: V.tensor_scalar(out=yv, in0=posf[64:96, seg], scalar1=cst[64:96, K_RP:K_RP + 1],
                                                    scalar2=cst[64:96, offc:offc + 1], op0=ALU.mult, op1=ALU.add),
                       reads=["posf", "cst"], writes=["tmpf"])
                sch.op(dve, lambda: V.tensor_scalar(out=ki, in0=yv, scalar1=1.0 / TWO_PI, scalar2=0.5, op0=ALU.mult, op1=ALU.add),
                       reads=["tmpf", "posf"], writes=["posi"])
                sch.op(dve, lambda: V.tensor_copy(out=kf, in_=ki), reads=["posi"], writes=[("xs", 0)])
                sch.op(dve, lambda: V.scalar_tensor_tensor(out=yv, in0=kf, scalar=-TWO_PI, in1=yv, op0=ALU.mult, op1=ALU.add),
                       reads=[("xs", 0), "tmpf"], writes=["tmpf"])
                sch.op(dve, lambda: V.tensor_scalar(out=mv, in0=yv, scalar1=-math.pi, scalar2=TWO_PI, op0=ALU.is_lt, op1=ALU.mult),
                       reads=["tmpf"], writes=[("xs", 1)])
                sch.op(dve, lambda: V.tensor_tensor(out=yv, in0=yv, in1=mv, op=ALU.add), reads=["tmpf", ("xs", 1)], writes=["tmpf"])
                sch.op(act, lambda: A.activation(out=tab[64:96, seg], in_=yv, func=AF.Sin), reads=["tmpf"], writes=[key])

        sch.op(dve, lambda: V.memset(Vaug[:, :, :, 64:65], 1.0), writes=["Vaug"])

        b1 = {}

        def b1_s1(tt):
            pz0, k0 = bank("z0", [0, 1])
            pz1, k1 = bank("z1", [2, 3])
            mm_acc(pz0[:, 0:384], [(hT[:, kc, tt * 128:(tt + 1) * 128], WinB[:, kc, 0:384]) for kc in range(8)],
                   k0, [("hT", tt), "W0"])
            mm_acc(pz1[:, 0:288], [(hT[:, kc, tt * 128:(tt + 1) * 128], WinB[:, kc, 384:672]) for kc in range(8)],
                   k1, [("hT", tt), "W0"])
            b1[tt] = (pz0, k0, pz1, k1)

        def b1_s2(tt):
            slot = tt % 2
            pz0, k0, pz1, k1 = b1[tt]
            st = stat[:, 16 + slot * 4:16 + slot * 4 + 2]
            junk = xn[:, slot * 1024:slot * 1024 + 384]
            sch.op(act, lambda: A.activation(out=junk, in_=pz0[:, 0:384], func=AF.Square, scale=384.0 ** -0.5,
                                             accum_out=st[:, 0:1]), reads=[k0], writes=[("xn", slot), ("bst", slot)])
            sch.op(act, lambda: A.activation(out=junk[:, 0:256], in_=pz1[:, 0:256], func=AF.Square, scale=1.0 / 16.0,
                                             accum_out=st[:, 1:2]), reads=[k1], writes=[("xn", slot), ("bst", slot)])
            sch.op(act, lambda: A.activation(out=st, in_=st, func=AF.Sqrt, bias=epsb[:], scale=1.0),
                   reads=[("bst", slot), "epsb"], writes=[("bst", slot)])
            sch.op(dve, lambda: V.reciprocal(out=st, in_=st), reads=[("bst", slot)], writes=[("bst", slot)])
            z = zst[slot]
            sch.op(act, lambda: A.activation(out=z[:, 0:384], in_=pz0[:, 0:384], func=AF.Copy, scale=st[:, 0:1]),
                   reads=[k0, ("bst", slot)], writes=[("zst", slot)])
            sch.op(dve, lambda: V.tensor_scalar(out=z[:, 384:640], in0=pz1[:, 0:256], scalar1=st[:, 1:2], scalar2=None,
                                                op0=ALU.mult), reads=[k1, ("bst", slot)], writes=[("zst", slot)])
            ks = kst[:, (slot * 2) * 96:(slot * 2 + 1) * 96]
            ks2 = kst[:, (slot * 2 + 1) * 96:(slot * 2 + 2) * 96]
            sch.op(act, lambda: A.copy(out=ks[:, 64:96], in_=pz1[:, 256:288]), reads=[k1], writes=[("kst", slot)])
            sch.op(dve, lambda: V.tensor_copy(out=ks2[:, 64:80], in_=pz1[:, 272:288]), reads=[k1], writes=[("kst2", slot)])
            sch.op(dve, lambda: V.tensor_copy(out=ks2[:, 80:96], in_=pz1[:, 256:272]), reads=[k1], writes=[("kst2", slot)])

        def b1_s3(tt):
            slot = tt % 2
            z = zst[slot]
            ks = kst[:, (slot * 2) * 96:(slot * 2 + 1) * 96]
            ks2 = kst[:, (slot * 2 + 1) * 96:(slot * 2 + 2) * 96]
            pt, ptkey = tbank()
            ptv = pt[:].rearrange("p (k t) -> p k t", k=8)
            for j in range(5):
                sch.op(pe, lambda j=j: T.transpose(ptv[:, j, :], z[:, j * 128:(j + 1) * 128], identb[:]),
                       reads=[("zst", slot), "ident"], writes=[ptkey], sig=False)
            sch.op(pe, lambda: T.transpose(ptv[0:96, 5, :], ks, identb[:]), reads=[("kst", slot), "ident"], writes=[ptkey], sig=False)
            sch.op(pe, lambda: T.transpose(ptv[0:96, 6, :], ks2, identb[:]), reads=[("kst2", slot), "ident"], writes=[ptkey])
            tsl = slice(tt * 128, (tt + 1) * 128)
            gq = gvecT[:, 24:27].unsqueeze(2).broadcast_to([128, 3, 128])
            gk = gvecT[:, 27:29].unsqueeze(2).broadcast_to([128, 2, 128])
            sch.op(dve, lambda: V.tensor_tensor(out=ckvnT[:, :, tsl], in0=ptv[:, 3:5, :], in1=gk, op=ALU.mult),
                   reads=[ptkey] + GV, writes=[("ckvnT", tt)])
            sch.op(dve, lambda: V.tensor_tensor(out=cqnT[:, :, tsl], in0=ptv[:, 0:3, :], in1=gq, op=ALU.mult),
                   reads=[ptkey] + GV, writes=[("cqnT", tt)])
            t1 = tmpf[64:96, slot * 128:(slot + 1) * 128]
            t2 = tmpf[64:96, 512 + slot * 128:512 + (slot + 1) * 128]
            sch.op(dve, lambda: V.tensor_tensor(out=t1, in0=ptv[64:96, 5, :], in1=cosT[64:96, tsl], op=ALU.mult),
                   reads=[ptkey, "cosT"], writes=["tmpf"])
            sch.op(dve, lambda: V.tensor_tensor(out=t2, in0=ptv[64:96, 6, :], in1=sinT[64:96, tsl], op=ALU.mult),
                   reads=[ptkey, "sinT"], writes=["tmpf"])
            sch.op(dve, lambda: V.tensor_tensor(out=KhT[0][64:96, tsl], in0=t1, in1=t2, op=ALU.add),
                   reads=["tmpf"], writes=[("KhT", 0)])
            sch.op(act, lambda: A.copy(out=KhT[1][64:96, tsl], in_=KhT[0][64:96, tsl]),
                   reads=[("KhT", 0)], writes=[("KhT", 1)])
            pv, kv = bank("zv", [4, 5])
            mm_acc(pv[:, :], [(ckvnT[:, kc, tsl], Wuv[:, kc, :]) for kc in range(2)], kv, [("ckvnT", tt), "W4"])
            sch.op(act, lambda: A.copy(out=Vaug[:, tt, :, 0:64], in_=pv[:].rearrange("p (h c) -> p h c", h=8)),
                   reads=[kv], writes=["Vaug"])

        for k in range(NT + 2):
            if k < NT:
                b1_s1(k)
            if 1 <= k <= NT:
                b1_s2(k - 1)
            if k >= 2:
                b1_s3(k - 2)

        if stage == 2:
            dump("cq", T_A[:, 0:10240], [("cqnT", t) for t in range(NT)] + [("ckvnT", t) for t in range(NT)])
            dump("kh", bigb[:, 0:2048], [("KhT", 0)])
            dump("cos", bigb[:, 8192:12288], ["cosT", "sinT"])
            dump("va", T_V[:], ["Vaug"])
        checkpoint(2)
        ALLCQ = [("cqnT", t) for t in range(NT)]
        ALLCKV = [("ckvnT", t) for t in range(NT)]
        att_scale = 96.0 ** -0.5
        def produce_kq(h):
            s_ = h % 2
            for blk in range(4):
                bs = slice(blk * 512, (blk + 1) * 512)
                pk, kk = bank("kq", [6, 7])
                mm_acc(pk[0:64, :], [(Wuk[:, kc, h * 64:(h + 1) * 64], ckvnT[:, kc, bs]) for kc in range(2)], kk,
                       ALLCKV[blk * 4:blk * 4 + 4] + ["W3"])
                sch.op(dve, lambda: V.tensor_copy(out=KhT[s_][0:64, bs], in_=pk[0:64, :]), reads=[kk], writes=[("KhT", s_)])
                pq, kq = bank("kq", [6, 7])
                pqs, kqs = bank("kq", [6, 7])
                mm_acc(pq[0:96, :], [(Wuq[:, kc, h * 96:(h + 1) * 96], cqnT[:, kc, bs]) for kc in range(3)], kq,
                       ALLCQ[blk * 4:blk * 4 + 4] + ["W1"])
                mm_acc(pqs[0:96, :], [(WuqS[:, kc, h * 96:(h + 1) * 96], cqnT[:, kc, bs]) for kc in range(3)], kqs,
                       ALLCQ[blk * 4:blk * 4 + 4] + W2K)
                sch.op(dve, lambda: V.tensor_copy(out=QhT[s_][0:64, bs], in_=pq[0:64, :]), reads=[kq], writes=[("QhT", s_)])
                t1 = tmpf[64:96, 0:512]
                t2 = tmpf[64:96, 512:1024]
                sch.op(dve, lambda: V.tensor_tensor(out=t1, in0=pq[64:96, :], in1=cosT[64:96, bs], op=ALU.mult),
                       reads=[kq, "cosT"], writes=["tmpf"])
                sch.op(dve, lambda: V.tensor_tensor(out=t2, in0=pqs[64:96, :], in1=sinT[64:96, bs], op=ALU.mult),
                       reads=[kqs, "sinT"], writes=["tmpf"])
                sch.op(dve, lambda: V.tensor_tensor(out=QhT[s_][64:96, bs], in0=t1, in1=t2, op=ALU.add),
                       reads=["tmpf"], writes=[("QhT", s_)])

        def attend(h):
            s_ = h % 2
            for qb in range(4):
                qs = slice(qb * 512, (qb + 1) * 512)
                pO, kO = bank("pO", [4, 5])
                pOv = pO[:, 0:260].rearrange("p (j c) -> p j c", j=4)

                def pv(kt, P_, ps_):
                    for j in range(4):
                        sch.op(pe, lambda j=j: T.matmul(pOv[:, j, :], lhsT=P_[:, j * 128:(j + 1) * 128], rhs=Vaug[:, kt, h, :],
                                                        start=(kt == 0 and j == 0), stop=(kt == 15), skip_group_check=True),
                               reads=[("PT", ps_), "Vaug"], writes=[kO], sig=(j == 3))

                prev = None
                for kp in range(8):
                    ip = rot.get("pspair", 0) % 2
                    rot["pspair"] = rot.get("pspair", 0) + 1
                    pSS = pdbl[ip]
                    kSS = [("pb", 2 * ip), ("pb", 2 * ip + 1)]
                    for u in range(2):
                        kt = 2 * kp + u
                        sch.op(pe, lambda: T.matmul(pSS[:, u * 512:(u + 1) * 512], lhsT=KhT[s_][0:96, kt * 128:(kt + 1) * 128],
                                                    rhs=QhT[s_][0:96, qs], start=True, stop=True),
                               reads=[("KhT", s_), ("QhT", s_)], writes=[kSS[u]])
                    ps_ = rot.get("ptslot", 0) % 3
                    rot["ptslot"] = rot.get("ptslot", 0) + 1
                    PP = PTt[:, ps_ * 1024:(ps_ + 1) * 1024]
                    sch.op(act, lambda: A.activation(out=PP, in_=pSS[:, :], func=AF.Exp, scale=att_scale),
                           reads=kSS, writes=[("PT", ps_)])
                    if prev is not None:
                        for a_ in prev:
                            pv(*a_)
                    prev = [(2 * kp + u, PP[:, u * 512:(u + 1) * 512], ps_) for u in range(2)]
                for a_ in prev:
                    pv(*a_)
                rec = stat[:, 32 + (qb % 2) * 4:36 + (qb % 2) * 4]
                sch.op(dve, lambda: V.reciprocal(out=rec, in_=pOv[:, :, 64]), reads=[kO], writes=[("rec", qb % 2)])
                sch.op(dve, lambda: V.tensor_tensor(out=otok[:, qb * 4:(qb + 1) * 4, h * 64:(h + 1) * 64], in0=pOv[:, :, 0:64],
                                                    in1=rec.unsqueeze(2).broadcast_to([128, 4, 64]), op=ALU.mult),
                       reads=[kO, ("rec", qb % 2)], writes=[("otok", qb)])

        produce_kq(0)
        for h in range(8):
            if h + 1 < 8:
                produce_kq(h + 1)
            attend(h)

        def flush_oT(br):
            for blk in range(4):
                sl = blk % 2
                sv = stg[:, sl * 512:(sl + 1) * 512]
                for j in range(4):
                    pt, ptkey = tbank()
                    ptv = pt[:].rearrange("p (k t) -> p k t", k=8)
                    for i in range(4):
                        sch.op(pe, lambda i=i: T.transpose(ptv[:, i, :], otok[:, blk * 4 + i, j * 128:(j + 1) * 128], identb[:]),
                               reads=[("otok", blk), "ident"], writes=[ptkey], sig=(i == 3))
                    st_ = stg[:, sl * 512:(sl + 1) * 512]
                    if j % 2 == 0:
                        sch.op(act, lambda: A.copy(out=st_.rearrange("p (i t) -> p i t", i=4), in_=ptv[:, 0:4, :]),
                               reads=[ptkey], writes=[("stg", sl)])
                    else:
                        sch.op(dve, lambda: V.tensor_copy(out=st_.rearrange("p (i t) -> p i t", i=4), in_=ptv[:, 0:4, :]),
                               reads=[ptkey], writes=[("stg", sl)])
                    sch.dma(sp, oT_d[br, :, j, blk * 512:(blk + 1) * 512], st_, reads=[("stg", sl)],
                            writes=[("oTd", br, blk, j)], grp=("stg", sl))
                    sl = (sl + 1) % 2

        if stage == 3:
            dump("otok", T_C[:], [("otok", q) for q in range(4)])
            dump("kh", bigb[:, 0:8192], [("KhT", 0), ("KhT", 1), ("QhT", 0), ("QhT", 1)])
        checkpoint(3)
        flush_oT(0)
        sch.barrier()

        checkpoint(4)
        WinU = T_W[:, 0:4096].rearrange("p (k n) -> p k n", k=8)
        WinVG = T_W[:, 4096:8448].rearrange("p (k n) -> p k n", k=8)
        WqBD = T_W[:, 8448:8960].rearrange("p (j n) -> p j n", j=4)
        WkBD = T_W[:, 8960:9472].rearrange("p (j n) -> p j n", j=4)
        for nm, lo, hi in (("W0", 0, 4096), ("W1", 4096, 8448), ("W1b", 4096, 8448), ("W3", 8448, 8960), ("W4", 8960, 9472)):
            sch.set_alias(nm, lo, hi)
        wload(WinU, win_b[:, C_MU:C_MU + 512], "W0", "win", 8)
        sch.dma(sp, WinVG[:, :, 0:512], win_b[:, C_MV:C_MV + 512].rearrange("(k p) n -> p k n", p=128),
                reads=[("w", "win")], writes=["W1"], grp="W1")
        sch.dma(sp, WinVG[:, :, 512:544], win_b[:, C_MG:C_MG + 32].rearrange("(k p) n -> p k n", p=128),
                reads=[("w", "win")], writes=["W1"], grp="W1")
        sch.op(dve, lambda: V.memset(T_W[:, 8448:9472], 0.0), writes=["W3", "W4"])
        for (Wt, src, key) in ((WqBD, wq_b, "W3"), (WkBD, wk_b, "W4")):
            s4 = src.rearrange("(j r c) d -> r c j d", j=4, r=2)
            for r in range(2):
                sch.dma(sp, Wt[r * 64:(r + 1) * 64, :, r * 64:(r + 1) * 64], s4[r], reads=[("w", "wq"), ("w", "wk")],
                        writes=[key], grp=key)
        uT = T_big[:].rearrange("p (c t) -> p c t", c=4)
        Hh = T_big[:, 0:8192].rearrange("p (t c) -> p t c", t=16)
        cT = T_C[:].rearrange("p (c t) -> p c t", c=4)
        vaug = Vaug
        accf = T_A[:].bitcast(F32)
        sch.op(dve, lambda: V.memset(uT[:, :, 14:16], 0.0), writes=["uTpad"])
        sch.op(dve, lambda: V.memset(uT[:, :, 2064:2066], 0.0), writes=["uTpad"])
        sch.op(dve, lambda: V.memset(vaug[:, :, :, 64:65], 1.0), writes=["Vaug"])
        checkpoint(4.1)
        for ct in range(4):
            for blk in range(4):
                pu, ku = bank("cu", [0, 1])
                mm_acc(pu[:, :], [(WinU[:, kc, ct * 128:(ct + 1) * 128], hT[:, kc, blk * 512:(blk + 1) * 512]) for kc in range(8)],
                       ku, htkeys(blk) + ["W0"])
                sch.op(act, lambda: A.copy(out=uT[:, ct, 16 + blk * 512:16 + (blk + 1) * 512], in_=pu[:, :]),
                       reads=[ku], writes=[("uT", ct)])
        checkpoint(4.2)
        for tt in range(NT):
            pv, kv = bank("cv", [2, 3])
            pg, kg = bank("cg", [4, 5])
            mm_acc(pv[:, :], [(hT[:, kc, tt * 128:(tt + 1) * 128], WinVG[:, kc, 0:512]) for kc in range(8)], kv, [("hT", tt), "W1"])
            mm_acc(pg[:, 0:32], [(hT[:, kc, tt * 128:(tt + 1) * 128], WinVG[:, kc, 512:544]) for kc in range(8)], kg,
                   [("hT", tt), "W1"])
            sch.op(act, lambda: A.copy(out=vaug[:, tt, :, 0:64], in_=pv[:].rearrange("p (h c) -> p h c", h=8)),
                   reads=[kv], writes=["Vaug"])
            Gt = gtmp[:, 0:32]
            sch.op(dve, lambda: V.tensor_tensor(out=Gt, in0=pg[:, 0:32], in1=gbias_bc[:], op=ALU.add),
                   reads=[kg, "gbias"], writes=["Gt"])
            G4 = Gt.rearrange("p (a h) -> p a h", a=4)
            Fv = gtmp[:, 0:32].rearrange("p (d a h) -> p d a h", d=2, a=2)[:, :, 1, :]
            Iv = gtmp[:, 0:32].rearrange("p (d a h) -> p d a h", d=2, a=2)[:, :, 0, :]
            sp_ = gtmp[:, 32:48].rearrange("p (d h) -> p d h", d=2)
            sch.op(act, lambda: A.activation(out=sp_, in_=Fv, func=AF.Exp, scale=-1.0), reads=["Gt"], writes=["spg"])
            sch.op(act, lambda: A.activation(out=sp_, in_=sp_, func=AF.Ln, bias=oneb[:], scale=1.0), reads=["spg", "oneb"], writes=["spg"])
            pc, kc_ = bank("cc", [0, 1])
            sch.op(pe, lambda: T.matmul(pc[:, 0:8], lhsT=cst[:, K_TF:K_TF + 128], rhs=gtmp[:, 32:40], start=True, stop=True),
                   reads=["spg", "cst"], writes=[kc_], sig=False)
            sch.op(pe, lambda: T.matmul(pc[:, 8:16], lhsT=cst[:, K_TB:K_TB + 128], rhs=gtmp[:, 40:48], start=True, stop=True),
                   reads=["spg", "cst"], writes=[kc_], sig=False)
            sch.op(pe, lambda: T.matmul(pc[:, 16:32], lhsT=cst[:, K_I0:K_I0 + 128], rhs=gtmp[:, 32:48], start=True, stop=True),
                   reads=["spg", "cst"], writes=[kc_], sig=False)
            sch.op(pe, lambda: T.matmul(pc[:, 32:48], lhsT=cst[:, K_I1:K_I1 + 128], rhs=gtmp[:, 32:48], start=True, stop=True),
                   reads=["spg", "cst"], writes=[kc_])
            tsl16 = slice(tt * 16, (tt + 1) * 16)
            ic = gtmp[:, 48:64].rearrange("p (d h) -> p d h", d=2)
            sch.op(dve, lambda: V.tensor_tensor(out=ic, in0=Iv, in1=pc[:, 0:16].rearrange("p (d h) -> p d h", d=2), op=ALU.add),
                   reads=["Gt", kc_], writes=["ic"])
            sch.op(act, lambda: A.activation(out=gA[:, tsl16], in_=gtmp[:, 48:64], func=AF.Exp), reads=["ic"], writes=[("gA", tt)])
            sch.op(act, lambda: A.activation(out=gE[:, tsl16], in_=pc[:, 0:16], func=AF.Exp, scale=-1.0), reads=[kc_], writes=[("gE", tt)])
            sch.op(act, lambda: A.activation(out=gDec[:, tt * 32:(tt + 1) * 32], in_=pc[:, 16:48], func=AF.Exp, scale=-1.0),
                   reads=[kc_], writes=[("gDec", tt)])
            sch.op(dve, lambda: V.tensor_tensor(out=gWt[0:64, tsl16], in0=gA[0:64, tsl16], in1=gDec[0:64, tt * 32:tt * 32 + 16], op=ALU.mult),
                   reads=[("gA", tt), ("gDec", tt)], writes=[("gW", tt)])
            sch.op(dve, lambda: V.tensor_tensor(out=gWt[64:128, tsl16], in0=gA[64:128, tsl16], in1=gDec[64:128, tt * 32 + 16:tt * 32 + 32],
                                                op=ALU.mult), reads=[("gA", tt), ("gDec", tt)], writes=[("gW", tt)])
        checkpoint(4.5)
        for ct in range(4):
            eng, E_ = dve, V
            acc = accf[:, (ct % 2) * 2048:(ct % 2 + 1) * 2048]
            ak = ("acc", ct % 2)
            sch.op(eng, lambda: E_.tensor_scalar(out=acc, in0=uT[:, ct, 14:14 + 2048], scalar1=convw[:, ct * 5:ct * 5 + 1], scalar2=None,
                                                 op0=ALU.mult), reads=[("uT", ct), "uTpad", "convw"], writes=[ak])
            for j in range(1, 5):
                sch.op(eng, lambda j=j: E_.scalar_tensor_tensor(out=acc, in0=uT[:, ct, 14 + j:14 + j + 2048],
                                                                scalar=convw[:, ct * 5 + j:ct * 5 + j + 1], in1=acc,
                                                                op0=ALU.mult, op1=ALU.add),
                       reads=[("uT", ct), "uTpad", "convw", ak], writes=[ak])
            sch.op(act, lambda: A.activation(out=cT[:, ct, :], in_=acc, func=AF.Silu), reads=[ak], writes=[("cT", ct)])
        sch.barrier()

        if stage == 5:
            dump("cT", T_C[:], [("cT", c_) for c_ in range(4)])
            dump("ga", gA[:], [("gA", t) for t in range(NT)])
            dump("ge", gE[:], [("gE", t) for t in range(NT)])
            dump("gw", gWt[:], [("gW", t) for t in range(NT)])
            dump("gd", gDec[:], [("gDec", t) for t in range(NT)])
        checkpoint(5)
        def tA(off, n):
            return T_A[:, off:off + n]
        qTz = [[[tA(((d * 2 + sl) * 2 + r) * 512, 512).rearrange("p (j t) -> p j t", j=4) for r in range(2)] for sl in range(2)]
               for d in range(2)]
        kTf = [tA(4096 + d * 512, 512).rearrange("p (j t) -> p j t", j=4) for d in range(2)]
        ktkz = [[tA(5120 + (d * 2 + r0) * 512, 512) for r0 in range(2)] for d in range(2)]
        PTm = [[tA(7168 + (d * 2 + r0) * 512, 512) for r0 in range(2)] for d in range(2)]
        vwf = [tA(9216 + d * 520, 520).rearrange("p (h c) -> p h c", h=8) for d in range(2)]
        vaf = [[T_W[:, 11288 + (d * 2 + sl) * 520:11288 + (d * 2 + sl + 1) * 520].rearrange("p (h c) -> p h c", h=8)
                for sl in range(2)] for d in range(2)]
        Cbf = [T_W[:, 10768 + d * 260:10768 + (d + 1) * 260].rearrange("p (j c) -> p j c", j=4) for d in range(2)]
        Cst = [T_W[:, 9728 + d * 520:9728 + (d + 1) * 520].bitcast(F32).rearrange("p (j c) -> p j c", j=4) for d in range(2)]
        pm = [cst[:, K_I0:K_I0 + 1], cst[:, K_I1:K_I1 + 1]]
        pm8 = stat[:, 28:30]
        sch.op(dve, lambda: V.tensor_scalar(out=pm8[:, 0:1], in0=pm[0], scalar1=0.125, scalar2=None, op0=ALU.mult), reads=["cst"], writes=["pm8"])
        sch.op(dve, lambda: V.tensor_scalar(out=pm8[:, 1:2], in0=pm[1], scalar1=0.125, scalar2=None, op0=ALU.mult), reads=["cst", "pm8"], writes=["pm8"])
        for d in range(2):
            sch.op(dve, lambda d=d: V.memset(Cst[d], 0.0), writes=[("Cst", d)])
            sch.op(dve, lambda d=d: V.memset(Cbf[d], 0.0), writes=[("Cbf", d)])
            for r0 in range(2):
                sch.op(dve, lambda d=d, r0=r0: V.memset(PTm[d][r0], 0.0), writes=[("PTm", d, r0)])

        pcb = [[(pbk[6], ("pt", 0)), (pbk[7], ("pt", 1))], [(pbk[1], ("pb", 1)), (pbk[3], ("pb", 3))]]

        def geom(d, i):
            c = i if d == 0 else 31 - i
            return c, c // 2, c % 2, (i // 2) % 2

        def prep(d, i):
            c, m, r0, sl = geom(d, i)
            rs = slice(r0 * 64, (r0 + 1) * 64)
            tsl = slice(m * 128, (m + 1) * 128)
            col = slice(m * 16 + d * 8, m * 16 + d * 8 + 8)
            if i % 2 == 0:
                pq, kq = bank("mj", [2])
                for j in range(4):
                    sch.op(pe, lambda j=j: T.matmul(pq[:, j * 128:(j + 1) * 128], lhsT=WqBD[:, j, :], rhs=cT[:, j, tsl], start=True, stop=True),
                           reads=[("cT", j), "W3"], writes=[kq], sig=(j == 3))
                pq3 = pq[:].rearrange("p (j t) -> p j t", j=4)
                for r in range(2):
                    sch.op(act, lambda r=r: A.activation(out=qTz[d][sl][r], in_=pq3, func=AF.Copy, scale=pm[r]),
                           reads=[kq, "cst"], writes=[("qTz", d, sl, r)])
                pk, kk = bank("mj", [2])
                for j in range(4):
                    sch.op(pe, lambda j=j: T.matmul(pk[:, j * 128:(j + 1) * 128], lhsT=WkBD[:, j, :], rhs=cT[:, j, tsl], start=True, stop=True),
                           reads=[("cT", j), "W4"], writes=[kk], sig=(j == 3))
                sch.op(act, lambda: A.mul(out=kTf[d], in_=pk[:].rearrange("p (j t) -> p j t", j=4), mul=0.125),
                       reads=[kk], writes=[("kTf", d)])
                pk2, kk2 = bank("mj", [2])
                for j in range(4):
                    sch.op(pe, lambda j=j: T.matmul(pk2[:, j * 128:(j + 1) * 128], lhsT=cT[:, j, tsl], rhs=WkBD[:, j, :], start=True, stop=True),
                           reads=[("cT", j), "W4"], writes=[kk2], sig=(j == 3))
                for rr in range(2):
                    sch.op(act, lambda rr=rr: A.activation(out=ktkz[d][rr], in_=pk2[:, :], func=AF.Copy, scale=pm8[:, rr:rr + 1]),
                           reads=[kk2, "pm8"], writes=[("ktkz", d, rr)])
                sch.op(pool, lambda: G.tensor_tensor(out=vaf[d][sl], in0=vaug[:, m, :, :],
                                                     in1=gA[:, col].unsqueeze(2).broadcast_to([128, 8, 65]), op=ALU.mult),
                       reads=["Vaug", ("gA", m)], writes=[("vaf", d, sl)])
                sch.op(pool, lambda: G.tensor_tensor(out=vwf[d], in0=vaug[:, m, :, :],
                                                     in1=gWt[:, col].unsqueeze(2).broadcast_to([128, 8, 65]), op=ALU.mult),
                       reads=["Vaug", ("gW", m)], writes=[("vwf", d)])
            pS, kS = pbk[0], ("pb", 0)
            pSv = pS[:].rearrange("p (h t) -> p h t", h=8)
            for h in range(8):
                j, r = h // 2, h % 2
                sch.op(pe, lambda h=h, j=j, r=r: T.matmul(pSv[rs, h, :], lhsT=kTf[d][:, j, rs], rhs=qTz[d][sl][r][:, j, rs],
                                                         start=True, stop=True),
                       reads=[("kTf", d), ("qTz", d, sl, r)], writes=[kS], sig=(h == 7))
            mk = cst[rs, (K_MF if d == 0 else K_MB):(K_MF if d == 0 else K_MB) + 64]
            sch.op(dve, lambda: V.tensor_tensor(out=PTm[d][r0][rs].rearrange("p (h t) -> p h t", h=8), in0=pSv[rs],
                                                in1=mk.unsqueeze(1).broadcast_to([64, 8, 64]), op=ALU.mult),
                   reads=[kS, "cst"], writes=[("PTm", d, r0)])
            pC, kC = pcb[d][i % 2]
            pCv = pC[:, 0:260].rearrange("p (j c) -> p j c", j=4)
            for h in range(8):
                j, r = h // 2, h % 2
                pr = slice(r * 64, (r + 1) * 64)
                sch.op(pe, lambda h=h, j=j, pr=pr: T.matmul(pCv[pr, j, :], lhsT=ktkz[d][r0][:, h * 64:(h + 1) * 64], rhs=vwf[d][:, h, :],
                                                           start=True, stop=True),
                       reads=[("ktkz", d, r0), ("vwf", d)], writes=[kC], sig=(h == 7))

        def recur(d, i):
            c, m, r0, sl = geom(d, i)
            rs = slice(r0 * 64, (r0 + 1) * 64)
            col = slice(m * 16 + d * 8, m * 16 + d * 8 + 8)
            pN0, kN0 = pbk[4], ("pb", 4)
            pN1, kN1 = pbk[5], ("pb", 5)
            for h in range(8):
                j, r = h // 2, h % 2
                pn, kn = (pN0, kN0) if h < 4 else (pN1, kN1)
                o_ = pn[rs, (h % 4) * 65:(h % 4 + 1) * 65]
                sch.op(pe, lambda h=h, o_=o_: T.matmul(o_, lhsT=PTm[d][r0][:, h * 64:(h + 1) * 64], rhs=vaf[d][sl][:, h, :],
                                                       start=True, stop=False),
                       reads=[("PTm", d, r0), ("vaf", d, sl)], writes=[kn], sig=False)
                sch.op(pe, lambda h=h, j=j, r=r, o_=o_: T.matmul(o_, lhsT=qTz[d][sl][r][:, j, rs], rhs=Cbf[d][:, j, :], start=False, stop=True),
                       reads=[("qTz", d, sl, r), ("Cbf", d)], writes=[kn], sig=(h % 4 == 3))
            pC, kC = pcb[d][i % 2]
            pCv = pC[:, 0:260].rearrange("p (j c) -> p j c", j=4)
            dcy = gDec[:, m * 32 + r0 * 16 + d * 8:m * 32 + r0 * 16 + d * 8 + 8]
            dv4 = dcy.rearrange("p (j r) -> p j r", r=2)
            for r in range(2):
                pr = slice(r * 64, (r + 1) * 64)
                sch.op(dve, lambda r=r, pr=pr: V.tensor_tensor(out=Cst[d][pr], in0=Cst[d][pr],
                                                              in1=dv4[pr, :, r].unsqueeze(2).broadcast_to([64, 4, 65]), op=ALU.mult),
                       reads=[("gDec", m), ("Cst", d)], writes=[("Cst", d)])
            sch.op(dve, lambda: V.tensor_tensor(out=Cst[d], in0=Cst[d], in1=pCv, op=ALU.add), reads=[kC, ("Cst", d)], writes=[("Cst", d)])
            sch.op(act, lambda: A.copy(out=Cbf[d], in_=Cst[d]), reads=[("Cst", d)], writes=[("Cbf", d)])
            ecol = gE[rs, col]
            dn = stat[rs, 64 + d * 16:72 + d * 16]
            dn2 = stat[rs, 72 + d * 16:80 + d * 16]
            for hb, (pn, kn) in enumerate(((pN0, kN0), (pN1, kN1))):
                pnv = pn[rs, 0:260].rearrange("p (h c) -> p h c", h=4)
                sch.op(act, lambda pnv=pnv, hb=hb: A.activation(out=dn[:, hb * 4:(hb + 1) * 4], in_=pnv[:, :, 64], func=AF.Abs),
                       reads=[kn], writes=[("dn", d)])
            sch.op(dve, lambda: V.reciprocal(out=dn, in_=dn), reads=[("dn", d)], writes=[("dn", d)])
            sch.op(dve, lambda: V.tensor_tensor(out=dn, in0=dn, in1=ecol, op=ALU.min), reads=[("dn", d), ("gE", m)], writes=[("dn", d)])
            first = (i < 16)
            if first:
                hn = Hh[rs, m, :].rearrange("p (h c) -> p h c", h=8)
                hk = ("H", m)
            else:
                hn = tmpf[rs, d * 512:(d + 1) * 512].rearrange("p (h c) -> p h c", h=8)
                hk = ("hn", d)
            for hb, (pn, kn) in enumerate(((pN0, kN0), (pN1, kN1))):
                pnv = pn[rs, 0:260].rearrange("p (h c) -> p h c", h=4)
                sch.op(dve, lambda pnv=pnv, hb=hb: V.tensor_tensor(out=hn[:, hb * 4:(hb + 1) * 4, :], in0=pnv[:, :, 0:64],
                                                                  in1=dn[:, hb * 4:(hb + 1) * 4].unsqueeze(2).broadcast_to([64, 4, 64]),
                                                                  op=ALU.mult), reads=[kn, ("dn", d)], writes=[hk])
            if not first:
                sch.op(pool, lambda: G.tensor_tensor(out=Hh[rs, m, :], in0=Hh[rs, m, :], in1=tmpf[rs, d * 512:(d + 1) * 512], op=ALU.add),
                       reads=[("hn", d), ("H", m)], writes=[("H", m)])

        for d in range(2):
            prep(d, 0)
        for i in range(32):
            for d in range(2):
                if i + 1 < 32:
                    prep(d, i + 1)
                recur(d, i)

        if stage == 6:
            dump("H", T_big[:, 0:8192], [("H", t) for t in range(NT)])
        checkpoint(6)
        WinO = T_W[:, 0:4096].rearrange("p (k n) -> p k n", k=8)
        wload(WinO, win_b[:, C_MO:C_MO + 512], "W0", "win", 8)
        sch.barrier()
        otok2 = T_A[:, 0:8192].rearrange("p (t c) -> p t c", t=16)
        for tt in range(NT):
            po, ko = bank("co", [0, 1])
            mm_acc(po[:, :], [(hT[:, kc, tt * 128:(tt + 1) * 128], WinO[:, kc, :]) for kc in range(8)], ko, [("hT", tt), "W0"])
            sg = tmpf[:, 512:1024]
            sch.op(act, lambda: A.activation(out=sg, in_=po[:, :], func=AF.Sigmoid), reads=[ko], writes=["sg"])
            sq = tmpf[:, 0:512]
            Ht = Hh[:, tt, :]
            sch.op(act, lambda: A.activation(out=sq, in_=Ht, func=AF.Square, scale=0.125), reads=[("H", tt)], writes=["sq"])
            ms = stat[:, 48:56]
            sch.op(dve, lambda: V.tensor_reduce(out=ms, in_=sq.rearrange("p (h c) -> p h c", h=8), axis=mybir.AxisListType.X, op=ALU.add),
                   reads=["sq"], writes=["ms"])
            sch.op(act, lambda: A.activation(out=ms, in_=ms, func=AF.Sqrt, bias=epsb[:], scale=1.0), reads=["ms", "epsb"], writes=["ms"])
            sch.op(dve, lambda: V.reciprocal(out=ms, in_=ms), reads=["ms"], writes=["ms"])
            sch.op(dve, lambda: V.tensor_tensor(out=sq.rearrange("p (h c) -> p h c", h=8), in0=Ht.rearrange("p (h c) -> p h c", h=8),
                                                in1=ms.unsqueeze(2).broadcast_to([128, 8, 64]), op=ALU.mult),
                   reads=[("H", tt), "ms"], writes=["sq"])
            sch.op(pool, lambda: G.tensor_tensor(out=sq, in0=sq, in1=ghead_bc[:], op=ALU.mult), reads=["sq", "ghead"], writes=["sq"])
            sch.op(dve, lambda: V.tensor_tensor(out=otok2[:, tt, :], in0=sq, in1=sg, op=ALU.mult), reads=["sq", "sg"],
                   writes=[("otok", tt // 4)])
        otok_save = otok
        otok = otok2
        flush_oT(1)
        otok = otok_save
        sch.barrier()

        checkpoint(7)
        WkvA = T_W[:, 0:4096].rearrange("p (k n) -> p k n", k=8)
        WkvB = T_W[:, 4096:8192].rearrange("p (k n) -> p k n", k=8)
        WinQ = T_W[:, 8192:12288].rearrange("p (k n) -> p k n", k=8)
        for nm, lo, hi in (("W0", 0, 4096), ("W1", 4096, 8192), ("W2", 8192, 12288)):
            sch.set_alias(nm, lo, hi)
        wload(WkvA, wkv_b[:, 0:512], "W0", "wkv", 8)
        wload(WkvB, wkv_b[:, 512:1024], "W1", "wkv", 8)
        wload(WinQ, win_b[:, C_MQ:C_MQ + 512], "W2", "win", 8)
        memT = T_A[:, 0:2048].rearrange("p (k t) -> p k t", k=8)
        kmT = T_A[:, 2048:3072].rearrange("p (h t) -> p h t", h=4)
        vm = T_A[:, 3072:4104].rearrange("p (t h c) -> p t h c", t=2, h=4)
        qm = [T_A[:, 4104 + i * 512:4104 + (i + 1) * 512] for i in range(2)]
        sch.op(dve, lambda: V.memset(vm[:, :, :, 128:129], 1.0), writes=["vm"])
        for t2 in range(2):
            slot = t2
            xsv = xs[:, slot * 1024:(slot + 1) * 1024]
            sch.dma(sp, xsv, mem_d[b, t2 * 128:(t2 + 1) * 128, :], writes=[("xs", slot)], grp=("xs", slot))
            rms_to_T(xsv, [("xs", slot)], 16, lambda t2=t2: memT[:, :, t2 * 128:(t2 + 1) * 128], [("memT", t2)], slot)
        checkpoint(7.1)
        MT = [("memT", 0), ("memT", 1)]
        for h in range(4):
            pk, kk = bank("dk", [0, 1])
            mm_acc(pk[:, 0:256], [(WkvA[:, kc, h * 128:(h + 1) * 128], memT[:, kc, :]) for kc in range(8)], kk, MT + ["W0"])
            sch.op(act, lambda: A.copy(out=kmT[:, h, :], in_=pk[:, 0:256]), reads=[kk], writes=["kmT"])
        for t2 in range(2):
            pv, kv = bank("dv", [2, 3])
            mm_acc(pv[:, :], [(memT[:, kc, t2 * 128:(t2 + 1) * 128], WkvB[:, kc, :]) for kc in range(8)], kv, MT + ["W1"])
            sch.op(act, lambda: A.copy(out=vm[:, t2, :, 0:128], in_=pv[:].rearrange("p (h c) -> p h c", h=4)), reads=[kv], writes=["vm"])
        checkpoint(7.2)
        mscale = 128.0 ** -0.5
        items = [(h, qb) for h in range(4) for qb in range(4)]
        dst_ = {}

        def d_q(i):
            h, qb = items[i]
            qs = slice(qb * 512, (qb + 1) * 512)
            pq, kq = bank("dq", [6, 7])
            mm_acc(pq[:, :], [(WinQ[:, kc, h * 128:(h + 1) * 128], hT[:, kc, qs]) for kc in range(8)], kq, htkeys(qb) + ["W2"])
            qi = i % 2
            sch.op(dve, lambda: V.tensor_copy(out=qm[qi], in_=pq[:, :]), reads=[kq], writes=[("qm", qi)])

        def d_st(i):
            h, qb = items[i]
            qi = i % 2
            ip = i % 2
            pSS = pdbl[ip]
            kSS = [("pb", 2 * ip), ("pb", 2 * ip + 1)]
            for kt in range(2):
                sch.op(pe, lambda kt=kt: T.matmul(pSS[:, kt * 512:(kt + 1) * 512], lhsT=kmT[:, h, kt * 128:(kt + 1) * 128], rhs=qm[qi],
                                                  start=True, stop=True), reads=["kmT", ("qm", qi)], writes=[kSS[kt]])
            ps_ = i % 3
            PP = PTt[:, ps_ * 1024:(ps_ + 1) * 1024]
            sch.op(act, lambda: A.activation(out=PP, in_=pSS[:, :], func=AF.Exp, scale=mscale), reads=kSS, writes=[("PT", ps_)])
            dst_[i] = (PP, ps_)

        def d_pv(i):
            h, qb = items[i]
            PP, ps_ = dst_[i]
            pOa, kOa = bank("dOa", [4])
            pOb, kOb = bank("dOb", [5])
            for kt in range(2):
                P_ = PP[:, kt * 512:(kt + 1) * 512]
                for j in range(4):
                    pO_, kO_ = (pOa, kOa) if j < 2 else (pOb, kOb)
                    sch.op(pe, lambda j=j, pO_=pO_, P_=P_: T.matmul(pO_[:, (j % 2) * 129:(j % 2 + 1) * 129], lhsT=P_[:, j * 128:(j + 1) * 128],
                                                                    rhs=vm[:, kt, h, :], start=(kt == 0 and j % 2 == 0), stop=(kt == 1),
                                                                    skip_group_check=True),
                           reads=[("PT", ps_), "vm"], writes=[kO_], sig=(j % 2 == 1))
            rec = stat[:, 32:36]
            for half, (pO_, kO_) in enumerate(((pOa, kOa), (pOb, kOb))):
                pv2 = pO_[:, 0:258].rearrange("p (j c) -> p j c", j=2)
                rc = rec[:, half * 2:half * 2 + 2]
                sch.op(dve, lambda: V.reciprocal(out=rc, in_=pv2[:, :, 128]), reads=[kO_], writes=[("rec", half)])
                sch.op(dve, lambda: V.tensor_tensor(out=otok[:, qb * 4 + half * 2:qb * 4 + half * 2 + 2, h * 128:(h + 1) * 128],
                                                    in0=pv2[:, :, 0:128], in1=rc.unsqueeze(2).broadcast_to([128, 2, 128]), op=ALU.mult),
                       reads=[kO_, ("rec", half)], writes=[("otok", qb)])

        d_q(0)
        d_st(0)
        for i in range(len(items)):
            if i + 1 < len(items):
                d_q(i + 1)
                d_st(i + 1)
            d_pv(i)
        checkpoint(7.4)
        flush_oT(2)
        sch.barrier()

        if dbg:
            for br in range(3):
                for j in range(4):
                    sch.dma(pool, dbg_d[br, :, j, :], oT_d[br, :, j, :], reads=[("oTd", br, k, j) for k in range(4)], writes=[("dbg", br, j)],
                            grp=("dbg", br, j), final=True)

        checkpoint(8)
        x1 = T_big[:, 0:4096].rearrange("p (t c) -> p t c", t=4)
        mrg = bigb[:, 8192:12288].rearrange("p (k t) -> p k t", k=8)
        h2T = bigb[:, 12288:16384].rearrange("p (k t) -> p k t", k=8)
        aT = T_A[:].rearrange("p (c t) -> p c t", c=NFF)
        oTs = [T_C[:, br * 2048:(br + 1) * 2048].rearrange("p (j t) -> p j t", j=4) for br in range(3)]
        sgb = [T_V[:, i * 512:(i + 1) * 512] for i in range(3)]
        prd = [T_V[:, 1536 + i * 1024:1536 + (i + 1) * 1024].bitcast(F32) for i in range(3)]
        WS = [T_W[:, i * 4608:(i + 1) * 4608] for i in range(3)]
        for i in range(3):
            sch.set_alias(("WS", i), i * 4608, (i + 1) * 4608)
        units = []

        def e1_unit(blk, f):
            bs = slice(blk * 512, (blk + 1) * 512)

            def load(sl):
                sch.dma(sp, WS[sl][:, :], wge_b[f], reads=[("w", "wge")], writes=[("WS", sl)], grp=("WS", sl))

            def consume(sl):
                if f == 0:
                    for br in range(3):
                        sch.dma(sp, oTs[br], oT_d[br, :, :, bs], reads=[("oTd", br, blk, j) for j in range(4)], writes=[("oTs", br)],
                                grp=("oTs", br))
                Wg_ = WS[sl][:, 0:3072].rearrange("p (k b n) -> p k b n", k=8, b=3)
                Wb_ = WS[sl][:, 3072:4608].rearrange("p (k b n) -> p k b n", k=4, b=3)
                for br in range(3):
                    pg, kg = bank("eg", [0, 1])
                    pb_, kb_ = bank("eb", [2, 3])
                    mm_acc(pg[:, :], [(Wg_[:, kc, br, :], hT[:, kc, bs]) for kc in range(8)], kg, htkeys(blk) + [("WS", sl)])
                    mm_acc(pb_[:, :], [(Wb_[:, kc, br, :], oTs[br][:, kc, :]) for kc in range(4)], kb_, [("oTs", br), ("WS", sl)])
                    sch.op(act, lambda: A.activation(out=sgb[br], in_=pg[:, :], func=AF.Sigmoid), reads=[kg], writes=[("sgb", br)])
                    sch.op(dve, lambda: V.tensor_tensor(out=prd[br], in0=pb_[:, :], in1=sgb[br], op=ALU.mult),
                           reads=[kb_, ("sgb", br)], writes=[("prd", br)])
                sch.op(pool, lambda: G.tensor_tensor(out=prd[0], in0=prd[0], in1=prd[1], op=ALU.add),
                       reads=[("prd", 0), ("prd", 1)], writes=[("prd", 0)])
                sch.op(pool, lambda: G.tensor_tensor(out=mrg[:, f, :], in0=prd[0], in1=prd[2], op=ALU.add),
                       reads=[("prd", 0), ("prd", 2)], writes=[("mrg", f)])
            return load, consume

        MRG = [("mrg", f) for f in range(8)]
        H2 = [("h2T", t) for t in range(4)]

        def e2_unit(blk, nh):
            def load(sl):
                sch.dma(sp, WS[sl][:, 0:4096].rearrange("p (k n) -> p k n", k=8),
                        wout_b[:, nh * 512:(nh + 1) * 512].rearrange("(k p) n -> p k n", p=128),
                        reads=[("w", "wout")], writes=[("WS", sl)], grp=("WS", sl))

            def consume(sl):
                Wo = WS[sl][:, 0:4096].rearrange("p (k n) -> p k n", k=8)
                for tt in range(4):
                    slot = tt % 2
                    gt = blk * 4 + tt
                    xh = xs[:, slot * 1024:slot * 1024 + 512]
                    sch.dma(sp, xh, x_d[b, gt * 128:(gt + 1) * 128, nh * 512:(nh + 1) * 512], writes=[("xs", slot)], grp=("xs", slot))
                    py, ky = bank("ey", [4, 5])
                    mm_acc(py[:, :], [(mrg[:, kc, tt * 128:(tt + 1) * 128], Wo[:, kc, :]) for kc in range(8)], ky, MRG + [("WS", sl)])
                    sch.op(dve, lambda: V.tensor_tensor(out=x1[:, tt, nh * 512:(nh + 1) * 512], in0=py[:, :], in1=xh,
                                                        op=ALU.add), reads=[ky, ("xs", slot)], writes=[("x1", tt)])
                if nh == 1:
                    for k in range(5):
                        if k < 4:
                            rms_a(x1[:, k, :], [("x1", k)], k % 2)
                        if k >= 1:
                            rms_b(8, lambda tt=k - 1: h2T[:, :, tt * 128:(tt + 1) * 128], [("h2T", k - 1)], (k - 1) % 2)
            return load, consume

        def e3a_unit(blk, u):
            def load(sl):
                sch.dma(sp, WS[sl][:, 0:4096], wgu_b[u], reads=[("w", "wgu")], writes=[("WS", sl)], grp=("WS", sl))

            def consume(sl):
                WF = WS[sl][:, 0:4096].rearrange("p (c g k n) -> p c g k n", c=2, g=2, k=8)
                for c2 in range(2):
                    c = 2 * u + c2
                    pg, kg = bank("fg", [0, 1])
                    pu, ku = bank("fu", [2, 3])
                    mm_acc(pg[:, :], [(WF[:, c2, 0, kc, :], h2T[:, kc, :]) for kc in range(8)], kg, H2 + [("WS", sl)])
                    mm_acc(pu[:, :], [(WF[:, c2, 1, kc, :], h2T[:, kc, :]) for kc in range(8)], ku, H2 + [("WS", sl)])
                    si = c % 3
                    sch.op(act, lambda: A.activation(out=prd[si], in_=pg[:, :], func=AF.Silu), reads=[kg], writes=[("prd", si)])
                    sch.op(dve, lambda: V.tensor_tensor(out=aT[:, c, :], in0=pu[:, :], in1=prd[si], op=ALU.mult),
                           reads=[ku, ("prd", si)], writes=[("aT", c)])
            return load, consume

        def e3b_unit(blk, u):
            c0 = 4 * u
            ncu = min(4, NFF - c0)

            def load(sl):
                sch.dma(sp, WS[sl][:, 0:ncu * 1024].rearrange("p (c n) -> p c n", c=ncu),
                        wd_b[c0 * 128:(c0 + ncu) * 128, :].rearrange("(c p) n -> p c n", p=128),
                        reads=[("w", "wd")], writes=[("WS", sl)], grp=("WS", sl))

            def consume(sl):
                Wd_ = WS[sl][:, 0:ncu * 1024].rearrange("p (c n) -> p c n", c=ncu)
                for tt in range(4):
                    for nh in range(2):
                        py, ky = bank("ey", [4, 5])
                        mm_acc(py[:, :], [(aT[:, c0 + c, tt * 128:(tt + 1) * 128], Wd_[:, c, nh * 512:(nh + 1) * 512]) for c in range(ncu)],
                               ky, [("aT", c0 + c) for c in range(ncu)] + [("WS", sl)])
                        sch.op(dve, lambda: V.tensor_tensor(out=x1[:, tt, nh * 512:(nh + 1) * 512], in0=py[:, :],
                                                            in1=x1[:, tt, nh * 512:(nh + 1) * 512], op=ALU.add),
                               reads=[ky, ("x1", tt)], writes=[("x1", tt)])
                if c0 + ncu == NFF:
                    for tt in range(4):
                        slot = tt % 2
                        gt = blk * 4 + tt
                        st = stat[:, 56 + slot * 2:56 + slot * 2 + 1]
                        xsv = xs[:, slot * 1024:(slot + 1) * 1024]
                        xnv = xn[:, slot * 1024:(slot + 1) * 1024]
                        sch.op(act, lambda: A.activation(out=xnv, in_=x1[:, tt, :], func=AF.Square, scale=1.0 / 32.0, accum_out=st),
                               reads=[("x1", tt)], writes=[("xn", slot), ("fst", slot)])
                        sch.op(act, lambda: A.activation(out=st, in_=st, func=AF.Sqrt, bias=epsb[:], scale=1.0),
                               reads=[("fst", slot), "epsb"], writes=[("fst", slot)])
                        sch.op(dve, lambda: V.reciprocal(out=st, in_=st), reads=[("fst", slot)], writes=[("fst", slot)])
                        sch.op(dve, lambda: V.scalar_tensor_tensor(out=xsv, in0=x1[:, tt, :], scalar=st, in1=gfin_bc[:],
                                                                   op0=ALU.mult, op1=ALU.mult),
                               reads=[("x1", tt), ("fst", slot), "gfin"], writes=[("xs", slot)])
                        sch.dma(sp, out_d[b, gt * 128:(gt + 1) * 128, :], xsv, reads=[("xs", slot)], grp=("xs", slot), final=True)
            return load, consume

        for blk in range(4):
            units += [e1_unit(blk, f) for f in range(8)]
            units += [e2_unit(blk, nh) for nh in range(2)]
            units += [e3a_unit(blk, u) for u in range(11)]
            units += [e3b_unit(blk, u) for u in range(6)]
        units[0][0](0)
        units[1][0](1)
        for ui, (ld_, cons_) in enumerate(units):
            cons_(ui % 3)
            if ui + 2 < len(units):
                units[ui + 2][0]((ui + 2) % 3)
        sch.barrier()


def make_consts():
    c = np.zeros((128, K_N), np.float32)
    c[:, K_ID:K_ID + 128] = np.eye(128, dtype=np.float32)
    p = np.arange(128)
    s = (p % 64)[:, None]
    t = np.arange(64)[None, :]
    c[:, K_MF:K_MF + 64] = (s <= t)
    c[:, K_MB:K_MB + 64] = (s >= t)
    ss = p[:, None]
    tt = p[None, :]
    same = (ss // 64) == (tt // 64)
    c[:, K_TF:K_TF + 128] = same & (ss <= tt)
    c[:, K_TB:K_TB + 128] = same & (ss >= tt)
    c[:, K_I0:K_I0 + 128] = (ss // 64 == 0) & (tt >= 0)
    c[:, K_I1:K_I1 + 128] = (ss // 64 == 1) & (tt >= 0)
    inv = (10000.0 ** (-np.arange(16, dtype=np.float32) / 16.0)).astype(np.float32)
    for i in range(32):
        c[64 + i, K_RP] = inv[i % 16]
        c[64 + i, K_RP + 1] = math.pi if i < 16 else 0.0
        c[64 + i, K_RP + 2] = 0.5 * math.pi
    c[:, K_RP + 3] = -math.pi
    return c


_CACHE = {}


def kernel(**inputs):
    f = lambda a: np.ascontiguousarray(np.asarray(a, dtype=np.float32))
    if "nc" not in _CACHE:
        _CACHE["nc"] = build_program()
    nc = _CACHE["nc"]
    shared = {
        "g_mix": f(inputs["g_mix"]).reshape(1, D),
        "w_in": f(inputs["w_in"]).reshape(D, N_IN),
        "mla_g_q": f(inputs["mla_g_q"]).reshape(1, 384),
        "mla_g_kv": f(inputs["mla_g_kv"]).reshape(1, 256),
        "mla_w_uq": f(inputs["mla_w_uq"]).reshape(384, 768),
        "mla_w_uk": f(inputs["mla_w_uk"]).reshape(256, 512),
        "mla_w_uv": f(inputs["mla_w_uv"]).reshape(256, 512),
        "ml_conv_w": f(inputs["ml_conv_w"]).reshape(5, 512),
        "ml_w_q": f(inputs["ml_w_q"]).reshape(512, 64),
        "ml_w_k": f(inputs["ml_w_k"]).reshape(512, 64),
        "ml_gate_bias": f(inputs["ml_gate_bias"]).reshape(1, 32),
        "ml_g_head": f(inputs["ml_g_head"]).reshape(1, 512),
        "mem_g": f(inputs["mem_g"]).reshape(1, D),
        "mem_w_kv": f(inputs["mem_w_kv"]).reshape(D, 1024),
        "w_branch": f(inputs["w_branch"]).reshape(1536, D),
        "w_out": f(inputs["w_out"]).reshape(D, D),
        "g_ffn": f(inputs["g_ffn"]).reshape(1, D),
        "w_ffn_gate": f(inputs["w_ffn_gate"]).reshape(D, DFF),
        "w_ffn_up": f(inputs["w_ffn_up"]).reshape(D, DFF),
        "w_ffn_down": f(inputs["w_ffn_down"]).reshape(DFF, D),
        "g_final": f(inputs["g_final"]).reshape(1, D),
        "cst": make_consts(),
    }
    x = f(inputs["x"])
    mem = f(inputs["mem"])
    pos = np.ascontiguousarray(np.asarray(inputs["positions"], dtype=np.int32))
    in_maps = []
    for c in range(NCORES):
        sl = slice(c * SEQ_PER_CORE, (c + 1) * SEQ_PER_CORE)
        m = dict(shared)
        m["x"] = x[sl]
        m["mem"] = mem[sl]
        m["positions"] = pos[sl]
        in_maps.append(m)
    res = run_bass_kernel_spmd(nc, in_maps, core_ids=list(range(NCORES)))
    out = np.concatenate([np.asarray(r["out"]) for r in res.results], axis=0)
    return out.astype(np.float32, copy=False)
```
